# Optimizing a Trainium2 kernel written in Bass

```python
import math
import jax
import jax.numpy as jnp
from jax import lax
import numpy as np

D_MODEL = 1024
BATCH = 16
SEQ = 2048
DEPTH = 2

GRID_W = 64
CTX_LEN = 256
N_MOD = 6
EPS = 1e-6
ROPE_BASE = 10000.0
ATTN_BLOCK = 128

SWA_HEADS = 4
SWA_KV_HEADS = 2
SWA_HEAD_DIM = 64
SWA_WINDOW = 128

GDN_HEADS = 4
GDN_HEAD_DIM = 128
GDN_CONV = 5
GDN_CHUNK = 64

MLA_HEADS = 4
MLA_Q_RANK = 256
MLA_KV_RANK = 128
MLA_NOPE = 64
MLA_ROPE = 32
MLA_V = 64
MLA_QK = MLA_NOPE + MLA_ROPE

N_EXPERTS = 32
TOP_K = 4
D_EXPERT = 1024
SWIGLU_LIMIT = 7.0
SWIGLU_ALPHA = 1.702
MOE_BLOCK = 256

SWA_Q = SWA_HEADS * SWA_HEAD_DIM
SWA_KV = SWA_KV_HEADS * SWA_HEAD_DIM
GDN_W = GDN_HEADS * GDN_HEAD_DIM
MLA_O = MLA_HEADS * MLA_V
D_MIX = SWA_Q + GDN_W + MLA_O
IN_SPLITS = (SWA_Q, SWA_KV, SWA_KV, GDN_W, GDN_W, GDN_W, GDN_W, 2 * GDN_HEADS, 2 * GDN_HEADS, MLA_Q_RANK, MLA_KV_RANK, MLA_ROPE)
N_IN = sum(IN_SPLITS)

kernel_name = 'hybrid_swa_gdn_mla_moe_dit'

F32 = jnp.float32


def rms_norm(x, gain):
    xf = x.astype(F32)
    y = xf * lax.rsqrt(jnp.mean(xf * xf, axis=-1, keepdims=True) + EPS)
    return (y * gain.astype(F32)).astype(x.dtype)


def l2_norm(x):
    xf = x.astype(F32)
    return xf * lax.rsqrt(jnp.sum(xf * xf, axis=-1, keepdims=True) + EPS)


def modulate(x, gain, shift, scale):
    return rms_norm(x, gain) * (1 + scale) + shift


def axial_rope_tables(n_lat, rot_dim):
    rows = n_lat // GRID_W
    t = jnp.arange(rows * GRID_W)
    row = (t // GRID_W).astype(F32)
    col = (t % GRID_W).astype(F32)
    n_freq = rot_dim // 4
    freq = ROPE_BASE ** (-jnp.arange(n_freq, dtype=F32) / n_freq)
    ang = jnp.stack([row[:, None] * freq, col[:, None] * freq], axis=1)
    return jnp.cos(ang), jnp.sin(ang)


def apply_axial_rope(x, cos, sin):
    b, n, h, r = x.shape
    xr = x.astype(F32).reshape(b, n, h, 2, 2, r // 4)
    x1, x2 = xr[..., 0, :], xr[..., 1, :]
    cs, sn = cos[None, :, None], sin[None, :, None]
    out = jnp.stack([x1 * cs - x2 * sn, x2 * cs + x1 * sn], axis=-2)
    return out.reshape(b, n, h, r).astype(x.dtype)


def swa_latent(q, k, v, kc, vc, sink):
    b, s, hq, dh = q.shape
    hkv = k.shape[2]
    grp = hq // hkv
    nb = s // ATTN_BLOCK
    nw = 3 * ATTN_BLOCK
    scale = dh ** -0.5
    qb = q.reshape(b, nb, ATTN_BLOCK, hkv, grp, dh)
    pad = ((0, 0), (ATTN_BLOCK, ATTN_BLOCK), (0, 0), (0, 0))
    kp = jnp.pad(k, pad).reshape(b, nb + 2, ATTN_BLOCK, hkv, dh)
    vp = jnp.pad(v, pad).reshape(b, nb + 2, ATTN_BLOCK, hkv, dh)
    kw = jnp.concatenate([kp[:, :-2], kp[:, 1:-1], kp[:, 2:]], axis=2)
    vw = jnp.concatenate([vp[:, :-2], vp[:, 1:-1], vp[:, 2:]], axis=2)
    q_off = jnp.arange(ATTN_BLOCK)
    k_off = jnp.arange(nw) - ATTN_BLOCK
    key_pos = jnp.arange(nb)[:, None] * ATTN_BLOCK + k_off[None, :]
    in_band = jnp.abs(k_off[None, :] - q_off[:, None]) <= SWA_WINDOW
    in_seq = (key_pos >= 0) & (key_pos < s)
    valid = in_band[None] & in_seq[:, None, :]
    s_win = jnp.einsum('bnqhgd,bnkhd->bnhgqk', qb, kw, preferred_element_type=F32) * scale
    s_win = jnp.where(valid[None, :, None, None], s_win, -jnp.inf)
    s_ctx = jnp.einsum('bnqhgd,bchd->bnhgqc', qb, kc, preferred_element_type=F32) * scale
    s_sink = jnp.broadcast_to(sink.astype(F32).reshape(hkv, grp)[:, :, None, None], s_win.shape[:-1] + (1,))
    p = jax.nn.softmax(jnp.concatenate([s_win, s_ctx, s_sink], axis=-1), axis=-1).astype(v.dtype)
    o = (jnp.einsum('bnhgqk,bnkhd->bnqhgd', p[..., :nw], vw)
         + jnp.einsum('bnhgqc,bchd->bnqhgd', p[..., nw:-1], vc))
    return o.reshape(b, s, hq * dh)


def context_attention(q, k, v, sink):
    b, n, hq, dq = q.shape
    hkv = k.shape[2]
    grp = hq // hkv
    qg = q.reshape(b, n, hkv, grp, dq)
    sc = jnp.einsum('bqhgd,bkhd->bhgqk', qg, k, preferred_element_type=F32) * dq ** -0.5
    if sink is not None:
        s_sink = jnp.broadcast_to(sink.astype(F32).reshape(hkv, grp)[:, :, None, None], sc.shape[:-1] + (1,))
        sc = jnp.concatenate([sc, s_sink], axis=-1)
    p = jax.nn.softmax(sc, axis=-1).astype(v.dtype)[..., :n]
    o = jnp.einsum('bhgqk,bkhd->bqhgd', p, v)
    return o.reshape(b, n, hq * v.shape[-1])


def dense_block_attention(q, k, v, kc, vc):
    b, s, h, dqk = q.shape
    dv = v.shape[-1]
    nb = s // ATTN_BLOCK
    scale = dqk ** -0.5
    qb = jnp.moveaxis(q.reshape(b, nb, ATTN_BLOCK, h, dqk), 1, 0)

    def attend(qi):
        sl = jnp.einsum('bqhd,bkhd->bhqk', qi, k, preferred_element_type=F32) * scale
        sc = jnp.einsum('bqhd,bchd->bhqc', qi, kc, preferred_element_type=F32) * scale
        p = jax.nn.softmax(jnp.concatenate([sl, sc], axis=-1), axis=-1).astype(v.dtype)
        return (jnp.einsum('bhqk,bkhd->bqhd', p[..., :s], v)
                + jnp.einsum('bhqc,bchd->bqhd', p[..., s:], vc))

    o = lax.map(attend, qb)
    return jnp.moveaxis(o, 0, 1).reshape(b, s, h * dv)


def short_conv(x, w):
    y = lax.conv_general_dilated(
        x, w[:, None, :].astype(x.dtype), window_strides=(1,),
        padding=[(GDN_CONV // 2, GDN_CONV // 2)],
        dimension_numbers=('NWC', 'WIO', 'NWC'), feature_group_count=x.shape[-1])
    return jax.nn.silu(y)


def gdn_chunk_scan(q, k, v, g, beta, state):
    b, n, h, _ = q.shape
    nc = n // GDN_CHUNK

    def chunks(t):
        t = t.reshape((b, nc, GDN_CHUNK, h) + t.shape[3:])
        return jnp.moveaxis(t, 3, 1)

    qc, kc, vc = chunks(q), chunks(k), chunks(v)
    gc = jnp.cumsum(chunks(g), axis=-1)
    bc = chunks(beta)
    idx = jnp.arange(GDN_CHUNK)
    incl = idx[:, None] >= idx[None, :]
    strict = idx[:, None] > idx[None, :]
    decay = jnp.exp(jnp.where(incl, gc[..., :, None] - gc[..., None, :], -jnp.inf))
    kk = jnp.einsum('bhctd,bhcid->bhcti', kc, kc)
    a_mat = jnp.where(strict, bc[..., :, None] * kk * decay, 0.0) + jnp.eye(GDN_CHUNK, dtype=F32)
    u = lax.linalg.triangular_solve(a_mat, vc * bc[..., None], left_side=True, lower=True, unit_diagonal=True)
    w = lax.linalg.triangular_solve(a_mat, kc * (bc * jnp.exp(gc))[..., None], left_side=True, lower=True, unit_diagonal=True)
    qk = jnp.einsum('bhctd,bhcid->bhcti', qc, kc) * decay
    q_dec = qc * jnp.exp(gc)[..., None]
    k_dec = kc * jnp.exp(gc[..., -1:] - gc)[..., None]
    g_end = jnp.exp(gc[..., -1])

    def step(s_prev, xs):
        u_c, w_c, qk_c, qd_c, kd_c, ge_c = xs
        v_new = u_c - jnp.einsum('bhtk,bhkv->bhtv', w_c, s_prev)
        o_c = jnp.einsum('bhtk,bhkv->bhtv', qd_c, s_prev) + jnp.einsum('bhti,bhiv->bhtv', qk_c, v_new)
        s_next = ge_c[..., None, None] * s_prev + jnp.einsum('bhtk,bhtv->bhkv', kd_c, v_new)
        return s_next, o_c

    xs = tuple(jnp.moveaxis(t, 2, 0) for t in (u, w, qk, q_dec, k_dec, g_end))
    state, o = lax.scan(step, state, xs)
    o = jnp.moveaxis(jnp.moveaxis(o, 0, 2), 1, 3)
    return o.reshape(b, n, h, o.shape[-1]), state


def orient(stream, d):
    q, k, v, g, beta = stream
    seq = (q, k, v, g[:, :, d], beta[:, :, d])
    if d == 1:
        return tuple(jnp.flip(t, axis=1) for t in seq)
    return seq


def bidirectional_gdn(lat, ctx):
    b, _, h, dk = lat[0].shape
    dv = lat[2].shape[-1]
    o_lat, o_ctx = [], []
    for d in range(2):
        state0 = jnp.zeros((b, h, dk, dv), F32)
        oc, s_ctx = gdn_chunk_scan(*orient(ctx, d), state0)
        ol, _ = gdn_chunk_scan(*orient(lat, d), s_ctx)
        if d == 1:
            oc, ol = jnp.flip(oc, axis=1), jnp.flip(ol, axis=1)
        o_lat.append(ol)
        o_ctx.append(oc)
    return o_lat[0] + o_lat[1], o_ctx[0] + o_ctx[1]


def token_mixers(p, pc, rope_a, rope_c, swa_q_norm, swa_k_norm, swa_sink, gdn_conv, gdn_a_log, gdn_dt_bias,
                 gdn_out_norm, mla_q_a_norm, mla_w_uq, mla_kv_a_norm, mla_w_ukv, mla_q_norm, mla_k_norm, with_ctx):
    b = p.shape[0]
    offsets = np.cumsum(IN_SPLITS)[:-1].tolist()
    aq, ak, av, gq, gk, gv, gz, ga, gb, cq, ckv, ckr = jnp.split(p, offsets, axis=-1)
    aq_c, ak_c, av_c, gq_c, gk_c, gv_c, gz_c, ga_c, gb_c, cq_c, ckv_c, ckr_c = jnp.split(pc, offsets, axis=-1)

    def swa_proj(q, k, v, rope):
        n = q.shape[1]
        q = rms_norm(q.reshape(b, n, SWA_HEADS, SWA_HEAD_DIM), swa_q_norm)
        k = rms_norm(k.reshape(b, n, SWA_KV_HEADS, SWA_HEAD_DIM), swa_k_norm)
        if rope is not None:
            q, k = apply_axial_rope(q, *rope), apply_axial_rope(k, *rope)
        return q, k, v.reshape(b, n, SWA_KV_HEADS, SWA_HEAD_DIM)

    qa, ka, va = swa_proj(aq, ak, av, rope_a)
    qa_c, ka_c, va_c = swa_proj(aq_c, ak_c, av_c, None)
    o_a = swa_latent(qa, ka, va, ka_c, va_c, swa_sink)

    def gdn_proj(q, k, v, a, bg):
        n = q.shape[1]
        q, k, v = jnp.split(short_conv(jnp.concatenate([q, k, v], axis=-1), gdn_conv), 3, axis=-1)
        hs = (b, n, GDN_HEADS, GDN_HEAD_DIM)
        q = l2_norm(q.reshape(hs)) * GDN_HEAD_DIM ** -0.5
        k = l2_norm(k.reshape(hs))
        v = v.reshape(hs).astype(F32)
        a = a.reshape(b, n, 2, GDN_HEADS).astype(F32)
        log_decay = -jnp.exp(gdn_a_log.astype(F32)) * jax.nn.softplus(a + gdn_dt_bias.astype(F32))
        beta = jax.nn.sigmoid(bg.reshape(b, n, 2, GDN_HEADS).astype(F32))
        return q, k, v, log_decay, beta

    def gdn_out(o, z):
        n = o.shape[1]
        gate = jax.nn.silu(z.reshape(b, n, GDN_HEADS, GDN_HEAD_DIM).astype(F32))
        return (rms_norm(o, gdn_out_norm) * gate).reshape(b, n, GDN_W).astype(p.dtype)

    ob, ob_c = bidirectional_gdn(gdn_proj(gq, gk, gv, ga, gb), gdn_proj(gq_c, gk_c, gv_c, ga_c, gb_c))
    o_b = gdn_out(ob, gz)

    def mla_proj(c_q, c_kv, k_rope, rope):
        n = c_q.shape[1]
        q = (rms_norm(c_q, mla_q_a_norm) @ mla_w_uq).reshape(b, n, MLA_HEADS, MLA_QK)
        kv = (rms_norm(c_kv, mla_kv_a_norm) @ mla_w_ukv).reshape(b, n, MLA_HEADS, MLA_NOPE + MLA_V)
        k_pe = jnp.broadcast_to(k_rope[:, :, None, :], (b, n, MLA_HEADS, MLA_ROPE))
        k = jnp.concatenate([kv[..., :MLA_NOPE], k_pe], axis=-1)
        q, k = rms_norm(q, mla_q_norm), rms_norm(k, mla_k_norm)
        if rope is not None:
            q = jnp.concatenate([q[..., :MLA_NOPE], apply_axial_rope(q[..., MLA_NOPE:], *rope)], axis=-1)
            k = jnp.concatenate([k[..., :MLA_NOPE], apply_axial_rope(k[..., MLA_NOPE:], *rope)], axis=-1)
        return q, k, kv[..., MLA_NOPE:]

    qm, km, vm = mla_proj(cq, ckv, ckr, rope_c)
    qm_c, km_c, vm_c = mla_proj(cq_c, ckv_c, ckr_c, None)
    o_c = dense_block_attention(qm, km, vm, km_c, vm_c)

    mix = jnp.concatenate([o_a, o_b, o_c], axis=-1)
    if not with_ctx:
        return mix, None
    mix_c = jnp.concatenate([context_attention(qa_c, ka_c, va_c, swa_sink), gdn_out(ob_c, gz_c),
                             context_attention(qm_c, km_c, vm_c, None)], axis=-1)
    return mix, mix_c


def clamped_swiglu(gu):
    x_glu = jnp.minimum(gu[..., ::2], SWIGLU_LIMIT)
    x_lin = jnp.clip(gu[..., 1::2], -SWIGLU_LIMIT, SWIGLU_LIMIT)
    return x_glu * jax.nn.sigmoid(SWIGLU_ALPHA * x_glu) * (x_lin + 1)


def moe_ffn(h, router_w, router_b, w_gu, b_gu, w_dn, b_dn):
    n, d = h.shape
    logits = jnp.dot(h, router_w, preferred_element_type=F32) + router_b.astype(F32)
    top_logit, top_e = lax.top_k(logits, TOP_K)
    gate = jax.nn.softmax(top_logit, axis=-1)
    nk = n * TOP_K
    e_flat = top_e.reshape(nk)
    tok_flat = jnp.arange(nk, dtype=jnp.int32) // TOP_K
    order = jnp.argsort(e_flat)
    e_sorted = e_flat[order]
    counts = jnp.bincount(e_flat, length=N_EXPERTS)
    padded = (counts + MOE_BLOCK - 1) // MOE_BLOCK * MOE_BLOCK
    pad_end = jnp.cumsum(padded)
    pad_start = pad_end - padded
    raw_start = jnp.cumsum(counts) - counts
    slot = pad_start[e_sorted] + jnp.arange(nk) - raw_start[e_sorted]
    n_blocks = -(-nk // MOE_BLOCK) + N_EXPERTS
    cap = n_blocks * MOE_BLOCK
    slot_tok = jnp.full((cap,), n, jnp.int32).at[slot].set(tok_flat[order])
    slot_gate = jnp.zeros((cap,), F32).at[slot].set(gate.reshape(nk)[order])
    block_e = jnp.minimum(jnp.searchsorted(pad_end, jnp.arange(n_blocks) * MOE_BLOCK, side='right'), N_EXPERTS - 1)
    h_pad = jnp.concatenate([h, jnp.zeros((1, d), h.dtype)], axis=0)

    def expert_block(args):
        toks, e = args
        xb = h_pad[toks]
        return clamped_swiglu(xb @ w_gu[e] + b_gu[e]) @ w_dn[e] + b_dn[e]

    y = lax.map(expert_block, (slot_tok.reshape(n_blocks, MOE_BLOCK), block_e))
    y = y.reshape(cap, d) * slot_gate[:, None].astype(y.dtype)
    return jnp.zeros((n + 1, d), y.dtype).at[slot_tok].add(y)[:n]


def setup_inputs(seed: int = 0) -> dict:
    key = jax.random.key(seed)
    ks = iter(jax.random.split(key, 32))
    L, d = DEPTH, D_MODEL

    def nrm(shape, scale):
        return jax.random.normal(next(ks), shape, F32) * scale

    def gain(shape):
        return 1.0 + nrm(shape, 0.02)

    a_log = jnp.log(jax.random.uniform(next(ks), (L, 2, GDN_HEADS), F32, 1.0, 16.0))
    dt = jnp.exp(jax.random.uniform(next(ks), (L, 2, GDN_HEADS), F32, math.log(1e-3), math.log(1e-1)))
    dt_bias = dt + jnp.log(-jnp.expm1(-dt))
    return {
        'x': nrm((BATCH, SEQ, d), 1.0),
        'c': nrm((BATCH, d), 1.0),
        'ctx': nrm((BATCH, CTX_LEN, d), 1.0),
        'c_ctx': nrm((d,), 1.0),
        'w_mod': nrm((L, d, N_MOD * d), 0.5 * d ** -0.5),
        'b_mod': nrm((L, N_MOD * d), 0.02),
        'norm1': gain((L, d)),
        'norm2': gain((L, d)),
        'w_in': nrm((L, d, N_IN), d ** -0.5),
        'w_out': nrm((L, D_MIX, d), D_MIX ** -0.5),
        'swa_q_norm': gain((L, SWA_HEAD_DIM)),
        'swa_k_norm': gain((L, SWA_HEAD_DIM)),
        'swa_sink': nrm((L, SWA_HEADS), 0.5),
        'gdn_conv': nrm((L, GDN_CONV, 3 * GDN_W), GDN_CONV ** -0.5),
        'gdn_a_log': a_log,
        'gdn_dt_bias': dt_bias,
        'gdn_out_norm': gain((L, GDN_HEAD_DIM)),
        'mla_q_a_norm': gain((L, MLA_Q_RANK)),
        'mla_w_uq': nrm((L, MLA_Q_RANK, MLA_HEADS * MLA_QK), MLA_Q_RANK ** -0.5),
        'mla_kv_a_norm': gain((L, MLA_KV_RANK)),
        'mla_w_ukv': nrm((L, MLA_KV_RANK, MLA_HEADS * (MLA_NOPE + MLA_V)), MLA_KV_RANK ** -0.5),
        'mla_q_norm': gain((L, MLA_QK)),
        'mla_k_norm': gain((L, MLA_QK)),
        'router_w': nrm((L, d, N_EXPERTS), d ** -0.5),
        'router_b': nrm((L, N_EXPERTS), 0.01),
        'exp_w_gu': nrm((L, N_EXPERTS, d, 2 * D_EXPERT), d ** -0.5),
        'exp_b_gu': nrm((L, N_EXPERTS, 2 * D_EXPERT), 0.02),
        'exp_w_dn': nrm((L, N_EXPERTS, D_EXPERT, d), D_EXPERT ** -0.5),
        'exp_b_dn': nrm((L, N_EXPERTS, d), 0.02),
    }


def reference(x, c, ctx, c_ctx, w_mod, b_mod, norm1, norm2, w_in, w_out, swa_q_norm, swa_k_norm, swa_sink,
              gdn_conv, gdn_a_log, gdn_dt_bias, gdn_out_norm, mla_q_a_norm, mla_w_uq, mla_kv_a_norm, mla_w_ukv,
              mla_q_norm, mla_k_norm, router_w, router_b, exp_w_gu, exp_b_gu, exp_w_dn, exp_b_dn):
    b, s, d = x.shape
    cl = ctx.shape[1]
    rope_a = axial_rope_tables(s, SWA_HEAD_DIM)
    rope_c = axial_rope_tables(s, MLA_ROPE)
    xc = ctx
    for l in range(DEPTH):
        with_ctx = l < DEPTH - 1
        mod = (jax.nn.silu(c) @ w_mod[l] + b_mod[l]).reshape(b, 1, N_MOD, d)
        mod_c = (jax.nn.silu(c_ctx) @ w_mod[l] + b_mod[l]).reshape(1, 1, N_MOD, d)
        h = modulate(x, norm1[l], mod[:, :, 0], mod[:, :, 1])
        hc = modulate(xc, norm1[l], mod_c[:, :, 0], mod_c[:, :, 1])
        mix, mix_c = token_mixers(h @ w_in[l], hc @ w_in[l], rope_a, rope_c, swa_q_norm[l], swa_k_norm[l],
                                  swa_sink[l], gdn_conv[l], gdn_a_log[l], gdn_dt_bias[l], gdn_out_norm[l],
                                  mla_q_a_norm[l], mla_w_uq[l], mla_kv_a_norm[l], mla_w_ukv[l], mla_q_norm[l],
                                  mla_k_norm[l], with_ctx)
        x = x + mod[:, :, 2] * (mix @ w_out[l])
        h = modulate(x, norm2[l], mod[:, :, 3], mod[:, :, 4])
        if with_ctx:
            xc = xc + mod_c[:, :, 2] * (mix_c @ w_out[l])
            hc = modulate(xc, norm2[l], mod_c[:, :, 3], mod_c[:, :, 4])
            y = moe_ffn(jnp.concatenate([h.reshape(b * s, d), hc.reshape(b * cl, d)], axis=0), router_w[l],
                        router_b[l], exp_w_gu[l], exp_b_gu[l], exp_w_dn[l], exp_b_dn[l])
            x = x + mod[:, :, 5] * y[:b * s].reshape(b, s, d)
            xc = xc + mod_c[:, :, 5] * y[b * s:].reshape(b, cl, d)
        else:
            y = moe_ffn(h.reshape(b * s, d), router_w[l], router_b[l], exp_w_gu[l], exp_b_gu[l],
                        exp_w_dn[l], exp_b_dn[l])
            x = x + mod[:, :, 5] * y.reshape(b, s, d)
    return x
```

```python
import contextlib
import numpy as np
import concourse.bass as bass
import concourse.mybir as mybir
from concourse.bass_utils import run_bass_kernel_spmd

F32 = mybir.dt.float32
BF16 = mybir.dt.bfloat16
AF = mybir.ActivationFunctionType
ALU = mybir.AluOpType

ENG = ["tensor", "vector", "scalar", "gpsimd", "sync"]
SEM_ROLL = 30000
NDSEM = 24


class Prog:
    def __init__(self, nc):
        self.nc = nc
        self.ops = {e: [] for e in ENG}
        self.cnt = {e: 0 for e in ENG}
        self.seen = {e: {p: 0 for p in ENG} for e in ENG}
        self.dseen = {e: set() for e in ENG}
        self.res = {}
        self.ndma = 0
        self.dma_issuer = {}
        self.arena = None
        self.off = 0
        self.peak = 0

    def set_arena(self, arena, nbytes):
        self.arena = arena
        self.arena_bytes = nbytes
        self.off = 0

    def alloc(self, n, dtype):
        esz = 2 if dtype == BF16 else 4
        nb = (n * esz + 63) // 64 * 64
        assert self.off + nb <= self.arena_bytes, ("arena overflow", self.off, nb)
        a = self.arena[:, self.off // 4:(self.off + nb) // 4]
        self.off += nb
        self.peak = max(self.peak, self.off)
        if dtype != F32:
            a = a.bitcast(dtype)
        return a[:, 0:n]

    def mark(self):
        return self.off

    def release(self, m):
        self.off = m

    def _need(self, reads, writes):
        toks = []
        for r in reads:
            s = self.res.get(r)
            if s and s["w"] is not None:
                toks.append(s["w"])
        for w in writes:
            s = self.res.get(w)
            if s:
                if s["w"] is not None:
                    toks.append(s["w"])
                for e, n in s["rc"].items():
                    toks.append(("c", e, n))
                for d in s["rd"]:
                    toks.append(("d", d))
        return toks

    def _emit_waits(self, eng, toks):
        lst = self.ops[eng]
        best = {}
        for t in toks:
            if t[0] == "c":
                _, p, n = t
                if p == "tensor" and eng == "tensor":
                    continue
                if self.seen[eng][p] < n:
                    best[p] = max(best.get(p, 0), n)
            else:
                d = t[1]
                if d not in self.dseen[eng]:
                    self.dseen[eng].add(d)
                    k, v = d % NDSEM, 16 * (d // NDSEM + 1)
                    lst.append(lambda e, S, k=k, v=v: e.wait_ge(S["d"][k], v))
        for p, n in best.items():
            self.seen[eng][p] = n
            k, v = (n - 1) // SEM_ROLL, (n - 1) % SEM_ROLL + 1
            lst.append(lambda e, S, p=p, k=k, v=v: e.wait_ge(S[p][k], v))

    def _commit(self, tok, reads, writes):
        for r in reads:
            s = self.res.setdefault(r, {"w": None, "rc": {}, "rd": set()})
            if tok[0] == "c":
                s["rc"][tok[1]] = tok[2]
            else:
                s["rd"].add(tok[1])
        for w in writes:
            self.res[w] = {"w": tok, "rc": {}, "rd": set()}

    @staticmethod
    def _bank_keys(reads, writes):
        reads, writes = list(reads), list(writes)
        for k in reads + writes:
            b = None
            if k.startswith("pq") and k[2:].isdigit():
                b = int(k[2:]) // 4
            elif k.startswith("ps") and k[2:].isdigit():
                b = int(k[2:])
            if b is not None and f"PB{b}" not in writes:
                writes.append(f"PB{b}")
        return reads, writes

    rec = None

    def replay_interleaved(self, bufs):
        self.rec = None
        n = max(len(b) for b in bufs)
        for i in range(n):
            for b in bufs:
                if i < len(b):
                    kind, args = b[i]
                    if kind == "op":
                        self.op(*args)
                    else:
                        self.dma(*args[:-1], **args[-1])

    def op(self, eng, fn, reads=(), writes=()):
        if self.rec is not None:
            self.rec.append(("op", (eng, fn, list(reads), list(writes))))
            return
        reads, writes = self._bank_keys(reads, writes)
        self._emit_waits(eng, self._need(reads, writes))
        self.cnt[eng] += 1
        n = self.cnt[eng]
        k = (n - 1) // SEM_ROLL
        self.ops[eng].append(lambda e, S, fn=fn, p=eng, k=k: fn(e).then_inc(S[p][k], 1))
        self._commit(("c", eng, n), reads, writes)

    def dma(self, eng, out, in_, reads=(), writes=(), **kw):
        if self.rec is not None:
            self.rec.append(("dma", (eng, out, in_, list(reads), list(writes), kw)))
            return
        d = self.ndma
        self.ndma += 1
        toks = self._need(reads, writes)
        if d >= NDSEM:
            toks.append(("d", d - NDSEM))
        self._emit_waits(eng, toks)
        k = d % NDSEM
        self.ops[eng].append(lambda e, S, k=k, out=out, in_=in_, kw=kw: e.dma_start(out=out, in_=in_, **kw).then_inc(S["d"][k], 16))
        self.dma_issuer[d] = eng
        self._commit(("d", d), reads, writes)
        return d

    def barrier(self):
        for e in ENG:
            toks = [("c", p, self.cnt[p]) for p in ENG if p != e and self.cnt[p] > 0]
            toks += [("d", d) for d in range(max(0, self.ndma - NDSEM), self.ndma)]
            self._emit_waits(e, toks)

    def finish(self):
        self._emit_waits("sync", [("d", d) for d in range(max(0, self.ndma - NDSEM), self.ndma)])
        nc = self.nc
        with contextlib.ExitStack() as st:
            S = {}
            for e in ENG:
                ns = self.cnt[e] // SEM_ROLL + 1
                S[e] = [st.enter_context(nc.semaphore(f"s_{e}_{i}")) for i in range(ns)]
            S["d"] = [st.enter_context(nc.semaphore(f"s_d_{i}")) for i in range(NDSEM)]
            block = st.enter_context(nc.Block())

            def run(engname):
                def _f(eng):
                    for f in self.ops[engname]:
                        f(eng, S)
                return _f

            block.sync(run("sync"))
            block.tensor(run("tensor"))
            block.vector(run("vector"))
            block.scalar(run("scalar"))
            block.gpsimd(run("gpsimd"))


D = 1024
T = 2304
NT = 18
CTX = 256
SEQ = 2048
EPS = 1e-6
BLKS = [(0, 256), (256, 512), (768, 512), (1280, 512), (1792, 512)]
NIN = 2992
NEXP = 32


def tile_blk(ti):
    return 0 if ti < 2 else 1 + (ti - 2) // 4


def host_consts():
    c = {}
    i = np.arange(128)
    U = (i[:, None] <= i[None, :]).astype(np.float32)
    L = (i[:, None] >= i[None, :]).astype(np.float32)
    SU = (i[:, None] < i[None, :]).astype(np.float32)
    SL = (i[:, None] > i[None, :]).astype(np.float32)
    ident = np.eye(128, dtype=np.float32)
    ones = np.ones((128, 128), np.float32)
    bd64 = np.kron(np.eye(2, dtype=np.float32), np.ones((64, 64), np.float32))

    def rmat(rot, off, size):
        R = np.zeros((size, size), np.float32)
        q = rot // 4
        for ax in range(2):
            b = off + ax * 2 * q
            for k in range(q):
                R[b + k, b + q + k] = -1.0
                R[b + q + k, b + k] = 1.0
        return R
    RA = np.kron(np.eye(2, dtype=np.float32), rmat(64, 0, 64))
    RC = np.zeros((128, 128), np.float32)
    RC[:96, :96] = rmat(32, 64, 96)
    E32 = np.zeros((128, 128), np.float32)
    for k in range(32):
        E32[k, 64 + k] = 1.0
    c["cf"] = np.concatenate([ident, U, L, SU, SL, ones, bd64, RA.T.copy(), RC.T.copy(), E32], axis=1).astype(np.float32)

    def tables(rot):
        nf = rot // 4
        t = np.arange(SEQ)
        row = (t // 64).astype(np.float32)
        col = (t % 64).astype(np.float32)
        freq = (np.float32(10000.0) ** (-np.arange(nf, dtype=np.float32) / np.float32(nf))).astype(np.float32)
        ang = np.stack([row[:, None] * freq, col[:, None] * freq], axis=1)
        cs, sn = np.cos(ang).astype(np.float32), np.sin(ang).astype(np.float32)
        C = np.concatenate([cs[:, 0], cs[:, 0], cs[:, 1], cs[:, 1]], axis=1).T
        S = np.concatenate([sn[:, 0], sn[:, 0], sn[:, 1], sn[:, 1]], axis=1).T
        return C, S
    CA, SA = tables(64)
    c["ropeA"] = np.concatenate([np.concatenate([CA, CA], 0), np.concatenate([SA, SA], 0)], axis=1).astype(np.float32)
    CC, SC = tables(32)
    cc = np.ones((128, SEQ), np.float32)
    sc = np.zeros((128, SEQ), np.float32)
    cc[64:96] = CC
    sc[64:96] = SC
    c["ropeC"] = np.concatenate([cc, sc], axis=1).astype(np.float32)
    return c


class K:
    def __init__(self, nseq=2, nlayers=2, phases="CABM", dbg=None, moe=True):
        self.nseq, self.nlayers, self.phases, self.dbg = nseq, nlayers, phases, dbg
        nc = self.nc = bass.Bass("TRN2", target_bir_lowering=False)
        self.P = Prog(nc)
        self.all_in = []

        def dt(n, s):
            a = nc.dram_tensor(n, list(s), F32, kind="ExternalInput").ap()
            self.all_in.append(a)
            return a
        self.x2 = dt("x2", (2, SEQ, D))
        self.ctx2 = dt("ctx2", (2, CTX, D))
        self.vp1 = dt("vp1", (128, 128))
        self.vp2 = dt("vp2", (32, 128))
        self.w_mod = dt("w_mod", (2, D, 6 * D))
        self.w_in = dt("w_in", (2, D, NIN))
        self.w_out = dt("w_out", (2, 1024, D))
        self.swa_q_norm = dt("swa_q_norm", (2, 64))
        self.swa_k_norm = dt("swa_k_norm", (2, 64))
        self.swa_sink = dt("swa_sink", (2, 4))
        self.gdn_conv = dt("gdn_conv", (2, 5, 1536))
        self.gdn_a_log = dt("gdn_a_log", (2, 8))
        self.gdn_dt_bias = dt("gdn_dt_bias", (2, 8))
        self.gdn_out_norm = dt("gdn_out_norm", (2, 128))
        self.mla_q_a_norm = dt("mla_q_a_norm", (2, 256))
        self.mla_w_uq = dt("mla_w_uq", (2, 256, 384))
        self.mla_kv_a_norm = dt("mla_kv_a_norm", (2, 128))
        self.mla_w_ukv = dt("mla_w_ukv", (2, 128, 512))
        self.mla_q_norm = dt("mla_q_norm", (2, 96))
        self.mla_k_norm = dt("mla_k_norm", (2, 96))
        self.router_w = dt("router_w", (2, D, NEXP))
        self.router_b = dt("router_b", (2, NEXP))
        self.exp_w_gu = dt("exp_w_gu", (2, NEXP, D, 2048) if moe else (2, 1, 8, 8))
        self.exp_b_gu = dt("exp_b_gu", (2, NEXP, 2048))
        self.exp_w_dn = dt("exp_w_dn", (2, NEXP, D, D) if moe else (2, 1, 8, 8))
        self.exp_b_dn = dt("exp_b_dn", (2, NEXP, D))
        self.cf = dt("cf", (128, 1280))
        self.ropeA = dt("ropeA", (128, 4096))
        self.ropeC = dt("ropeC", (128, 4096))
        self.out = nc.dram_tensor("out", [2, SEQ, D], F32, kind="ExternalOutput").ap()
        self.dbg_out = None
        if dbg:
            self.dbg_out = nc.dram_tensor("dbg", [128, dbg], F32, kind="ExternalOutput").ap()
        self.uid = 0

    def MM(self, out, lhsT, rhs, start=True, stop=True, r=(), w=()):
        self.P.op("tensor", lambda e, o=out, a=lhsT, b=rhs, s=start, t=stop: e.matmul(o, a, b, start=s, stop=t), r, w)

    def TR(self, out, in_, r=(), w=()):
        npar = in_.shape[0]
        idn = self.ident[0:npar, 0:npar]
        self.P.op("tensor", lambda e, o=out, a=in_: e.transpose(o, a, idn), list(r) + ["const"], w)

    def ACT(self, out, in_, func, r=(), w=(), scale=None, bias=None, eng="scalar"):
        kw = {}
        if scale is not None:
            kw["scale"] = scale
        if bias is not None:
            kw["bias"] = bias
        self.P.op("scalar", lambda e, o=out, a=in_, f=func, kw=kw: e.activation(o, a, f, **kw), r, w)

    def TS(self, eng, out, in0, s1, op0, s2=None, op1=None, r=(), w=()):
        if op1 is None:
            self.P.op(eng, lambda e, o=out, a=in0: e.tensor_scalar(out=o, in0=a, scalar1=s1, scalar2=None, op0=op0), r, w)
        else:
            self.P.op(eng, lambda e, o=out, a=in0: e.tensor_scalar(out=o, in0=a, scalar1=s1, scalar2=s2, op0=op0, op1=op1), r, w)

    def TT(self, eng, out, a, b, op, r=(), w=()):
        self.P.op(eng, lambda e, o=out, x=a, y=b: e.tensor_tensor(out=o, in0=x, in1=y, op=op), r, w)

    def STT(self, out, a, s, b, op0, op1, r=(), w=()):
        self.P.op("vector", lambda e, o=out, x=a, y=b: e.scalar_tensor_tensor(out=o, in0=x, scalar=s, in1=y, op0=op0, op1=op1), r, w)

    def CP(self, eng, out, in_, r=(), w=()):
        if eng == "scalar":
            self.P.op(eng, lambda e, o=out, a=in_: e.copy(o, a), r, w)
        else:
            self.P.op(eng, lambda e, o=out, a=in_: e.tensor_copy(o, a), r, w)

    def RECIP(self, out, in_, r=(), w=()):
        self.P.op("vector", lambda e, o=out, a=in_: e.reciprocal(o, a), r, w)

    def MEMSET(self, eng, ap, val, w=()):
        self.P.op(eng, lambda e, a=ap: e.memset(a, val), (), w)

    def DMA(self, out, in_, r=(), w=(), eng="sync"):
        self.P.dma(eng, out, in_, r, w, allow_slow_non_contiguous=True)

    def key(self, base):
        self.uid += 1
        return f"{base}#{self.uid}"

    def dump(self, ap, ncols, r, col0=0, rows=128):
        P = self.P
        m = P.mark()
        tmp = P.alloc(64, F32)
        for c0 in range(0, ncols, 64):
            n = min(64, ncols - c0)
            k = self.key("dump")
            self.CP("vector", tmp[0:rows, 0:n], ap[:, c0:c0 + n], r=list(r) + ["dumptmp"], w=[k, "dumptmp"])
            self.DMA(self.dbg_out[0:rows, col0 + c0:col0 + c0 + n], tmp[0:rows, 0:n], r=[k], w=[self.key("dbgout"), "dumptmp"])
        P.barrier()
        P.release(m)

    def setup(self):
        P = self.P
        cf = P.alloc(1280, F32)
        self.DMA(cf, self.cf, w=["const"])
        self.ident = cf[:, 0:128]
        self.U, self.L = cf[:, 128:256], cf[:, 256:384]
        self.SU, self.SL = cf[:, 384:512], cf[:, 512:640]
        self.ones = cf[:, 640:768]
        cb = P.alloc(640, BF16)
        self.CP("vector", cb, cf[:, 640:1280], r=["const"], w=["constb"])
        self.ones_b, self.bd64_b = cb[:, 0:128], cb[:, 128:256]
        self.RAT_b, self.RCT_b, self.E32_b = cb[:, 256:384], cb[:, 384:512], cb[:, 512:640]
        junk = P.alloc(64, F32)
        for i, a in enumerate(self.all_in):
            fl = a
            while len(fl.shape) > 1:
                fl = fl[0]
            self.DMA(junk[0:1, i:i + 1], fl[0:1].rearrange("(a b) -> a b", a=1), w=["junk"])
        self.eps = P.alloc(1, F32)
        self.MEMSET("vector", self.eps, EPS, w=["const2"])
        vt = P.alloc(128, F32)
        self.DMA(vt, self.vp1, w=["vt"])
        self.VP = P.alloc(128, F32)
        self.TR(self.ps[:, 0, 0:128], vt, r=["vt"], w=["ps0"])
        self.CP("vector", self.VP, self.ps[:, 0, 0:128], r=["ps0"], w=["VP"])
        vt2 = P.alloc(128, F32)
        self.DMA(vt2[0:32, :], self.vp2, w=["vt2"])
        self.CS = P.alloc(32, F32)
        self.TR(self.ps[:, 1, 0:32], vt2[0:32, :], r=["vt2"], w=["ps1"])
        self.ACT(self.CS, self.ps[:, 1, 0:32], AF.Silu, r=["ps1"], w=["CS"])
        self.xT = P.alloc(8 * T, F32).rearrange("p (c t) -> p c t", c=8)
        self.hT = P.alloc(8 * T, BF16).rearrange("p (c t) -> p c t", c=8)
        self.stg = [P.alloc(1024, F32), P.alloc(1024, F32)]
        self.stg_i = 0
        self.MOD = [P.alloc(96, F32).rearrange("p (j y) -> p j y", y=2) for _ in range(2)]
        self.A1 = [P.alloc(16, F32).rearrange("p (c y) -> p c y", y=2) for _ in range(2)]
        self.A2 = [P.alloc(16, F32).rearrange("p (c y) -> p c y", y=2) for _ in range(2)]

    def stage(self):
        i = self.stg_i
        self.stg_i ^= 1
        return self.stg[i], f"stg{i}"

    def load_cast(self, dst, src, ncols, eng="gpsimd", dkey=None, parts=128):
        st, sk = self.stage()
        self.DMA(st[0:parts, 0:ncols], src, w=[sk])
        self.CP(eng, dst, st[0:parts, 0:ncols], r=[sk], w=[dkey])

    def load_w_cols(self, dst, wsrc, c0, ncols, dkey, kc=8):
        per = max(1, 1024 // ncols)
        for k0 in range(0, kc, per):
            k1 = min(kc, k0 + per)
            st, sk = self.stage()
            sv = st[:, 0:(k1 - k0) * ncols].rearrange("p (k n) -> p k n", n=ncols)
            self.DMA(sv, wsrc[k0 * 128:k1 * 128, c0:c0 + ncols].rearrange("(k p) n -> p k n", p=128), w=[sk])
            self.CP("gpsimd", dst[:, k0:k1, :], sv, r=[sk], w=[dkey])

    def load_x(self, s):
        P = self.P
        m = P.mark()
        bufs = [P.alloc(1024, F32), P.alloc(1024, F32)]
        for ti in range(NT):
            b = bufs[ti % 2]
            bk = f"xl{ti % 2}"
            src = self.ctx2[s, ti * 128:(ti + 1) * 128, :] if ti < 2 else self.x2[s, (ti - 2) * 128:(ti - 1) * 128, :]
            self.DMA(b, src, w=[bk])
            for half in range(2):
                bank = 2 * (ti % 2) + half
                for c4 in range(4):
                    c = half * 4 + c4
                    self.TR(self.ps[:, bank, c4 * 128:(c4 + 1) * 128], b[:, c * 128:(c + 1) * 128], r=[bk], w=[f"ps{bank}"])
                eng = "vector" if half == 0 else "scalar"
                self.CP(eng, self.xT[:, half * 4:half * 4 + 4, ti * 128:(ti + 1) * 128],
                        self.ps[:, bank, :].rearrange("p (c t) -> p c t", c=4), r=[f"ps{bank}"], w=[f"xT{tile_blk(ti)}"])
        P.barrier()
        P.release(m)

    def store_x(self, s):
        P = self.P
        m = P.mark()
        bufs = [P.alloc(1024, F32), P.alloc(1024, F32)]
        for ti in range(2, NT):
            b = bufs[ti % 2]
            bk = f"xs{ti % 2}"
            for half in range(2):
                bank = 2 * (ti % 2) + half
                for c4 in range(4):
                    c = half * 4 + c4
                    self.TR(self.ps[:, bank, c4 * 128:(c4 + 1) * 128], self.xT[:, c, ti * 128:(ti + 1) * 128], r=[f"xT{tile_blk(ti)}"], w=[f"ps{bank}"])
                eng = "vector" if half == 0 else "scalar"
                self.CP(eng, b[:, half * 512:(half + 1) * 512], self.ps[:, bank, :], r=[f"ps{bank}"], w=[bk])
            self.DMA(self.out[s, (ti - 2) * 128:(ti - 1) * 128, :], b, r=[bk], w=[self.key("out")])
        P.barrier()
        P.release(m)

    def mods(self, s, l):
        P = self.P
        m = P.mark()
        sc = P.alloc(16, F32).rearrange("p (k y) -> p k y", y=2)
        kk = self.key("sc")
        self.CP("vector", sc[:, :, 0], self.CS[:, s * 8:s * 8 + 8], r=["CS"], w=[kk])
        self.CP("vector", sc[:, :, 1], self.CS[:, 16:24], r=["CS"], w=[kk])
        MOD = self.MOD[l]
        for j in range(48):
            st, sk = self.stage()
            sv = st[:, 0:1024].rearrange("p (k n) -> p k n", n=128)
            self.DMA(sv, self.w_mod[l, :, j * 128:(j + 1) * 128].rearrange("(k p) n -> p k n", p=128), w=[sk])
            for k in range(8):
                self.MM(self.ps[:, 7, 2 * j:2 * j + 2], sv[:, k, :], sc[:, k, :], start=(k == 0), stop=(k == 7), r=[sk, kk], w=["ps7"])
        mk = f"MOD{l}"
        bm = self.VP[:, l * 64:l * 64 + 48]
        pv = self.ps[:, 7, 0:96].rearrange("p (j y) -> p j y", y=2)
        for y in range(2):
            self.TT("vector", MOD[:, :, y], pv[:, :, y], bm, ALU.add, r=["ps7", "VP"], w=[mk])
        n1 = self.VP[:, l * 64 + 48:l * 64 + 56]
        n2 = self.VP[:, l * 64 + 56:l * 64 + 64]
        for y in range(2):
            self.STT(self.A1[l][:, :, y], MOD[:, 8:16, y], 1.0, n1, ALU.add, ALU.mult, r=[mk, "VP"], w=[mk + "a"])
            self.STT(self.A2[l][:, :, y], MOD[:, 32:40, y], 1.0, n2, ALU.add, ALU.mult, r=[mk, "VP"], w=[mk + "a"])
        P.barrier()
        P.release(m)

    def norm_mod(self, l, A, shift_j, router=None):
        P = self.P
        m = P.mark()
        sq = P.alloc(8 * 512, BF16).rearrange("p (c t) -> p c t", c=8)
        sd = P.alloc(512, F32)
        rs = P.alloc(512, F32)
        tmp = [P.alloc(512, F32), P.alloc(512, F32)]
        MOD = self.MOD[l]
        for bi, (t0, n) in enumerate(BLKS):
            y = 1 if bi == 0 else 0
            xk, hk = f"xT{bi}", f"hT{bi}"
            for c in range(8):
                self.ACT(sq[:, c, 0:n], self.xT[:, c, t0:t0 + n], AF.Square, r=[xk], w=[f"sq{c}"])
            for c in range(8):
                self.MM(self.ps[:, 0, 0:n], self.ones_b, sq[:, c, 0:n], start=(c == 0), stop=(c == 7), r=[f"sq{c}", "constb"], w=["ps0"])
            self.ACT(sd[:, 0:n], self.ps[:, 0, 0:n], AF.Sqrt, scale=1.0 / D, bias=self.eps, r=["ps0", "const2"], w=["sd"])
            self.RECIP(rs[:, 0:n], sd[:, 0:n], r=["sd"], w=["rs"])
            for c in range(8):
                tb, tk = tmp[c % 2], f"nt{c % 2}"
                if router is not None:
                    tb, tk = router["h2f"][:, c, :], f"h2f{c}"
                self.STT(tb[:, 0:n], self.xT[:, c, t0:t0 + n], A[:, c, y:y + 1], rs[:, 0:n], ALU.mult, ALU.mult, r=[xk, "rs", f"MOD{l}a"], w=[tk])
                if router is not None:
                    self.TS("vector", tb[:, 0:n], tb[:, 0:n], MOD[:, shift_j + c, y:y + 1], ALU.add, r=[tk, f"MOD{l}"], w=[tk])
                    self.CP("scalar", self.hT[:, c, t0:t0 + n], tb[:, 0:n], r=[tk], w=[hk])
                else:
                    self.ACT(self.hT[:, c, t0:t0 + n], tb[:, 0:n], AF.Identity, bias=MOD[:, shift_j + c, y:y + 1], r=[tk, f"MOD{l}"], w=[hk])
            if router is not None:
                router["fn"](bi, t0, n)
        P.barrier()
        P.release(m)

    def head_norm_rope(self, src, npar, n, dst, onesl, inv_dim, gain, RT, rope_dram, t0, sk, dk, W):
        ps = self.ps
        sq, sd, rs, qn, t1, t2, rb = W["sq"], W["sd"], W["rs"], W["qn"], W["t1"], W["t2"], W["rb"]
        self.ACT(sq[0:npar, 0:n], src, AF.Square, r=[sk], w=["w_sq"])
        self.MM(ps[0:npar, 5, 0:n], onesl[0:npar, 0:npar], sq[0:npar, 0:n], r=["w_sq", "constb"], w=["ps5"])
        self.ACT(sd[0:npar, 0:n], ps[0:npar, 5, 0:n], AF.Sqrt, scale=inv_dim, bias=self.eps[0:npar, :], r=["ps5", "const2"], w=["w_sd"])
        self.RECIP(rs[0:npar, 0:n], sd[0:npar, 0:n], r=["w_sd"], w=["w_rs"])
        if t0 < CTX:
            self.STT(dst, src, gain, rs[0:npar, 0:n], ALU.mult, ALU.mult, r=[sk, "w_rs", "gains"], w=[dk])
            return
        self.STT(qn[0:npar, 0:n], src, gain, rs[0:npar, 0:n], ALU.mult, ALU.mult, r=[sk, "w_rs", "gains"], w=["w_qn"])
        self.MM(ps[0:npar, 6, 0:n], RT[0:npar, 0:npar], qn[0:npar, 0:n], r=["w_qn", "constb"], w=["ps6"])
        self.DMA(rb[:, 0, 0:n], rope_dram[:, t0 - CTX:t0 - CTX + n], w=["w_rb"])
        self.DMA(rb[:, 1, 0:n], rope_dram[:, SEQ + t0 - CTX:SEQ + t0 - CTX + n], w=["w_rb"])
        self.TT("gpsimd", t1[0:npar, 0:n], qn[0:npar, 0:n], rb[0:npar, 0, 0:n], ALU.mult, r=["w_qn", "w_rb"], w=["w_t1"])
        self.TT("vector", t2[0:npar, 0:n], ps[0:npar, 6, 0:n], rb[0:npar, 1, 0:n], ALU.mult, r=["ps6", "w_rb"], w=["w_t2"])
        self.TT("vector", dst, t1[0:npar, 0:n], t2[0:npar, 0:n], ALU.add, r=["w_t1", "w_t2"], w=[dk])

    def norm_temps(self):
        P = self.P
        return dict(sq=P.alloc(512, BF16), sd=P.alloc(512, F32), rs=P.alloc(512, F32), qn=P.alloc(512, BF16),
                    t1=P.alloc(512, BF16), t2=P.alloc(512, BF16), rb=P.alloc(1024, F32).rearrange("p (a n) -> p a n", a=2))

    def out_proj(self, l, Wo, kparts, mix, mixk, wk, nacc=1):
        ps = self.ps
        MOD = self.MOD[l]
        cnt = 0
        for bi, (t0, n) in enumerate(BLKS):
            y = 1 if bi == 0 else 0
            for dc in range(8):
                bank = cnt % 4
                cnt += 1
                for a in range(nacc):
                    self.MM(ps[:, bank, 0:n], Wo[0:kparts, a, dc * 128:(dc + 1) * 128], mix[0:kparts, a, t0:t0 + n],
                            start=(a == 0), stop=(a == nacc - 1), r=[wk, mixk], w=[f"ps{bank}"])
                self.STT(self.xT[:, dc, t0:t0 + n], ps[:, bank, 0:n], MOD[:, 16 + dc, y:y + 1], self.xT[:, dc, t0:t0 + n],
                         ALU.mult, ALU.add, r=[f"ps{bank}", f"MOD{l}", f"xT{bi}"], w=[f"xT{bi}"])

    def mixer_C(self, l):
        P, ps = self.P, self.ps
        m = P.mark()
        WC = P.alloc(8 * 416, BF16).rearrange("p (k n) -> p k n", k=8)
        self.load_w_cols(WC, self.w_in[l], 2576, 416, "WC")
        Wuq = P.alloc(2 * 384, BF16).rearrange("p (k n) -> p k n", k=2)
        self.load_w_cols(Wuq, self.mla_w_uq[l], 0, 384, "Wuq", kc=2)
        Wk = P.alloc(4 * 96, BF16).rearrange("p (h n) -> p h n", h=4)
        Wv = P.alloc(256, BF16).rearrange("p (h n) -> p h n", h=4)
        self.MEMSET("vector", Wk, 0.0, w=["Wk"])
        st, sk = self.stage()
        self.DMA(st[:, 0:512], self.mla_w_ukv[l], w=[sk])
        sv = st[:, 0:512].rearrange("p (h n) -> p h n", h=4)
        self.CP("gpsimd", Wk[:, :, 0:64], sv[:, :, 0:64], r=[sk], w=["Wk"])
        self.CP("gpsimd", Wv, sv[:, :, 64:128], r=[sk], w=["Wv"])
        g = P.alloc(8, F32)
        self.DMA(g[:, 0:2], self.mla_q_a_norm[l].rearrange("(c p) -> p c", p=128), w=["gains"])
        self.DMA(g[:, 2:3], self.mla_kv_a_norm[l].rearrange("(p o) -> p o", o=1), w=["gains"])
        self.DMA(g[0:96, 5:6], self.mla_q_norm[l].rearrange("(p o) -> p o", o=1), w=["gains"])
        self.DMA(g[0:96, 4:5], self.mla_k_norm[l].rearrange("(p o) -> p o", o=1), w=["gains"])
        self.TS("vector", g[0:96, 3:4], g[0:96, 5:6], 96.0 ** -0.5, ALU.mult, r=["gains"], w=["gains"])
        cqn = P.alloc(2 * T, BF16).rearrange("p (c t) -> p c t", c=2)
        ckvn = P.alloc(T, BF16)
        krT = P.alloc(T, BF16)
        W = self.norm_temps()
        for bi, (t0, n) in enumerate(BLKS):
            hk = f"hT{bi}"
            for o, (c0, npar) in enumerate([(0, 128), (128, 128), (256, 128), (384, 32)]):
                for k in range(8):
                    self.MM(ps[0:npar, o, 0:n], WC[:, k, c0:c0 + npar], self.hT[:, k, t0:t0 + n], start=(k == 0), stop=(k == 7), r=["WC", hk], w=[f"ps{o}"])
            for c in range(2):
                self.ACT(W["sq"][:, 0:n], ps[:, c, 0:n], AF.Square, r=[f"ps{c}"], w=["w_sq"])
                self.MM(ps[:, 5, 0:n], self.ones_b, W["sq"][:, 0:n], start=(c == 0), stop=(c == 1), r=["w_sq", "constb"], w=["ps5"])
            self.ACT(W["sd"][:, 0:n], ps[:, 5, 0:n], AF.Sqrt, scale=1.0 / 256, bias=self.eps, r=["ps5", "const2"], w=["w_sd"])
            self.RECIP(W["rs"][:, 0:n], W["sd"][:, 0:n], r=["w_sd"], w=["w_rs"])
            for c in range(2):
                self.STT(cqn[:, c, t0:t0 + n], ps[:, c, 0:n], g[:, c:c + 1], W["rs"][:, 0:n], ALU.mult, ALU.mult, r=[f"ps{c}", "w_rs", "gains"], w=[f"cqn{bi}"])
            self.ACT(W["sq"][:, 0:n], ps[:, 2, 0:n], AF.Square, r=["ps2"], w=["w_sq"])
            self.MM(ps[:, 5, 0:n], self.ones_b, W["sq"][:, 0:n], r=["w_sq", "constb"], w=["ps5"])
            self.ACT(W["sd"][:, 0:n], ps[:, 5, 0:n], AF.Sqrt, scale=1.0 / 128, bias=self.eps, r=["ps5", "const2"], w=["w_sd"])
            self.RECIP(W["rs"][:, 0:n], W["sd"][:, 0:n], r=["w_sd"], w=["w_rs"])
            self.STT(ckvn[:, t0:t0 + n], ps[:, 2, 0:n], g[:, 2:3], W["rs"][:, 0:n], ALU.mult, ALU.mult, r=["ps2", "w_rs", "gains"], w=[f"ckvn{bi}"])
            self.CP("scalar", krT[0:32, t0:t0 + n], ps[0:32, 3, 0:n], r=["ps3"], w=[f"krT{bi}"])
        allc = [f"cqn{b}" for b in range(5)]
        allkv = [f"ckvn{b}" for b in range(5)]
        qr = P.alloc(T, BF16)
        kr = P.alloc(T, BF16)
        Vh = P.alloc(NT * 64, BF16).rearrange("p (t d) -> p t d", d=64)
        mixh = P.alloc(T, BF16)
        WoC = P.alloc(1024, BF16)
        Eb = [P.alloc(512, BF16) for _ in range(3)]
        rd = P.alloc(512, F32)
        for h in range(4):
            self.load_cast(WoC[0:64, :], self.w_out[l, 768 + h * 64:768 + (h + 1) * 64, :], 1024, dkey="WoC", parts=64)
            for bi, (t0, n) in enumerate(BLKS):
                for k in range(2):
                    self.MM(ps[0:96, 0, 0:n], Wuq[:, k, h * 96:(h + 1) * 96], cqn[:, k, t0:t0 + n], start=(k == 0), stop=(k == 1), r=["Wuq", f"cqn{bi}"], w=["ps0"])
                self.head_norm_rope(ps[0:96, 0, 0:n], 96, n, qr[0:96, t0:t0 + n], self.ones_b, 1.0 / 96, g[0:96, 3:4], self.RCT_b, self.ropeC, t0, "ps0", f"qr{bi}", W)
                self.MM(ps[0:96, 1, 0:n], Wk[:, h, :], ckvn[:, t0:t0 + n], start=True, stop=False, r=["Wk", f"ckvn{bi}"], w=["ps1"])
                self.MM(ps[0:96, 1, 0:n], self.E32_b[0:32, 0:96], krT[0:32, t0:t0 + n], start=False, stop=True, r=["constb", f"krT{bi}"], w=["ps1"])
                self.head_norm_rope(ps[0:96, 1, 0:n], 96, n, kr[0:96, t0:t0 + n], self.ones_b, 1.0 / 96, g[0:96, 4:5], self.RCT_b, self.ropeC, t0, "ps1", f"kr{bi}", W)
            for ti in range(NT):
                bank = 2 + ti % 2
                self.MM(ps[:, bank, 0:64], ckvn[:, ti * 128:(ti + 1) * 128], Wv[:, h, :], r=allkv + ["Wv"], w=[f"ps{bank}"])
                self.CP("scalar", Vh[:, ti, :], ps[:, bank, 0:64], r=[f"ps{bank}"], w=["Vh"])
            allq = [f"qr{b}" for b in range(5)]
            allk = [f"kr{b}" for b in range(5)]
            for qi, (t0, n) in enumerate(BLKS):
                kts = [0, 1] if qi == 0 else list(range(NT))
                ob, db = (4, 5) if qi % 2 == 0 else (6, 7)
                for idx, kt in enumerate(kts):
                    sb = idx % 4
                    E, ek = Eb[idx % 3], f"E{idx % 3}"
                    self.MM(ps[:, sb, 0:n], kr[0:96, kt * 128:(kt + 1) * 128], qr[0:96, t0:t0 + n], r=allk + [f"qr{qi}"], w=[f"ps{sb}"])
                    self.ACT(E[:, 0:n], ps[:, sb, 0:n], AF.Exp, r=[f"ps{sb}"], w=[ek])
                    self.MM(ps[0:64, ob, 0:n], Vh[:, kt, :], E[:, 0:n], start=(idx == 0), stop=(idx == len(kts) - 1), r=["Vh", ek], w=[f"ps{ob}"])
                    self.MM(ps[0:64, db, 0:n], self.ones_b[:, 0:64], E[:, 0:n], start=(idx == 0), stop=(idx == len(kts) - 1), r=["constb", ek], w=[f"ps{db}"])
                self.RECIP(rd[0:64, 0:n], ps[0:64, db, 0:n], r=[f"ps{db}"], w=["rd"])
                self.TT("vector", mixh[0:64, t0:t0 + n], ps[0:64, ob, 0:n], rd[0:64, 0:n], ALU.mult, r=[f"ps{ob}", "rd"], w=[f"mixh{qi}"])
            if self.dbg and self.dbg_what == f"C{h}":
                self.dump(mixh[0:64, :], T, [f"mixh{b}" for b in range(5)], rows=64)
            self.out_proj_heads(l, WoC, mixh)
        P.barrier()
        P.release(m)

    def out_proj_heads(self, l, Wo, mixh):
        ps = self.ps
        MOD = self.MOD[l]
        cnt = 0
        for bi, (t0, n) in enumerate(BLKS):
            y = 1 if bi == 0 else 0
            for dc in range(8):
                bank = cnt % 4
                cnt += 1
                self.MM(ps[:, bank, 0:n], Wo[0:64, dc * 128:(dc + 1) * 128], mixh[0:64, t0:t0 + n], r=["WoC", f"mixh{bi}"], w=[f"ps{bank}"])
                self.STT(self.xT[:, dc, t0:t0 + n], ps[:, bank, 0:n], MOD[:, 16 + dc, y:y + 1], self.xT[:, dc, t0:t0 + n],
                         ALU.mult, ALU.add, r=[f"ps{bank}", f"MOD{l}", f"xT{bi}"], w=[f"xT{bi}"])

    def build(self, dbg_what=None):
        nc, P = self.nc, self.P
        self.dbg_what = dbg_what
        NB = 48800
        with nc.sbuf_tensor("arena", [128, NB], F32) as arena, nc.psum_tensor("ps", [128, 8, 512], F32) as ps:
            self.ps = ps
            P.set_arena(arena, NB * 4)
            self.setup()
            if dbg_what == "setup":
                self.dump(self.VP, 128, ["VP"])
                P.finish()
                return nc
            for s in range(self.nseq):
                self.load_x(s)
                if dbg_what == "loadx":
                    self.dump(self.xT[:, 0, :], T, [f"xT{b}" for b in range(5)])
                    P.finish()
                    return nc
                for l in range(self.nlayers):
                    self.mods(s, l)
                if dbg_what == "mods":
                    self.dump(self.MOD[0].rearrange("p j y -> p (j y)"), 96, ["MOD0"])
                    P.finish()
                    return nc
                for l in range(self.nlayers):
                    self.layer(s, l)
                    if self.stop:
                        break
                if self.stop:
                    break
                self.store_x(s)
            P.finish()
        return nc

    stop = False

    def layer(self, s, l):
        ph = self.phases
        allx = [f"xT{b}" for b in range(5)]
        allh = [f"hT{b}" for b in range(5)]
        self.norm_mod(l, self.A1[l], 0)
        if self.dbg_what == "h":
            self.dump(self.hT[:, 0, :], T, allh)
            self.stop = True
            return
        if "C" in ph:
            self.mixer_C(l)
        if "A" in ph:
            self.mixer_A(l)
        if "B" in ph:
            self.mixer_B(l)
        if self.dbg_what == "xa":
            self.dump(self.xT[:, 0, :], T, allx)
            self.stop = True
            return
        if "M" in ph:
            self.moe(l)
        if self.dbg_what == "xb":
            self.dump(self.xT[:, 0, :], T, allx)
            self.stop = True
            return


def make_inputs(inputs, core, consts):
    b0 = 2 * core
    f = lambda k: np.ascontiguousarray(inputs[k], dtype=np.float32)
    vp1 = np.concatenate([np.concatenate([f("b_mod")[l].reshape(48, 128), f("norm1")[l].reshape(8, 128), f("norm2")[l].reshape(8, 128)], 0) for l in range(2)], 0)
    vp2 = np.zeros((32, 128), np.float32)
    vp2[0:8] = f("c")[b0].reshape(8, 128)
    vp2[8:16] = f("c")[b0 + 1].reshape(8, 128)
    vp2[16:24] = f("c_ctx").reshape(8, 128)
    m = {"x2": f("x")[b0:b0 + 2], "ctx2": f("ctx")[b0:b0 + 2], "vp1": vp1, "vp2": vp2}
    for k in ["w_mod", "w_in", "w_out", "swa_q_norm", "swa_k_norm", "swa_sink", "gdn_conv", "gdn_out_norm", "mla_q_a_norm", "mla_w_uq",
              "mla_kv_a_norm", "mla_w_ukv", "mla_q_norm", "mla_k_norm", "router_w", "router_b", "exp_w_gu", "exp_b_gu", "exp_w_dn", "exp_b_dn"]:
        m[k] = f(k)
    m["gdn_a_log"] = f("gdn_a_log").reshape(2, 8)
    m["gdn_dt_bias"] = f("gdn_dt_bias").reshape(2, 8)
    m.update(consts)
    return m


def kernel(**inputs):
    k = K(nseq=2, nlayers=2, phases="CABM")
    nc = k.build()
    consts = host_consts()
    in_maps = [make_inputs(inputs, c, consts) for c in range(8)]
    res = run_bass_kernel_spmd(nc, in_maps, core_ids=list(range(8)))
    return np.concatenate([r["out"] for r in res.results], axis=0)


def _mixer_A(self, l):
    P, ps = self.P, self.ps
    m = P.mark()
    WA = P.alloc(8 * 512, BF16).rearrange("p (k n) -> p k n", k=8)
    for d0, c0, nc_ in [(0, 0, 64), (64, 128, 64), (128, 64, 64), (192, 192, 64), (256, 256, 256)]:
        self.load_w_cols(WA[:, :, d0:d0 + nc_], self.w_in[l], c0, nc_, "WA")
    g = P.alloc(8, F32)
    for hh in range(2):
        self.DMA(g[hh * 64:(hh + 1) * 64, 2:3], self.swa_q_norm[l].rearrange("(p o) -> p o", o=1), w=["gains"])
        self.DMA(g[hh * 64:(hh + 1) * 64, 1:2], self.swa_k_norm[l].rearrange("(p o) -> p o", o=1), w=["gains"])
    self.TS("vector", g[:, 0:1], g[:, 2:3], 0.125, ALU.mult, r=["gains"], w=["gains"])
    sk = P.alloc(4, F32)
    self.DMA(sk, self.swa_sink[l].rearrange("(a h) -> a h", a=1).broadcast_to([128, 4]), w=["sk0"])
    self.ACT(sk, sk, AF.Exp, r=["sk0"], w=["sk"])
    qr = P.alloc(2 * T, BF16).rearrange("p (c t) -> p c t", c=2)
    kr = P.alloc(T, BF16)
    V = P.alloc(NT * 128, BF16).rearrange("p (t d) -> p t d", d=128)
    W = self.norm_temps()
    for bi, (t0, n) in enumerate(BLKS):
        for o, c0 in enumerate([0, 128, 256]):
            for k in range(8):
                self.MM(ps[:, o, 0:n], WA[:, k, c0:c0 + 128], self.hT[:, k, t0:t0 + n], start=(k == 0), stop=(k == 7), r=["WA", f"hT{bi}"], w=[f"ps{o}"])
            dst = kr[:, t0:t0 + n] if o == 2 else qr[:, o, t0:t0 + n]
            self.head_norm_rope(ps[:, o, 0:n], 128, n, dst, self.bd64_b, 1.0 / 64, g[:, (1 if o == 2 else 0):(2 if o == 2 else 1)], self.RAT_b, self.ropeA, t0, f"ps{o}", "qkA", W)
    for ti in range(NT):
        bank = 3 + ti % 2
        for k in range(8):
            self.MM(ps[:, bank, 0:128], self.hT[:, k, ti * 128:(ti + 1) * 128], WA[:, k, 384:512], start=(k == 0), stop=(k == 7), r=["WA", f"hT{tile_blk(ti)}"], w=[f"ps{bank}"])
        self.CP("scalar", V[:, ti, :], ps[:, bank, 0:128], r=[f"ps{bank}"], w=["VA"])
    mixg = P.alloc(2 * T, BF16).rearrange("p (c t) -> p c t", c=2)
    WoA = P.alloc(2 * 1024, BF16).rearrange("p (c n) -> p c n", c=2)
    Eb = [P.alloc(5 * 256, BF16).rearrange("p (k n) -> p k n", k=5) for _ in range(2)]
    dn = P.alloc(256, F32)
    for gi in range(2):
        gp = slice(gi * 64, (gi + 1) * 64)
        for j in range(2):
            hd = 2 * gi + j
            self.load_cast(WoA[0:64, j, :], self.w_out[l, hd * 64:(hd + 1) * 64, :], 1024, dkey="WoA", parts=64)
        for ti in range(NT):
            if ti < 2:
                kts = [(0, None), (1, None)]
            else:
                kts = ([(ti - 1, self.L)] if ti - 1 >= 2 else []) + [(ti, None)] + ([(ti + 1, self.U)] if ti + 1 < NT else []) + [(0, None), (1, None)]
            b0 = 0 if ti % 2 == 0 else 3
            E, ek = Eb[ti % 2], f"EA{ti % 2}"
            qrhs = qr[gp, :, ti * 128:(ti + 1) * 128]
            nk = len(kts)
            for idx, (kt, mk) in enumerate(kts):
                bank = b0 + idx // 2
                c0 = (idx % 2) * 256
                self.MM(ps[:, bank, c0:c0 + 256].rearrange("p (j t) -> p j t", j=2), kr[gp, kt * 128:(kt + 1) * 128], qrhs, r=["qkA"], w=[f"ps{bank}"])
            psflat = ps[:, b0:b0 + 3, :].rearrange("p b n -> p (b n)")
            self.ACT(E[:, 0:nk, :].rearrange("p k n -> p (k n)"), psflat[:, 0:nk * 256], AF.Exp, r=[f"ps{b0}", f"ps{b0 + 1}", f"ps{b0 + 2}"], w=[ek])
            for idx, (kt, mk) in enumerate(kts):
                if mk is not None:
                    ev = E[:, idx, :].rearrange("p (j t) -> p j t", j=2)
                    self.TT("gpsimd", ev, ev, mk.rearrange("p (a t) -> p a t", a=1).broadcast_to([128, 2, 128]), ALU.mult, r=[ek, "const"], w=[ek])
            for idx, (kt, mk) in enumerate(kts):
                self.MM(ps[0:64, 6, 0:256], V[:, kt, gi * 64:(gi + 1) * 64], E[:, idx, :], start=(idx == 0), stop=(idx == nk - 1), r=["VA", ek], w=["ps6"])
            for idx, (kt, mk) in enumerate(kts):
                self.MM(ps[0:64, 7, 0:256], self.ones_b[:, 0:64], E[:, idx, :], start=(idx == 0), stop=(idx == nk - 1), r=["constb", ek], w=["ps7"])
            for j in range(2):
                self.TS("vector", dn[0:64, j * 128:(j + 1) * 128], ps[0:64, 7, j * 128:(j + 1) * 128], sk[0:64, 2 * gi + j:2 * gi + j + 1], ALU.add, r=["ps7", "sk"], w=["dnA"])
            self.RECIP(dn[0:64, :], dn[0:64, :], r=["dnA"], w=["dnA"])
            self.TT("vector", mixg[0:64, :, ti * 128:(ti + 1) * 128], ps[0:64, 6, 0:256].rearrange("p (j t) -> p j t", j=2),
                    dn[0:64, :].rearrange("p (j t) -> p j t", j=2), ALU.mult, r=["ps6", "dnA"], w=["mixg"])
        if self.dbg and self.dbg_what == f"A{gi}":
            self.dump(mixg[0:64, 0, :], T, ["mixg"], rows=64)
        self.out_proj(l, WoA, 64, mixg, "mixg", "WoA", nacc=2)
    P.barrier()
    P.release(m)


K.mixer_A = _mixer_A


def _mixer_B(self, l):
    P, ps = self.P, self.ps
    m = P.mark()
    psq = ps.rearrange("p b (q n) -> p (b q) n", q=4)
    allh = [f"hT{b}" for b in range(5)]
    Wab = P.alloc(8 * 16, BF16).rearrange("p (k n) -> p k n", k=8)
    self.load_w_cols(Wab, self.w_in[l], 2560, 16, "Wab")
    for ti in range(NT):
        for k in range(8):
            self.MM(ps[:, 0, ti * 16:(ti + 1) * 16], self.hT[:, k, ti * 128:(ti + 1) * 128], Wab[:, k, :], start=(k == 0), stop=(k == 7), r=["Wab"] + allh, w=["ps0"])
    abv = ps[:, 0, 0:NT * 16].rearrange("p (t j) -> p t j", j=16)
    one = P.alloc(1, F32)
    self.MEMSET("vector", one, 1.0, w=["one"])
    arr = lambda: P.alloc(NT * 8, F32).rearrange("p (t j) -> p t j", j=8)
    dtb, alog, X, AX, G, BETA, GC, GCL, BEG, KD = [arr() for _ in range(10)]
    self.DMA(dtb, self.gdn_dt_bias[l].rearrange("(a b j) -> a b j", a=1, b=1).broadcast_to([128, NT, 8]), w=["dtb"])
    self.DMA(alog, self.gdn_a_log[l].rearrange("(a b j) -> a b j", a=1, b=1).broadcast_to([128, NT, 8]), w=["alog"])
    self.TT("vector", X, abv[:, :, 0:8], dtb, ALU.add, r=["ps0", "dtb"], w=["gX"])
    self.TS("vector", AX, X, -1.0, ALU.mult, r=["gX"], w=["gAX"])
    self.TT("vector", AX, AX, X, ALU.max, r=["gX", "gAX"], w=["gAX"])
    self.ACT(AX, AX, AF.Exp, scale=-1.0, r=["gAX"], w=["gAX"])
    self.ACT(AX, AX, AF.Ln, bias=one, r=["gAX", "one"], w=["gAX"])
    self.STT(X, X, 0.0, AX, ALU.max, ALU.add, r=["gX", "gAX"], w=["gX"])
    self.ACT(alog, alog, AF.Exp, r=["alog"], w=["alog"])
    self.TS("vector", alog, alog, -1.0, ALU.mult, r=["alog"], w=["alog"])
    self.TT("vector", G, X, alog, ALU.mult, r=["gX", "alog"], w=["gG"])
    self.ACT(BETA, abv[:, :, 8:16], AF.Sigmoid, r=["ps0"], w=["gB"])
    if self.dbg_what == "BG3a":
        P.barrier()
        self.dump(G.rearrange("p t j -> p (t j)"), NT * 8, ["gG"], col0=256)
        self.dump(BETA.rearrange("p t j -> p (t j)"), NT * 8, ["gB"], col0=768)
        P.release(m)
        return
    for ti in range(NT):
        self.MM(ps[:, 1, ti * 8:ti * 8 + 4], self.U, G[:, ti, 0:4], r=["gG", "const"], w=["ps1"])
        self.MM(ps[:, 1, ti * 8 + 4:ti * 8 + 8], self.L, G[:, ti, 4:8], r=["gG", "const"], w=["ps1"])
        self.MM(ps[:, 2, ti * 8:ti * 8 + 8], self.ones, G[:, ti, :], r=["gG", "const"], w=["ps2"])
    self.CP("vector", GC, ps[:, 1, 0:NT * 8].rearrange("p (t j) -> p t j", j=8), r=["ps1"], w=["gGC"])
    self.CP("vector", GCL, ps[:, 2, 0:NT * 8].rearrange("p (t j) -> p t j", j=8), r=["ps2"], w=["gGCL"])
    if self.dbg_what == "BG3b":
        P.barrier()
        self.dump(GC.rearrange("p t j -> p (t j)"), NT * 8, ["gGC"], col0=256)
        self.dump(GCL.rearrange("p t j -> p (t j)"), NT * 8, ["gGCL"], col0=768)
        P.release(m)
        return
    self.ACT(BEG, GC, AF.Exp, r=["gGC"], w=["gBEG"])
    self.TT("vector", BEG, BEG, BETA, ALU.mult, r=["gBEG", "gB"], w=["gBEG"])
    self.TT("vector", KD, GCL, GC, ALU.subtract, r=["gGC", "gGCL"], w=["gKD"])
    self.ACT(KD, KD, AF.Exp, r=["gKD"], w=["gKD"])
    self.ACT(GCL, GCL, AF.Exp, r=["gGCL", "gKD"], w=["gGCL"])
    P.barrier()
    if self.dbg_what == "BG3":
        self.dump(KD.rearrange("p t j -> p (t j)"), NT * 8, ["gKD"])
        self.dump(G.rearrange("p t j -> p (t j)"), NT * 8, ["gG"], col0=256)
        self.dump(GC.rearrange("p t j -> p (t j)"), NT * 8, ["gGC"], col0=512)
        self.dump(BETA.rearrange("p t j -> p (t j)"), NT * 8, ["gB"], col0=768)
        P.release(m)
        return
    gains = P.alloc(2, F32)
    self.DMA(gains[:, 0:1], self.gdn_out_norm[l].rearrange("(p o) -> p o", o=1), w=["gains"])
    WG = P.alloc(8 * 128, BF16).rearrange("p (k n) -> p k n", k=8)
    raw = P.alloc(T, BF16)
    kT = P.alloc(T, F32)
    qT = P.alloc(T, BF16)
    vT = P.alloc(T, BF16)
    Oacc = P.alloc(T, BF16)
    cw = P.alloc(16, F32)
    y = P.alloc(512, F32)
    ys = P.alloc(512, F32)
    sqb = P.alloc(512, BF16)
    Wo = P.alloc(1024, BF16)
    names = ["qf", "vf", "Gtri", "t1", "t2", "e2m", "QKDT", "N", "M", "eg", "qdec", "kbg", "kdec", "bv", "R0", "R1", "N2", "M2", "U_", "WT", "VN", "S0", "S1"]
    Bs = [{nm: P.alloc(128, F32) for nm in names} for _ in range(2)]
    allraw = [f"raw{b}" for b in range(5)]
    for h in range(4):
        for Xi, (dstT, colbase) in enumerate([(qT, 512), (kT, 1024), (vT, 1536)]):
            self.load_w_cols(WG, self.w_in[l], colbase + h * 128, 128, "WG")
            self.DMA(cw[:, Xi * 5:(Xi + 1) * 5], self.gdn_conv[l, :, Xi * 512 + h * 128:Xi * 512 + (h + 1) * 128].rearrange("w p -> p w"), w=["cw"])
            for bi, (t0, n) in enumerate(BLKS):
                bank = bi % 2
                for k in range(8):
                    self.MM(ps[:, bank, 0:n], WG[:, k, :], self.hT[:, k, t0:t0 + n], start=(k == 0), stop=(k == 7), r=["WG", f"hT{bi}"], w=[f"pq{bank * 4}"])
                self.CP("scalar", raw[:, t0:t0 + n], ps[:, bank, 0:n], r=[f"pq{bank * 4}"], w=[f"raw{bi}"])
            for bi, (t0, n) in enumerate(BLKS):
                s0, s1 = (0, CTX) if bi == 0 else (CTX, T)
                self.TS("vector", y[:, 0:n], raw[:, t0:t0 + n], cw[:, Xi * 5 + 2:Xi * 5 + 3], ALU.mult, r=allraw + ["cw"], w=["cy"])
                for j in (0, 1, 3, 4):
                    a, b = max(t0, s0 - (j - 2)), min(t0 + n, s1 - (j - 2))
                    self.STT(y[:, a - t0:b - t0], raw[:, a + j - 2:b + j - 2], cw[:, Xi * 5 + j:Xi * 5 + j + 1], y[:, a - t0:b - t0], ALU.mult, ALU.add, r=allraw + ["cw", "cy"], w=["cy"])
                if Xi == 2:
                    self.ACT(vT[:, t0:t0 + n], y[:, 0:n], AF.Silu, r=["cy"], w=["gv"])
                    continue
                self.ACT(ys[:, 0:n], y[:, 0:n], AF.Silu, r=["cy"], w=["cys"])
                self.ACT(sqb[:, 0:n], ys[:, 0:n], AF.Square, r=["cys"], w=["csq"])
                self.MM(ps[:, 2, 0:n], self.ones_b, sqb[:, 0:n], r=["csq", "constb"], w=["pq8"])
                self.ACT(y[:, 0:n], ps[:, 2, 0:n], AF.Sqrt, bias=self.eps, r=["pq8", "const2", "cy"], w=["cy"])
                self.RECIP(y[:, 0:n], y[:, 0:n], r=["cy"], w=["cy"])
                self.STT(dstT[:, t0:t0 + n], ys[:, 0:n], (128.0 ** -0.5 if Xi == 0 else 1.0), y[:, 0:n], ALU.mult, ALU.mult, r=["cys", "cy"], w=["gq" if Xi == 0 else "gk"])
        P.barrier()
        if self.dbg_what == "Bproj":
            self.dump(kT, T, ["gk"])
            P.release(m)
            return
        order = [list(range(NT)), [1, 0] + list(range(NT - 1, 1, -1))]
        Scur = [Bs[0]["S0"], Bs[1]["S0"]]
        Snxt = [Bs[0]["S1"], Bs[1]["S1"]]
        for d in range(2):
            self.MEMSET("vector", Scur[d], 0.0, w=[f"S{d}"])
        touched = set()
        for step in range(NT):
            bufs = []
            for d in range(2):
                P.rec = []
                bufs.append(P.rec)
                ti = order[d][step]
                B = Bs[d]
                j = d * 4 + h
                q0 = d * 16
                cs = slice(ti * 128, (ti + 1) * 128)
                kt = kT[:, cs]
                Tri = self.U if d == 0 else self.L
                MS = self.SL if d == 0 else self.SU
                MIT = self.U if d == 0 else self.L
                gcol, gccol, bcol = G[:, ti, j:j + 1], GC[:, ti, j:j + 1], BETA[:, ti, j:j + 1]
                K_ = lambda nm: f"b{d}{nm}"
                SLT = lambda i: psq[:, q0 + i, :]
                SK = lambda i: f"pq{q0 + i}"
                self.CP("gpsimd", B["qf"], qT[:, cs], r=["gq"], w=[K_("qf")])
                self.CP("gpsimd", B["vf"], vT[:, cs], r=["gv"], w=[K_("vf")])
                self.MM(SLT(0), kt, kt, r=["gk"], w=[SK(0)])
                self.MM(SLT(1), kt, B["qf"], r=["gk", K_("qf")], w=[SK(1)])
                self.TS("vector", B["Gtri"], Tri, gcol, ALU.mult, r=["const", "gG"], w=[K_("Gtri")])
                self.MM(SLT(2), self.ones, B["Gtri"], r=["const", K_("Gtri")], w=[SK(2)])
                self.TS("vector", B["t1"], SLT(2), gccol, ALU.subtract, 0.0, ALU.max, r=[SK(2), "gGC"], w=[K_("t1")])
                self.ACT(B["t1"], B["t1"], AF.Exp, scale=-1.0, r=[K_("t1")], w=[K_("t1")])
                self.TT("gpsimd", B["t1"], B["t1"], MS, ALU.mult, r=[K_("t1"), "const"], w=[K_("t1")])
                self.TS("vector", B["t2"], SLT(2), gccol, ALU.subtract, 0.0, ALU.min, r=[SK(2), "gGC"], w=[K_("t2")])
                self.ACT(B["t2"], B["t2"], AF.Exp, r=[K_("t2")], w=[K_("t2")])
                self.TT("gpsimd", B["e2m"], B["t2"], MIT, ALU.mult, r=[K_("t2"), "const"], w=[K_("e2m")])
                self.TT("vector", B["QKDT"], SLT(1), B["e2m"], ALU.mult, r=[SK(1), K_("e2m")], w=[K_("QKDT")])
                self.STT(B["N"], SLT(0), bcol, B["t1"], ALU.mult, ALU.mult, r=[SK(0), "gB", K_("t1")], w=[K_("N")])
                self.TR(SLT(3), B["N"], r=[K_("N")], w=[SK(3)])
                self.CP("scalar", B["M"], SLT(3), r=[SK(3)], w=[K_("M")])
                self.ACT(B["eg"], SLT(2), AF.Exp, r=[SK(2)], w=[K_("eg")])
                self.TT("vector", B["qdec"], B["qf"], B["eg"], ALU.mult, r=[K_("qf"), K_("eg")], w=[K_("qdec")])
                self.TR(SLT(4), kt, r=["gk"], w=[SK(4)])
                self.TR(SLT(5), B["vf"], r=[K_("vf")], w=[SK(5)])
                self.TS("vector", B["kbg"], SLT(4), BEG[:, ti, j:j + 1], ALU.mult, r=[SK(4), "gBEG"], w=[K_("kbg")])
                self.TS("vector", B["kdec"], SLT(4), KD[:, ti, j:j + 1], ALU.mult, r=[SK(4), "gKD"], w=[K_("kdec")])
                self.TS("vector", B["bv"], SLT(5), bcol, ALU.mult, r=[SK(5), "gB"], w=[K_("bv")])
                self.TT("vector", B["R0"], self.ident, B["M"], ALU.subtract, r=["const", K_("M")], w=[K_("R0")])
                Nc, Mc, Rc = ("N", "M", "R0")
                Nn, Mn, Rn = ("N2", "M2", "R1")
                for lev in range(1, 7):
                    self.MM(SLT(6), B[Mc], B[Nc], r=[K_(Mc), K_(Nc)], w=[SK(6)])
                    self.CP("vector", B[Nn], SLT(6), r=[SK(6)], w=[K_(Nn)])
                    if lev < 6:
                        self.MM(SLT(7), B[Nc], B[Mc], r=[K_(Mc), K_(Nc)], w=[SK(7)])
                        self.CP("scalar", B[Mn], SLT(7), r=[SK(7)], w=[K_(Mn)])
                    self.MM(SLT(8), B[Nn], B[Rc], r=[K_(Nn), K_(Rc)], w=[SK(8)])
                    self.TT("vector", B[Rn], SLT(8), B[Rc], ALU.add, r=[SK(8), K_(Rc)], w=[K_(Rn)])
                    Nc, Nn = Nn, Nc
                    Mc, Mn = Mn, Mc
                    Rc, Rn = Rn, Rc
                TTk = Rc
                self.MM(SLT(9), B[TTk], B["bv"], r=[K_(TTk), K_("bv")], w=[SK(9)])
                self.CP("scalar", B["U_"], SLT(9), r=[SK(9)], w=[K_("U_")])
                self.MM(SLT(10), B["kbg"], B[TTk], r=[K_(TTk), K_("kbg")], w=[SK(10)])
                self.CP("vector", B["WT"], SLT(10), r=[SK(10)], w=[K_("WT")])
                S_, Sn_ = Scur[d], Snxt[d]
                self.MM(SLT(11), B["WT"], S_, r=[K_("WT"), f"S{d}"], w=[SK(11)])
                self.TT("vector", B["VN"], B["U_"], SLT(11), ALU.subtract, r=[K_("U_"), SK(11)], w=[K_("VN")])
                self.MM(SLT(12), S_, B["qdec"], start=True, stop=False, r=[f"S{d}", K_("qdec")], w=[SK(12)])
                self.MM(SLT(12), B["VN"], B["QKDT"], start=False, stop=True, r=[K_("VN"), K_("QKDT")], w=[SK(12)])
                if ti not in touched:
                    touched.add(ti)
                    self.CP("scalar", Oacc[:, cs], SLT(12), r=[SK(12)], w=[f"Oacc{ti}"])
                else:
                    self.TT("vector", Oacc[:, cs], SLT(12), Oacc[:, cs], ALU.add, r=[SK(12), f"Oacc{ti}"], w=[f"Oacc{ti}"])
                self.MM(SLT(13), B["kdec"], B["VN"], r=[K_("kdec"), K_("VN")], w=[SK(13)])
                self.STT(Sn_, S_, GCL[:, ti, j:j + 1], SLT(13), ALU.mult, ALU.add, r=[f"S{d}", "gGCL", SK(13)], w=[f"S{d}"])
                Scur[d], Snxt[d] = Sn_, S_
            P.replay_interleaved(bufs)
        P.barrier()
        self.load_w_cols(WG, self.w_in[l], 2048 + h * 128, 128, "WG")
        self.load_cast(Wo, self.w_out[l, 256 + h * 128:256 + (h + 1) * 128, :], 1024, dkey="WoB")
        MOD = self.MOD[l]
        for bi, (t0, n) in enumerate(BLKS):
            yy = 1 if bi == 0 else 0
            oa = Oacc[:, t0:t0 + n]
            self.ACT(sqb[:, 0:n], oa, AF.Square, w=["csq"])
            self.MM(ps[:, 2, 0:n], self.ones_b, sqb[:, 0:n], r=["csq", "constb"], w=["pq8"])
            self.ACT(y[:, 0:n], ps[:, 2, 0:n], AF.Sqrt, scale=1.0 / 128, bias=self.eps, r=["pq8", "const2"], w=["cy"])
            self.RECIP(y[:, 0:n], y[:, 0:n], r=["cy"], w=["cy"])
            self.STT(ys[:, 0:n], oa, gains[:, 0:1], y[:, 0:n], ALU.mult, ALU.mult, r=["cy", "gains"], w=["cys"])
            for k in range(8):
                self.MM(ps[:, 3, 0:n], WG[:, k, :], self.hT[:, k, t0:t0 + n], start=(k == 0), stop=(k == 7), r=["WG", f"hT{bi}"], w=["pq12"])
            self.ACT(y[:, 0:n], ps[:, 3, 0:n], AF.Silu, r=["pq12", "cy"], w=["cy"])
            self.TT("vector", sqb[:, 0:n], ys[:, 0:n], y[:, 0:n], ALU.mult, r=["cys", "cy"], w=["csq"])
            if self.dbg and self.dbg_what == f"B{h}":
                self.dump(sqb[:, 0:n], n, ["csq"], col0=t0)
            for dc in range(8):
                bank = 4 + dc % 4
                self.MM(ps[:, bank, 0:n], Wo[:, dc * 128:(dc + 1) * 128], sqb[:, 0:n], r=["WoB", "csq"], w=[f"pq{bank * 4}"])
                self.STT(self.xT[:, dc, t0:t0 + n], ps[:, bank, 0:n], MOD[:, 16 + dc, yy:yy + 1], self.xT[:, dc, t0:t0 + n],
                         ALU.mult, ALU.add, r=[f"pq{bank * 4}", f"MOD{l}", f"xT{bi}"], w=[f"xT{bi}"])
        P.barrier()
    P.release(m)


K.mixer_B = _mixer_B


def _moe(self, l):
    P, ps = self.P, self.ps
    m0 = P.mark()
    gateT = P.alloc(T, BF16)
    m = P.mark()
    RW = P.alloc(8 * 32, F32).rearrange("p (k e) -> p k e", k=8)
    self.DMA(RW, self.router_w[l].rearrange("(k p) e -> p k e", p=128), w=["RW"])
    rb = P.alloc(32, F32)
    self.DMA(rb, self.router_b[l].rearrange("(a e) -> a e", a=1).broadcast_to([128, 32]), w=["rb"])
    h2f = P.alloc(8 * 512, F32).rearrange("p (c t) -> p c t", c=8)
    lg, msk, ex = P.alloc(32, F32), P.alloc(32, F32), P.alloc(32, F32)
    top8, dn = P.alloc(8, F32), P.alloc(1, F32)

    def router_fn(bi, t0, n):
        for tt in range(n // 128):
            cs = slice(tt * 128, (tt + 1) * 128)
            for c in range(8):
                self.MM(ps[:, 1, 0:32], h2f[:, c, cs], RW[:, c, :], start=(c == 0), stop=(c == 7), r=[f"h2f{c}", "RW"], w=["ps1"])
            self.TT("vector", lg, ps[:, 1, 0:32], rb, ALU.add, r=["ps1", "rb"], w=["lg"])
            self.P.op("vector", lambda e: e.max(out=top8, in_=lg), ["lg"], ["top8"])
            self.TS("vector", msk, lg, top8[:, 3:4], ALU.is_ge, r=["lg", "top8"], w=["msk"])
            self.TS("vector", ex, lg, top8[:, 0:1], ALU.subtract, r=["lg", "top8"], w=["ex"])
            self.ACT(ex, ex, AF.Exp, r=["ex"], w=["ex"])
            self.TT("vector", ex, ex, msk, ALU.mult, r=["ex", "msk"], w=["ex"])
            self.P.op("vector", lambda e: e.reduce_sum(out=dn, in_=ex, axis=mybir.AxisListType.X), ["ex"], ["dn"])
            self.RECIP(dn, dn, r=["dn"], w=["dn"])
            self.TS("vector", ex, ex, dn[:, 0:1], ALU.mult, r=["ex", "dn"], w=["ex"])
            self.TR(ps[0:32, 2, 0:128], ex, r=["ex"], w=["ps2"])
            self.CP("scalar", gateT[0:32, t0 + tt * 128:t0 + (tt + 1) * 128], ps[0:32, 2, 0:128], r=["ps2"], w=["gateT"])

    self.norm_mod(l, self.A2[l], 24, router=dict(h2f=h2f, fn=router_fn))
    P.release(m)
    if self.dbg_what == "gate":
        self.dump(gateT[0:32, :], T, ["gateT"], rows=32)
        P.release(m0)
        return
    bsb = P.alloc(2048, F32)
    self.DMA(bsb[0:32, :], self.exp_b_gu[l], w=["bsb"])
    bv = bsb[0:32, :].rearrange("e (j p s) -> e j s p", j=8, s=2)
    for j in range(8):
        for s_ in range(2):
            self.TR(ps[:, 3, (j * 2 + s_) * 32:(j * 2 + s_ + 1) * 32], bv[:, j, s_, :], r=["bsb"], w=["ps3"])
    bguT = P.alloc(512, F32).rearrange("p (a e) -> p a e", e=32)
    self.CP("vector", bguT, ps[:, 3, :].rearrange("p (a e) -> p a e", e=32), r=["ps3"], w=["bguT"])
    bdn = P.alloc(1024, BF16)
    self.load_cast(bdn[0:32, :], self.exp_b_dn[l], 1024, dkey="bdn", parts=32)
    MOD = self.MOD[l]
    for bi, (t0, n) in enumerate(BLKS):
        yy = 1 if bi == 0 else 0
        for dc in range(8):
            bank = 4 + dc % 2
            self.MM(ps[:, bank, 0:n], bdn[0:32, dc * 128:(dc + 1) * 128], gateT[0:32, t0:t0 + n], r=["bdn", "gateT"], w=[f"ps{bank}"])
            self.STT(self.xT[:, dc, t0:t0 + n], ps[:, bank, 0:n], MOD[:, 40 + dc, yy:yy + 1], self.xT[:, dc, t0:t0 + n],
                     ALU.mult, ALU.add, r=[f"ps{bank}", f"MOD{l}", f"xT{bi}"], w=[f"xT{bi}"])
    Wgu = [P.alloc(8 * 512, BF16).rearrange("p (k s f) -> p k s f", k=8, s=2) for _ in range(2)]
    Wdn = [P.alloc(2 * 1024, BF16).rearrange("p (j n) -> p j n", j=2) for _ in range(2)]
    act = [P.alloc(2 * 512, BF16).rearrange("p (j n) -> p j n", j=2) for _ in range(2)]
    gsel = P.alloc(512, BF16)
    tg = [[P.alloc(512, BF16) for _ in range(2)] for _ in range(2)]
    tsg = [[P.alloc(512, BF16) for _ in range(2)] for _ in range(2)]
    tl = [[P.alloc(512, BF16) for _ in range(2)] for _ in range(2)]
    bl1 = P.alloc(8 * 32, F32).rearrange("p (j e) -> p j e", e=32)
    self.TS("vector", bl1, bguT.rearrange("p (j s) e -> p j s e", s=2)[:, :, 1, :], 1.0, ALU.add, r=["bguT"], w=["bl1"])
    nexp = NEXP if self.moe_experts is None else self.moe_experts
    units = [(e_, q) for e_ in range(nexp) for q in range(4)]

    def load_unit(ui):
        e_, q = units[ui]
        wi = ui % 2
        for k0 in range(0, 8, 2):
            st, sk = self.stage()
            sv = st[:, 0:1024].rearrange("p (k n) -> p k n", k=2)
            self.DMA(sv, self.exp_w_gu[l, e_, k0 * 128:(k0 + 2) * 128, q * 512:(q + 1) * 512].rearrange("(k p) n -> p k n", p=128), w=[sk])
            self.CP("gpsimd", Wgu[wi][:, k0:k0 + 2, :, :], sv.rearrange("p k (f s) -> p k s f", s=2), r=[sk], w=[f"Wgu{wi}"])
        for jj in range(2):
            st, sk = self.stage()
            self.DMA(st[:, 0:1024], self.exp_w_dn[l, e_, q * 256 + jj * 128:q * 256 + (jj + 1) * 128, :], w=[sk])
            self.CP("scalar", Wdn[wi][:, jj, :], st[:, 0:1024], r=[sk], w=[f"Wdn{wi}"])

    def gu_swiglu(ui, bi, ai):
        e_, q = units[ui]
        wi = ui % 2
        t0, n = BLKS[bi]
        wgk = f"Wgu{wi}"
        self.TS("vector", gsel[0:32, 0:n], gateT[0:32, t0:t0 + n], self.ident[0:32, e_:e_ + 1], ALU.mult, r=["gateT", "const"], w=["gsel"])
        self.MM(ps[:, 6, 0:n], self.ones_b[0:32, :], gsel[0:32, 0:n], r=["gsel", "constb"], w=["ps6"])
        for jj in range(2):
            bg_, bl_ = 2 * jj, 2 * jj + 1
            for s_ in range(2):
                for k in range(8):
                    self.MM(ps[:, 2 * jj + s_, 0:n], Wgu[wi][:, k, s_, jj * 128:(jj + 1) * 128], self.hT[:, k, t0:t0 + n],
                            start=(k == 0), stop=(k == 7), r=[wgk, f"hT{bi}"], w=[f"ps{2 * jj + s_}"])
            fj = 2 * q + jj
            G_, S_, L_ = tg[ai][jj], tsg[ai][jj], tl[ai][jj]
            kk = f"{ai}{jj}"
            self.TS("vector", G_[:, 0:n], ps[:, bg_, 0:n], bguT[:, fj * 2, e_:e_ + 1], ALU.add, 7.0, ALU.min, r=[f"ps{bg_}", "bguT"], w=["tg" + kk])
            self.ACT(S_[:, 0:n], G_[:, 0:n], AF.Sigmoid, scale=1.702, r=["tg" + kk], w=["tsg" + kk])
            self.TS("vector", L_[:, 0:n], ps[:, bl_, 0:n], bl1[:, fj, e_:e_ + 1], ALU.add, 8.0, ALU.min, r=[f"ps{bl_}", "bl1"], w=["tl" + kk])
            self.STT(L_[:, 0:n], L_[:, 0:n], -6.0, ps[:, 6, 0:n], ALU.max, ALU.mult, r=["tl" + kk, "ps6"], w=["tl" + kk])
            self.TT("gpsimd", G_[:, 0:n], G_[:, 0:n], S_[:, 0:n], ALU.mult, r=["tg" + kk, "tsg" + kk], w=["tg" + kk])
            self.TT("gpsimd", act[ai][:, jj, 0:n], G_[:, 0:n], L_[:, 0:n], ALU.mult, r=["tg" + kk, "tl" + kk], w=[f"act{ai}"])

    def dn_update(ui, bi, ai):
        wi = ui % 2
        t0, n = BLKS[bi]
        yy = 1 if bi == 0 else 0
        for dc in range(8):
            bank = 4 + dc % 2
            for jj in range(2):
                self.MM(ps[:, bank, 0:n], Wdn[wi][:, jj, dc * 128:(dc + 1) * 128], act[ai][:, jj, 0:n], start=(jj == 0), stop=(jj == 1), r=[f"Wdn{wi}", f"act{ai}"], w=[f"ps{bank}"])
            self.STT(self.xT[:, dc, t0:t0 + n], ps[:, bank, 0:n], MOD[:, 40 + dc, yy:yy + 1], self.xT[:, dc, t0:t0 + n],
                     ALU.mult, ALU.add, r=[f"ps{bank}", f"MOD{l}", f"xT{bi}"], w=[f"xT{bi}"])

    load_unit(0)
    prev = None
    step = 0
    blist = list(range(len(BLKS)))
    if l == self.nlayers - 1 and self.dbg_what is None:
        blist = blist[1:]
    for ui in range(len(units)):
        for bi in blist:
            ai = step % 2
            step += 1
            gu_swiglu(ui, bi, ai)
            if bi == blist[1] and ui + 1 < len(units):
                load_unit(ui + 1)
            if prev is not None:
                dn_update(*prev)
            prev = (ui, bi, ai)
    dn_update(*prev)
    P.barrier()
    P.release(m0)


K.moe = _moe
K.moe_experts = None
```

```python
import contextlib
import numpy as np
import concourse.bass as bass
import concourse.mybir as mybir
from concourse.bass_utils import run_bass_kernel_spmd

F32 = mybir.dt.float32
BF16 = mybir.dt.bfloat16
AF = mybir.ActivationFunctionType
ALU = mybir.AluOpType

ENG = ["tensor", "vector", "scalar", "gpsimd", "sync"]
SEM_ROLL = 30000
NDSEM = 24


class Prog:
    def __init__(self, nc):
        self.nc = nc
        self.ops = {e: [] for e in ENG}
        self.cnt = {e: 0 for e in ENG}
        self.seen = {e: {p: 0 for p in ENG} for e in ENG}
        self.dseen = {e: set() for e in ENG}
        self.res = {}
        self.ndma = 0
        self.dma_issuer = {}
        self.arena = None
        self.off = 0
        self.peak = 0

    def set_arena(self, arena, nbytes):
        self.arena = arena
        self.arena_bytes = nbytes
        self.off = 0

    def alloc(self, n, dtype):
        esz = 2 if dtype == BF16 else 4
        nb = (n * esz + 63) // 64 * 64
        assert self.off + nb <= self.arena_bytes, ("arena overflow", self.off, nb)
        a = self.arena[:, self.off // 4:(self.off + nb) // 4]
        self.off += nb
        self.peak = max(self.peak, self.off)
        if dtype != F32:
            a = a.bitcast(dtype)
        return a[:, 0:n]

    def mark(self):
        return self.off

    def release(self, m):
        self.off = m

    def _need(self, reads, writes):
        toks = []
        for r in reads:
            s = self.res.get(r)
            if s and s["w"] is not None:
                toks.append(s["w"])
        for w in writes:
            s = self.res.get(w)
            if s:
                if s["w"] is not None:
                    toks.append(s["w"])
                for e, n in s["rc"].items():
                    toks.append(("c", e, n))
                for d in s["rd"]:
                    toks.append(("d", d))
        return toks

    def _emit_waits(self, eng, toks):
        lst = self.ops[eng]
        best = {}
        for t in toks:
            if t[0] == "c":
                _, p, n = t
                if p == "tensor" and eng == "tensor":
                    continue
                if self.seen[eng][p] < n:
                    best[p] = max(best.get(p, 0), n)
            else:
                d = t[1]
                if d not in self.dseen[eng]:
                    self.dseen[eng].add(d)
                    k, v = d % NDSEM, 16 * (d // NDSEM + 1)
                    lst.append(lambda e, S, k=k, v=v: e.wait_ge(S["d"][k], v))
        for p, n in best.items():
            self.seen[eng][p] = n
            k, v = (n - 1) // SEM_ROLL, (n - 1) % SEM_ROLL + 1
            lst.append(lambda e, S, p=p, k=k, v=v: e.wait_ge(S[p][k], v))

    def _commit(self, tok, reads, writes):
        for r in reads:
            s = self.res.setdefault(r, {"w": None, "rc": {}, "rd": set()})
            if tok[0] == "c":
                s["rc"][tok[1]] = tok[2]
            else:
                s["rd"].add(tok[1])
        for w in writes:
            self.res[w] = {"w": tok, "rc": {}, "rd": set()}

    @staticmethod
    def _bank_keys(reads, writes):
        reads, writes = list(reads), list(writes)
        for k in reads + writes:
            b = None
            if k.startswith("pq") and k[2:].isdigit():
                b = int(k[2:]) // 4
            elif k.startswith("ps") and k[2:].isdigit():
                b = int(k[2:])
            if b is not None and f"PB{b}" not in writes:
                writes.append(f"PB{b}")
        return reads, writes

    rec = None

    def replay_interleaved(self, bufs):
        self.rec = None
        n = max(len(b) for b in bufs)
        for i in range(n):
            for b in bufs:
                if i < len(b):
                    kind, args = b[i]
                    if kind == "op":
                        self.op(*args)
                    else:
                        self.dma(*args[:-1], **args[-1])

    def op(self, eng, fn, reads=(), writes=()):
        if self.rec is not None:
            self.rec.append(("op", (eng, fn, list(reads), list(writes))))
            return
        reads, writes = self._bank_keys(reads, writes)
        self._emit_waits(eng, self._need(reads, writes))
        self.cnt[eng] += 1
        n = self.cnt[eng]
        k = (n - 1) // SEM_ROLL
        self.ops[eng].append(lambda e, S, fn=fn, p=eng, k=k: fn(e).then_inc(S[p][k], 1))
        self._commit(("c", eng, n), reads, writes)

    def dma(self, eng, out, in_, reads=(), writes=(), **kw):
        if self.rec is not None:
            self.rec.append(("dma", (eng, out, in_, list(reads), list(writes), kw)))
            return
        d = self.ndma
        self.ndma += 1
        toks = self._need(reads, writes)
        if d >= NDSEM:
            toks.append(("d", d - NDSEM))
        self._emit_waits(eng, toks)
        k = d % NDSEM
        self.ops[eng].append(lambda e, S, k=k, out=out, in_=in_, kw=kw: e.dma_start(out=out, in_=in_, **kw).then_inc(S["d"][k], 16))
        self.dma_issuer[d] = eng
        self._commit(("d", d), reads, writes)
        return d

    def barrier(self):
        for e in ENG:
            toks = [("c", p, self.cnt[p]) for p in ENG if p != e and self.cnt[p] > 0]
            toks += [("d", d) for d in range(max(0, self.ndma - NDSEM), self.ndma)]
            self._emit_waits(e, toks)

    def finish(self):
        self._emit_waits("sync", [("d", d) for d in range(max(0, self.ndma - NDSEM), self.ndma)])
        nc = self.nc
        with contextlib.ExitStack() as st:
            S = {}
            for e in ENG:
                ns = self.cnt[e] // SEM_ROLL + 1
                S[e] = [st.enter_context(nc.semaphore(f"s_{e}_{i}")) for i in range(ns)]
            S["d"] = [st.enter_context(nc.semaphore(f"s_d_{i}")) for i in range(NDSEM)]
            block = st.enter_context(nc.Block())

            def run(engname):
                def _f(eng):
                    for f in self.ops[engname]:
                        f(eng, S)
                return _f

            block.sync(run("sync"))
            block.tensor(run("tensor"))
            block.vector(run("vector"))
            block.scalar(run("scalar"))
            block.gpsimd(run("gpsimd"))


D = 1024
T = 2304
NT = 18
CTX = 256
SEQ = 2048
EPS = 1e-6
BLKS = [(0, 256), (256, 512), (768, 512), (1280, 512), (1792, 512)]
NIN = 2992
NEXP = 32


def tile_blk(ti):
    return 0 if ti < 2 else 1 + (ti - 2) // 4


def host_consts():
    c = {}
    i = np.arange(128)
    U = (i[:, None] <= i[None, :]).astype(np.float32)
    L = (i[:, None] >= i[None, :]).astype(np.float32)
    SU = (i[:, None] < i[None, :]).astype(np.float32)
    SL = (i[:, None] > i[None, :]).astype(np.float32)
    ident = np.eye(128, dtype=np.float32)
    ones = np.ones((128, 128), np.float32)
    bd64 = np.kron(np.eye(2, dtype=np.float32), np.ones((64, 64), np.float32))

    def rmat(rot, off, size):
        R = np.zeros((size, size), np.float32)
        q = rot // 4
        for ax in range(2):
            b = off + ax * 2 * q
            for k in range(q):
                R[b + k, b + q + k] = -1.0
                R[b + q + k, b + k] = 1.0
        return R
    RA = np.kron(np.eye(2, dtype=np.float32), rmat(64, 0, 64))
    RC = np.zeros((128, 128), np.float32)
    RC[:96, :96] = rmat(32, 64, 96)
    E32 = np.zeros((128, 128), np.float32)
    for k in range(32):
        E32[k, 64 + k] = 1.0
    c["cf"] = np.concatenate([ident, U, L, SU, SL, ones, bd64, RA.T.copy(), RC.T.copy(), E32], axis=1).astype(np.float32)

    def tables(rot):
        nf = rot // 4
        t = np.arange(SEQ)
        row = (t // 64).astype(np.float32)
        col = (t % 64).astype(np.float32)
        freq = (np.float32(10000.0) ** (-np.arange(nf, dtype=np.float32) / np.float32(nf))).astype(np.float32)
        ang = np.stack([row[:, None] * freq, col[:, None] * freq], axis=1)
        cs, sn = np.cos(ang).astype(np.float32), np.sin(ang).astype(np.float32)
        C = np.concatenate([cs[:, 0], cs[:, 0], cs[:, 1], cs[:, 1]], axis=1).T
        S = np.concatenate([sn[:, 0], sn[:, 0], sn[:, 1], sn[:, 1]], axis=1).T
        return C, S
    CA, SA = tables(64)
    c["ropeA"] = np.concatenate([np.concatenate([CA, CA], 0), np.concatenate([SA, SA], 0)], axis=1).astype(np.float32)
    CC, SC = tables(32)
    cc = np.ones((128, SEQ), np.float32)
    sc = np.zeros((128, SEQ), np.float32)
    cc[64:96] = CC
    sc[64:96] = SC
    c["ropeC"] = np.concatenate([cc, sc], axis=1).astype(np.float32)
    return c


class K:
    def __init__(self, nseq=2, nlayers=2, phases="CABM", dbg=None, moe=True):
        self.nseq, self.nlayers, self.phases, self.dbg = nseq, nlayers, phases, dbg
        nc = self.nc = bass.Bass("TRN2", target_bir_lowering=False)
        self.P = Prog(nc)
        self.all_in = []

        def dt(n, s):
            a = nc.dram_tensor(n, list(s), F32, kind="ExternalInput").ap()
            self.all_in.append(a)
            return a
        self.x2 = dt("x2", (2, SEQ, D))
        self.ctx2 = dt("ctx2", (2, CTX, D))
        self.vp1 = dt("vp1", (128, 128))
        self.vp2 = dt("vp2", (32, 128))
        self.w_mod = dt("w_mod", (2, D, 6 * D))
        self.w_in = dt("w_in", (2, D, NIN))
        self.w_out = dt("w_out", (2, 1024, D))
        self.swa_q_norm = dt("swa_q_norm", (2, 64))
        self.swa_k_norm = dt("swa_k_norm", (2, 64))
        self.swa_sink = dt("swa_sink", (2, 4))
        self.gdn_conv = dt("gdn_conv", (2, 5, 1536))
        self.gdn_a_log = dt("gdn_a_log", (2, 8))
        self.gdn_dt_bias = dt("gdn_dt_bias", (2, 8))
        self.gdn_out_norm = dt("gdn_out_norm", (2, 128))
        self.mla_q_a_norm = dt("mla_q_a_norm", (2, 256))
        self.mla_w_uq = dt("mla_w_uq", (2, 256, 384))
        self.mla_kv_a_norm = dt("mla_kv_a_norm", (2, 128))
        self.mla_w_ukv = dt("mla_w_ukv", (2, 128, 512))
        self.mla_q_norm = dt("mla_q_norm", (2, 96))
        self.mla_k_norm = dt("mla_k_norm", (2, 96))
        self.router_w = dt("router_w", (2, D, NEXP))
        self.router_b = dt("router_b", (2, NEXP))
        self.exp_w_gu = dt("exp_w_gu", (2, NEXP, D, 2048) if moe else (2, 1, 8, 8))
        self.exp_b_gu = dt("exp_b_gu", (2, NEXP, 2048))
        self.exp_w_dn = dt("exp_w_dn", (2, NEXP, D, D) if moe else (2, 1, 8, 8))
        self.exp_b_dn = dt("exp_b_dn", (2, NEXP, D))
        self.cf = dt("cf", (128, 1280))
        self.ropeA = dt("ropeA", (128, 4096))
        self.ropeC = dt("ropeC", (128, 4096))
        self.out = nc.dram_tensor("out", [2, SEQ, D], F32, kind="ExternalOutput").ap()
        self.dbg_out = None
        if dbg:
            self.dbg_out = nc.dram_tensor("dbg", [128, dbg], F32, kind="ExternalOutput").ap()
        self.uid = 0

    def MM(self, out, lhsT, rhs, start=True, stop=True, r=(), w=()):
        self.P.op("tensor", lambda e, o=out, a=lhsT, b=rhs, s=start, t=stop: e.matmul(o, a, b, start=s, stop=t), r, w)

    def TR(self, out, in_, r=(), w=()):
        npar = in_.shape[0]
        idn = self.ident[0:npar, 0:npar]
        self.P.op("tensor", lambda e, o=out, a=in_: e.transpose(o, a, idn), list(r) + ["const"], w)

    def ACT(self, out, in_, func, r=(), w=(), scale=None, bias=None, eng="scalar"):
        kw = {}
        if scale is not None:
            kw["scale"] = scale
        if bias is not None:
            kw["bias"] = bias
        self.P.op("scalar", lambda e, o=out, a=in_, f=func, kw=kw: e.activation(o, a, f, **kw), r, w)

    def TS(self, eng, out, in0, s1, op0, s2=None, op1=None, r=(), w=()):
        if op1 is None:
            self.P.op(eng, lambda e, o=out, a=in0: e.tensor_scalar(out=o, in0=a, scalar1=s1, scalar2=None, op0=op0), r, w)
        else:
            self.P.op(eng, lambda e, o=out, a=in0: e.tensor_scalar(out=o, in0=a, scalar1=s1, scalar2=s2, op0=op0, op1=op1), r, w)

    def TT(self, eng, out, a, b, op, r=(), w=()):
        self.P.op(eng, lambda e, o=out, x=a, y=b: e.tensor_tensor(out=o, in0=x, in1=y, op=op), r, w)

    def STT(self, out, a, s, b, op0, op1, r=(), w=()):
        self.P.op("vector", lambda e, o=out, x=a, y=b: e.scalar_tensor_tensor(out=o, in0=x, scalar=s, in1=y, op0=op0, op1=op1), r, w)

    def CP(self, eng, out, in_, r=(), w=()):
        if eng == "scalar":
            self.P.op(eng, lambda e, o=out, a=in_: e.copy(o, a), r, w)
        else:
            self.P.op(eng, lambda e, o=out, a=in_: e.tensor_copy(o, a), r, w)

    def RECIP(self, out, in_, r=(), w=()):
        self.P.op("vector", lambda e, o=out, a=in_: e.reciprocal(o, a), r, w)

    def MEMSET(self, eng, ap, val, w=()):
        self.P.op(eng, lambda e, a=ap: e.memset(a, val), (), w)

    def DMA(self, out, in_, r=(), w=(), eng="sync"):
        self.P.dma(eng, out, in_, r, w, allow_slow_non_contiguous=True)

    def key(self, base):
        self.uid += 1
        return f"{base}#{self.uid}"

    def dump(self, ap, ncols, r, col0=0, rows=128):
        P = self.P
        m = P.mark()
        tmp = P.alloc(64, F32)
        for c0 in range(0, ncols, 64):
            n = min(64, ncols - c0)
            k = self.key("dump")
            self.CP("vector", tmp[0:rows, 0:n], ap[:, c0:c0 + n], r=list(r) + ["dumptmp"], w=[k, "dumptmp"])
            self.DMA(self.dbg_out[0:rows, col0 + c0:col0 + c0 + n], tmp[0:rows, 0:n], r=[k], w=[self.key("dbgout"), "dumptmp"])
        P.barrier()
        P.release(m)

    def setup(self):
        P = self.P
        cf = P.alloc(1280, F32)
        self.DMA(cf, self.cf, w=["const"])
        self.ident = cf[:, 0:128]
        self.U, self.L = cf[:, 128:256], cf[:, 256:384]
        self.SU, self.SL = cf[:, 384:512], cf[:, 512:640]
        self.ones = cf[:, 640:768]
        cb = P.alloc(640, BF16)
        self.CP("vector", cb, cf[:, 640:1280], r=["const"], w=["constb"])
        self.ones_b, self.bd64_b = cb[:, 0:128], cb[:, 128:256]
        self.RAT_b, self.RCT_b, self.E32_b = cb[:, 256:384], cb[:, 384:512], cb[:, 512:640]
        junk = P.alloc(64, F32)
        for i, a in enumerate(self.all_in):
            fl = a
            while len(fl.shape) > 1:
                fl = fl[0]
            self.DMA(junk[0:1, i:i + 1], fl[0:1].rearrange("(a b) -> a b", a=1), w=["junk"])
        self.eps = P.alloc(1, F32)
        self.MEMSET("vector", self.eps, EPS, w=["const2"])
        vt = P.alloc(128, F32)
        self.DMA(vt, self.vp1, w=["vt"])
        self.VP = P.alloc(128, F32)
        self.TR(self.ps[:, 0, 0:128], vt, r=["vt"], w=["ps0"])
        self.CP("vector", self.VP, self.ps[:, 0, 0:128], r=["ps0"], w=["VP"])
        vt2 = P.alloc(128, F32)
        self.DMA(vt2[0:32, :], self.vp2, w=["vt2"])
        self.CS = P.alloc(32, F32)
        self.TR(self.ps[:, 1, 0:32], vt2[0:32, :], r=["vt2"], w=["ps1"])
        self.ACT(self.CS, self.ps[:, 1, 0:32], AF.Silu, r=["ps1"], w=["CS"])
        self.xT = P.alloc(8 * T, F32).rearrange("p (c t) -> p c t", c=8)
        self.hT = P.alloc(8 * T, BF16).rearrange("p (c t) -> p c t", c=8)
        self.stg = [P.alloc(1024, F32), P.alloc(1024, F32)]
        self.stg_i = 0
        self.MOD = [P.alloc(96, F32).rearrange("p (j y) -> p j y", y=2) for _ in range(2)]
        self.A1 = [P.alloc(16, F32).rearrange("p (c y) -> p c y", y=2) for _ in range(2)]
        self.A2 = [P.alloc(16, F32).rearrange("p (c y) -> p c y", y=2) for _ in range(2)]

    def stage(self):
        i = self.stg_i
        self.stg_i ^= 1
        return self.stg[i], f"stg{i}"

    def load_cast(self, dst, src, ncols, eng="gpsimd", dkey=None, parts=128):
        st, sk = self.stage()
        self.DMA(st[0:parts, 0:ncols], src, w=[sk])
        self.CP(eng, dst, st[0:parts, 0:ncols], r=[sk], w=[dkey])

    def load_w_cols(self, dst, wsrc, c0, ncols, dkey, kc=8):
        per = max(1, 1024 // ncols)
        for k0 in range(0, kc, per):
            k1 = min(kc, k0 + per)
            st, sk = self.stage()
            sv = st[:, 0:(k1 - k0) * ncols].rearrange("p (k n) -> p k n", n=ncols)
            self.DMA(sv, wsrc[k0 * 128:k1 * 128, c0:c0 + ncols].rearrange("(k p) n -> p k n", p=128), w=[sk])
            self.CP("gpsimd", dst[:, k0:k1, :], sv, r=[sk], w=[dkey])

    def load_x(self, s):
        P = self.P
        m = P.mark()
        bufs = [P.alloc(1024, F32), P.alloc(1024, F32)]
        for ti in range(NT):
            b = bufs[ti % 2]
            bk = f"xl{ti % 2}"
            src = self.ctx2[s, ti * 128:(ti + 1) * 128, :] if ti < 2 else self.x2[s, (ti - 2) * 128:(ti - 1) * 128, :]
            self.DMA(b, src, w=[bk])
            for half in range(2):
                bank = 2 * (ti % 2) + half
                for c4 in range(4):
                    c = half * 4 + c4
                    self.TR(self.ps[:, bank, c4 * 128:(c4 + 1) * 128], b[:, c * 128:(c + 1) * 128], r=[bk], w=[f"ps{bank}"])
                eng = "vector" if half == 0 else "scalar"
                self.CP(eng, self.xT[:, half * 4:half * 4 + 4, ti * 128:(ti + 1) * 128],
                        self.ps[:, bank, :].rearrange("p (c t) -> p c t", c=4), r=[f"ps{bank}"], w=[f"xT{tile_blk(ti)}"])
        P.barrier()
        P.release(m)

    def store_x(self, s):
        P = self.P
        m = P.mark()
        bufs = [P.alloc(1024, F32), P.alloc(1024, F32)]
        for ti in range(2, NT):
            b = bufs[ti % 2]
            bk = f"xs{ti % 2}"
            for half in range(2):
                bank = 2 * (ti % 2) + half
                for c4 in range(4):
                    c = half * 4 + c4
                    self.TR(self.ps[:, bank, c4 * 128:(c4 + 1) * 128], self.xT[:, c, ti * 128:(ti + 1) * 128], r=[f"xT{tile_blk(ti)}"], w=[f"ps{bank}"])
                eng = "vector" if half == 0 else "scalar"
                self.CP(eng, b[:, half * 512:(half + 1) * 512], self.ps[:, bank, :], r=[f"ps{bank}"], w=[bk])
            self.DMA(self.out[s, (ti - 2) * 128:(ti - 1) * 128, :], b, r=[bk], w=[self.key("out")])
        P.barrier()
        P.release(m)

    def mods(self, s, l):
        P = self.P
        m = P.mark()
        sc = P.alloc(16, F32).rearrange("p (k y) -> p k y", y=2)
        kk = self.key("sc")
        self.CP("vector", sc[:, :, 0], self.CS[:, s * 8:s * 8 + 8], r=["CS"], w=[kk])
        self.CP("vector", sc[:, :, 1], self.CS[:, 16:24], r=["CS"], w=[kk])
        MOD = self.MOD[l]
        for j in range(48):
            st, sk = self.stage()
            sv = st[:, 0:1024].rearrange("p (k n) -> p k n", n=128)
            self.DMA(sv, self.w_mod[l, :, j * 128:(j + 1) * 128].rearrange("(k p) n -> p k n", p=128), w=[sk])
            for k in range(8):
                self.MM(self.ps[:, 7, 2 * j:2 * j + 2], sv[:, k, :], sc[:, k, :], start=(k == 0), stop=(k == 7), r=[sk, kk], w=["ps7"])
        mk = f"MOD{l}"
        bm = self.VP[:, l * 64:l * 64 + 48]
        pv = self.ps[:, 7, 0:96].rearrange("p (j y) -> p j y", y=2)
        for y in range(2):
            self.TT("vector", MOD[:, :, y], pv[:, :, y], bm, ALU.add, r=["ps7", "VP"], w=[mk])
        n1 = self.VP[:, l * 64 + 48:l * 64 + 56]
        n2 = self.VP[:, l * 64 + 56:l * 64 + 64]
        for y in range(2):
            self.STT(self.A1[l][:, :, y], MOD[:, 8:16, y], 1.0, n1, ALU.add, ALU.mult, r=[mk, "VP"], w=[mk + "a"])
            self.STT(self.A2[l][:, :, y], MOD[:, 32:40, y], 1.0, n2, ALU.add, ALU.mult, r=[mk, "VP"], w=[mk + "a"])
        P.barrier()
        P.release(m)

    def norm_mod(self, l, A, shift_j, router=None):
        P = self.P
        m = P.mark()
        sq = P.alloc(8 * 512, BF16).rearrange("p (c t) -> p c t", c=8)
        sd = P.alloc(512, F32)
        rs = P.alloc(512, F32)
        tmp = [P.alloc(512, F32), P.alloc(512, F32)]
        MOD = self.MOD[l]
        for bi, (t0, n) in enumerate(BLKS):
            y = 1 if bi == 0 else 0
            xk, hk = f"xT{bi}", f"hT{bi}"
            for c in range(8):
                self.ACT(sq[:, c, 0:n], self.xT[:, c, t0:t0 + n], AF.Square, r=[xk], w=[f"sq{c}"])
            for c in range(8):
                self.MM(self.ps[:, 0, 0:n], self.ones_b, sq[:, c, 0:n], start=(c == 0), stop=(c == 7), r=[f"sq{c}", "constb"], w=["ps0"])
            self.ACT(sd[:, 0:n], self.ps[:, 0, 0:n], AF.Sqrt, scale=1.0 / D, bias=self.eps, r=["ps0", "const2"], w=["sd"])
            self.RECIP(rs[:, 0:n], sd[:, 0:n], r=["sd"], w=["rs"])
            for c in range(8):
                tb, tk = tmp[c % 2], f"nt{c % 2}"
                if router is not None:
                    tb, tk = router["h2f"][:, c, :], f"h2f{c}"
                self.STT(tb[:, 0:n], self.xT[:, c, t0:t0 + n], A[:, c, y:y + 1], rs[:, 0:n], ALU.mult, ALU.mult, r=[xk, "rs", f"MOD{l}a"], w=[tk])
                if router is not None:
                    self.TS("vector", tb[:, 0:n], tb[:, 0:n], MOD[:, shift_j + c, y:y + 1], ALU.add, r=[tk, f"MOD{l}"], w=[tk])
                    self.CP("scalar", self.hT[:, c, t0:t0 + n], tb[:, 0:n], r=[tk], w=[hk])
                else:
                    self.ACT(self.hT[:, c, t0:t0 + n], tb[:, 0:n], AF.Identity, bias=MOD[:, shift_j + c, y:y + 1], r=[tk, f"MOD{l}"], w=[hk])
            if router is not None:
                router["fn"](bi, t0, n)
        P.barrier()
        P.release(m)

    def head_norm_rope(self, src, npar, n, dst, onesl, inv_dim, gain, RT, rope_dram, t0, sk, dk, W):
        ps = self.ps
        sq, sd, rs, qn, t1, t2, rb = W["sq"], W["sd"], W["rs"], W["qn"], W["t1"], W["t2"], W["rb"]
        self.ACT(sq[0:npar, 0:n], src, AF.Square, r=[sk], w=["w_sq"])
        self.MM(ps[0:npar, 5, 0:n], onesl[0:npar, 0:npar], sq[0:npar, 0:n], r=["w_sq", "constb"], w=["ps5"])
        self.ACT(sd[0:npar, 0:n], ps[0:npar, 5, 0:n], AF.Sqrt, scale=inv_dim, bias=self.eps[0:npar, :], r=["ps5", "const2"], w=["w_sd"])
        self.RECIP(rs[0:npar, 0:n], sd[0:npar, 0:n], r=["w_sd"], w=["w_rs"])
        if t0 < CTX:
            self.STT(dst, src, gain, rs[0:npar, 0:n], ALU.mult, ALU.mult, r=[sk, "w_rs", "gains"], w=[dk])
            return
        self.STT(qn[0:npar, 0:n], src, gain, rs[0:npar, 0:n], ALU.mult, ALU.mult, r=[sk, "w_rs", "gains"], w=["w_qn"])
        self.MM(ps[0:npar, 6, 0:n], RT[0:npar, 0:npar], qn[0:npar, 0:n], r=["w_qn", "constb"], w=["ps6"])
        self.DMA(rb[:, 0, 0:n], rope_dram[:, t0 - CTX:t0 - CTX + n], w=["w_rb"])
        self.DMA(rb[:, 1, 0:n], rope_dram[:, SEQ + t0 - CTX:SEQ + t0 - CTX + n], w=["w_rb"])
        self.TT("gpsimd", t1[0:npar, 0:n], qn[0:npar, 0:n], rb[0:npar, 0, 0:n], ALU.mult, r=["w_qn", "w_rb"], w=["w_t1"])
        self.TT("vector", t2[0:npar, 0:n], ps[0:npar, 6, 0:n], rb[0:npar, 1, 0:n], ALU.mult, r=["ps6", "w_rb"], w=["w_t2"])
        self.TT("vector", dst, t1[0:npar, 0:n], t2[0:npar, 0:n], ALU.add, r=["w_t1", "w_t2"], w=[dk])

    def norm_temps(self):
        P = self.P
        return dict(sq=P.alloc(512, BF16), sd=P.alloc(512, F32), rs=P.alloc(512, F32), qn=P.alloc(512, BF16),
                    t1=P.alloc(512, BF16), t2=P.alloc(512, BF16), rb=P.alloc(1024, F32).rearrange("p (a n) -> p a n", a=2))

    def out_proj(self, l, Wo, kparts, mix, mixk, wk, nacc=1):
        ps = self.ps
        MOD = self.MOD[l]
        cnt = 0
        for bi, (t0, n) in enumerate(BLKS):
            y = 1 if bi == 0 else 0
            for dc in range(8):
                bank = cnt % 4
                cnt += 1
                for a in range(nacc):
                    self.MM(ps[:, bank, 0:n], Wo[0:kparts, a, dc * 128:(dc + 1) * 128], mix[0:kparts, a, t0:t0 + n],
                            start=(a == 0), stop=(a == nacc - 1), r=[wk, mixk], w=[f"ps{bank}"])
                self.STT(self.xT[:, dc, t0:t0 + n], ps[:, bank, 0:n], MOD[:, 16 + dc, y:y + 1], self.xT[:, dc, t0:t0 + n],
                         ALU.mult, ALU.add, r=[f"ps{bank}", f"MOD{l}", f"xT{bi}"], w=[f"xT{bi}"])

    def mixer_C(self, l):
        P, ps = self.P, self.ps
        m = P.mark()
        WC = P.alloc(8 * 416, BF16).rearrange("p (k n) -> p k n", k=8)
        self.load_w_cols(WC, self.w_in[l], 2576, 416, "WC")
        Wuq = P.alloc(2 * 384, BF16).rearrange("p (k n) -> p k n", k=2)
        self.load_w_cols(Wuq, self.mla_w_uq[l], 0, 384, "Wuq", kc=2)
        Wk = P.alloc(4 * 96, BF16).rearrange("p (h n) -> p h n", h=4)
        Wv = P.alloc(256, BF16).rearrange("p (h n) -> p h n", h=4)
        self.MEMSET("vector", Wk, 0.0, w=["Wk"])
        st, sk = self.stage()
        self.DMA(st[:, 0:512], self.mla_w_ukv[l], w=[sk])
        sv = st[:, 0:512].rearrange("p (h n) -> p h n", h=4)
        self.CP("gpsimd", Wk[:, :, 0:64], sv[:, :, 0:64], r=[sk], w=["Wk"])
        self.CP("gpsimd", Wv, sv[:, :, 64:128], r=[sk], w=["Wv"])
        g = P.alloc(8, F32)
        self.DMA(g[:, 0:2], self.mla_q_a_norm[l].rearrange("(c p) -> p c", p=128), w=["gains"])
        self.DMA(g[:, 2:3], self.mla_kv_a_norm[l].rearrange("(p o) -> p o", o=1), w=["gains"])
        self.DMA(g[0:96, 5:6], self.mla_q_norm[l].rearrange("(p o) -> p o", o=1), w=["gains"])
        self.DMA(g[0:96, 4:5], self.mla_k_norm[l].rearrange("(p o) -> p o", o=1), w=["gains"])
        self.TS("vector", g[0:96, 3:4], g[0:96, 5:6], 96.0 ** -0.5, ALU.mult, r=["gains"], w=["gains"])
        cqn = P.alloc(2 * T, BF16).rearrange("p (c t) -> p c t", c=2)
        ckvn = P.alloc(T, BF16)
        krT = P.alloc(T, BF16)
        W = self.norm_temps()
        for bi, (t0, n) in enumerate(BLKS):
            hk = f"hT{bi}"
            for o, (c0, npar) in enumerate([(0, 128), (128, 128), (256, 128), (384, 32)]):
                for k in range(8):
                    self.MM(ps[0:npar, o, 0:n], WC[:, k, c0:c0 + npar], self.hT[:, k, t0:t0 + n], start=(k == 0), stop=(k == 7), r=["WC", hk], w=[f"ps{o}"])
            for c in range(2):
                self.ACT(W["sq"][:, 0:n], ps[:, c, 0:n], AF.Square, r=[f"ps{c}"], w=["w_sq"])
                self.MM(ps[:, 5, 0:n], self.ones_b, W["sq"][:, 0:n], start=(c == 0), stop=(c == 1), r=["w_sq", "constb"], w=["ps5"])
            self.ACT(W["sd"][:, 0:n], ps[:, 5, 0:n], AF.Sqrt, scale=1.0 / 256, bias=self.eps, r=["ps5", "const2"], w=["w_sd"])
            self.RECIP(W["rs"][:, 0:n], W["sd"][:, 0:n], r=["w_sd"], w=["w_rs"])
            for c in range(2):
                self.STT(cqn[:, c, t0:t0 + n], ps[:, c, 0:n], g[:, c:c + 1], W["rs"][:, 0:n], ALU.mult, ALU.mult, r=[f"ps{c}", "w_rs", "gains"], w=[f"cqn{bi}"])
            self.ACT(W["sq"][:, 0:n], ps[:, 2, 0:n], AF.Square, r=["ps2"], w=["w_sq"])
            self.MM(ps[:, 5, 0:n], self.ones_b, W["sq"][:, 0:n], r=["w_sq", "constb"], w=["ps5"])
            self.ACT(W["sd"][:, 0:n], ps[:, 5, 0:n], AF.Sqrt, scale=1.0 / 128, bias=self.eps, r=["ps5", "const2"], w=["w_sd"])
            self.RECIP(W["rs"][:, 0:n], W["sd"][:, 0:n], r=["w_sd"], w=["w_rs"])
            self.STT(ckvn[:, t0:t0 + n], ps[:, 2, 0:n], g[:, 2:3], W["rs"][:, 0:n], ALU.mult, ALU.mult, r=["ps2", "w_rs", "gains"], w=[f"ckvn{bi}"])
            self.CP("scalar", krT[0:32, t0:t0 + n], ps[0:32, 3, 0:n], r=["ps3"], w=[f"krT{bi}"])
        allc = [f"cqn{b}" for b in range(5)]
        allkv = [f"ckvn{b}" for b in range(5)]
        qr = P.alloc(T, BF16)
        kr = P.alloc(T, BF16)
        Vh = P.alloc(NT * 64, BF16).rearrange("p (t d) -> p t d", d=64)
        mixh = P.alloc(T, BF16)
        WoC = P.alloc(1024, BF16)
        Eb = [P.alloc(512, BF16) for _ in range(3)]
        rd = P.alloc(512, F32)
        for h in range(4):
            self.load_cast(WoC[0:64, :], self.w_out[l, 768 + h * 64:768 + (h + 1) * 64, :], 1024, dkey="WoC", parts=64)
            for bi, (t0, n) in enumerate(BLKS):
                for k in range(2):
                    self.MM(ps[0:96, 0, 0:n], Wuq[:, k, h * 96:(h + 1) * 96], cqn[:, k, t0:t0 + n], start=(k == 0), stop=(k == 1), r=["Wuq", f"cqn{bi}"], w=["ps0"])
                self.head_norm_rope(ps[0:96, 0, 0:n], 96, n, qr[0:96, t0:t0 + n], self.ones_b, 1.0 / 96, g[0:96, 3:4], self.RCT_b, self.ropeC, t0, "ps0", f"qr{bi}", W)
                self.MM(ps[0:96, 1, 0:n], Wk[:, h, :], ckvn[:, t0:t0 + n], start=True, stop=False, r=["Wk", f"ckvn{bi}"], w=["ps1"])
                self.MM(ps[0:96, 1, 0:n], self.E32_b[0:32, 0:96], krT[0:32, t0:t0 + n], start=False, stop=True, r=["constb", f"krT{bi}"], w=["ps1"])
                self.head_norm_rope(ps[0:96, 1, 0:n], 96, n, kr[0:96, t0:t0 + n], self.ones_b, 1.0 / 96, g[0:96, 4:5], self.RCT_b, self.ropeC, t0, "ps1", f"kr{bi}", W)
            for ti in range(NT):
                bank = 2 + ti % 2
                self.MM(ps[:, bank, 0:64], ckvn[:, ti * 128:(ti + 1) * 128], Wv[:, h, :], r=allkv + ["Wv"], w=[f"ps{bank}"])
                self.CP("scalar", Vh[:, ti, :], ps[:, bank, 0:64], r=[f"ps{bank}"], w=["Vh"])
            allq = [f"qr{b}" for b in range(5)]
            allk = [f"kr{b}" for b in range(5)]
            for qi, (t0, n) in enumerate(BLKS):
                kts = [0, 1] if qi == 0 else list(range(NT))
                ob, db = (4, 5) if qi % 2 == 0 else (6, 7)
                for idx, kt in enumerate(kts):
                    sb = idx % 4
                    E, ek = Eb[idx % 3], f"E{idx % 3}"
                    self.MM(ps[:, sb, 0:n], kr[0:96, kt * 128:(kt + 1) * 128], qr[0:96, t0:t0 + n], r=allk + [f"qr{qi}"], w=[f"ps{sb}"])
                    self.ACT(E[:, 0:n], ps[:, sb, 0:n], AF.Exp, r=[f"ps{sb}"], w=[ek])
                    self.MM(ps[0:64, ob, 0:n], Vh[:, kt, :], E[:, 0:n], start=(idx == 0), stop=(idx == len(kts) - 1), r=["Vh", ek], w=[f"ps{ob}"])
                    self.MM(ps[0:64, db, 0:n], self.ones_b[:, 0:64], E[:, 0:n], start=(idx == 0), stop=(idx == len(kts) - 1), r=["constb", ek], w=[f"ps{db}"])
                self.RECIP(rd[0:64, 0:n], ps[0:64, db, 0:n], r=[f"ps{db}"], w=["rd"])
                self.TT("vector", mixh[0:64, t0:t0 + n], ps[0:64, ob, 0:n], rd[0:64, 0:n], ALU.mult, r=[f"ps{ob}", "rd"], w=[f"mixh{qi}"])
            if self.dbg and self.dbg_what == f"C{h}":
                self.dump(mixh[0:64, :], T, [f"mixh{b}" for b in range(5)], rows=64)
            self.out_proj_heads(l, WoC, mixh)
        P.barrier()
        P.release(m)

    def out_proj_heads(self, l, Wo, mixh):
        ps = self.ps
        MOD = self.MOD[l]
        cnt = 0
        for bi, (t0, n) in enumerate(BLKS):
            y = 1 if bi == 0 else 0
            for dc in range(8):
                bank = cnt % 4
                cnt += 1
                self.MM(ps[:, bank, 0:n], Wo[0:64, dc * 128:(dc + 1) * 128], mixh[0:64, t0:t0 + n], r=["WoC", f"mixh{bi}"], w=[f"ps{bank}"])
                self.STT(self.xT[:, dc, t0:t0 + n], ps[:, bank, 0:n], MOD[:, 16 + dc, y:y + 1], self.xT[:, dc, t0:t0 + n],
                         ALU.mult, ALU.add, r=[f"ps{bank}", f"MOD{l}", f"xT{bi}"], w=[f"xT{bi}"])

    def build(self, dbg_what=None):
        nc, P = self.nc, self.P
        self.dbg_what = dbg_what
        NB = 48800
        with nc.sbuf_tensor("arena", [128, NB], F32) as arena, nc.psum_tensor("ps", [128, 8, 512], F32) as ps:
            self.ps = ps
            P.set_arena(arena, NB * 4)
            self.setup()
            if dbg_what == "setup":
                self.dump(self.VP, 128, ["VP"])
                P.finish()
                return nc
            for s in range(self.nseq):
                self.load_x(s)
                if dbg_what == "loadx":
                    self.dump(self.xT[:, 0, :], T, [f"xT{b}" for b in range(5)])
                    P.finish()
                    return nc
                for l in range(self.nlayers):
                    self.mods(s, l)
                if dbg_what == "mods":
                    self.dump(self.MOD[0].rearrange("p j y -> p (j y)"), 96, ["MOD0"])
                    P.finish()
                    return nc
                for l in range(self.nlayers):
                    self.layer(s, l)
                    if self.stop:
                        break
                if self.stop:
                    break
                self.store_x(s)
            P.finish()
        return nc

    stop = False

    def layer(self, s, l):
        ph = self.phases
        allx = [f"xT{b}" for b in range(5)]
        allh = [f"hT{b}" for b in range(5)]
        self.norm_mod(l, self.A1[l], 0)
        if self.dbg_what == "h":
            self.dump(self.hT[:, 0, :], T, allh)
            self.stop = True
            return
        if "C" in ph:
            self.mixer_C(l)
        if "A" in ph:
            self.mixer_A(l)
        if "B" in ph:
            self.mixer_B(l)
        if self.dbg_what == "xa":
            self.dump(self.xT[:, 0, :], T, allx)
            self.stop = True
            return
        if "M" in ph:
            self.moe(l)
        if self.dbg_what == "xb":
            self.dump(self.xT[:, 0, :], T, allx)
            self.stop = True
            return


def make_inputs(inputs, core, consts):
    b0 = 2 * core
    f = lambda k: np.ascontiguousarray(inputs[k], dtype=np.float32)
    vp1 = np.concatenate([np.concatenate([f("b_mod")[l].reshape(48, 128), f("norm1")[l].reshape(8, 128), f("norm2")[l].reshape(8, 128)], 0) for l in range(2)], 0)
    vp2 = np.zeros((32, 128), np.float32)
    vp2[0:8] = f("c")[b0].reshape(8, 128)
    vp2[8:16] = f("c")[b0 + 1].reshape(8, 128)
    vp2[16:24] = f("c_ctx").reshape(8, 128)
    m = {"x2": f("x")[b0:b0 + 2], "ctx2": f("ctx")[b0:b0 + 2], "vp1": vp1, "vp2": vp2}
    for k in ["w_mod", "w_in", "w_out", "swa_q_norm", "swa_k_norm", "swa_sink", "gdn_conv", "gdn_out_norm", "mla_q_a_norm", "mla_w_uq",
              "mla_kv_a_norm", "mla_w_ukv", "mla_q_norm", "mla_k_norm", "router_w", "router_b", "exp_w_gu", "exp_b_gu", "exp_w_dn", "exp_b_dn"]:
        m[k] = f(k)
    m["gdn_a_log"] = f("gdn_a_log").reshape(2, 8)
    m["gdn_dt_bias"] = f("gdn_dt_bias").reshape(2, 8)
    m.update(consts)
    return m


def kernel(**inputs):
    k = K(nseq=2, nlayers=2, phases="CABM")
    nc = k.build()
    consts = host_consts()
    in_maps = [make_inputs(inputs, c, consts) for c in range(8)]
    res = run_bass_kernel_spmd(nc, in_maps, core_ids=list(range(8)))
    return np.concatenate([r["out"] for r in res.results], axis=0)


def _mixer_A(self, l):
    P, ps = self.P, self.ps
    m = P.mark()
    WA = P.alloc(8 * 512, BF16).rearrange("p (k n) -> p k n", k=8)
    for d0, c0, nc_ in [(0, 0, 64), (64, 128, 64), (128, 64, 64), (192, 192, 64), (256, 256, 256)]:
        self.load_w_cols(WA[:, :, d0:d0 + nc_], self.w_in[l], c0, nc_, "WA")
    g = P.alloc(8, F32)
    for hh in range(2):
        self.DMA(g[hh * 64:(hh + 1) * 64, 2:3], self.swa_q_norm[l].rearrange("(p o) -> p o", o=1), w=["gains"])
        self.DMA(g[hh * 64:(hh + 1) * 64, 1:2], self.swa_k_norm[l].rearrange("(p o) -> p o", o=1), w=["gains"])
    self.TS("vector", g[:, 0:1], g[:, 2:3], 0.125, ALU.mult, r=["gains"], w=["gains"])
    sk = P.alloc(4, F32)
    self.DMA(sk, self.swa_sink[l].rearrange("(a h) -> a h", a=1).broadcast_to([128, 4]), w=["sk0"])
    self.ACT(sk, sk, AF.Exp, r=["sk0"], w=["sk"])
    qr = P.alloc(2 * T, BF16).rearrange("p (c t) -> p c t", c=2)
    kr = P.alloc(T, BF16)
    V = P.alloc(NT * 128, BF16).rearrange("p (t d) -> p t d", d=128)
    W = self.norm_temps()
    for bi, (t0, n) in enumerate(BLKS):
        for o, c0 in enumerate([0, 128, 256]):
            for k in range(8):
                self.MM(ps[:, o, 0:n], WA[:, k, c0:c0 + 128], self.hT[:, k, t0:t0 + n], start=(k == 0), stop=(k == 7), r=["WA", f"hT{bi}"], w=[f"ps{o}"])
            dst = kr[:, t0:t0 + n] if o == 2 else qr[:, o, t0:t0 + n]
            self.head_norm_rope(ps[:, o, 0:n], 128, n, dst, self.bd64_b, 1.0 / 64, g[:, (1 if o == 2 else 0):(2 if o == 2 else 1)], self.RAT_b, self.ropeA, t0, f"ps{o}", "qkA", W)
    for ti in range(NT):
        bank = 3 + ti % 2
        for k in range(8):
            self.MM(ps[:, bank, 0:128], self.hT[:, k, ti * 128:(ti + 1) * 128], WA[:, k, 384:512], start=(k == 0), stop=(k == 7), r=["WA", f"hT{tile_blk(ti)}"], w=[f"ps{bank}"])
        self.CP("scalar", V[:, ti, :], ps[:, bank, 0:128], r=[f"ps{bank}"], w=["VA"])
    mixg = P.alloc(2 * T, BF16).rearrange("p (c t) -> p c t", c=2)
    WoA = P.alloc(2 * 1024, BF16).rearrange("p (c n) -> p c n", c=2)
    Eb = [P.alloc(5 * 256, BF16).rearrange("p (k n) -> p k n", k=5) for _ in range(2)]
    dn = P.alloc(256, F32)
    for gi in range(2):
        gp = slice(gi * 64, (gi + 1) * 64)
        for j in range(2):
            hd = 2 * gi + j
            self.load_cast(WoA[0:64, j, :], self.w_out[l, hd * 64:(hd + 1) * 64, :], 1024, dkey="WoA", parts=64)
        for ti in range(NT):
            if ti < 2:
                kts = [(0, None), (1, None)]
            else:
                kts = ([(ti - 1, self.L)] if ti - 1 >= 2 else []) + [(ti, None)] + ([(ti + 1, self.U)] if ti + 1 < NT else []) + [(0, None), (1, None)]
            b0 = 0 if ti % 2 == 0 else 3
            E, ek = Eb[ti % 2], f"EA{ti % 2}"
            qrhs = qr[gp, :, ti * 128:(ti + 1) * 128]
            nk = len(kts)
            for idx, (kt, mk) in enumerate(kts):
                bank = b0 + idx // 2
                c0 = (idx % 2) * 256
                self.MM(ps[:, bank, c0:c0 + 256].rearrange("p (j t) -> p j t", j=2), kr[gp, kt * 128:(kt + 1) * 128], qrhs, r=["qkA"], w=[f"ps{bank}"])
            psflat = ps[:, b0:b0 + 3, :].rearrange("p b n -> p (b n)")
            self.ACT(E[:, 0:nk, :].rearrange("p k n -> p (k n)"), psflat[:, 0:nk * 256], AF.Exp, r=[f"ps{b0}", f"ps{b0 + 1}", f"ps{b0 + 2}"], w=[ek])
            for idx, (kt, mk) in enumerate(kts):
                if mk is not None:
                    ev = E[:, idx, :].rearrange("p (j t) -> p j t", j=2)
                    self.TT("gpsimd", ev, ev, mk.rearrange("p (a t) -> p a t", a=1).broadcast_to([128, 2, 128]), ALU.mult, r=[ek, "const"], w=[ek])
            for idx, (kt, mk) in enumerate(kts):
                self.MM(ps[0:64, 6, 0:256], V[:, kt, gi * 64:(gi + 1) * 64], E[:, idx, :], start=(idx == 0), stop=(idx == nk - 1), r=["VA", ek], w=["ps6"])
            for idx, (kt, mk) in enumerate(kts):
                self.MM(ps[0:64, 7, 0:256], self.ones_b[:, 0:64], E[:, idx, :], start=(idx == 0), stop=(idx == nk - 1), r=["constb", ek], w=["ps7"])
            for j in range(2):
                self.TS("vector", dn[0:64, j * 128:(j + 1) * 128], ps[0:64, 7, j * 128:(j + 1) * 128], sk[0:64, 2 * gi + j:2 * gi + j + 1], ALU.add, r=["ps7", "sk"], w=["dnA"])
            self.RECIP(dn[0:64, :], dn[0:64, :], r=["dnA"], w=["dnA"])
            self.TT("vector", mixg[0:64, :, ti * 128:(ti + 1) * 128], ps[0:64, 6, 0:256].rearrange("p (j t) -> p j t", j=2),
                    dn[0:64, :].rearrange("p (j t) -> p j t", j=2), ALU.mult, r=["ps6", "dnA"], w=["mixg"])
        if self.dbg and self.dbg_what == f"A{gi}":
            self.dump(mixg[0:64, 0, :], T, ["mixg"], rows=64)
        self.out_proj(l, WoA, 64, mixg, "mixg", "WoA", nacc=2)
    P.barrier()
    P.release(m)


K.mixer_A = _mixer_A


def _mixer_B(self, l):
    P, ps = self.P, self.ps
    m = P.mark()
    psq = ps.rearrange("p b (q n) -> p (b q) n", q=4)
    allh = [f"hT{b}" for b in range(5)]
    Wab = P.alloc(8 * 16, BF16).rearrange("p (k n) -> p k n", k=8)
    self.load_w_cols(Wab, self.w_in[l], 2560, 16, "Wab")
    for ti in range(NT):
        for k in range(8):
            self.MM(ps[:, 0, ti * 16:(ti + 1) * 16], self.hT[:, k, ti * 128:(ti + 1) * 128], Wab[:, k, :], start=(k == 0), stop=(k == 7), r=["Wab"] + allh, w=["ps0"])
    abv = ps[:, 0, 0:NT * 16].rearrange("p (t j) -> p t j", j=16)
    one = P.alloc(1, F32)
    self.MEMSET("vector", one, 1.0, w=["one"])
    arr = lambda: P.alloc(NT * 8, F32).rearrange("p (t j) -> p t j", j=8)
    dtb, alog, X, AX, G, BETA, GC, GCL, BEG, KD = [arr() for _ in range(10)]
    self.DMA(dtb, self.gdn_dt_bias[l].rearrange("(a b j) -> a b j", a=1, b=1).broadcast_to([128, NT, 8]), w=["dtb"])
    self.DMA(alog, self.gdn_a_log[l].rearrange("(a b j) -> a b j", a=1, b=1).broadcast_to([128, NT, 8]), w=["alog"])
    self.TT("vector", X, abv[:, :, 0:8], dtb, ALU.add, r=["ps0", "dtb"], w=["gX"])
    self.TS("vector", AX, X, -1.0, ALU.mult, r=["gX"], w=["gAX"])
    self.TT("vector", AX, AX, X, ALU.max, r=["gX", "gAX"], w=["gAX"])
    self.ACT(AX, AX, AF.Exp, scale=-1.0, r=["gAX"], w=["gAX"])
    self.ACT(AX, AX, AF.Ln, bias=one, r=["gAX", "one"], w=["gAX"])
    self.STT(X, X, 0.0, AX, ALU.max, ALU.add, r=["gX", "gAX"], w=["gX"])
    self.ACT(alog, alog, AF.Exp, r=["alog"], w=["alog"])
    self.TS("vector", alog, alog, -1.0, ALU.mult, r=["alog"], w=["alog"])
    self.TT("vector", G, X, alog, ALU.mult, r=["gX", "alog"], w=["gG"])
    self.ACT(BETA, abv[:, :, 8:16], AF.Sigmoid, r=["ps0"], w=["gB"])
    if self.dbg_what == "BG3a":
        P.barrier()
        self.dump(G.rearrange("p t j -> p (t j)"), NT * 8, ["gG"], col0=256)
        self.dump(BETA.rearrange("p t j -> p (t j)"), NT * 8, ["gB"], col0=768)
        P.release(m)
        return
    for ti in range(NT):
        self.MM(ps[:, 1, ti * 8:ti * 8 + 4], self.U, G[:, ti, 0:4], r=["gG", "const"], w=["ps1"])
        self.MM(ps[:, 1, ti * 8 + 4:ti * 8 + 8], self.L, G[:, ti, 4:8], r=["gG", "const"], w=["ps1"])
        self.MM(ps[:, 2, ti * 8:ti * 8 + 8], self.ones, G[:, ti, :], r=["gG", "const"], w=["ps2"])
    self.CP("vector", GC, ps[:, 1, 0:NT * 8].rearrange("p (t j) -> p t j", j=8), r=["ps1"], w=["gGC"])
    self.CP("vector", GCL, ps[:, 2, 0:NT * 8].rearrange("p (t j) -> p t j", j=8), r=["ps2"], w=["gGCL"])
    if self.dbg_what == "BG3b":
        P.barrier()
        self.dump(GC.rearrange("p t j -> p (t j)"), NT * 8, ["gGC"], col0=256)
        self.dump(GCL.rearrange("p t j -> p (t j)"), NT * 8, ["gGCL"], col0=768)
        P.release(m)
        return
    self.ACT(BEG, GC, AF.Exp, r=["gGC"], w=["gBEG"])
    self.TT("vector", BEG, BEG, BETA, ALU.mult, r=["gBEG", "gB"], w=["gBEG"])
    self.TT("vector", KD, GCL, GC, ALU.subtract, r=["gGC", "gGCL"], w=["gKD"])
    self.ACT(KD, KD, AF.Exp, r=["gKD"], w=["gKD"])
    self.ACT(GCL, GCL, AF.Exp, r=["gGCL", "gKD"], w=["gGCL"])
    P.barrier()
    if self.dbg_what == "BG3":
        self.dump(KD.rearrange("p t j -> p (t j)"), NT * 8, ["gKD"])
        self.dump(G.rearrange("p t j -> p (t j)"), NT * 8, ["gG"], col0=256)
        self.dump(GC.rearrange("p t j -> p (t j)"), NT * 8, ["gGC"], col0=512)
        self.dump(BETA.rearrange("p t j -> p (t j)"), NT * 8, ["gB"], col0=768)
        P.release(m)
        return
    gains = P.alloc(2, F32)
    self.DMA(gains[:, 0:1], self.gdn_out_norm[l].rearrange("(p o) -> p o", o=1), w=["gains"])
    WG = P.alloc(8 * 128, BF16).rearrange("p (k n) -> p k n", k=8)
    raw = P.alloc(T, BF16)
    kT = P.alloc(T, F32)
    qT = P.alloc(T, BF16)
    vT = P.alloc(T, BF16)
    Oacc = P.alloc(T, BF16)
    cw = P.alloc(16, F32)
    y = P.alloc(512, F32)
    ys = P.alloc(512, F32)
    sqb = P.alloc(512, BF16)
    Wo = P.alloc(1024, BF16)
    names = ["qf", "vf", "Gtri", "t1", "t2", "e2m", "QKDT", "N", "M", "eg", "qdec", "kbg", "kdec", "bv", "R0", "R1", "N2", "M2", "U_", "WT", "VN", "S0", "S1"]
    Bs = [{nm: P.alloc(128, F32) for nm in names} for _ in range(2)]
    allraw = [f"raw{b}" for b in range(5)]
    for h in range(4):
        for Xi, (dstT, colbase) in enumerate([(qT, 512), (kT, 1024), (vT, 1536)]):
            self.load_w_cols(WG, self.w_in[l], colbase + h * 128, 128, "WG")
            self.DMA(cw[:, Xi * 5:(Xi + 1) * 5], self.gdn_conv[l, :, Xi * 512 + h * 128:Xi * 512 + (h + 1) * 128].rearrange("w p -> p w"), w=["cw"])
            for bi, (t0, n) in enumerate(BLKS):
                bank = bi % 2
                for k in range(8):
                    self.MM(ps[:, bank, 0:n], WG[:, k, :], self.hT[:, k, t0:t0 + n], start=(k == 0), stop=(k == 7), r=["WG", f"hT{bi}"], w=[f"pq{bank * 4}"])
                self.CP("scalar", raw[:, t0:t0 + n], ps[:, bank, 0:n], r=[f"pq{bank * 4}"], w=[f"raw{bi}"])
            for bi, (t0, n) in enumerate(BLKS):
                s0, s1 = (0, CTX) if bi == 0 else (CTX, T)
                self.TS("vector", y[:, 0:n], raw[:, t0:t0 + n], cw[:, Xi * 5 + 2:Xi * 5 + 3], ALU.mult, r=allraw + ["cw"], w=["cy"])
                for j in (0, 1, 3, 4):
                    a, b = max(t0, s0 - (j - 2)), min(t0 + n, s1 - (j - 2))
                    self.STT(y[:, a - t0:b - t0], raw[:, a + j - 2:b + j - 2], cw[:, Xi * 5 + j:Xi * 5 + j + 1], y[:, a - t0:b - t0], ALU.mult, ALU.add, r=allraw + ["cw", "cy"], w=["cy"])
                if Xi == 2:
                    self.ACT(vT[:, t0:t0 + n], y[:, 0:n], AF.Silu, r=["cy"], w=["gv"])
                    continue
                self.ACT(ys[:, 0:n], y[:, 0:n], AF.Silu, r=["cy"], w=["cys"])
                self.ACT(sqb[:, 0:n], ys[:, 0:n], AF.Square, r=["cys"], w=["csq"])
                self.MM(ps[:, 2, 0:n], self.ones_b, sqb[:, 0:n], r=["csq", "constb"], w=["pq8"])
                self.ACT(y[:, 0:n], ps[:, 2, 0:n], AF.Sqrt, bias=self.eps, r=["pq8", "const2", "cy"], w=["cy"])
                self.RECIP(y[:, 0:n], y[:, 0:n], r=["cy"], w=["cy"])
                self.STT(dstT[:, t0:t0 + n], ys[:, 0:n], (128.0 ** -0.5 if Xi == 0 else 1.0), y[:, 0:n], ALU.mult, ALU.mult, r=["cys", "cy"], w=["gq" if Xi == 0 else "gk"])
        P.barrier()
        if self.dbg_what == "Bproj":
            self.dump(kT, T, ["gk"])
            P.release(m)
            return
        order = [list(range(NT)), [1, 0] + list(range(NT - 1, 1, -1))]
        Scur = [Bs[0]["S0"], Bs[1]["S0"]]
        Snxt = [Bs[0]["S1"], Bs[1]["S1"]]
        for d in range(2):
            self.MEMSET("vector", Scur[d], 0.0, w=[f"S{d}"])
        touched = set()
        for step in range(NT):
            bufs = []
            for d in range(2):
                P.rec = []
                bufs.append(P.rec)
                ti = order[d][step]
                B = Bs[d]
                j = d * 4 + h
                q0 = d * 16
                cs = slice(ti * 128, (ti + 1) * 128)
                kt = kT[:, cs]
                Tri = self.U if d == 0 else self.L
                MS = self.SL if d == 0 else self.SU
                MIT = self.U if d == 0 else self.L
                gcol, gccol, bcol = G[:, ti, j:j + 1], GC[:, ti, j:j + 1], BETA[:, ti, j:j + 1]
                K_ = lambda nm: f"b{d}{nm}"
                SLT = lambda i: psq[:, q0 + i, :]
                SK = lambda i: f"pq{q0 + i}"
                self.CP("gpsimd", B["qf"], qT[:, cs], r=["gq"], w=[K_("qf")])
                self.CP("gpsimd", B["vf"], vT[:, cs], r=["gv"], w=[K_("vf")])
                self.MM(SLT(0), kt, kt, r=["gk"], w=[SK(0)])
                self.MM(SLT(1), kt, B["qf"], r=["gk", K_("qf")], w=[SK(1)])
                self.TS("vector", B["Gtri"], Tri, gcol, ALU.mult, r=["const", "gG"], w=[K_("Gtri")])
                self.MM(SLT(2), self.ones, B["Gtri"], r=["const", K_("Gtri")], w=[SK(2)])
                self.TS("vector", B["t1"], SLT(2), gccol, ALU.subtract, 0.0, ALU.max, r=[SK(2), "gGC"], w=[K_("t1")])
                self.ACT(B["t1"], B["t1"], AF.Exp, scale=-1.0, r=[K_("t1")], w=[K_("t1")])
                self.TT("gpsimd", B["t1"], B["t1"], MS, ALU.mult, r=[K_("t1"), "const"], w=[K_("t1")])
                self.TS("vector", B["t2"], SLT(2), gccol, ALU.subtract, 0.0, ALU.min, r=[SK(2), "gGC"], w=[K_("t2")])
                self.ACT(B["t2"], B["t2"], AF.Exp, r=[K_("t2")], w=[K_("t2")])
                self.TT("gpsimd", B["e2m"], B["t2"], MIT, ALU.mult, r=[K_("t2"), "const"], w=[K_("e2m")])
                self.TT("vector", B["QKDT"], SLT(1), B["e2m"], ALU.mult, r=[SK(1), K_("e2m")], w=[K_("QKDT")])
                self.STT(B["N"], SLT(0), bcol, B["t1"], ALU.mult, ALU.mult, r=[SK(0), "gB", K_("t1")], w=[K_("N")])
                self.TR(SLT(3), B["N"], r=[K_("N")], w=[SK(3)])
                self.CP("scalar", B["M"], SLT(3), r=[SK(3)], w=[K_("M")])
                self.ACT(B["eg"], SLT(2), AF.Exp, r=[SK(2)], w=[K_("eg")])
                self.TT("vector", B["qdec"], B["qf"], B["eg"], ALU.mult, r=[K_("qf"), K_("eg")], w=[K_("qdec")])
                self.TR(SLT(4), kt, r=["gk"], w=[SK(4)])
                self.TR(SLT(5), B["vf"], r=[K_("vf")], w=[SK(5)])
                self.TS("vector", B["kbg"], SLT(4), BEG[:, ti, j:j + 1], ALU.mult, r=[SK(4), "gBEG"], w=[K_("kbg")])
                self.TS("vector", B["kdec"], SLT(4), KD[:, ti, j:j + 1], ALU.mult, r=[SK(4), "gKD"], w=[K_("kdec")])
                self.TS("vector", B["bv"], SLT(5), bcol, ALU.mult, r=[SK(5), "gB"], w=[K_("bv")])
                self.TT("vector", B["R0"], self.ident, B["M"], ALU.subtract, r=["const", K_("M")], w=[K_("R0")])
                Nc, Mc, Rc = ("N", "M", "R0")
                Nn, Mn, Rn = ("N2", "M2", "R1")
                for lev in range(1, 7):
                    self.MM(SLT(6), B[Mc], B[Nc], r=[K_(Mc), K_(Nc)], w=[SK(6)])
                    self.CP("vector", B[Nn], SLT(6), r=[SK(6)], w=[K_(Nn)])
                    if lev < 6:
                        self.MM(SLT(7), B[Nc], B[Mc], r=[K_(Mc), K_(Nc)], w=[SK(7)])
                        self.CP("scalar", B[Mn], SLT(7), r=[SK(7)], w=[K_(Mn)])
                    self.MM(SLT(8), B[Nn], B[Rc], r=[K_(Nn), K_(Rc)], w=[SK(8)])
                    self.TT("vector", B[Rn], SLT(8), B[Rc], ALU.add, r=[SK(8), K_(Rc)], w=[K_(Rn)])
                    Nc, Nn = Nn, Nc
                    Mc, Mn = Mn, Mc
                    Rc, Rn = Rn, Rc
                TTk = Rc
                self.MM(SLT(9), B[TTk], B["bv"], r=[K_(TTk), K_("bv")], w=[SK(9)])
                self.CP("scalar", B["U_"], SLT(9), r=[SK(9)], w=[K_("U_")])
                self.MM(SLT(10), B["kbg"], B[TTk], r=[K_(TTk), K_("kbg")], w=[SK(10)])
                self.CP("vector", B["WT"], SLT(10), r=[SK(10)], w=[K_("WT")])
                S_, Sn_ = Scur[d], Snxt[d]
                self.MM(SLT(11), B["WT"], S_, r=[K_("WT"), f"S{d}"], w=[SK(11)])
                self.TT("vector", B["VN"], B["U_"], SLT(11), ALU.subtract, r=[K_("U_"), SK(11)], w=[K_("VN")])
                self.MM(SLT(12), S_, B["qdec"], start=True, stop=False, r=[f"S{d}", K_("qdec")], w=[SK(12)])
                self.MM(SLT(12), B["VN"], B["QKDT"], start=False, stop=True, r=[K_("VN"), K_("QKDT")], w=[SK(12)])
                if ti not in touched:
                    touched.add(ti)
                    self.CP("scalar", Oacc[:, cs], SLT(12), r=[SK(12)], w=[f"Oacc{ti}"])
                else:
                    self.TT("vector", Oacc[:, cs], SLT(12), Oacc[:, cs], ALU.add, r=[SK(12), f"Oacc{ti}"], w=[f"Oacc{ti}"])
                self.MM(SLT(13), B["kdec"], B["VN"], r=[K_("kdec"), K_("VN")], w=[SK(13)])
                self.STT(Sn_, S_, GCL[:, ti, j:j + 1], SLT(13), ALU.mult, ALU.add, r=[f"S{d}", "gGCL", SK(13)], w=[f"S{d}"])
                Scur[d], Snxt[d] = Sn_, S_
            P.replay_interleaved(bufs)
        P.barrier()
        self.load_w_cols(WG, self.w_in[l], 2048 + h * 128, 128, "WG")
        self.load_cast(Wo, self.w_out[l, 256 + h * 128:256 + (h + 1) * 128, :], 1024, dkey="WoB")
        MOD = self.MOD[l]
        for bi, (t0, n) in enumerate(BLKS):
            yy = 1 if bi == 0 else 0
            oa = Oacc[:, t0:t0 + n]
            self.ACT(sqb[:, 0:n], oa, AF.Square, w=["csq"])
            self.MM(ps[:, 2, 0:n], self.ones_b, sqb[:, 0:n], r=["csq", "constb"], w=["pq8"])
            self.ACT(y[:, 0:n], ps[:, 2, 0:n], AF.Sqrt, scale=1.0 / 128, bias=self.eps, r=["pq8", "const2"], w=["cy"])
            self.RECIP(y[:, 0:n], y[:, 0:n], r=["cy"], w=["cy"])
            self.STT(ys[:, 0:n], oa, gains[:, 0:1], y[:, 0:n], ALU.mult, ALU.mult, r=["cy", "gains"], w=["cys"])
            for k in range(8):
                self.MM(ps[:, 3, 0:n], WG[:, k, :], self.hT[:, k, t0:t0 + n], start=(k == 0), stop=(k == 7), r=["WG", f"hT{bi}"], w=["pq12"])
            self.ACT(y[:, 0:n], ps[:, 3, 0:n], AF.Silu, r=["pq12", "cy"], w=["cy"])
            self.TT("vector", sqb[:, 0:n], ys[:, 0:n], y[:, 0:n], ALU.mult, r=["cys", "cy"], w=["csq"])
            if self.dbg and self.dbg_what == f"B{h}":
                self.dump(sqb[:, 0:n], n, ["csq"], col0=t0)
            for dc in range(8):
                bank = 4 + dc % 4
                self.MM(ps[:, bank, 0:n], Wo[:, dc * 128:(dc + 1) * 128], sqb[:, 0:n], r=["WoB", "csq"], w=[f"pq{bank * 4}"])
                self.STT(self.xT[:, dc, t0:t0 + n], ps[:, bank, 0:n], MOD[:, 16 + dc, yy:yy + 1], self.xT[:, dc, t0:t0 + n],
                         ALU.mult, ALU.add, r=[f"pq{bank * 4}", f"MOD{l}", f"xT{bi}"], w=[f"xT{bi}"])
        P.barrier()
    P.release(m)


K.mixer_B = _mixer_B


def _moe(self, l):
    P, ps = self.P, self.ps
    m0 = P.mark()
    gateT = P.alloc(T, BF16)
    m = P.mark()
    RW = P.alloc(8 * 32, F32).rearrange("p (k e) -> p k e", k=8)
    self.DMA(RW, self.router_w[l].rearrange("(k p) e -> p k e", p=128), w=["RW"])
    rb = P.alloc(32, F32)
    self.DMA(rb, self.router_b[l].rearrange("(a e) -> a e", a=1).broadcast_to([128, 32]), w=["rb"])
    h2f = P.alloc(8 * 512, F32).rearrange("p (c t) -> p c t", c=8)
    lg, msk, ex = P.alloc(32, F32), P.alloc(32, F32), P.alloc(32, F32)
    top8, dn = P.alloc(8, F32), P.alloc(1, F32)

    def router_fn(bi, t0, n):
        for tt in range(n // 128):
            cs = slice(tt * 128, (tt + 1) * 128)
            for c in range(8):
                self.MM(ps[:, 1, 0:32], h2f[:, c, cs], RW[:, c, :], start=(c == 0), stop=(c == 7), r=[f"h2f{c}", "RW"], w=["ps1"])
            self.TT("vector", lg, ps[:, 1, 0:32], rb, ALU.add, r=["ps1", "rb"], w=["lg"])
            self.P.op("vector", lambda e: e.max(out=top8, in_=lg), ["lg"], ["top8"])
            self.TS("vector", msk, lg, top8[:, 3:4], ALU.is_ge, r=["lg", "top8"], w=["msk"])
            self.TS("vector", ex, lg, top8[:, 0:1], ALU.subtract, r=["lg", "top8"], w=["ex"])
            self.ACT(ex, ex, AF.Exp, r=["ex"], w=["ex"])
            self.TT("vector", ex, ex, msk, ALU.mult, r=["ex", "msk"], w=["ex"])
            self.P.op("vector", lambda e: e.reduce_sum(out=dn, in_=ex, axis=mybir.AxisListType.X), ["ex"], ["dn"])
            self.RECIP(dn, dn, r=["dn"], w=["dn"])
            self.TS("vector", ex, ex, dn[:, 0:1], ALU.mult, r=["ex", "dn"], w=["ex"])
            self.TR(ps[0:32, 2, 0:128], ex, r=["ex"], w=["ps2"])
            self.CP("scalar", gateT[0:32, t0 + tt * 128:t0 + (tt + 1) * 128], ps[0:32, 2, 0:128], r=["ps2"], w=["gateT"])

    self.norm_mod(l, self.A2[l], 24, router=dict(h2f=h2f, fn=router_fn))
    P.release(m)
    if self.dbg_what == "gate":
        self.dump(gateT[0:32, :], T, ["gateT"], rows=32)
        P.release(m0)
        return
    bsb = P.alloc(2048, F32)
    self.DMA(bsb[0:32, :], self.exp_b_gu[l], w=["bsb"])
    bv = bsb[0:32, :].rearrange("e (j p s) -> e j s p", j=8, s=2)
    for j in range(8):
        for s_ in range(2):
            self.TR(ps[:, 3, (j * 2 + s_) * 32:(j * 2 + s_ + 1) * 32], bv[:, j, s_, :], r=["bsb"], w=["ps3"])
    bguT = P.alloc(512, F32).rearrange("p (a e) -> p a e", e=32)
    self.CP("vector", bguT, ps[:, 3, :].rearrange("p (a e) -> p a e", e=32), r=["ps3"], w=["bguT"])
    bdn = P.alloc(1024, BF16)
    self.load_cast(bdn[0:32, :], self.exp_b_dn[l], 1024, dkey="bdn", parts=32)
    MOD = self.MOD[l]
    for bi, (t0, n) in enumerate(BLKS):
        yy = 1 if bi == 0 else 0
        for dc in range(8):
            bank = 4 + dc % 2
            self.MM(ps[:, bank, 0:n], bdn[0:32, dc * 128:(dc + 1) * 128], gateT[0:32, t0:t0 + n], r=["bdn", "gateT"], w=[f"ps{bank}"])
            self.STT(self.xT[:, dc, t0:t0 + n], ps[:, bank, 0:n], MOD[:, 40 + dc, yy:yy + 1], self.xT[:, dc, t0:t0 + n],
                     ALU.mult, ALU.add, r=[f"ps{bank}", f"MOD{l}", f"xT{bi}"], w=[f"xT{bi}"])
    Wgu = [P.alloc(8 * 512, BF16).rearrange("p (k s f) -> p k s f", k=8, s=2) for _ in range(2)]
    Wdn = [P.alloc(2 * 1024, BF16).rearrange("p (j n) -> p j n", j=2) for _ in range(2)]
    act = [P.alloc(2 * 512, BF16).rearrange("p (j n) -> p j n", j=2) for _ in range(3)]
    gsel = P.alloc(512, BF16)
    tg = [[P.alloc(512, BF16) for _ in range(2)] for _ in range(2)]
    tsg = [[P.alloc(512, BF16) for _ in range(2)] for _ in range(2)]
    tl = [[P.alloc(512, BF16) for _ in range(2)] for _ in range(2)]
    bl1 = P.alloc(8 * 32, F32).rearrange("p (j e) -> p j e", e=32)
    self.TS("vector", bl1, bguT.rearrange("p (j s) e -> p j s e", s=2)[:, :, 1, :], 1.0, ALU.add, r=["bguT"], w=["bl1"])
    nexp = NEXP if self.moe_experts is None else self.moe_experts
    units = [(e_, q) for e_ in range(nexp) for q in range(4)]

    def load_unit(ui):
        e_, q = units[ui]
        wi = ui % 2
        for k0 in range(0, 8, 2):
            st, sk = self.stage()
            sv = st[:, 0:1024].rearrange("p (k n) -> p k n", k=2)
            self.DMA(sv, self.exp_w_gu[l, e_, k0 * 128:(k0 + 2) * 128, q * 512:(q + 1) * 512].rearrange("(k p) n -> p k n", p=128), w=[sk])
            self.CP("gpsimd", Wgu[wi][:, k0:k0 + 2, :, :], sv.rearrange("p k (f s) -> p k s f", s=2), r=[sk], w=[f"Wgu{wi}"])
        for jj in range(2):
            st, sk = self.stage()
            self.DMA(st[:, 0:1024], self.exp_w_dn[l, e_, q * 256 + jj * 128:q * 256 + (jj + 1) * 128, :], w=[sk])
            self.CP("scalar", Wdn[wi][:, jj, :], st[:, 0:1024], r=[sk], w=[f"Wdn{wi}"])

    def gu_swiglu(ui, bi, ai):
        e_, q = units[ui]
        wi = ui % 2
        t0, n = BLKS[bi]
        wgk = f"Wgu{wi}"
        self.TS("vector", gsel[0:32, 0:n], gateT[0:32, t0:t0 + n], self.ident[0:32, e_:e_ + 1], ALU.mult, r=["gateT", "const"], w=["gsel"])
        for jj in range(2):
            bg_, bl_ = 2 * jj, 2 * jj + 1
            for s_ in range(2):
                for k in range(8):
                    self.MM(ps[:, 2 * jj + s_, 0:n], Wgu[wi][:, k, s_, jj * 128:(jj + 1) * 128], self.hT[:, k, t0:t0 + n],
                            start=(k == 0), stop=(k == 7), r=[wgk, f"hT{bi}"], w=[f"ps{2 * jj + s_}"])
            if jj == 0:
                self.MM(ps[:, 6, 0:n], self.ones_b[0:32, :], gsel[0:32, 0:n], r=["gsel", "constb"], w=["ps6"])
            fj = 2 * q + jj
            G_, S_, L_ = tg[ai % 2][jj], tsg[ai % 2][jj], tl[ai % 2][jj]
            kk = f"{ai % 2}{jj}"
            self.TS("vector", G_[:, 0:n], ps[:, bg_, 0:n], bguT[:, fj * 2, e_:e_ + 1], ALU.add, 7.0, ALU.min, r=[f"ps{bg_}", "bguT"], w=["tg" + kk])
            self.ACT(S_[:, 0:n], G_[:, 0:n], AF.Gelu_apprx_sigmoid, r=["tg" + kk], w=["tsg" + kk])
            self.TS("vector", L_[:, 0:n], ps[:, bl_, 0:n], bl1[:, fj, e_:e_ + 1], ALU.add, 8.0, ALU.min, r=[f"ps{bl_}", "bl1"], w=["tl" + kk])
            self.STT(L_[:, 0:n], L_[:, 0:n], -6.0, ps[:, 6, 0:n], ALU.max, ALU.mult, r=["tl" + kk, "ps6"], w=["tl" + kk])
            self.TT("gpsimd", act[ai][:, jj, 0:n], S_[:, 0:n], L_[:, 0:n], ALU.mult, r=["tsg" + kk, "tl" + kk], w=[f"act{ai}"])

    def dn_update(ui, bi, ai):
        wi = ui % 2
        t0, n = BLKS[bi]
        yy = 1 if bi == 0 else 0
        for dc in range(8):
            bank = 4 + dc % 2
            for jj in range(2):
                self.MM(ps[:, bank, 0:n], Wdn[wi][:, jj, dc * 128:(dc + 1) * 128], act[ai][:, jj, 0:n], start=(jj == 0), stop=(jj == 1), r=[f"Wdn{wi}", f"act{ai}"], w=[f"ps{bank}"])
            if dc in ():
                xi = xcnt[0] % 2
                xcnt[0] += 1
                self.ACT(xtmp[xi][:, 0:n], ps[:, bank, 0:n], AF.Copy, scale=MOD[:, 40 + dc, yy:yy + 1], r=[f"ps{bank}", f"MOD{l}"], w=[f"xtmp{xi}"])
                self.TT("gpsimd", self.xT[:, dc, t0:t0 + n], self.xT[:, dc, t0:t0 + n], xtmp[xi][:, 0:n], ALU.add, r=[f"xtmp{xi}", f"xTm{bi}_{dc}"], w=[f"xTm{bi}_{dc}"])
            else:
                self.STT(self.xT[:, dc, t0:t0 + n], ps[:, bank, 0:n], MOD[:, 40 + dc, yy:yy + 1], self.xT[:, dc, t0:t0 + n],
                         ALU.mult, ALU.add, r=[f"ps{bank}", f"MOD{l}", f"xTm{bi}_{dc}"], w=[f"xTm{bi}_{dc}"])

    xtmp = [P.alloc(512, F32), P.alloc(512, F32)]
    xcnt = [0]
    load_unit(0)
    pend = []
    step = 0
    blist = list(range(len(BLKS)))
    if l == self.nlayers - 1 and self.dbg_what is None:
        blist = blist[1:]
    for ui in range(len(units)):
        for bi in blist:
            ai = step % 2
            step += 1
            ai = step % 3
            P.rec = bufA = []
            gu_swiglu(ui, bi, ai)
            P.rec = bufB = []
            if len(pend) == 2:
                dn_update(*pend.pop(0))
            P.rec = None
            groups = [bufB[i:i + 3] for i in range(0, len(bufB), 3)]
            nmm = 0
            for kind, args in bufA:
                P.op(*args)
                if args[0] == "tensor":
                    nmm += 1
                    if nmm % 4 == 0 and groups:
                        for k2, a2 in groups.pop(0):
                            P.op(*a2)
            for g_ in groups:
                for k2, a2 in g_:
                    P.op(*a2)
            if bi == blist[1] and ui + 1 < len(units):
                load_unit(ui + 1)
            pend.append((ui, bi, ai))
    for p_ in pend:
        dn_update(*p_)
    P.barrier()
    P.release(m0)


K.moe = _moe
K.moe_experts = None
```

```python
import contextlib
import numpy as np
import concourse.bass as bass
import concourse.mybir as mybir
from concourse.bass_utils import run_bass_kernel_spmd

F32 = mybir.dt.float32
BF16 = mybir.dt.bfloat16
AF = mybir.ActivationFunctionType
ALU = mybir.AluOpType

ENG = ["tensor", "vector", "scalar", "gpsimd", "sync"]
SEM_ROLL = 30000
NDSEM = 24


class Prog:
    def __init__(self, nc):
        self.nc = nc
        self.ops = {e: [] for e in ENG}
        self.cnt = {e: 0 for e in ENG}
        self.seen = {e: {p: 0 for p in ENG} for e in ENG}
        self.dseen = {e: set() for e in ENG}
        self.res = {}
        self.ndma = 0
        self.dma_issuer = {}
        self.arena = None
        self.off = 0
        self.peak = 0

    def set_arena(self, arena, nbytes):
        self.arena = arena
        self.arena_bytes = nbytes
        self.off = 0

    def alloc(self, n, dtype):
        esz = 2 if dtype == BF16 else 4
        nb = (n * esz + 63) // 64 * 64
        assert self.off + nb <= self.arena_bytes, ("arena overflow", self.off, nb)
        a = self.arena[:, self.off // 4:(self.off + nb) // 4]
        self.off += nb
        self.peak = max(self.peak, self.off)
        if dtype != F32:
            a = a.bitcast(dtype)
        return a[:, 0:n]

    def mark(self):
        return self.off

    def release(self, m):
        self.off = m

    def _need(self, reads, writes):
        toks = []
        for r in reads:
            s = self.res.get(r)
            if s and s["w"] is not None:
                toks.append(s["w"])
        for w in writes:
            s = self.res.get(w)
            if s:
                if s["w"] is not None:
                    toks.append(s["w"])
                for e, n in s["rc"].items():
                    toks.append(("c", e, n))
                for d in s["rd"]:
                    toks.append(("d", d))
        return toks

    def _emit_waits(self, eng, toks):
        lst = self.ops[eng]
        best = {}
        for t in toks:
            if t[0] == "c":
                _, p, n = t
                if p == "tensor" and eng == "tensor":
                    continue
                if self.seen[eng][p] < n:
                    best[p] = max(best.get(p, 0), n)
            else:
                d = t[1]
                if d not in self.dseen[eng]:
                    self.dseen[eng].add(d)
                    k, v = d % NDSEM, 16 * (d // NDSEM + 1)
                    lst.append(lambda e, S, k=k, v=v: e.wait_ge(S["d"][k], v))
        for p, n in best.items():
            self.seen[eng][p] = n
            k, v = (n - 1) // SEM_ROLL, (n - 1) % SEM_ROLL + 1
            lst.append(lambda e, S, p=p, k=k, v=v: e.wait_ge(S[p][k], v))

    def _commit(self, tok, reads, writes):
        for r in reads:
            s = self.res.setdefault(r, {"w": None, "rc": {}, "rd": set()})
            if tok[0] == "c":
                s["rc"][tok[1]] = tok[2]
            else:
                s["rd"].add(tok[1])
        for w in writes:
            self.res[w] = {"w": tok, "rc": {}, "rd": set()}

    @staticmethod
    def _bank_keys(reads, writes):
        reads, writes = list(reads), list(writes)
        for k in reads + writes:
            b = None
            if k.startswith("pq") and k[2:].isdigit():
                b = int(k[2:]) // 4
            elif k.startswith("ps") and k[2:].isdigit():
                b = int(k[2:])
            if b is not None and f"PB{b}" not in writes:
                writes.append(f"PB{b}")
        return reads, writes

    rec = None

    def replay_interleaved(self, bufs):
        self.rec = None
        n = max(len(b) for b in bufs)
        for i in range(n):
            for b in bufs:
                if i < len(b):
                    kind, args = b[i]
                    if kind == "op":
                        self.op(*args)
                    else:
                        self.dma(*args[:-1], **args[-1])

    def op(self, eng, fn, reads=(), writes=()):
        if self.rec is not None:
            self.rec.append(("op", (eng, fn, list(reads), list(writes))))
            return
        reads, writes = self._bank_keys(reads, writes)
        self._emit_waits(eng, self._need(reads, writes))
        self.cnt[eng] += 1
        n = self.cnt[eng]
        k = (n - 1) // SEM_ROLL
        self.ops[eng].append(lambda e, S, fn=fn, p=eng, k=k: fn(e).then_inc(S[p][k], 1))
        self._commit(("c", eng, n), reads, writes)

    def dma(self, eng, out, in_, reads=(), writes=(), **kw):
        if self.rec is not None:
            self.rec.append(("dma", (eng, out, in_, list(reads), list(writes), kw)))
            return
        d = self.ndma
        self.ndma += 1
        toks = self._need(reads, writes)
        if d >= NDSEM:
            toks.append(("d", d - NDSEM))
        self._emit_waits(eng, toks)
        k = d % NDSEM
        self.ops[eng].append(lambda e, S, k=k, out=out, in_=in_, kw=kw: e.dma_start(out=out, in_=in_, **kw).then_inc(S["d"][k], 16))
        self.dma_issuer[d] = eng
        self._commit(("d", d), reads, writes)
        return d

    def barrier(self):
        for e in ENG:
            toks = [("c", p, self.cnt[p]) for p in ENG if p != e and self.cnt[p] > 0]
            toks += [("d", d) for d in range(max(0, self.ndma - NDSEM), self.ndma)]
            self._emit_waits(e, toks)

    def finish(self):
        self._emit_waits("sync", [("d", d) for d in range(max(0, self.ndma - NDSEM), self.ndma)])
        nc = self.nc
        with contextlib.ExitStack() as st:
            S = {}
            for e in ENG:
                ns = self.cnt[e] // SEM_ROLL + 1
                S[e] = [st.enter_context(nc.semaphore(f"s_{e}_{i}")) for i in range(ns)]
            S["d"] = [st.enter_context(nc.semaphore(f"s_d_{i}")) for i in range(NDSEM)]
            block = st.enter_context(nc.Block())

            def run(engname):
                def _f(eng):
                    for f in self.ops[engname]:
                        f(eng, S)
                return _f

            block.sync(run("sync"))
            block.tensor(run("tensor"))
            block.vector(run("vector"))
            block.scalar(run("scalar"))
            block.gpsimd(run("gpsimd"))


D = 1024
T = 2304
NT = 18
CTX = 256
SEQ = 2048
EPS = 1e-6
BLKS = [(0, 256), (256, 512), (768, 512), (1280, 512), (1792, 512)]
NIN = 2992
NEXP = 32


def tile_blk(ti):
    return 0 if ti < 2 else 1 + (ti - 2) // 4


def host_consts():
    c = {}
    i = np.arange(128)
    U = (i[:, None] <= i[None, :]).astype(np.float32)
    L = (i[:, None] >= i[None, :]).astype(np.float32)
    SU = (i[:, None] < i[None, :]).astype(np.float32)
    SL = (i[:, None] > i[None, :]).astype(np.float32)
    ident = np.eye(128, dtype=np.float32)
    ones = np.ones((128, 128), np.float32)
    bd64 = np.kron(np.eye(2, dtype=np.float32), np.ones((64, 64), np.float32))

    def rmat(rot, off, size):
        R = np.zeros((size, size), np.float32)
        q = rot // 4
        for ax in range(2):
            b = off + ax * 2 * q
            for k in range(q):
                R[b + k, b + q + k] = -1.0
                R[b + q + k, b + k] = 1.0
        return R
    RA = np.kron(np.eye(2, dtype=np.float32), rmat(64, 0, 64))
    RC = np.zeros((128, 128), np.float32)
    RC[:96, :96] = rmat(32, 64, 96)
    E32 = np.zeros((128, 128), np.float32)
    for k in range(32):
        E32[k, 64 + k] = 1.0
    c["cf"] = np.concatenate([ident, U, L, SU, SL, ones, bd64, RA.T.copy(), RC.T.copy(), E32], axis=1).astype(np.float32)

    def tables(rot):
        nf = rot // 4
        t = np.arange(SEQ)
        row = (t // 64).astype(np.float32)
        col = (t % 64).astype(np.float32)
        freq = (np.float32(10000.0) ** (-np.arange(nf, dtype=np.float32) / np.float32(nf))).astype(np.float32)
        ang = np.stack([row[:, None] * freq, col[:, None] * freq], axis=1)
        cs, sn = np.cos(ang).astype(np.float32), np.sin(ang).astype(np.float32)
        C = np.concatenate([cs[:, 0], cs[:, 0], cs[:, 1], cs[:, 1]], axis=1).T
        S = np.concatenate([sn[:, 0], sn[:, 0], sn[:, 1], sn[:, 1]], axis=1).T
        return C, S
    CA, SA = tables(64)
    c["ropeA"] = np.concatenate([np.concatenate([CA, CA], 0), np.concatenate([SA, SA], 0)], axis=1).astype(np.float32)
    CC, SC = tables(32)
    cc = np.ones((128, SEQ), np.float32)
    sc = np.zeros((128, SEQ), np.float32)
    cc[64:96] = CC
    sc[64:96] = SC
    c["ropeC"] = np.concatenate([cc, sc], axis=1).astype(np.float32)
    return c


class K:
    def __init__(self, nseq=2, nlayers=2, phases="CABM", dbg=None, moe=True):
        self.nseq, self.nlayers, self.phases, self.dbg = nseq, nlayers, phases, dbg
        nc = self.nc = bass.Bass("TRN2", target_bir_lowering=False)
        self.P = Prog(nc)
        self.all_in = []

        def dt(n, s):
            a = nc.dram_tensor(n, list(s), F32, kind="ExternalInput").ap()
            self.all_in.append(a)
            return a
        self.x2 = dt("x2", (2, SEQ, D))
        self.ctx2 = dt("ctx2", (2, CTX, D))
        self.vp1 = dt("vp1", (128, 128))
        self.vp2 = dt("vp2", (32, 128))
        self.w_mod = dt("w_mod", (2, D, 6 * D))
        self.w_in = dt("w_in", (2, D, NIN))
        self.w_out = dt("w_out", (2, 1024, D))
        self.swa_q_norm = dt("swa_q_norm", (2, 64))
        self.swa_k_norm = dt("swa_k_norm", (2, 64))
        self.swa_sink = dt("swa_sink", (2, 4))
        self.gdn_conv = dt("gdn_conv", (2, 5, 1536))
        self.gdn_a_log = dt("gdn_a_log", (2, 8))
        self.gdn_dt_bias = dt("gdn_dt_bias", (2, 8))
        self.gdn_out_norm = dt("gdn_out_norm", (2, 128))
        self.mla_q_a_norm = dt("mla_q_a_norm", (2, 256))
        self.mla_w_uq = dt("mla_w_uq", (2, 256, 384))
        self.mla_kv_a_norm = dt("mla_kv_a_norm", (2, 128))
        self.mla_w_ukv = dt("mla_w_ukv", (2, 128, 512))
        self.mla_q_norm = dt("mla_q_norm", (2, 96))
        self.mla_k_norm = dt("mla_k_norm", (2, 96))
        self.router_w = dt("router_w", (2, D, NEXP))
        self.router_b = dt("router_b", (2, NEXP))
        self.exp_w_gu = dt("exp_w_gu", (2, NEXP, D, 2048) if moe else (2, 1, 8, 8))
        self.exp_b_gu = dt("exp_b_gu", (2, NEXP, 2048))
        self.exp_w_dn = dt("exp_w_dn", (2, NEXP, D, D) if moe else (2, 1, 8, 8))
        self.exp_b_dn = dt("exp_b_dn", (2, NEXP, D))
        self.cf = dt("cf", (128, 1280))
        self.ropeA = dt("ropeA", (128, 4096))
        self.ropeC = dt("ropeC", (128, 4096))
        self.out = nc.dram_tensor("out", [2, SEQ, D], F32, kind="ExternalOutput").ap()
        self.dbg_out = None
        if dbg:
            self.dbg_out = nc.dram_tensor("dbg", [128, dbg], F32, kind="ExternalOutput").ap()
        self.uid = 0

    def MM(self, out, lhsT, rhs, start=True, stop=True, r=(), w=()):
        self.P.op("tensor", lambda e, o=out, a=lhsT, b=rhs, s=start, t=stop: e.matmul(o, a, b, start=s, stop=t), r, w)

    def TR(self, out, in_, r=(), w=()):
        npar = in_.shape[0]
        idn = self.ident[0:npar, 0:npar]
        self.P.op("tensor", lambda e, o=out, a=in_: e.transpose(o, a, idn), list(r) + ["const"], w)

    def ACT(self, out, in_, func, r=(), w=(), scale=None, bias=None, eng="scalar"):
        kw = {}
        if scale is not None:
            kw["scale"] = scale
        if bias is not None:
            kw["bias"] = bias
        self.P.op("scalar", lambda e, o=out, a=in_, f=func, kw=kw: e.activation(o, a, f, **kw), r, w)

    def TS(self, eng, out, in0, s1, op0, s2=None, op1=None, r=(), w=()):
        if op1 is None:
            self.P.op(eng, lambda e, o=out, a=in0: e.tensor_scalar(out=o, in0=a, scalar1=s1, scalar2=None, op0=op0), r, w)
        else:
            self.P.op(eng, lambda e, o=out, a=in0: e.tensor_scalar(out=o, in0=a, scalar1=s1, scalar2=s2, op0=op0, op1=op1), r, w)

    def TT(self, eng, out, a, b, op, r=(), w=()):
        self.P.op(eng, lambda e, o=out, x=a, y=b: e.tensor_tensor(out=o, in0=x, in1=y, op=op), r, w)

    def STT(self, out, a, s, b, op0, op1, r=(), w=()):
        self.P.op("vector", lambda e, o=out, x=a, y=b: e.scalar_tensor_tensor(out=o, in0=x, scalar=s, in1=y, op0=op0, op1=op1), r, w)

    def CP(self, eng, out, in_, r=(), w=()):
        if eng == "scalar":
            self.P.op(eng, lambda e, o=out, a=in_: e.copy(o, a), r, w)
        else:
            self.P.op(eng, lambda e, o=out, a=in_: e.tensor_copy(o, a), r, w)

    def RECIP(self, out, in_, r=(), w=()):
        self.P.op("vector", lambda e, o=out, a=in_: e.reciprocal(o, a), r, w)

    def MEMSET(self, eng, ap, val, w=()):
        self.P.op(eng, lambda e, a=ap: e.memset(a, val), (), w)

    def DMA(self, out, in_, r=(), w=(), eng="sync"):
        self.P.dma(eng, out, in_, r, w, allow_slow_non_contiguous=True)

    def key(self, base):
        self.uid += 1
        return f"{base}#{self.uid}"

    def dump(self, ap, ncols, r, col0=0, rows=128):
        P = self.P
        m = P.mark()
        tmp = P.alloc(64, F32)
        for c0 in range(0, ncols, 64):
            n = min(64, ncols - c0)
            k = self.key("dump")
            self.CP("vector", tmp[0:rows, 0:n], ap[:, c0:c0 + n], r=list(r) + ["dumptmp"], w=[k, "dumptmp"])
            self.DMA(self.dbg_out[0:rows, col0 + c0:col0 + c0 + n], tmp[0:rows, 0:n], r=[k], w=[self.key("dbgout"), "dumptmp"])
        P.barrier()
        P.release(m)

    def setup(self):
        P = self.P
        cf = P.alloc(1280, F32)
        self.DMA(cf, self.cf, w=["const"])
        self.ident = cf[:, 0:128]
        self.U, self.L = cf[:, 128:256], cf[:, 256:384]
        self.SU, self.SL = cf[:, 384:512], cf[:, 512:640]
        self.ones = cf[:, 640:768]
        cb = P.alloc(640, BF16)
        self.CP("vector", cb, cf[:, 640:1280], r=["const"], w=["constb"])
        self.ones_b, self.bd64_b = cb[:, 0:128], cb[:, 128:256]
        self.RAT_b, self.RCT_b, self.E32_b = cb[:, 256:384], cb[:, 384:512], cb[:, 512:640]
        junk = P.alloc(64, F32)
        for i, a in enumerate(self.all_in):
            fl = a
            while len(fl.shape) > 1:
                fl = fl[0]
            self.DMA(junk[0:1, i:i + 1], fl[0:1].rearrange("(a b) -> a b", a=1), w=["junk"])
        self.eps = P.alloc(1, F32)
        self.MEMSET("vector", self.eps, EPS, w=["const2"])
        vt = P.alloc(128, F32)
        self.DMA(vt, self.vp1, w=["vt"])
        self.VP = P.alloc(128, F32)
        self.TR(self.ps[:, 0, 0:128], vt, r=["vt"], w=["ps0"])
        self.CP("vector", self.VP, self.ps[:, 0, 0:128], r=["ps0"], w=["VP"])
        vt2 = P.alloc(128, F32)
        self.DMA(vt2[0:32, :], self.vp2, w=["vt2"])
        self.CS = P.alloc(32, F32)
        self.TR(self.ps[:, 1, 0:32], vt2[0:32, :], r=["vt2"], w=["ps1"])
        self.ACT(self.CS, self.ps[:, 1, 0:32], AF.Silu, r=["ps1"], w=["CS"])
        self.xT = P.alloc(8 * T, F32).rearrange("p (c t) -> p c t", c=8)
        self.hT = P.alloc(8 * T, BF16).rearrange("p (c t) -> p c t", c=8)
        self.stg = [P.alloc(1024, F32), P.alloc(1024, F32)]
        self.stg_i = 0
        self.MOD = [P.alloc(96, F32).rearrange("p (j y) -> p j y", y=2) for _ in range(2)]
        self.A1 = [P.alloc(16, F32).rearrange("p (c y) -> p c y", y=2) for _ in range(2)]
        self.A2 = [P.alloc(16, F32).rearrange("p (c y) -> p c y", y=2) for _ in range(2)]

    def stage(self):
        i = self.stg_i
        self.stg_i ^= 1
        return self.stg[i], f"stg{i}"

    def load_cast(self, dst, src, ncols, eng="gpsimd", dkey=None, parts=128):
        st, sk = self.stage()
        self.DMA(st[0:parts, 0:ncols], src, w=[sk])
        self.CP(eng, dst, st[0:parts, 0:ncols], r=[sk], w=[dkey])

    def load_w_cols(self, dst, wsrc, c0, ncols, dkey, kc=8):
        per = max(1, 1024 // ncols)
        for k0 in range(0, kc, per):
            k1 = min(kc, k0 + per)
            st, sk = self.stage()
            sv = st[:, 0:(k1 - k0) * ncols].rearrange("p (k n) -> p k n", n=ncols)
            self.DMA(sv, wsrc[k0 * 128:k1 * 128, c0:c0 + ncols].rearrange("(k p) n -> p k n", p=128), w=[sk])
            self.CP("gpsimd", dst[:, k0:k1, :], sv, r=[sk], w=[dkey])

    def load_x(self, s):
        P = self.P
        m = P.mark()
        bufs = [P.alloc(1024, F32), P.alloc(1024, F32)]
        for ti in range(NT):
            b = bufs[ti % 2]
            bk = f"xl{ti % 2}"
            src = self.ctx2[s, ti * 128:(ti + 1) * 128, :] if ti < 2 else self.x2[s, (ti - 2) * 128:(ti - 1) * 128, :]
            self.DMA(b, src, w=[bk])
            for half in range(2):
                bank = 2 * (ti % 2) + half
                for c4 in range(4):
                    c = half * 4 + c4
                    self.TR(self.ps[:, bank, c4 * 128:(c4 + 1) * 128], b[:, c * 128:(c + 1) * 128], r=[bk], w=[f"ps{bank}"])
                eng = "vector" if half == 0 else "scalar"
                self.CP(eng, self.xT[:, half * 4:half * 4 + 4, ti * 128:(ti + 1) * 128],
                        self.ps[:, bank, :].rearrange("p (c t) -> p c t", c=4), r=[f"ps{bank}"], w=[f"xT{tile_blk(ti)}"])
        P.barrier()
        P.release(m)

    def store_x(self, s):
        P = self.P
        m = P.mark()
        bufs = [P.alloc(1024, F32), P.alloc(1024, F32)]
        for ti in range(2, NT):
            b = bufs[ti % 2]
            bk = f"xs{ti % 2}"
            for half in range(2):
                bank = 2 * (ti % 2) + half
                for c4 in range(4):
                    c = half * 4 + c4
                    self.TR(self.ps[:, bank, c4 * 128:(c4 + 1) * 128], self.xT[:, c, ti * 128:(ti + 1) * 128], r=[f"xT{tile_blk(ti)}"], w=[f"ps{bank}"])
                eng = "vector" if half == 0 else "scalar"
                self.CP(eng, b[:, half * 512:(half + 1) * 512], self.ps[:, bank, :], r=[f"ps{bank}"], w=[bk])
            self.DMA(self.out[s, (ti - 2) * 128:(ti - 1) * 128, :], b, r=[bk], w=[self.key("out")])
        P.barrier()
        P.release(m)

    def mods(self, s, l):
        P = self.P
        m = P.mark()
        sc = P.alloc(16, F32).rearrange("p (k y) -> p k y", y=2)
        kk = self.key("sc")
        self.CP("vector", sc[:, :, 0], self.CS[:, s * 8:s * 8 + 8], r=["CS"], w=[kk])
        self.CP("vector", sc[:, :, 1], self.CS[:, 16:24], r=["CS"], w=[kk])
        MOD = self.MOD[l]
        for j in range(48):
            st, sk = self.stage()
            sv = st[:, 0:1024].rearrange("p (k n) -> p k n", n=128)
            self.DMA(sv, self.w_mod[l, :, j * 128:(j + 1) * 128].rearrange("(k p) n -> p k n", p=128), w=[sk])
            for k in range(8):
                self.MM(self.ps[:, 7, 2 * j:2 * j + 2], sv[:, k, :], sc[:, k, :], start=(k == 0), stop=(k == 7), r=[sk, kk], w=["ps7"])
        mk = f"MOD{l}"
        bm = self.VP[:, l * 64:l * 64 + 48]
        pv = self.ps[:, 7, 0:96].rearrange("p (j y) -> p j y", y=2)
        for y in range(2):
            self.TT("vector", MOD[:, :, y], pv[:, :, y], bm, ALU.add, r=["ps7", "VP"], w=[mk])
        n1 = self.VP[:, l * 64 + 48:l * 64 + 56]
        n2 = self.VP[:, l * 64 + 56:l * 64 + 64]
        for y in range(2):
            self.STT(self.A1[l][:, :, y], MOD[:, 8:16, y], 1.0, n1, ALU.add, ALU.mult, r=[mk, "VP"], w=[mk + "a"])
            self.STT(self.A2[l][:, :, y], MOD[:, 32:40, y], 1.0, n2, ALU.add, ALU.mult, r=[mk, "VP"], w=[mk + "a"])
        P.barrier()
        P.release(m)

    def norm_mod(self, l, A, shift_j, router=None):
        P = self.P
        m = P.mark()
        sq = P.alloc(8 * 512, BF16).rearrange("p (c t) -> p c t", c=8)
        sd = P.alloc(512, F32)
        rs = P.alloc(512, F32)
        tmp = [P.alloc(512, F32), P.alloc(512, F32)]
        MOD = self.MOD[l]
        for bi, (t0, n) in enumerate(BLKS):
            y = 1 if bi == 0 else 0
            xk, hk = f"xT{bi}", f"hT{bi}"
            for c in range(8):
                self.ACT(sq[:, c, 0:n], self.xT[:, c, t0:t0 + n], AF.Square, r=[xk], w=[f"sq{c}"])
            for c in range(8):
                self.MM(self.ps[:, 0, 0:n], self.ones_b, sq[:, c, 0:n], start=(c == 0), stop=(c == 7), r=[f"sq{c}", "constb"], w=["ps0"])
            self.ACT(sd[:, 0:n], self.ps[:, 0, 0:n], AF.Sqrt, scale=1.0 / D, bias=self.eps, r=["ps0", "const2"], w=["sd"])
            self.RECIP(rs[:, 0:n], sd[:, 0:n], r=["sd"], w=["rs"])
            for c in range(8):
                tb, tk = tmp[c % 2], f"nt{c % 2}"
                if router is not None:
                    tb, tk = router["h2f"][:, c, :], f"h2f{c}"
                self.STT(tb[:, 0:n], self.xT[:, c, t0:t0 + n], A[:, c, y:y + 1], rs[:, 0:n], ALU.mult, ALU.mult, r=[xk, "rs", f"MOD{l}a"], w=[tk])
                if router is not None:
                    self.TS("vector", tb[:, 0:n], tb[:, 0:n], MOD[:, shift_j + c, y:y + 1], ALU.add, r=[tk, f"MOD{l}"], w=[tk])
                    self.CP("scalar", self.hT[:, c, t0:t0 + n], tb[:, 0:n], r=[tk], w=[hk])
                else:
                    self.ACT(self.hT[:, c, t0:t0 + n], tb[:, 0:n], AF.Identity, bias=MOD[:, shift_j + c, y:y + 1], r=[tk, f"MOD{l}"], w=[hk])
            if router is not None:
                router["fn"](bi, t0, n)
        P.barrier()
        P.release(m)

    def head_norm_rope(self, src, npar, n, dst, onesl, inv_dim, gain, RT, rope_dram, t0, sk, dk, W):
        ps = self.ps
        sq, sd, rs, qn, t1, t2, rb = W["sq"], W["sd"], W["rs"], W["qn"], W["t1"], W["t2"], W["rb"]
        self.ACT(sq[0:npar, 0:n], src, AF.Square, r=[sk], w=["w_sq"])
        self.MM(ps[0:npar, 5, 0:n], onesl[0:npar, 0:npar], sq[0:npar, 0:n], r=["w_sq", "constb"], w=["ps5"])
        self.ACT(sd[0:npar, 0:n], ps[0:npar, 5, 0:n], AF.Sqrt, scale=inv_dim, bias=self.eps[0:npar, :], r=["ps5", "const2"], w=["w_sd"])
        self.RECIP(rs[0:npar, 0:n], sd[0:npar, 0:n], r=["w_sd"], w=["w_rs"])
        if t0 < CTX:
            self.STT(dst, src, gain, rs[0:npar, 0:n], ALU.mult, ALU.mult, r=[sk, "w_rs", "gains"], w=[dk])
            return
        self.STT(qn[0:npar, 0:n], src, gain, rs[0:npar, 0:n], ALU.mult, ALU.mult, r=[sk, "w_rs", "gains"], w=["w_qn"])
        self.MM(ps[0:npar, 6, 0:n], RT[0:npar, 0:npar], qn[0:npar, 0:n], r=["w_qn", "constb"], w=["ps6"])
        self.DMA(rb[:, 0, 0:n], rope_dram[:, t0 - CTX:t0 - CTX + n], w=["w_rb"])
        self.DMA(rb[:, 1, 0:n], rope_dram[:, SEQ + t0 - CTX:SEQ + t0 - CTX + n], w=["w_rb"])
        self.TT("gpsimd", t1[0:npar, 0:n], qn[0:npar, 0:n], rb[0:npar, 0, 0:n], ALU.mult, r=["w_qn", "w_rb"], w=["w_t1"])
        self.TT("vector", t2[0:npar, 0:n], ps[0:npar, 6, 0:n], rb[0:npar, 1, 0:n], ALU.mult, r=["ps6", "w_rb"], w=["w_t2"])
        self.TT("vector", dst, t1[0:npar, 0:n], t2[0:npar, 0:n], ALU.add, r=["w_t1", "w_t2"], w=[dk])

    def norm_temps(self):
        P = self.P
        return dict(sq=P.alloc(512, BF16), sd=P.alloc(512, F32), rs=P.alloc(512, F32), qn=P.alloc(512, BF16),
                    t1=P.alloc(512, BF16), t2=P.alloc(512, BF16), rb=P.alloc(1024, F32).rearrange("p (a n) -> p a n", a=2))

    def out_proj(self, l, Wo, kparts, mix, mixk, wk, nacc=1):
        ps = self.ps
        MOD = self.MOD[l]
        cnt = 0
        for bi, (t0, n) in enumerate(BLKS):
            y = 1 if bi == 0 else 0
            for dc in range(8):
                bank = cnt % 4
                cnt += 1
                for a in range(nacc):
                    self.MM(ps[:, bank, 0:n], Wo[0:kparts, a, dc * 128:(dc + 1) * 128], mix[0:kparts, a, t0:t0 + n],
                            start=(a == 0), stop=(a == nacc - 1), r=[wk, mixk], w=[f"ps{bank}"])
                self.STT(self.xT[:, dc, t0:t0 + n], ps[:, bank, 0:n], MOD[:, 16 + dc, y:y + 1], self.xT[:, dc, t0:t0 + n],
                         ALU.mult, ALU.add, r=[f"ps{bank}", f"MOD{l}", f"xT{bi}"], w=[f"xT{bi}"])

    def mixer_C(self, l):
        P, ps = self.P, self.ps
        m = P.mark()
        WC = P.alloc(8 * 416, BF16).rearrange("p (k n) -> p k n", k=8)
        self.load_w_cols(WC, self.w_in[l], 2576, 416, "WC")
        Wuq = P.alloc(2 * 384, BF16).rearrange("p (k n) -> p k n", k=2)
        self.load_w_cols(Wuq, self.mla_w_uq[l], 0, 384, "Wuq", kc=2)
        Wk = P.alloc(4 * 96, BF16).rearrange("p (h n) -> p h n", h=4)
        Wv = P.alloc(256, BF16).rearrange("p (h n) -> p h n", h=4)
        self.MEMSET("vector", Wk, 0.0, w=["Wk"])
        st, sk = self.stage()
        self.DMA(st[:, 0:512], self.mla_w_ukv[l], w=[sk])
        sv = st[:, 0:512].rearrange("p (h n) -> p h n", h=4)
        self.CP("gpsimd", Wk[:, :, 0:64], sv[:, :, 0:64], r=[sk], w=["Wk"])
        self.CP("gpsimd", Wv, sv[:, :, 64:128], r=[sk], w=["Wv"])
        g = P.alloc(8, F32)
        self.DMA(g[:, 0:2], self.mla_q_a_norm[l].rearrange("(c p) -> p c", p=128), w=["gains"])
        self.DMA(g[:, 2:3], self.mla_kv_a_norm[l].rearrange("(p o) -> p o", o=1), w=["gains"])
        self.DMA(g[0:96, 5:6], self.mla_q_norm[l].rearrange("(p o) -> p o", o=1), w=["gains"])
        self.DMA(g[0:96, 4:5], self.mla_k_norm[l].rearrange("(p o) -> p o", o=1), w=["gains"])
        self.TS("vector", g[0:96, 3:4], g[0:96, 5:6], 96.0 ** -0.5, ALU.mult, r=["gains"], w=["gains"])
        cqn = P.alloc(2 * T, BF16).rearrange("p (c t) -> p c t", c=2)
        ckvn = P.alloc(T, BF16)
        krT = P.alloc(T, BF16)
        W = self.norm_temps()
        for bi, (t0, n) in enumerate(BLKS):
            hk = f"hT{bi}"
            for o, (c0, npar) in enumerate([(0, 128), (128, 128), (256, 128), (384, 32)]):
                for k in range(8):
                    self.MM(ps[0:npar, o, 0:n], WC[:, k, c0:c0 + npar], self.hT[:, k, t0:t0 + n], start=(k == 0), stop=(k == 7), r=["WC", hk], w=[f"ps{o}"])
            for c in range(2):
                self.ACT(W["sq"][:, 0:n], ps[:, c, 0:n], AF.Square, r=[f"ps{c}"], w=["w_sq"])
                self.MM(ps[:, 5, 0:n], self.ones_b, W["sq"][:, 0:n], start=(c == 0), stop=(c == 1), r=["w_sq", "constb"], w=["ps5"])
            self.ACT(W["sd"][:, 0:n], ps[:, 5, 0:n], AF.Sqrt, scale=1.0 / 256, bias=self.eps, r=["ps5", "const2"], w=["w_sd"])
            self.RECIP(W["rs"][:, 0:n], W["sd"][:, 0:n], r=["w_sd"], w=["w_rs"])
            for c in range(2):
                self.STT(cqn[:, c, t0:t0 + n], ps[:, c, 0:n], g[:, c:c + 1], W["rs"][:, 0:n], ALU.mult, ALU.mult, r=[f"ps{c}", "w_rs", "gains"], w=[f"cqn{bi}"])
            self.ACT(W["sq"][:, 0:n], ps[:, 2, 0:n], AF.Square, r=["ps2"], w=["w_sq"])
            self.MM(ps[:, 5, 0:n], self.ones_b, W["sq"][:, 0:n], r=["w_sq", "constb"], w=["ps5"])
            self.ACT(W["sd"][:, 0:n], ps[:, 5, 0:n], AF.Sqrt, scale=1.0 / 128, bias=self.eps, r=["ps5", "const2"], w=["w_sd"])
            self.RECIP(W["rs"][:, 0:n], W["sd"][:, 0:n], r=["w_sd"], w=["w_rs"])
            self.STT(ckvn[:, t0:t0 + n], ps[:, 2, 0:n], g[:, 2:3], W["rs"][:, 0:n], ALU.mult, ALU.mult, r=["ps2", "w_rs", "gains"], w=[f"ckvn{bi}"])
            self.CP("scalar", krT[0:32, t0:t0 + n], ps[0:32, 3, 0:n], r=["ps3"], w=[f"krT{bi}"])
        allc = [f"cqn{b}" for b in range(5)]
        allkv = [f"ckvn{b}" for b in range(5)]
        qr = P.alloc(T, BF16)
        kr = P.alloc(T, BF16)
        Vh = P.alloc(NT * 64, BF16).rearrange("p (t d) -> p t d", d=64)
        mixh = P.alloc(T, BF16)
        WoC = P.alloc(1024, BF16)
        Eb = [P.alloc(512, BF16) for _ in range(3)]
        rd = P.alloc(512, F32)
        for h in range(4):
            self.load_cast(WoC[0:64, :], self.w_out[l, 768 + h * 64:768 + (h + 1) * 64, :], 1024, dkey="WoC", parts=64)
            for bi, (t0, n) in enumerate(BLKS):
                for k in range(2):
                    self.MM(ps[0:96, 0, 0:n], Wuq[:, k, h * 96:(h + 1) * 96], cqn[:, k, t0:t0 + n], start=(k == 0), stop=(k == 1), r=["Wuq", f"cqn{bi}"], w=["ps0"])
                self.head_norm_rope(ps[0:96, 0, 0:n], 96, n, qr[0:96, t0:t0 + n], self.ones_b, 1.0 / 96, g[0:96, 3:4], self.RCT_b, self.ropeC, t0, "ps0", f"qr{bi}", W)
                self.MM(ps[0:96, 1, 0:n], Wk[:, h, :], ckvn[:, t0:t0 + n], start=True, stop=False, r=["Wk", f"ckvn{bi}"], w=["ps1"])
                self.MM(ps[0:96, 1, 0:n], self.E32_b[0:32, 0:96], krT[0:32, t0:t0 + n], start=False, stop=True, r=["constb", f"krT{bi}"], w=["ps1"])
                self.head_norm_rope(ps[0:96, 1, 0:n], 96, n, kr[0:96, t0:t0 + n], self.ones_b, 1.0 / 96, g[0:96, 4:5], self.RCT_b, self.ropeC, t0, "ps1", f"kr{bi}", W)
            for ti in range(NT):
                bank = 2 + ti % 2
                self.MM(ps[:, bank, 0:64], ckvn[:, ti * 128:(ti + 1) * 128], Wv[:, h, :], r=allkv + ["Wv"], w=[f"ps{bank}"])
                self.CP("scalar", Vh[:, ti, :], ps[:, bank, 0:64], r=[f"ps{bank}"], w=["Vh"])
            allq = [f"qr{b}" for b in range(5)]
            allk = [f"kr{b}" for b in range(5)]
            for qi, (t0, n) in enumerate(BLKS):
                kts = [0, 1] if qi == 0 else list(range(NT))
                ob, db = (4, 5) if qi % 2 == 0 else (6, 7)
                for idx, kt in enumerate(kts):
                    sb = idx % 4
                    E, ek = Eb[idx % 3], f"E{idx % 3}"
                    self.MM(ps[:, sb, 0:n], kr[0:96, kt * 128:(kt + 1) * 128], qr[0:96, t0:t0 + n], r=allk + [f"qr{qi}"], w=[f"ps{sb}"])
                    self.ACT(E[:, 0:n], ps[:, sb, 0:n], AF.Exp, r=[f"ps{sb}"], w=[ek])
                    self.MM(ps[0:64, ob, 0:n], Vh[:, kt, :], E[:, 0:n], start=(idx == 0), stop=(idx == len(kts) - 1), r=["Vh", ek], w=[f"ps{ob}"])
                    self.MM(ps[0:64, db, 0:n], self.ones_b[:, 0:64], E[:, 0:n], start=(idx == 0), stop=(idx == len(kts) - 1), r=["constb", ek], w=[f"ps{db}"])
                self.RECIP(rd[0:64, 0:n], ps[0:64, db, 0:n], r=[f"ps{db}"], w=["rd"])
                self.TT("vector", mixh[0:64, t0:t0 + n], ps[0:64, ob, 0:n], rd[0:64, 0:n], ALU.mult, r=[f"ps{ob}", "rd"], w=[f"mixh{qi}"])
            if self.dbg and self.dbg_what == f"C{h}":
                self.dump(mixh[0:64, :], T, [f"mixh{b}" for b in range(5)], rows=64)
            self.out_proj_heads(l, WoC, mixh)
        P.barrier()
        P.release(m)

    def out_proj_heads(self, l, Wo, mixh):
        ps = self.ps
        MOD = self.MOD[l]
        cnt = 0
        for bi, (t0, n) in enumerate(BLKS):
            y = 1 if bi == 0 else 0
            for dc in range(8):
                bank = cnt % 4
                cnt += 1
                self.MM(ps[:, bank, 0:n], Wo[0:64, dc * 128:(dc + 1) * 128], mixh[0:64, t0:t0 + n], r=["WoC", f"mixh{bi}"], w=[f"ps{bank}"])
                self.STT(self.xT[:, dc, t0:t0 + n], ps[:, bank, 0:n], MOD[:, 16 + dc, y:y + 1], self.xT[:, dc, t0:t0 + n],
                         ALU.mult, ALU.add, r=[f"ps{bank}", f"MOD{l}", f"xT{bi}"], w=[f"xT{bi}"])

    def build(self, dbg_what=None):
        nc, P = self.nc, self.P
        self.dbg_what = dbg_what
        NB = 48800
        with nc.sbuf_tensor("arena", [128, NB], F32) as arena, nc.psum_tensor("ps", [128, 8, 512], F32) as ps:
            self.ps = ps
            P.set_arena(arena, NB * 4)
            self.setup()
            if dbg_what == "setup":
                self.dump(self.VP, 128, ["VP"])
                P.finish()
                return nc
            for s in range(self.nseq):
                self.load_x(s)
                if dbg_what == "loadx":
                    self.dump(self.xT[:, 0, :], T, [f"xT{b}" for b in range(5)])
                    P.finish()
                    return nc
                for l in range(self.nlayers):
                    self.mods(s, l)
                if dbg_what == "mods":
                    self.dump(self.MOD[0].rearrange("p j y -> p (j y)"), 96, ["MOD0"])
                    P.finish()
                    return nc
                for l in range(self.nlayers):
                    self.layer(s, l)
                    if self.stop:
                        break
                if self.stop:
                    break
                self.store_x(s)
            P.finish()
        return nc

    stop = False

    def layer(self, s, l):
        ph = self.phases
        allx = [f"xT{b}" for b in range(5)]
        allh = [f"hT{b}" for b in range(5)]
        self.norm_mod(l, self.A1[l], 0)
        if self.dbg_what == "h":
            self.dump(self.hT[:, 0, :], T, allh)
            self.stop = True
            return
        if "C" in ph:
            self.mixer_C(l)
        if "A" in ph:
            self.mixer_A(l)
        if "B" in ph:
            self.mixer_B(l)
        if self.dbg_what == "xa":
            self.dump(self.xT[:, 0, :], T, allx)
            self.stop = True
            return
        if "M" in ph:
            self.moe(l)
        if self.dbg_what == "xb":
            self.dump(self.xT[:, 0, :], T, allx)
            self.stop = True
            return


def make_inputs(inputs, core, consts):
    b0 = 2 * core
    f = lambda k: np.ascontiguousarray(inputs[k], dtype=np.float32)
    vp1 = np.concatenate([np.concatenate([f("b_mod")[l].reshape(48, 128), f("norm1")[l].reshape(8, 128), f("norm2")[l].reshape(8, 128)], 0) for l in range(2)], 0)
    vp2 = np.zeros((32, 128), np.float32)
    vp2[0:8] = f("c")[b0].reshape(8, 128)
    vp2[8:16] = f("c")[b0 + 1].reshape(8, 128)
    vp2[16:24] = f("c_ctx").reshape(8, 128)
    m = {"x2": f("x")[b0:b0 + 2], "ctx2": f("ctx")[b0:b0 + 2], "vp1": vp1, "vp2": vp2}
    for k in ["w_mod", "w_in", "w_out", "swa_q_norm", "swa_k_norm", "swa_sink", "gdn_conv", "gdn_out_norm", "mla_q_a_norm", "mla_w_uq",
              "mla_kv_a_norm", "mla_w_ukv", "mla_q_norm", "mla_k_norm", "router_w", "router_b", "exp_w_gu", "exp_b_gu", "exp_w_dn", "exp_b_dn"]:
        m[k] = f(k)
    m["gdn_a_log"] = f("gdn_a_log").reshape(2, 8)
    m["gdn_dt_bias"] = f("gdn_dt_bias").reshape(2, 8)
    m.update(consts)
    return m


def kernel(**inputs):
    k = K(nseq=2, nlayers=2, phases="CABM")
    nc = k.build()
    consts = host_consts()
    in_maps = [make_inputs(inputs, c, consts) for c in range(8)]
    res = run_bass_kernel_spmd(nc, in_maps, core_ids=list(range(8)))
    return np.concatenate([r["out"] for r in res.results], axis=0)


def _mixer_A(self, l):
    P, ps = self.P, self.ps
    m = P.mark()
    WA = P.alloc(8 * 512, BF16).rearrange("p (k n) -> p k n", k=8)
    for d0, c0, nc_ in [(0, 0, 64), (64, 128, 64), (128, 64, 64), (192, 192, 64), (256, 256, 256)]:
        self.load_w_cols(WA[:, :, d0:d0 + nc_], self.w_in[l], c0, nc_, "WA")
    g = P.alloc(8, F32)
    for hh in range(2):
        self.DMA(g[hh * 64:(hh + 1) * 64, 2:3], self.swa_q_norm[l].rearrange("(p o) -> p o", o=1), w=["gains"])
        self.DMA(g[hh * 64:(hh + 1) * 64, 1:2], self.swa_k_norm[l].rearrange("(p o) -> p o", o=1), w=["gains"])
    self.TS("vector", g[:, 0:1], g[:, 2:3], 0.125, ALU.mult, r=["gains"], w=["gains"])
    sk = P.alloc(4, F32)
    self.DMA(sk, self.swa_sink[l].rearrange("(a h) -> a h", a=1).broadcast_to([128, 4]), w=["sk0"])
    self.ACT(sk, sk, AF.Exp, r=["sk0"], w=["sk"])
    qr = P.alloc(2 * T, BF16).rearrange("p (c t) -> p c t", c=2)
    kr = P.alloc(T, BF16)
    V = P.alloc(NT * 128, BF16).rearrange("p (t d) -> p t d", d=128)
    W = self.norm_temps()
    for bi, (t0, n) in enumerate(BLKS):
        for o, c0 in enumerate([0, 128, 256]):
            for k in range(8):
                self.MM(ps[:, o, 0:n], WA[:, k, c0:c0 + 128], self.hT[:, k, t0:t0 + n], start=(k == 0), stop=(k == 7), r=["WA", f"hT{bi}"], w=[f"ps{o}"])
            dst = kr[:, t0:t0 + n] if o == 2 else qr[:, o, t0:t0 + n]
            self.head_norm_rope(ps[:, o, 0:n], 128, n, dst, self.bd64_b, 1.0 / 64, g[:, (1 if o == 2 else 0):(2 if o == 2 else 1)], self.RAT_b, self.ropeA, t0, f"ps{o}", "qkA", W)
    for ti in range(NT):
        bank = 3 + ti % 2
        for k in range(8):
            self.MM(ps[:, bank, 0:128], self.hT[:, k, ti * 128:(ti + 1) * 128], WA[:, k, 384:512], start=(k == 0), stop=(k == 7), r=["WA", f"hT{tile_blk(ti)}"], w=[f"ps{bank}"])
        self.CP("scalar", V[:, ti, :], ps[:, bank, 0:128], r=[f"ps{bank}"], w=["VA"])
    mixg = P.alloc(2 * T, BF16).rearrange("p (c t) -> p c t", c=2)
    WoA = P.alloc(2 * 1024, BF16).rearrange("p (c n) -> p c n", c=2)
    Eb = [P.alloc(5 * 256, BF16).rearrange("p (k n) -> p k n", k=5) for _ in range(2)]
    dn = P.alloc(256, F32)
    for gi in range(2):
        gp = slice(gi * 64, (gi + 1) * 64)
        for j in range(2):
            hd = 2 * gi + j
            self.load_cast(WoA[0:64, j, :], self.w_out[l, hd * 64:(hd + 1) * 64, :], 1024, dkey="WoA", parts=64)
        for ti in range(NT):
            if ti < 2:
                kts = [(0, None), (1, None)]
            else:
                kts = ([(ti - 1, self.L)] if ti - 1 >= 2 else []) + [(ti, None)] + ([(ti + 1, self.U)] if ti + 1 < NT else []) + [(0, None), (1, None)]
            b0 = 0 if ti % 2 == 0 else 3
            E, ek = Eb[ti % 2], f"EA{ti % 2}"
            qrhs = qr[gp, :, ti * 128:(ti + 1) * 128]
            nk = len(kts)
            for idx, (kt, mk) in enumerate(kts):
                bank = b0 + idx // 2
                c0 = (idx % 2) * 256
                self.MM(ps[:, bank, c0:c0 + 256].rearrange("p (j t) -> p j t", j=2), kr[gp, kt * 128:(kt + 1) * 128], qrhs, r=["qkA"], w=[f"ps{bank}"])
            psflat = ps[:, b0:b0 + 3, :].rearrange("p b n -> p (b n)")
            self.ACT(E[:, 0:nk, :].rearrange("p k n -> p (k n)"), psflat[:, 0:nk * 256], AF.Exp, r=[f"ps{b0}", f"ps{b0 + 1}", f"ps{b0 + 2}"], w=[ek])
            for idx, (kt, mk) in enumerate(kts):
                if mk is not None:
                    ev = E[:, idx, :].rearrange("p (j t) -> p j t", j=2)
                    self.TT("gpsimd", ev, ev, mk.rearrange("p (a t) -> p a t", a=1).broadcast_to([128, 2, 128]), ALU.mult, r=[ek, "const"], w=[ek])
            for idx, (kt, mk) in enumerate(kts):
                self.MM(ps[0:64, 6, 0:256], V[:, kt, gi * 64:(gi + 1) * 64], E[:, idx, :], start=(idx == 0), stop=(idx == nk - 1), r=["VA", ek], w=["ps6"])
            for idx, (kt, mk) in enumerate(kts):
                self.MM(ps[0:64, 7, 0:256], self.ones_b[:, 0:64], E[:, idx, :], start=(idx == 0), stop=(idx == nk - 1), r=["constb", ek], w=["ps7"])
            for j in range(2):
                self.TS("vector", dn[0:64, j * 128:(j + 1) * 128], ps[0:64, 7, j * 128:(j + 1) * 128], sk[0:64, 2 * gi + j:2 * gi + j + 1], ALU.add, r=["ps7", "sk"], w=["dnA"])
            self.RECIP(dn[0:64, :], dn[0:64, :], r=["dnA"], w=["dnA"])
            self.TT("vector", mixg[0:64, :, ti * 128:(ti + 1) * 128], ps[0:64, 6, 0:256].rearrange("p (j t) -> p j t", j=2),
                    dn[0:64, :].rearrange("p (j t) -> p j t", j=2), ALU.mult, r=["ps6", "dnA"], w=["mixg"])
        if self.dbg and self.dbg_what == f"A{gi}":
            self.dump(mixg[0:64, 0, :], T, ["mixg"], rows=64)
        self.out_proj(l, WoA, 64, mixg, "mixg", "WoA", nacc=2)
    P.barrier()
    P.release(m)


K.mixer_A = _mixer_A


def _mixer_B(self, l):
    P, ps = self.P, self.ps
    m = P.mark()
    psq = ps.rearrange("p b (q n) -> p (b q) n", q=4)
    allh = [f"hT{b}" for b in range(5)]
    Wab = P.alloc(8 * 16, BF16).rearrange("p (k n) -> p k n", k=8)
    self.load_w_cols(Wab, self.w_in[l], 2560, 16, "Wab")
    for ti in range(NT):
        for k in range(8):
            self.MM(ps[:, 0, ti * 16:(ti + 1) * 16], self.hT[:, k, ti * 128:(ti + 1) * 128], Wab[:, k, :], start=(k == 0), stop=(k == 7), r=["Wab"] + allh, w=["ps0"])
    abv = ps[:, 0, 0:NT * 16].rearrange("p (t j) -> p t j", j=16)
    one = P.alloc(1, F32)
    self.MEMSET("vector", one, 1.0, w=["one"])
    arr = lambda: P.alloc(NT * 8, F32).rearrange("p (t j) -> p t j", j=8)
    dtb, alog, X, AX, G, BETA, GC, GCL, BEG, KD = [arr() for _ in range(10)]
    self.DMA(dtb, self.gdn_dt_bias[l].rearrange("(a b j) -> a b j", a=1, b=1).broadcast_to([128, NT, 8]), w=["dtb"])
    self.DMA(alog, self.gdn_a_log[l].rearrange("(a b j) -> a b j", a=1, b=1).broadcast_to([128, NT, 8]), w=["alog"])
    self.TT("vector", X, abv[:, :, 0:8], dtb, ALU.add, r=["ps0", "dtb"], w=["gX"])
    self.TS("vector", AX, X, -1.0, ALU.mult, r=["gX"], w=["gAX"])
    self.TT("vector", AX, AX, X, ALU.max, r=["gX", "gAX"], w=["gAX"])
    self.ACT(AX, AX, AF.Exp, scale=-1.0, r=["gAX"], w=["gAX"])
    self.ACT(AX, AX, AF.Ln, bias=one, r=["gAX", "one"], w=["gAX"])
    self.STT(X, X, 0.0, AX, ALU.max, ALU.add, r=["gX", "gAX"], w=["gX"])
    self.ACT(alog, alog, AF.Exp, r=["alog"], w=["alog"])
    self.TS("vector", alog, alog, -1.0, ALU.mult, r=["alog"], w=["alog"])
    self.TT("vector", G, X, alog, ALU.mult, r=["gX", "alog"], w=["gG"])
    self.ACT(BETA, abv[:, :, 8:16], AF.Sigmoid, r=["ps0"], w=["gB"])
    if self.dbg_what == "BG3a":
        P.barrier()
        self.dump(G.rearrange("p t j -> p (t j)"), NT * 8, ["gG"], col0=256)
        self.dump(BETA.rearrange("p t j -> p (t j)"), NT * 8, ["gB"], col0=768)
        P.release(m)
        return
    for ti in range(NT):
        self.MM(ps[:, 1, ti * 8:ti * 8 + 4], self.U, G[:, ti, 0:4], r=["gG", "const"], w=["ps1"])
        self.MM(ps[:, 1, ti * 8 + 4:ti * 8 + 8], self.L, G[:, ti, 4:8], r=["gG", "const"], w=["ps1"])
        self.MM(ps[:, 2, ti * 8:ti * 8 + 8], self.ones, G[:, ti, :], r=["gG", "const"], w=["ps2"])
    self.CP("vector", GC, ps[:, 1, 0:NT * 8].rearrange("p (t j) -> p t j", j=8), r=["ps1"], w=["gGC"])
    self.CP("vector", GCL, ps[:, 2, 0:NT * 8].rearrange("p (t j) -> p t j", j=8), r=["ps2"], w=["gGCL"])
    if self.dbg_what == "BG3b":
        P.barrier()
        self.dump(GC.rearrange("p t j -> p (t j)"), NT * 8, ["gGC"], col0=256)
        self.dump(GCL.rearrange("p t j -> p (t j)"), NT * 8, ["gGCL"], col0=768)
        P.release(m)
        return
    self.ACT(BEG, GC, AF.Exp, r=["gGC"], w=["gBEG"])
    self.TT("vector", BEG, BEG, BETA, ALU.mult, r=["gBEG", "gB"], w=["gBEG"])
    self.TT("vector", KD, GCL, GC, ALU.subtract, r=["gGC", "gGCL"], w=["gKD"])
    self.ACT(KD, KD, AF.Exp, r=["gKD"], w=["gKD"])
    self.ACT(GCL, GCL, AF.Exp, r=["gGCL", "gKD"], w=["gGCL"])
    P.barrier()
    if self.dbg_what == "BG3":
        self.dump(KD.rearrange("p t j -> p (t j)"), NT * 8, ["gKD"])
        self.dump(G.rearrange("p t j -> p (t j)"), NT * 8, ["gG"], col0=256)
        self.dump(GC.rearrange("p t j -> p (t j)"), NT * 8, ["gGC"], col0=512)
        self.dump(BETA.rearrange("p t j -> p (t j)"), NT * 8, ["gB"], col0=768)
        P.release(m)
        return
    gains = P.alloc(2, F32)
    self.DMA(gains[:, 0:1], self.gdn_out_norm[l].rearrange("(p o) -> p o", o=1), w=["gains"])
    WG = P.alloc(8 * 128, BF16).rearrange("p (k n) -> p k n", k=8)
    raw = P.alloc(T, BF16)
    kT = P.alloc(T, F32)
    qT = P.alloc(T, BF16)
    vT = P.alloc(T, BF16)
    Oacc = P.alloc(T, BF16)
    cw = P.alloc(16, F32)
    y = P.alloc(512, F32)
    ys = P.alloc(512, F32)
    sqb = P.alloc(512, BF16)
    Wo = P.alloc(1024, BF16)
    names = ["qf", "vf", "Gtri", "t1", "t2", "e2m", "QKDT", "N", "M", "eg", "qdec", "kbg", "kdec", "bv", "R0", "R1", "N2", "M2", "U_", "WT", "VN", "S0", "S1"]
    Bs = [{nm: P.alloc(128, F32) for nm in names} for _ in range(2)]
    allraw = [f"raw{b}" for b in range(5)]
    for h in range(4):
        for Xi, (dstT, colbase) in enumerate([(qT, 512), (kT, 1024), (vT, 1536)]):
            self.load_w_cols(WG, self.w_in[l], colbase + h * 128, 128, "WG")
            self.DMA(cw[:, Xi * 5:(Xi + 1) * 5], self.gdn_conv[l, :, Xi * 512 + h * 128:Xi * 512 + (h + 1) * 128].rearrange("w p -> p w"), w=["cw"])
            for bi, (t0, n) in enumerate(BLKS):
                bank = bi % 2
                for k in range(8):
                    self.MM(ps[:, bank, 0:n], WG[:, k, :], self.hT[:, k, t0:t0 + n], start=(k == 0), stop=(k == 7), r=["WG", f"hT{bi}"], w=[f"pq{bank * 4}"])
                self.CP("scalar", raw[:, t0:t0 + n], ps[:, bank, 0:n], r=[f"pq{bank * 4}"], w=[f"raw{bi}"])
            for bi, (t0, n) in enumerate(BLKS):
                s0, s1 = (0, CTX) if bi == 0 else (CTX, T)
                self.TS("vector", y[:, 0:n], raw[:, t0:t0 + n], cw[:, Xi * 5 + 2:Xi * 5 + 3], ALU.mult, r=allraw + ["cw"], w=["cy"])
                for j in (0, 1, 3, 4):
                    a, b = max(t0, s0 - (j - 2)), min(t0 + n, s1 - (j - 2))
                    self.STT(y[:, a - t0:b - t0], raw[:, a + j - 2:b + j - 2], cw[:, Xi * 5 + j:Xi * 5 + j + 1], y[:, a - t0:b - t0], ALU.mult, ALU.add, r=allraw + ["cw", "cy"], w=["cy"])
                if Xi == 2:
                    self.ACT(vT[:, t0:t0 + n], y[:, 0:n], AF.Silu, r=["cy"], w=["gv"])
                    continue
                self.ACT(ys[:, 0:n], y[:, 0:n], AF.Silu, r=["cy"], w=["cys"])
                self.ACT(sqb[:, 0:n], ys[:, 0:n], AF.Square, r=["cys"], w=["csq"])
                self.MM(ps[:, 2, 0:n], self.ones_b, sqb[:, 0:n], r=["csq", "constb"], w=["pq8"])
                self.ACT(y[:, 0:n], ps[:, 2, 0:n], AF.Sqrt, bias=self.eps, r=["pq8", "const2", "cy"], w=["cy"])
                self.RECIP(y[:, 0:n], y[:, 0:n], r=["cy"], w=["cy"])
                self.STT(dstT[:, t0:t0 + n], ys[:, 0:n], (128.0 ** -0.5 if Xi == 0 else 1.0), y[:, 0:n], ALU.mult, ALU.mult, r=["cys", "cy"], w=["gq" if Xi == 0 else "gk"])
        P.barrier()
        if self.dbg_what == "Bproj":
            self.dump(kT, T, ["gk"])
            P.release(m)
            return
        order = [list(range(NT)), [1, 0] + list(range(NT - 1, 1, -1))]
        Scur = [Bs[0]["S0"], Bs[1]["S0"]]
        Snxt = [Bs[0]["S1"], Bs[1]["S1"]]
        for d in range(2):
            self.MEMSET("vector", Scur[d], 0.0, w=[f"S{d}"])
        touched = set()
        for step in range(NT):
            bufs = []
            for d in range(2):
                P.rec = []
                bufs.append(P.rec)
                ti = order[d][step]
                B = Bs[d]
                j = d * 4 + h
                q0 = d * 16
                cs = slice(ti * 128, (ti + 1) * 128)
                kt = kT[:, cs]
                Tri = self.U if d == 0 else self.L
                MS = self.SL if d == 0 else self.SU
                MIT = self.U if d == 0 else self.L
                gcol, gccol, bcol = G[:, ti, j:j + 1], GC[:, ti, j:j + 1], BETA[:, ti, j:j + 1]
                K_ = lambda nm: f"b{d}{nm}"
                SLT = lambda i: psq[:, q0 + i, :]
                SK = lambda i: f"pq{q0 + i}"
                self.CP("gpsimd", B["qf"], qT[:, cs], r=["gq"], w=[K_("qf")])
                self.CP("gpsimd", B["vf"], vT[:, cs], r=["gv"], w=[K_("vf")])
                self.MM(SLT(0), kt, kt, r=["gk"], w=[SK(0)])
                self.MM(SLT(1), kt, B["qf"], r=["gk", K_("qf")], w=[SK(1)])
                self.TS("vector", B["Gtri"], Tri, gcol, ALU.mult, r=["const", "gG"], w=[K_("Gtri")])
                self.MM(SLT(2), self.ones, B["Gtri"], r=["const", K_("Gtri")], w=[SK(2)])
                self.TS("vector", B["t1"], SLT(2), gccol, ALU.subtract, 0.0, ALU.max, r=[SK(2), "gGC"], w=[K_("t1")])
                self.ACT(B["t1"], B["t1"], AF.Exp, scale=-1.0, r=[K_("t1")], w=[K_("t1")])
                self.TT("gpsimd", B["t1"], B["t1"], MS, ALU.mult, r=[K_("t1"), "const"], w=[K_("t1")])
                self.TS("vector", B["t2"], SLT(2), gccol, ALU.subtract, 0.0, ALU.min, r=[SK(2), "gGC"], w=[K_("t2")])
                self.ACT(B["t2"], B["t2"], AF.Exp, r=[K_("t2")], w=[K_("t2")])
                self.TT("gpsimd", B["e2m"], B["t2"], MIT, ALU.mult, r=[K_("t2"), "const"], w=[K_("e2m")])
                self.TT("vector", B["QKDT"], SLT(1), B["e2m"], ALU.mult, r=[SK(1), K_("e2m")], w=[K_("QKDT")])
                self.STT(B["N"], SLT(0), bcol, B["t1"], ALU.mult, ALU.mult, r=[SK(0), "gB", K_("t1")], w=[K_("N")])
                self.TR(SLT(3), B["N"], r=[K_("N")], w=[SK(3)])
                self.CP("scalar", B["M"], SLT(3), r=[SK(3)], w=[K_("M")])
                self.ACT(B["eg"], SLT(2), AF.Exp, r=[SK(2)], w=[K_("eg")])
                self.TT("vector", B["qdec"], B["qf"], B["eg"], ALU.mult, r=[K_("qf"), K_("eg")], w=[K_("qdec")])
                self.TR(SLT(4), kt, r=["gk"], w=[SK(4)])
                self.TR(SLT(5), B["vf"], r=[K_("vf")], w=[SK(5)])
                self.TS("vector", B["kbg"], SLT(4), BEG[:, ti, j:j + 1], ALU.mult, r=[SK(4), "gBEG"], w=[K_("kbg")])
                self.TS("vector", B["kdec"], SLT(4), KD[:, ti, j:j + 1], ALU.mult, r=[SK(4), "gKD"], w=[K_("kdec")])
                self.TS("vector", B["bv"], SLT(5), bcol, ALU.mult, r=[SK(5), "gB"], w=[K_("bv")])
                self.TT("vector", B["R0"], self.ident, B["M"], ALU.subtract, r=["const", K_("M")], w=[K_("R0")])
                Nc, Mc, Rc = ("N", "M", "R0")
                Nn, Mn, Rn = ("N2", "M2", "R1")
                for lev in range(1, 7):
                    self.MM(SLT(6), B[Mc], B[Nc], r=[K_(Mc), K_(Nc)], w=[SK(6)])
                    self.CP("vector", B[Nn], SLT(6), r=[SK(6)], w=[K_(Nn)])
                    if lev < 6:
                        self.MM(SLT(14), B[Nc], B[Mc], r=[K_(Mc), K_(Nc)], w=[SK(14)])
                        self.CP("scalar", B[Mn], SLT(14), r=[SK(14)], w=[K_(Mn)])
                    self.MM(SLT(8), B[Nn], B[Rc], r=[K_(Nn), K_(Rc)], w=[SK(8)])
                    self.TT("vector", B[Rn], SLT(8), B[Rc], ALU.add, r=[SK(8), K_(Rc)], w=[K_(Rn)])
                    Nc, Nn = Nn, Nc
                    Mc, Mn = Mn, Mc
                    Rc, Rn = Rn, Rc
                TTk = Rc
                self.MM(SLT(9), B[TTk], B["bv"], r=[K_(TTk), K_("bv")], w=[SK(9)])
                self.CP("scalar", B["U_"], SLT(9), r=[SK(9)], w=[K_("U_")])
                self.MM(SLT(15), B["kbg"], B[TTk], r=[K_(TTk), K_("kbg")], w=[SK(15)])
                self.CP("vector", B["WT"], SLT(15), r=[SK(15)], w=[K_("WT")])
                S_, Sn_ = Scur[d], Snxt[d]
                self.MM(SLT(11), B["WT"], S_, r=[K_("WT"), f"S{d}"], w=[SK(11)])
                self.TT("vector", B["VN"], B["U_"], SLT(11), ALU.subtract, r=[K_("U_"), SK(11)], w=[K_("VN")])
                self.MM(SLT(12), S_, B["qdec"], start=True, stop=False, r=[f"S{d}", K_("qdec")], w=[SK(12)])
                self.MM(SLT(12), B["VN"], B["QKDT"], start=False, stop=True, r=[K_("VN"), K_("QKDT")], w=[SK(12)])
                if ti not in touched:
                    touched.add(ti)
                    self.CP("scalar", Oacc[:, cs], SLT(12), r=[SK(12)], w=[f"Oacc{ti}"])
                else:
                    self.TT("vector", Oacc[:, cs], SLT(12), Oacc[:, cs], ALU.add, r=[SK(12), f"Oacc{ti}"], w=[f"Oacc{ti}"])
                self.MM(SLT(9 + 0), B["kdec"], B["VN"], r=[K_("kdec"), K_("VN")], w=[SK(9 + 0)])
                self.STT(Sn_, S_, GCL[:, ti, j:j + 1], SLT(9 + 0), ALU.mult, ALU.add, r=[f"S{d}", "gGCL", SK(9 + 0)], w=[f"S{d}"])
                Scur[d], Snxt[d] = Sn_, S_
            P.replay_interleaved(bufs)
        P.barrier()
        self.load_w_cols(WG, self.w_in[l], 2048 + h * 128, 128, "WG")
        self.load_cast(Wo, self.w_out[l, 256 + h * 128:256 + (h + 1) * 128, :], 1024, dkey="WoB")
        MOD = self.MOD[l]
        for bi, (t0, n) in enumerate(BLKS):
            yy = 1 if bi == 0 else 0
            oa = Oacc[:, t0:t0 + n]
            self.ACT(sqb[:, 0:n], oa, AF.Square, w=["csq"])
            self.MM(ps[:, 2, 0:n], self.ones_b, sqb[:, 0:n], r=["csq", "constb"], w=["pq8"])
            self.ACT(y[:, 0:n], ps[:, 2, 0:n], AF.Sqrt, scale=1.0 / 128, bias=self.eps, r=["pq8", "const2"], w=["cy"])
            self.RECIP(y[:, 0:n], y[:, 0:n], r=["cy"], w=["cy"])
            self.STT(ys[:, 0:n], oa, gains[:, 0:1], y[:, 0:n], ALU.mult, ALU.mult, r=["cy", "gains"], w=["cys"])
            for k in range(8):
                self.MM(ps[:, 3, 0:n], WG[:, k, :], self.hT[:, k, t0:t0 + n], start=(k == 0), stop=(k == 7), r=["WG", f"hT{bi}"], w=["pq12"])
            self.ACT(y[:, 0:n], ps[:, 3, 0:n], AF.Silu, r=["pq12", "cy"], w=["cy"])
            self.TT("vector", sqb[:, 0:n], ys[:, 0:n], y[:, 0:n], ALU.mult, r=["cys", "cy"], w=["csq"])
            if self.dbg and self.dbg_what == f"B{h}":
                self.dump(sqb[:, 0:n], n, ["csq"], col0=t0)
            for dc in range(8):
                bank = 4 + dc % 4
                self.MM(ps[:, bank, 0:n], Wo[:, dc * 128:(dc + 1) * 128], sqb[:, 0:n], r=["WoB", "csq"], w=[f"pq{bank * 4}"])
                self.STT(self.xT[:, dc, t0:t0 + n], ps[:, bank, 0:n], MOD[:, 16 + dc, yy:yy + 1], self.xT[:, dc, t0:t0 + n],
                         ALU.mult, ALU.add, r=[f"pq{bank * 4}", f"MOD{l}", f"xT{bi}"], w=[f"xT{bi}"])
        P.barrier()
    P.release(m)


K.mixer_B = _mixer_B


def _moe(self, l):
    P, ps = self.P, self.ps
    m0 = P.mark()
    gateT = P.alloc(T, BF16)
    m = P.mark()
    RW = P.alloc(8 * 32, F32).rearrange("p (k e) -> p k e", k=8)
    self.DMA(RW, self.router_w[l].rearrange("(k p) e -> p k e", p=128), w=["RW"])
    rb = P.alloc(32, F32)
    self.DMA(rb, self.router_b[l].rearrange("(a e) -> a e", a=1).broadcast_to([128, 32]), w=["rb"])
    h2f = P.alloc(8 * 512, F32).rearrange("p (c t) -> p c t", c=8)
    lg, msk, ex = P.alloc(32, F32), P.alloc(32, F32), P.alloc(32, F32)
    top8, dn = P.alloc(8, F32), P.alloc(1, F32)

    def router_fn(bi, t0, n):
        for tt in range(n // 128):
            cs = slice(tt * 128, (tt + 1) * 128)
            for c in range(8):
                self.MM(ps[:, 1, 0:32], h2f[:, c, cs], RW[:, c, :], start=(c == 0), stop=(c == 7), r=[f"h2f{c}", "RW"], w=["ps1"])
            self.TT("vector", lg, ps[:, 1, 0:32], rb, ALU.add, r=["ps1", "rb"], w=["lg"])
            self.P.op("vector", lambda e: e.max(out=top8, in_=lg), ["lg"], ["top8"])
            self.TS("vector", msk, lg, top8[:, 3:4], ALU.is_ge, r=["lg", "top8"], w=["msk"])
            self.TS("vector", ex, lg, top8[:, 0:1], ALU.subtract, r=["lg", "top8"], w=["ex"])
            self.ACT(ex, ex, AF.Exp, r=["ex"], w=["ex"])
            self.TT("vector", ex, ex, msk, ALU.mult, r=["ex", "msk"], w=["ex"])
            self.P.op("vector", lambda e: e.reduce_sum(out=dn, in_=ex, axis=mybir.AxisListType.X), ["ex"], ["dn"])
            self.RECIP(dn, dn, r=["dn"], w=["dn"])
            self.TS("vector", ex, ex, dn[:, 0:1], ALU.mult, r=["ex", "dn"], w=["ex"])
            self.TR(ps[0:32, 2, 0:128], ex, r=["ex"], w=["ps2"])
            self.CP("scalar", gateT[0:32, t0 + tt * 128:t0 + (tt + 1) * 128], ps[0:32, 2, 0:128], r=["ps2"], w=["gateT"])

    self.norm_mod(l, self.A2[l], 24, router=dict(h2f=h2f, fn=router_fn))
    P.release(m)
    if self.dbg_what == "gate":
        self.dump(gateT[0:32, :], T, ["gateT"], rows=32)
        P.release(m0)
        return
    bsb = P.alloc(2048, F32)
    self.DMA(bsb[0:32, :], self.exp_b_gu[l], w=["bsb"])
    bv = bsb[0:32, :].rearrange("e (j p s) -> e j s p", j=8, s=2)
    for j in range(8):
        for s_ in range(2):
            self.TR(ps[:, 3, (j * 2 + s_) * 32:(j * 2 + s_ + 1) * 32], bv[:, j, s_, :], r=["bsb"], w=["ps3"])
    bguT = P.alloc(512, F32).rearrange("p (a e) -> p a e", e=32)
    self.CP("vector", bguT, ps[:, 3, :].rearrange("p (a e) -> p a e", e=32), r=["ps3"], w=["bguT"])
    bdn = P.alloc(1024, BF16)
    self.load_cast(bdn[0:32, :], self.exp_b_dn[l], 1024, dkey="bdn", parts=32)
    MOD = self.MOD[l]
    for bi, (t0, n) in enumerate(BLKS):
        yy = 1 if bi == 0 else 0
        for dc in range(8):
            bank = 4 + dc % 2
            self.MM(ps[:, bank, 0:n], bdn[0:32, dc * 128:(dc + 1) * 128], gateT[0:32, t0:t0 + n], r=["bdn", "gateT"], w=[f"ps{bank}"])
            self.STT(self.xT[:, dc, t0:t0 + n], ps[:, bank, 0:n], MOD[:, 40 + dc, yy:yy + 1], self.xT[:, dc, t0:t0 + n],
                     ALU.mult, ALU.add, r=[f"ps{bank}", f"MOD{l}", f"xT{bi}"], w=[f"xT{bi}"])
    Wgu = [P.alloc(8 * 512, BF16).rearrange("p (k s f) -> p k s f", k=8, s=2) for _ in range(2)]
    Wdn = [P.alloc(2 * 1024, BF16).rearrange("p (j n) -> p j n", j=2) for _ in range(2)]
    act = [P.alloc(2 * 512, BF16).rearrange("p (j n) -> p j n", j=2) for _ in range(3)]
    gsel = P.alloc(512, BF16)
    tg = [[P.alloc(512, BF16) for _ in range(2)] for _ in range(2)]
    tsg = [[P.alloc(512, BF16) for _ in range(2)] for _ in range(2)]
    tl = [[P.alloc(512, BF16) for _ in range(2)] for _ in range(2)]
    bl1 = P.alloc(8 * 32, F32).rearrange("p (j e) -> p j e", e=32)
    self.TS("vector", bl1, bguT.rearrange("p (j s) e -> p j s e", s=2)[:, :, 1, :], 1.0, ALU.add, r=["bguT"], w=["bl1"])
    nexp = NEXP if self.moe_experts is None else self.moe_experts
    units = [(e_, q) for e_ in range(nexp) for q in range(4)]

    def load_unit(ui):
        e_, q = units[ui]
        wi = ui % 2
        for k0 in range(0, 8, 2):
            st, sk = self.stage()
            sv = st[:, 0:1024].rearrange("p (k n) -> p k n", k=2)
            self.DMA(sv, self.exp_w_gu[l, e_, k0 * 128:(k0 + 2) * 128, q * 512:(q + 1) * 512].rearrange("(k p) n -> p k n", p=128), w=[sk])
            self.CP("gpsimd", Wgu[wi][:, k0:k0 + 2, :, :], sv.rearrange("p k (f s) -> p k s f", s=2), r=[sk], w=[f"Wgu{wi}"])
        for jj in range(2):
            st, sk = self.stage()
            self.DMA(st[:, 0:1024], self.exp_w_dn[l, e_, q * 256 + jj * 128:q * 256 + (jj + 1) * 128, :], w=[sk])
            self.CP("scalar", Wdn[wi][:, jj, :], st[:, 0:1024], r=[sk], w=[f"Wdn{wi}"])

    def gu_swiglu(ui, bi, ai):
        e_, q = units[ui]
        wi = ui % 2
        t0, n = BLKS[bi]
        wgk = f"Wgu{wi}"
        self.TS("vector", gsel[0:32, 0:n], gateT[0:32, t0:t0 + n], self.ident[0:32, e_:e_ + 1], ALU.mult, r=["gateT", "const"], w=["gsel"])
        for jj in range(2):
            bg_, bl_ = 2 * jj, 2 * jj + 1
            for s_ in range(2):
                for k in range(8):
                    self.MM(ps[:, 2 * jj + s_, 0:n], Wgu[wi][:, k, s_, jj * 128:(jj + 1) * 128], self.hT[:, k, t0:t0 + n],
                            start=(k == 0), stop=(k == 7), r=[wgk, f"hT{bi}"], w=[f"ps{2 * jj + s_}"])
            if jj == 0:
                self.MM(ps[:, 6, 0:n], self.ones_b[0:32, :], gsel[0:32, 0:n], r=["gsel", "constb"], w=["ps6"])
            fj = 2 * q + jj
            G_, S_, L_ = tg[ai % 2][jj], tsg[ai % 2][jj], tl[ai % 2][jj]
            kk = f"{ai % 2}{jj}"
            self.TS("vector", G_[:, 0:n], ps[:, bg_, 0:n], bguT[:, fj * 2, e_:e_ + 1], ALU.add, 7.0, ALU.min, r=[f"ps{bg_}", "bguT"], w=["tg" + kk])
            self.ACT(S_[:, 0:n], G_[:, 0:n], AF.Gelu_apprx_sigmoid, r=["tg" + kk], w=["tsg" + kk])
            self.TS("vector", L_[:, 0:n], ps[:, bl_, 0:n], bl1[:, fj, e_:e_ + 1], ALU.add, 8.0, ALU.min, r=[f"ps{bl_}", "bl1"], w=["tl" + kk])
            self.STT(L_[:, 0:n], L_[:, 0:n], -6.0, ps[:, 6, 0:n], ALU.max, ALU.mult, r=["tl" + kk, "ps6"], w=["tl" + kk])
            self.TT("gpsimd", act[ai][:, jj, 0:n], S_[:, 0:n], L_[:, 0:n], ALU.mult, r=["tsg" + kk, "tl" + kk], w=[f"act{ai}"])

    def dn_update(ui, bi, ai):
        wi = ui % 2
        t0, n = BLKS[bi]
        yy = 1 if bi == 0 else 0
        for dc in range(8):
            bank = 4 + dc % 2
            for jj in range(2):
                self.MM(ps[:, bank, 0:n], Wdn[wi][:, jj, dc * 128:(dc + 1) * 128], act[ai][:, jj, 0:n], start=(jj == 0), stop=(jj == 1), r=[f"Wdn{wi}", f"act{ai}"], w=[f"ps{bank}"])
            if dc in ():
                xi = xcnt[0] % 2
                xcnt[0] += 1
                self.ACT(xtmp[xi][:, 0:n], ps[:, bank, 0:n], AF.Copy, scale=MOD[:, 40 + dc, yy:yy + 1], r=[f"ps{bank}", f"MOD{l}"], w=[f"xtmp{xi}"])
                self.TT("gpsimd", self.xT[:, dc, t0:t0 + n], self.xT[:, dc, t0:t0 + n], xtmp[xi][:, 0:n], ALU.add, r=[f"xtmp{xi}", f"xTm{bi}_{dc}"], w=[f"xTm{bi}_{dc}"])
            else:
                self.STT(self.xT[:, dc, t0:t0 + n], ps[:, bank, 0:n], MOD[:, 40 + dc, yy:yy + 1], self.xT[:, dc, t0:t0 + n],
                         ALU.mult, ALU.add, r=[f"ps{bank}", f"MOD{l}", f"xTm{bi}_{dc}"], w=[f"xTm{bi}_{dc}"])

    xtmp = [P.alloc(512, F32), P.alloc(512, F32)]
    xcnt = [0]
    load_unit(0)
    pend = []
    step = 0
    blist = list(range(len(BLKS)))
    if l == self.nlayers - 1 and self.dbg_what is None:
        blist = blist[1:]
    for ui in range(len(units)):
        for bi in blist:
            ai = step % 2
            step += 1
            ai = step % 3
            P.rec = bufA = []
            gu_swiglu(ui, bi, ai)
            P.rec = bufB = []
            if len(pend) == 2:
                dn_update(*pend.pop(0))
            P.rec = None
            groups = [bufB[i:i + 3] for i in range(0, len(bufB), 3)]
            nmm = 0
            for kind, args in bufA:
                P.op(*args)
                if args[0] == "tensor":
                    nmm += 1
                    if nmm % 4 == 0 and groups:
                        for k2, a2 in groups.pop(0):
                            P.op(*a2)
            for g_ in groups:
                for k2, a2 in g_:
                    P.op(*a2)
            if bi == blist[1] and ui + 1 < len(units):
                load_unit(ui + 1)
            pend.append((ui, bi, ai))
    for p_ in pend:
        dn_update(*p_)
    P.barrier()
    P.release(m0)


K.moe = _moe
K.moe_experts = None
```

```python
import contextlib
import numpy as np
import concourse.bass as bass
import concourse.mybir as mybir
from concourse.bass_utils import run_bass_kernel_spmd

F32 = mybir.dt.float32
BF16 = mybir.dt.bfloat16
AF = mybir.ActivationFunctionType
ALU = mybir.AluOpType

ENG = ["tensor", "vector", "scalar", "gpsimd", "sync"]
SEM_ROLL = 30000
NDSEM = 24


class Prog:
    def __init__(self, nc):
        self.nc = nc
        self.ops = {e: [] for e in ENG}
        self.cnt = {e: 0 for e in ENG}
        self.seen = {e: {p: 0 for p in ENG} for e in ENG}
        self.dseen = {e: set() for e in ENG}
        self.res = {}
        self.ndma = 0
        self.dma_issuer = {}
        self.arena = None
        self.off = 0
        self.peak = 0

    def set_arena(self, arena, nbytes):
        self.arena = arena
        self.arena_bytes = nbytes
        self.off = 0

    def alloc(self, n, dtype):
        esz = 2 if dtype == BF16 else 4
        nb = (n * esz + 63) // 64 * 64
        assert self.off + nb <= self.arena_bytes, ("arena overflow", self.off, nb)
        a = self.arena[:, self.off // 4:(self.off + nb) // 4]
        self.off += nb
        self.peak = max(self.peak, self.off)
        if dtype != F32:
            a = a.bitcast(dtype)
        return a[:, 0:n]

    def mark(self):
        return self.off

    def release(self, m):
        self.off = m

    def _need(self, reads, writes):
        toks = []
        for r in reads:
            s = self.res.get(r)
            if s and s["w"] is not None:
                toks.append(s["w"])
        for w in writes:
            s = self.res.get(w)
            if s:
                if s["w"] is not None:
                    toks.append(s["w"])
                for e, n in s["rc"].items():
                    toks.append(("c", e, n))
                for d in s["rd"]:
                    toks.append(("d", d))
        return toks

    def _emit_waits(self, eng, toks):
        lst = self.ops[eng]
        best = {}
        for t in toks:
            if t[0] == "c":
                _, p, n = t
                if p == "tensor" and eng == "tensor":
                    continue
                if self.seen[eng][p] < n:
                    best[p] = max(best.get(p, 0), n)
            else:
                d = t[1]
                if d not in self.dseen[eng]:
                    self.dseen[eng].add(d)
                    k, v = d % NDSEM, 16 * (d // NDSEM + 1)
                    lst.append(lambda e, S, k=k, v=v: e.wait_ge(S["d"][k], v))
        for p, n in best.items():
            self.seen[eng][p] = n
            k, v = (n - 1) // SEM_ROLL, (n - 1) % SEM_ROLL + 1
            lst.append(lambda e, S, p=p, k=k, v=v: e.wait_ge(S[p][k], v))

    def _commit(self, tok, reads, writes):
        for r in reads:
            s = self.res.setdefault(r, {"w": None, "rc": {}, "rd": set()})
            if tok[0] == "c":
                s["rc"][tok[1]] = tok[2]
            else:
                s["rd"].add(tok[1])
        for w in writes:
            self.res[w] = {"w": tok, "rc": {}, "rd": set()}

    @staticmethod
    def _bank_keys(reads, writes):
        reads, writes = list(reads), list(writes)
        for k in reads + writes:
            b = None
            if k.startswith("pq") and k[2:].isdigit():
                b = int(k[2:]) // 4
            elif k.startswith("ps") and k[2:].isdigit():
                b = int(k[2:])
            if b is not None and f"PB{b}" not in writes:
                writes.append(f"PB{b}")
        return reads, writes

    rec = None

    def replay_interleaved(self, bufs):
        self.rec = None
        n = max(len(b) for b in bufs)
        for i in range(n):
            for b in bufs:
                if i < len(b):
                    kind, args = b[i]
                    if kind == "op":
                        self.op(*args)
                    else:
                        self.dma(*args[:-1], **args[-1])

    def op(self, eng, fn, reads=(), writes=()):
        if self.rec is not None:
            self.rec.append(("op", (eng, fn, list(reads), list(writes))))
            return
        reads, writes = self._bank_keys(reads, writes)
        self._emit_waits(eng, self._need(reads, writes))
        self.cnt[eng] += 1
        n = self.cnt[eng]
        k = (n - 1) // SEM_ROLL
        self.ops[eng].append(lambda e, S, fn=fn, p=eng, k=k: fn(e).then_inc(S[p][k], 1))
        self._commit(("c", eng, n), reads, writes)

    def dma(self, eng, out, in_, reads=(), writes=(), **kw):
        if self.rec is not None:
            self.rec.append(("dma", (eng, out, in_, list(reads), list(writes), kw)))
            return
        d = self.ndma
        self.ndma += 1
        toks = self._need(reads, writes)
        if d >= NDSEM:
            toks.append(("d", d - NDSEM))
        self._emit_waits(eng, toks)
        k = d % NDSEM
        self.ops[eng].append(lambda e, S, k=k, out=out, in_=in_, kw=kw: e.dma_start(out=out, in_=in_, **kw).then_inc(S["d"][k], 16))
        self.dma_issuer[d] = eng
        self._commit(("d", d), reads, writes)
        return d

    def barrier(self):
        for e in ENG:
            toks = [("c", p, self.cnt[p]) for p in ENG if p != e and self.cnt[p] > 0]
            toks += [("d", d) for d in range(max(0, self.ndma - NDSEM), self.ndma)]
            self._emit_waits(e, toks)

    def finish(self):
        self._emit_waits("sync", [("d", d) for d in range(max(0, self.ndma - NDSEM), self.ndma)])
        nc = self.nc
        with contextlib.ExitStack() as st:
            S = {}
            for e in ENG:
                ns = self.cnt[e] // SEM_ROLL + 1
                S[e] = [st.enter_context(nc.semaphore(f"s_{e}_{i}")) for i in range(ns)]
            S["d"] = [st.enter_context(nc.semaphore(f"s_d_{i}")) for i in range(NDSEM)]
            block = st.enter_context(nc.Block())

            def run(engname):
                def _f(eng):
                    for f in self.ops[engname]:
                        f(eng, S)
                return _f

            block.sync(run("sync"))
            block.tensor(run("tensor"))
            block.vector(run("vector"))
            block.scalar(run("scalar"))
            block.gpsimd(run("gpsimd"))


D = 1024
T = 2304
NT = 18
CTX = 256
SEQ = 2048
EPS = 1e-6
BLKS = [(0, 256), (256, 512), (768, 512), (1280, 512), (1792, 512)]
NIN = 2992
NEXP = 32


def tile_blk(ti):
    return 0 if ti < 2 else 1 + (ti - 2) // 4


def host_consts():
    c = {}
    i = np.arange(128)
    U = (i[:, None] <= i[None, :]).astype(np.float32)
    L = (i[:, None] >= i[None, :]).astype(np.float32)
    SU = (i[:, None] < i[None, :]).astype(np.float32)
    SL = (i[:, None] > i[None, :]).astype(np.float32)
    ident = np.eye(128, dtype=np.float32)
    ones = np.ones((128, 128), np.float32)
    bd64 = np.kron(np.eye(2, dtype=np.float32), np.ones((64, 64), np.float32))

    def rmat(rot, off, size):
        R = np.zeros((size, size), np.float32)
        q = rot // 4
        for ax in range(2):
            b = off + ax * 2 * q
            for k in range(q):
                R[b + k, b + q + k] = -1.0
                R[b + q + k, b + k] = 1.0
        return R
    RA = np.kron(np.eye(2, dtype=np.float32), rmat(64, 0, 64))
    RC = np.zeros((128, 128), np.float32)
    RC[:96, :96] = rmat(32, 64, 96)
    E32 = np.zeros((128, 128), np.float32)
    for k in range(32):
        E32[k, 64 + k] = 1.0
    BIG = np.float32(1.0e4)
    c["cf"] = np.concatenate([ident, U, L, SU, SL, ones, bd64, RA.T.copy(), RC.T.copy(), E32], axis=1).astype(np.float32)
    c["gmask"] = np.concatenate([BIG * (1 - SL), BIG * (1 - SU), -BIG * (1 - U), -BIG * (1 - L)], axis=1).astype(np.float32)

    def tables(rot):
        nf = rot // 4
        t = np.arange(SEQ)
        row = (t // 64).astype(np.float32)
        col = (t % 64).astype(np.float32)
        freq = (np.float32(10000.0) ** (-np.arange(nf, dtype=np.float32) / np.float32(nf))).astype(np.float32)
        ang = np.stack([row[:, None] * freq, col[:, None] * freq], axis=1)
        cs, sn = np.cos(ang).astype(np.float32), np.sin(ang).astype(np.float32)
        C = np.concatenate([cs[:, 0], cs[:, 0], cs[:, 1], cs[:, 1]], axis=1).T
        S = np.concatenate([sn[:, 0], sn[:, 0], sn[:, 1], sn[:, 1]], axis=1).T
        return C, S
    CA, SA = tables(64)
    c["ropeA"] = np.concatenate([np.concatenate([CA, CA], 0), np.concatenate([SA, SA], 0)], axis=1).astype(np.float32)
    CC, SC = tables(32)
    cc = np.ones((128, SEQ), np.float32)
    sc = np.zeros((128, SEQ), np.float32)
    cc[64:96] = CC
    sc[64:96] = SC
    c["ropeC"] = np.concatenate([cc, sc], axis=1).astype(np.float32)
    return c


class K:
    def __init__(self, nseq=2, nlayers=2, phases="CABM", dbg=None, moe=True):
        self.nseq, self.nlayers, self.phases, self.dbg = nseq, nlayers, phases, dbg
        nc = self.nc = bass.Bass("TRN2", target_bir_lowering=False)
        self.P = Prog(nc)
        self.all_in = []

        def dt(n, s):
            a = nc.dram_tensor(n, list(s), F32, kind="ExternalInput").ap()
            self.all_in.append(a)
            return a
        self.x2 = dt("x2", (2, SEQ, D))
        self.ctx2 = dt("ctx2", (2, CTX, D))
        self.vp1 = dt("vp1", (128, 128))
        self.vp2 = dt("vp2", (32, 128))
        self.w_mod = dt("w_mod", (2, D, 6 * D))
        self.w_in = dt("w_in", (2, D, NIN))
        self.w_out = dt("w_out", (2, 1024, D))
        self.swa_q_norm = dt("swa_q_norm", (2, 64))
        self.swa_k_norm = dt("swa_k_norm", (2, 64))
        self.swa_sink = dt("swa_sink", (2, 4))
        self.gdn_conv = dt("gdn_conv", (2, 5, 1536))
        self.gdn_a_log = dt("gdn_a_log", (2, 8))
        self.gdn_dt_bias = dt("gdn_dt_bias", (2, 8))
        self.gdn_out_norm = dt("gdn_out_norm", (2, 128))
        self.mla_q_a_norm = dt("mla_q_a_norm", (2, 256))
        self.mla_w_uq = dt("mla_w_uq", (2, 256, 384))
        self.mla_kv_a_norm = dt("mla_kv_a_norm", (2, 128))
        self.mla_w_ukv = dt("mla_w_ukv", (2, 128, 512))
        self.mla_q_norm = dt("mla_q_norm", (2, 96))
        self.mla_k_norm = dt("mla_k_norm", (2, 96))
        self.router_w = dt("router_w", (2, D, NEXP))
        self.router_b = dt("router_b", (2, NEXP))
        self.exp_w_gu = dt("exp_w_gu", (2, NEXP, D, 2048) if moe else (2, 1, 8, 8))
        self.exp_b_gu = dt("exp_b_gu", (2, NEXP, 2048))
        self.exp_w_dn = dt("exp_w_dn", (2, NEXP, D, D) if moe else (2, 1, 8, 8))
        self.exp_b_dn = dt("exp_b_dn", (2, NEXP, D))
        self.cf = dt("cf", (128, 1280))
        self.gmask_d = dt("gmask", (128, 512))
        self.ropeA = dt("ropeA", (128, 4096))
        self.ropeC = dt("ropeC", (128, 4096))
        self.out = nc.dram_tensor("out", [2, SEQ, D], F32, kind="ExternalOutput").ap()
        self.dbg_out = None
        if dbg:
            self.dbg_out = nc.dram_tensor("dbg", [128, dbg], F32, kind="ExternalOutput").ap()
        self.uid = 0

    def MM(self, out, lhsT, rhs, start=True, stop=True, r=(), w=()):
        self.P.op("tensor", lambda e, o=out, a=lhsT, b=rhs, s=start, t=stop: e.matmul(o, a, b, start=s, stop=t), r, w)

    def TR(self, out, in_, r=(), w=()):
        npar = in_.shape[0]
        idn = self.ident[0:npar, 0:npar]
        self.P.op("tensor", lambda e, o=out, a=in_: e.transpose(o, a, idn), list(r) + ["const"], w)

    def ACT(self, out, in_, func, r=(), w=(), scale=None, bias=None, eng="scalar"):
        kw = {}
        if scale is not None:
            kw["scale"] = scale
        if bias is not None:
            kw["bias"] = bias
        self.P.op("scalar", lambda e, o=out, a=in_, f=func, kw=kw: e.activation(o, a, f, **kw), r, w)

    def TS(self, eng, out, in0, s1, op0, s2=None, op1=None, r=(), w=()):
        if op1 is None:
            self.P.op(eng, lambda e, o=out, a=in0: e.tensor_scalar(out=o, in0=a, scalar1=s1, scalar2=None, op0=op0), r, w)
        else:
            self.P.op(eng, lambda e, o=out, a=in0: e.tensor_scalar(out=o, in0=a, scalar1=s1, scalar2=s2, op0=op0, op1=op1), r, w)

    def TT(self, eng, out, a, b, op, r=(), w=()):
        self.P.op(eng, lambda e, o=out, x=a, y=b: e.tensor_tensor(out=o, in0=x, in1=y, op=op), r, w)

    def STT(self, out, a, s, b, op0, op1, r=(), w=()):
        self.P.op("vector", lambda e, o=out, x=a, y=b: e.scalar_tensor_tensor(out=o, in0=x, scalar=s, in1=y, op0=op0, op1=op1), r, w)

    def CP(self, eng, out, in_, r=(), w=()):
        if eng == "scalar":
            self.P.op(eng, lambda e, o=out, a=in_: e.copy(o, a), r, w)
        else:
            self.P.op(eng, lambda e, o=out, a=in_: e.tensor_copy(o, a), r, w)

    def RECIP(self, out, in_, r=(), w=()):
        self.P.op("vector", lambda e, o=out, a=in_: e.reciprocal(o, a), r, w)

    def MEMSET(self, eng, ap, val, w=()):
        self.P.op(eng, lambda e, a=ap: e.memset(a, val), (), w)

    def DMA(self, out, in_, r=(), w=(), eng="sync"):
        self.P.dma(eng, out, in_, r, w, allow_slow_non_contiguous=True)

    def key(self, base):
        self.uid += 1
        return f"{base}#{self.uid}"

    def dump(self, ap, ncols, r, col0=0, rows=128):
        P = self.P
        m = P.mark()
        tmp = P.alloc(64, F32)
        for c0 in range(0, ncols, 64):
            n = min(64, ncols - c0)
            k = self.key("dump")
            self.CP("vector", tmp[0:rows, 0:n], ap[:, c0:c0 + n], r=list(r) + ["dumptmp"], w=[k, "dumptmp"])
            self.DMA(self.dbg_out[0:rows, col0 + c0:col0 + c0 + n], tmp[0:rows, 0:n], r=[k], w=[self.key("dbgout"), "dumptmp"])
        P.barrier()
        P.release(m)

    def setup(self):
        P = self.P
        cf = P.alloc(1280, F32)
        self.DMA(cf, self.cf, w=["const"])
        self.ident = cf[:, 0:128]
        self.U, self.L = cf[:, 128:256], cf[:, 256:384]
        self.SU, self.SL = cf[:, 384:512], cf[:, 512:640]
        self.ones = cf[:, 640:768]
        cb = P.alloc(640, BF16)
        self.CP("vector", cb, cf[:, 640:1280], r=["const"], w=["constb"])
        self.ones_b, self.bd64_b = cb[:, 0:128], cb[:, 128:256]
        self.RAT_b, self.RCT_b, self.E32_b = cb[:, 256:384], cb[:, 384:512], cb[:, 512:640]
        junk = P.alloc(64, F32)
        for i, a in enumerate(self.all_in):
            fl = a
            while len(fl.shape) > 1:
                fl = fl[0]
            self.DMA(junk[0:1, i:i + 1], fl[0:1].rearrange("(a b) -> a b", a=1), w=["junk"])
        self.eps = P.alloc(1, F32)
        self.MEMSET("vector", self.eps, EPS, w=["const2"])
        vt = P.alloc(128, F32)
        self.DMA(vt, self.vp1, w=["vt"])
        self.VP = P.alloc(128, F32)
        self.TR(self.ps[:, 0, 0:128], vt, r=["vt"], w=["ps0"])
        self.CP("vector", self.VP, self.ps[:, 0, 0:128], r=["ps0"], w=["VP"])
        vt2 = P.alloc(128, F32)
        self.DMA(vt2[0:32, :], self.vp2, w=["vt2"])
        self.CS = P.alloc(32, F32)
        self.TR(self.ps[:, 1, 0:32], vt2[0:32, :], r=["vt2"], w=["ps1"])
        self.ACT(self.CS, self.ps[:, 1, 0:32], AF.Silu, r=["ps1"], w=["CS"])
        self.xT = P.alloc(8 * T, F32).rearrange("p (c t) -> p c t", c=8)
        self.hT = P.alloc(8 * T, BF16).rearrange("p (c t) -> p c t", c=8)
        self.stg = [P.alloc(1024, F32), P.alloc(1024, F32)]
        self.stg_i = 0
        self.MOD = [P.alloc(96, F32).rearrange("p (j y) -> p j y", y=2) for _ in range(2)]
        self.A1 = [P.alloc(16, F32).rearrange("p (c y) -> p c y", y=2) for _ in range(2)]
        self.A2 = [P.alloc(16, F32).rearrange("p (c y) -> p c y", y=2) for _ in range(2)]

    def stage(self):
        i = self.stg_i
        self.stg_i ^= 1
        return self.stg[i], f"stg{i}"

    def load_cast(self, dst, src, ncols, eng="gpsimd", dkey=None, parts=128):
        st, sk = self.stage()
        self.DMA(st[0:parts, 0:ncols], src, w=[sk])
        self.CP(eng, dst, st[0:parts, 0:ncols], r=[sk], w=[dkey])

    def load_w_cols(self, dst, wsrc, c0, ncols, dkey, kc=8):
        per = max(1, 1024 // ncols)
        for k0 in range(0, kc, per):
            k1 = min(kc, k0 + per)
            st, sk = self.stage()
            sv = st[:, 0:(k1 - k0) * ncols].rearrange("p (k n) -> p k n", n=ncols)
            self.DMA(sv, wsrc[k0 * 128:k1 * 128, c0:c0 + ncols].rearrange("(k p) n -> p k n", p=128), w=[sk])
            self.CP("gpsimd", dst[:, k0:k1, :], sv, r=[sk], w=[dkey])

    def load_x(self, s):
        P = self.P
        m = P.mark()
        bufs = [P.alloc(1024, F32), P.alloc(1024, F32)]
        for ti in range(NT):
            b = bufs[ti % 2]
            bk = f"xl{ti % 2}"
            src = self.ctx2[s, ti * 128:(ti + 1) * 128, :] if ti < 2 else self.x2[s, (ti - 2) * 128:(ti - 1) * 128, :]
            self.DMA(b, src, w=[bk])
            for half in range(2):
                bank = 2 * (ti % 2) + half
                for c4 in range(4):
                    c = half * 4 + c4
                    self.TR(self.ps[:, bank, c4 * 128:(c4 + 1) * 128], b[:, c * 128:(c + 1) * 128], r=[bk], w=[f"ps{bank}"])
                eng = "vector" if half == 0 else "scalar"
                self.CP(eng, self.xT[:, half * 4:half * 4 + 4, ti * 128:(ti + 1) * 128],
                        self.ps[:, bank, :].rearrange("p (c t) -> p c t", c=4), r=[f"ps{bank}"], w=[f"xT{tile_blk(ti)}"])
        P.barrier()
        P.release(m)

    def store_x(self, s):
        P = self.P
        m = P.mark()
        bufs = [P.alloc(1024, F32), P.alloc(1024, F32)]
        for ti in range(2, NT):
            b = bufs[ti % 2]
            bk = f"xs{ti % 2}"
            for half in range(2):
                bank = 2 * (ti % 2) + half
                for c4 in range(4):
                    c = half * 4 + c4
                    self.TR(self.ps[:, bank, c4 * 128:(c4 + 1) * 128], self.xT[:, c, ti * 128:(ti + 1) * 128], r=[f"xT{tile_blk(ti)}"], w=[f"ps{bank}"])
                eng = "vector" if half == 0 else "scalar"
                self.CP(eng, b[:, half * 512:(half + 1) * 512], self.ps[:, bank, :], r=[f"ps{bank}"], w=[bk])
            self.DMA(self.out[s, (ti - 2) * 128:(ti - 1) * 128, :], b, r=[bk], w=[self.key("out")])
        P.barrier()
        P.release(m)

    def mods(self, s, l):
        P = self.P
        m = P.mark()
        sc = P.alloc(16, F32).rearrange("p (k y) -> p k y", y=2)
        kk = self.key("sc")
        self.CP("vector", sc[:, :, 0], self.CS[:, s * 8:s * 8 + 8], r=["CS"], w=[kk])
        self.CP("vector", sc[:, :, 1], self.CS[:, 16:24], r=["CS"], w=[kk])
        MOD = self.MOD[l]
        for j in range(48):
            st, sk = self.stage()
            sv = st[:, 0:1024].rearrange("p (k n) -> p k n", n=128)
            self.DMA(sv, self.w_mod[l, :, j * 128:(j + 1) * 128].rearrange("(k p) n -> p k n", p=128), w=[sk])
            for k in range(8):
                self.MM(self.ps[:, 7, 2 * j:2 * j + 2], sv[:, k, :], sc[:, k, :], start=(k == 0), stop=(k == 7), r=[sk, kk], w=["ps7"])
        mk = f"MOD{l}"
        bm = self.VP[:, l * 64:l * 64 + 48]
        pv = self.ps[:, 7, 0:96].rearrange("p (j y) -> p j y", y=2)
        for y in range(2):
            self.TT("vector", MOD[:, :, y], pv[:, :, y], bm, ALU.add, r=["ps7", "VP"], w=[mk])
        n1 = self.VP[:, l * 64 + 48:l * 64 + 56]
        n2 = self.VP[:, l * 64 + 56:l * 64 + 64]
        for y in range(2):
            self.STT(self.A1[l][:, :, y], MOD[:, 8:16, y], 1.0, n1, ALU.add, ALU.mult, r=[mk, "VP"], w=[mk + "a"])
            self.STT(self.A2[l][:, :, y], MOD[:, 32:40, y], 1.0, n2, ALU.add, ALU.mult, r=[mk, "VP"], w=[mk + "a"])
        P.barrier()
        P.release(m)

    def norm_mod(self, l, A, shift_j, router=None):
        P = self.P
        m = P.mark()
        sq = P.alloc(8 * 512, BF16).rearrange("p (c t) -> p c t", c=8)
        sd = P.alloc(512, F32)
        rs = P.alloc(512, F32)
        tmp = [P.alloc(512, F32), P.alloc(512, F32)]
        MOD = self.MOD[l]
        for bi, (t0, n) in enumerate(BLKS):
            y = 1 if bi == 0 else 0
            xk, hk = f"xT{bi}", f"hT{bi}"
            for c in range(8):
                self.ACT(sq[:, c, 0:n], self.xT[:, c, t0:t0 + n], AF.Square, r=[xk], w=[f"sq{c}"])
            for c in range(8):
                self.MM(self.ps[:, 0, 0:n], self.ones_b, sq[:, c, 0:n], start=(c == 0), stop=(c == 7), r=[f"sq{c}", "constb"], w=["ps0"])
            self.ACT(sd[:, 0:n], self.ps[:, 0, 0:n], AF.Sqrt, scale=1.0 / D, bias=self.eps, r=["ps0", "const2"], w=["sd"])
            self.RECIP(rs[:, 0:n], sd[:, 0:n], r=["sd"], w=["rs"])
            for c in range(8):
                tb, tk = tmp[c % 2], f"nt{c % 2}"
                if router is not None:
                    tb, tk = router["h2f"][:, c, :], f"h2f{c}"
                self.STT(tb[:, 0:n], self.xT[:, c, t0:t0 + n], A[:, c, y:y + 1], rs[:, 0:n], ALU.mult, ALU.mult, r=[xk, "rs", f"MOD{l}a"], w=[tk])
                if router is not None:
                    self.TS("vector", tb[:, 0:n], tb[:, 0:n], MOD[:, shift_j + c, y:y + 1], ALU.add, r=[tk, f"MOD{l}"], w=[tk])
                    self.CP("scalar", self.hT[:, c, t0:t0 + n], tb[:, 0:n], r=[tk], w=[hk])
                else:
                    self.ACT(self.hT[:, c, t0:t0 + n], tb[:, 0:n], AF.Identity, bias=MOD[:, shift_j + c, y:y + 1], r=[tk, f"MOD{l}"], w=[hk])
            if router is not None:
                router["fn"](bi, t0, n)
        P.barrier()
        P.release(m)

    def head_norm_rope(self, src, npar, n, dst, onesl, inv_dim, gain, RT, rope_dram, t0, sk, dk, W):
        ps = self.ps
        sq, sd, rs, qn, t1, t2, rb = W["sq"], W["sd"], W["rs"], W["qn"], W["t1"], W["t2"], W["rb"]
        self.ACT(sq[0:npar, 0:n], src, AF.Square, r=[sk], w=["w_sq"])
        self.MM(ps[0:npar, 5, 0:n], onesl[0:npar, 0:npar], sq[0:npar, 0:n], r=["w_sq", "constb"], w=["ps5"])
        self.ACT(sd[0:npar, 0:n], ps[0:npar, 5, 0:n], AF.Sqrt, scale=inv_dim, bias=self.eps[0:npar, :], r=["ps5", "const2"], w=["w_sd"])
        self.RECIP(rs[0:npar, 0:n], sd[0:npar, 0:n], r=["w_sd"], w=["w_rs"])
        if t0 < CTX:
            self.STT(dst, src, gain, rs[0:npar, 0:n], ALU.mult, ALU.mult, r=[sk, "w_rs", "gains"], w=[dk])
            return
        self.STT(qn[0:npar, 0:n], src, gain, rs[0:npar, 0:n], ALU.mult, ALU.mult, r=[sk, "w_rs", "gains"], w=["w_qn"])
        self.MM(ps[0:npar, 6, 0:n], RT[0:npar, 0:npar], qn[0:npar, 0:n], r=["w_qn", "constb"], w=["ps6"])
        self.DMA(rb[:, 0, 0:n], rope_dram[:, t0 - CTX:t0 - CTX + n], w=["w_rb"])
        self.DMA(rb[:, 1, 0:n], rope_dram[:, SEQ + t0 - CTX:SEQ + t0 - CTX + n], w=["w_rb"])
        self.TT("gpsimd", t1[0:npar, 0:n], qn[0:npar, 0:n], rb[0:npar, 0, 0:n], ALU.mult, r=["w_qn", "w_rb"], w=["w_t1"])
        self.TT("vector", t2[0:npar, 0:n], ps[0:npar, 6, 0:n], rb[0:npar, 1, 0:n], ALU.mult, r=["ps6", "w_rb"], w=["w_t2"])
        self.TT("vector", dst, t1[0:npar, 0:n], t2[0:npar, 0:n], ALU.add, r=["w_t1", "w_t2"], w=[dk])

    def norm_temps(self):
        P = self.P
        return dict(sq=P.alloc(512, BF16), sd=P.alloc(512, F32), rs=P.alloc(512, F32), qn=P.alloc(512, BF16),
                    t1=P.alloc(512, BF16), t2=P.alloc(512, BF16), rb=P.alloc(1024, F32).rearrange("p (a n) -> p a n", a=2))

    def out_proj(self, l, Wo, kparts, mix, mixk, wk, nacc=1):
        ps = self.ps
        MOD = self.MOD[l]
        cnt = 0
        for bi, (t0, n) in enumerate(BLKS):
            y = 1 if bi == 0 else 0
            for dc in range(8):
                bank = cnt % 4
                cnt += 1
                for a in range(nacc):
                    self.MM(ps[:, bank, 0:n], Wo[0:kparts, a, dc * 128:(dc + 1) * 128], mix[0:kparts, a, t0:t0 + n],
                            start=(a == 0), stop=(a == nacc - 1), r=[wk, mixk], w=[f"ps{bank}"])
                self.STT(self.xT[:, dc, t0:t0 + n], ps[:, bank, 0:n], MOD[:, 16 + dc, y:y + 1], self.xT[:, dc, t0:t0 + n],
                         ALU.mult, ALU.add, r=[f"ps{bank}", f"MOD{l}", f"xT{bi}"], w=[f"xT{bi}"])

    def mixer_C(self, l):
        P, ps = self.P, self.ps
        m = P.mark()
        WC = P.alloc(8 * 416, BF16).rearrange("p (k n) -> p k n", k=8)
        self.load_w_cols(WC, self.w_in[l], 2576, 416, "WC")
        Wuq = P.alloc(2 * 384, BF16).rearrange("p (k n) -> p k n", k=2)
        self.load_w_cols(Wuq, self.mla_w_uq[l], 0, 384, "Wuq", kc=2)
        Wk = P.alloc(4 * 96, BF16).rearrange("p (h n) -> p h n", h=4)
        Wv = P.alloc(256, BF16).rearrange("p (h n) -> p h n", h=4)
        self.MEMSET("vector", Wk, 0.0, w=["Wk"])
        st, sk = self.stage()
        self.DMA(st[:, 0:512], self.mla_w_ukv[l], w=[sk])
        sv = st[:, 0:512].rearrange("p (h n) -> p h n", h=4)
        self.CP("gpsimd", Wk[:, :, 0:64], sv[:, :, 0:64], r=[sk], w=["Wk"])
        self.CP("gpsimd", Wv, sv[:, :, 64:128], r=[sk], w=["Wv"])
        g = P.alloc(8, F32)
        self.DMA(g[:, 0:2], self.mla_q_a_norm[l].rearrange("(c p) -> p c", p=128), w=["gains"])
        self.DMA(g[:, 2:3], self.mla_kv_a_norm[l].rearrange("(p o) -> p o", o=1), w=["gains"])
        self.DMA(g[0:96, 5:6], self.mla_q_norm[l].rearrange("(p o) -> p o", o=1), w=["gains"])
        self.DMA(g[0:96, 4:5], self.mla_k_norm[l].rearrange("(p o) -> p o", o=1), w=["gains"])
        self.TS("vector", g[0:96, 3:4], g[0:96, 5:6], 96.0 ** -0.5, ALU.mult, r=["gains"], w=["gains"])
        cqn = P.alloc(2 * T, BF16).rearrange("p (c t) -> p c t", c=2)
        ckvn = P.alloc(T, BF16)
        krT = P.alloc(T, BF16)
        W = self.norm_temps()
        for bi, (t0, n) in enumerate(BLKS):
            hk = f"hT{bi}"
            for o, (c0, npar) in enumerate([(0, 128), (128, 128), (256, 128), (384, 32)]):
                for k in range(8):
                    self.MM(ps[0:npar, o, 0:n], WC[:, k, c0:c0 + npar], self.hT[:, k, t0:t0 + n], start=(k == 0), stop=(k == 7), r=["WC", hk], w=[f"ps{o}"])
            for c in range(2):
                self.ACT(W["sq"][:, 0:n], ps[:, c, 0:n], AF.Square, r=[f"ps{c}"], w=["w_sq"])
                self.MM(ps[:, 5, 0:n], self.ones_b, W["sq"][:, 0:n], start=(c == 0), stop=(c == 1), r=["w_sq", "constb"], w=["ps5"])
            self.ACT(W["sd"][:, 0:n], ps[:, 5, 0:n], AF.Sqrt, scale=1.0 / 256, bias=self.eps, r=["ps5", "const2"], w=["w_sd"])
            self.RECIP(W["rs"][:, 0:n], W["sd"][:, 0:n], r=["w_sd"], w=["w_rs"])
            for c in range(2):
                self.STT(cqn[:, c, t0:t0 + n], ps[:, c, 0:n], g[:, c:c + 1], W["rs"][:, 0:n], ALU.mult, ALU.mult, r=[f"ps{c}", "w_rs", "gains"], w=[f"cqn{bi}"])
            self.ACT(W["sq"][:, 0:n], ps[:, 2, 0:n], AF.Square, r=["ps2"], w=["w_sq"])
            self.MM(ps[:, 5, 0:n], self.ones_b, W["sq"][:, 0:n], r=["w_sq", "constb"], w=["ps5"])
            self.ACT(W["sd"][:, 0:n], ps[:, 5, 0:n], AF.Sqrt, scale=1.0 / 128, bias=self.eps, r=["ps5", "const2"], w=["w_sd"])
            self.RECIP(W["rs"][:, 0:n], W["sd"][:, 0:n], r=["w_sd"], w=["w_rs"])
            self.STT(ckvn[:, t0:t0 + n], ps[:, 2, 0:n], g[:, 2:3], W["rs"][:, 0:n], ALU.mult, ALU.mult, r=["ps2", "w_rs", "gains"], w=[f"ckvn{bi}"])
            self.CP("scalar", krT[0:32, t0:t0 + n], ps[0:32, 3, 0:n], r=["ps3"], w=[f"krT{bi}"])
        allc = [f"cqn{b}" for b in range(5)]
        allkv = [f"ckvn{b}" for b in range(5)]
        qr = P.alloc(T, BF16)
        kr = P.alloc(T, BF16)
        Vh = P.alloc(NT * 64, BF16).rearrange("p (t d) -> p t d", d=64)
        mixh = P.alloc(T, BF16)
        WoC = P.alloc(1024, BF16)
        Eb = [P.alloc(512, BF16) for _ in range(3)]
        rd = P.alloc(512, F32)
        for h in range(4):
            self.load_cast(WoC[0:64, :], self.w_out[l, 768 + h * 64:768 + (h + 1) * 64, :], 1024, dkey="WoC", parts=64)
            for bi, (t0, n) in enumerate(BLKS):
                for k in range(2):
                    self.MM(ps[0:96, 0, 0:n], Wuq[:, k, h * 96:(h + 1) * 96], cqn[:, k, t0:t0 + n], start=(k == 0), stop=(k == 1), r=["Wuq", f"cqn{bi}"], w=["ps0"])
                self.head_norm_rope(ps[0:96, 0, 0:n], 96, n, qr[0:96, t0:t0 + n], self.ones_b, 1.0 / 96, g[0:96, 3:4], self.RCT_b, self.ropeC, t0, "ps0", f"qr{bi}", W)
                self.MM(ps[0:96, 1, 0:n], Wk[:, h, :], ckvn[:, t0:t0 + n], start=True, stop=False, r=["Wk", f"ckvn{bi}"], w=["ps1"])
                self.MM(ps[0:96, 1, 0:n], self.E32_b[0:32, 0:96], krT[0:32, t0:t0 + n], start=False, stop=True, r=["constb", f"krT{bi}"], w=["ps1"])
                self.head_norm_rope(ps[0:96, 1, 0:n], 96, n, kr[0:96, t0:t0 + n], self.ones_b, 1.0 / 96, g[0:96, 4:5], self.RCT_b, self.ropeC, t0, "ps1", f"kr{bi}", W)
            for ti in range(NT):
                bank = 2 + ti % 2
                self.MM(ps[:, bank, 0:64], ckvn[:, ti * 128:(ti + 1) * 128], Wv[:, h, :], r=allkv + ["Wv"], w=[f"ps{bank}"])
                self.CP("scalar", Vh[:, ti, :], ps[:, bank, 0:64], r=[f"ps{bank}"], w=["Vh"])
            allq = [f"qr{b}" for b in range(5)]
            allk = [f"kr{b}" for b in range(5)]
            for qi, (t0, n) in enumerate(BLKS):
                kts = [0, 1] if qi == 0 else list(range(NT))
                ob, db = (4, 5) if qi % 2 == 0 else (6, 7)
                for idx, kt in enumerate(kts):
                    sb = idx % 4
                    E, ek = Eb[idx % 3], f"E{idx % 3}"
                    self.MM(ps[:, sb, 0:n], kr[0:96, kt * 128:(kt + 1) * 128], qr[0:96, t0:t0 + n], r=allk + [f"qr{qi}"], w=[f"ps{sb}"])
                    self.ACT(E[:, 0:n], ps[:, sb, 0:n], AF.Exp, r=[f"ps{sb}"], w=[ek])
                    self.MM(ps[0:64, ob, 0:n], Vh[:, kt, :], E[:, 0:n], start=(idx == 0), stop=(idx == len(kts) - 1), r=["Vh", ek], w=[f"ps{ob}"])
                    self.MM(ps[0:64, db, 0:n], self.ones_b[:, 0:64], E[:, 0:n], start=(idx == 0), stop=(idx == len(kts) - 1), r=["constb", ek], w=[f"ps{db}"])
                self.RECIP(rd[0:64, 0:n], ps[0:64, db, 0:n], r=[f"ps{db}"], w=["rd"])
                self.TT("vector", mixh[0:64, t0:t0 + n], ps[0:64, ob, 0:n], rd[0:64, 0:n], ALU.mult, r=[f"ps{ob}", "rd"], w=[f"mixh{qi}"])
            if self.dbg and self.dbg_what == f"C{h}":
                self.dump(mixh[0:64, :], T, [f"mixh{b}" for b in range(5)], rows=64)
            self.out_proj_heads(l, WoC, mixh)
        P.barrier()
        P.release(m)

    def out_proj_heads(self, l, Wo, mixh):
        ps = self.ps
        MOD = self.MOD[l]
        cnt = 0
        for bi, (t0, n) in enumerate(BLKS):
            y = 1 if bi == 0 else 0
            for dc in range(8):
                bank = cnt % 4
                cnt += 1
                self.MM(ps[:, bank, 0:n], Wo[0:64, dc * 128:(dc + 1) * 128], mixh[0:64, t0:t0 + n], r=["WoC", f"mixh{bi}"], w=[f"ps{bank}"])
                self.STT(self.xT[:, dc, t0:t0 + n], ps[:, bank, 0:n], MOD[:, 16 + dc, y:y + 1], self.xT[:, dc, t0:t0 + n],
                         ALU.mult, ALU.add, r=[f"ps{bank}", f"MOD{l}", f"xT{bi}"], w=[f"xT{bi}"])

    def build(self, dbg_what=None):
        nc, P = self.nc, self.P
        self.dbg_what = dbg_what
        NB = 48800
        with nc.sbuf_tensor("arena", [128, NB], F32) as arena, nc.psum_tensor("ps", [128, 8, 512], F32) as ps:
            self.ps = ps
            P.set_arena(arena, NB * 4)
            self.setup()
            if dbg_what == "setup":
                self.dump(self.VP, 128, ["VP"])
                P.finish()
                return nc
            for s in range(self.nseq):
                self.load_x(s)
                if dbg_what == "loadx":
                    self.dump(self.xT[:, 0, :], T, [f"xT{b}" for b in range(5)])
                    P.finish()
                    return nc
                for l in range(self.nlayers):
                    self.mods(s, l)
                if dbg_what == "mods":
                    self.dump(self.MOD[0].rearrange("p j y -> p (j y)"), 96, ["MOD0"])
                    P.finish()
                    return nc
                for l in range(self.nlayers):
                    self.layer(s, l)
                    if self.stop:
                        break
                if self.stop:
                    break
                self.store_x(s)
            P.finish()
        return nc

    stop = False

    def layer(self, s, l):
        ph = self.phases
        allx = [f"xT{b}" for b in range(5)]
        allh = [f"hT{b}" for b in range(5)]
        self.norm_mod(l, self.A1[l], 0)
        if self.dbg_what == "h":
            self.dump(self.hT[:, 0, :], T, allh)
            self.stop = True
            return
        if "C" in ph:
            self.mixer_C(l)
        if "A" in ph:
            self.mixer_A(l)
        if "B" in ph:
            self.mixer_B(l)
        if self.dbg_what == "xa":
            self.dump(self.xT[:, 0, :], T, allx)
            self.stop = True
            return
        if "M" in ph:
            self.moe(l)
        if self.dbg_what == "xb":
            self.dump(self.xT[:, 0, :], T, allx)
            self.stop = True
            return


def make_inputs(inputs, core, consts):
    b0 = 2 * core
    f = lambda k: np.ascontiguousarray(inputs[k], dtype=np.float32)
    vp1 = np.concatenate([np.concatenate([f("b_mod")[l].reshape(48, 128), f("norm1")[l].reshape(8, 128), f("norm2")[l].reshape(8, 128)], 0) for l in range(2)], 0)
    vp2 = np.zeros((32, 128), np.float32)
    vp2[0:8] = f("c")[b0].reshape(8, 128)
    vp2[8:16] = f("c")[b0 + 1].reshape(8, 128)
    vp2[16:24] = f("c_ctx").reshape(8, 128)
    m = {"x2": f("x")[b0:b0 + 2], "ctx2": f("ctx")[b0:b0 + 2], "vp1": vp1, "vp2": vp2}
    for k in ["w_mod", "w_in", "w_out", "swa_q_norm", "swa_k_norm", "swa_sink", "gdn_conv", "gdn_out_norm", "mla_q_a_norm", "mla_w_uq",
              "mla_kv_a_norm", "mla_w_ukv", "mla_q_norm", "mla_k_norm", "router_w", "router_b", "exp_w_gu", "exp_b_gu", "exp_w_dn", "exp_b_dn"]:
        m[k] = f(k)
    m["gdn_a_log"] = f("gdn_a_log").reshape(2, 8)
    m["gdn_dt_bias"] = f("gdn_dt_bias").reshape(2, 8)
    m.update(consts)
    return m


def kernel(**inputs):
    k = K(nseq=2, nlayers=2, phases="CABM")
    nc = k.build()
    consts = host_consts()
    in_maps = [make_inputs(inputs, c, consts) for c in range(8)]
    res = run_bass_kernel_spmd(nc, in_maps, core_ids=list(range(8)))
    return np.concatenate([r["out"] for r in res.results], axis=0)


def _mixer_A(self, l):
    P, ps = self.P, self.ps
    m = P.mark()
    WA = P.alloc(8 * 512, BF16).rearrange("p (k n) -> p k n", k=8)
    for d0, c0, nc_ in [(0, 0, 64), (64, 128, 64), (128, 64, 64), (192, 192, 64), (256, 256, 256)]:
        self.load_w_cols(WA[:, :, d0:d0 + nc_], self.w_in[l], c0, nc_, "WA")
    g = P.alloc(8, F32)
    for hh in range(2):
        self.DMA(g[hh * 64:(hh + 1) * 64, 2:3], self.swa_q_norm[l].rearrange("(p o) -> p o", o=1), w=["gains"])
        self.DMA(g[hh * 64:(hh + 1) * 64, 1:2], self.swa_k_norm[l].rearrange("(p o) -> p o", o=1), w=["gains"])
    self.TS("vector", g[:, 0:1], g[:, 2:3], 0.125, ALU.mult, r=["gains"], w=["gains"])
    sk = P.alloc(4, F32)
    self.DMA(sk, self.swa_sink[l].rearrange("(a h) -> a h", a=1).broadcast_to([128, 4]), w=["sk0"])
    self.ACT(sk, sk, AF.Exp, r=["sk0"], w=["sk"])
    qr = P.alloc(2 * T, BF16).rearrange("p (c t) -> p c t", c=2)
    kr = P.alloc(T, BF16)
    V = P.alloc(NT * 128, BF16).rearrange("p (t d) -> p t d", d=128)
    W = self.norm_temps()
    for bi, (t0, n) in enumerate(BLKS):
        for o, c0 in enumerate([0, 128, 256]):
            for k in range(8):
                self.MM(ps[:, o, 0:n], WA[:, k, c0:c0 + 128], self.hT[:, k, t0:t0 + n], start=(k == 0), stop=(k == 7), r=["WA", f"hT{bi}"], w=[f"ps{o}"])
            dst = kr[:, t0:t0 + n] if o == 2 else qr[:, o, t0:t0 + n]
            self.head_norm_rope(ps[:, o, 0:n], 128, n, dst, self.bd64_b, 1.0 / 64, g[:, (1 if o == 2 else 0):(2 if o == 2 else 1)], self.RAT_b, self.ropeA, t0, f"ps{o}", "qkA", W)
    for ti in range(NT):
        bank = 3 + ti % 2
        for k in range(8):
            self.MM(ps[:, bank, 0:128], self.hT[:, k, ti * 128:(ti + 1) * 128], WA[:, k, 384:512], start=(k == 0), stop=(k == 7), r=["WA", f"hT{tile_blk(ti)}"], w=[f"ps{bank}"])
        self.CP("scalar", V[:, ti, :], ps[:, bank, 0:128], r=[f"ps{bank}"], w=["VA"])
    mixg = P.alloc(2 * T, BF16).rearrange("p (c t) -> p c t", c=2)
    WoA = P.alloc(2 * 1024, BF16).rearrange("p (c n) -> p c n", c=2)
    Eb = [P.alloc(5 * 256, BF16).rearrange("p (k n) -> p k n", k=5) for _ in range(2)]
    dn = P.alloc(256, F32)
    for gi in range(2):
        gp = slice(gi * 64, (gi + 1) * 64)
        for j in range(2):
            hd = 2 * gi + j
            self.load_cast(WoA[0:64, j, :], self.w_out[l, hd * 64:(hd + 1) * 64, :], 1024, dkey="WoA", parts=64)
        for ti in range(NT):
            if ti < 2:
                kts = [(0, None), (1, None)]
            else:
                kts = ([(ti - 1, self.L)] if ti - 1 >= 2 else []) + [(ti, None)] + ([(ti + 1, self.U)] if ti + 1 < NT else []) + [(0, None), (1, None)]
            b0 = 0 if ti % 2 == 0 else 3
            E, ek = Eb[ti % 2], f"EA{ti % 2}"
            qrhs = qr[gp, :, ti * 128:(ti + 1) * 128]
            nk = len(kts)
            for idx, (kt, mk) in enumerate(kts):
                bank = b0 + idx // 2
                c0 = (idx % 2) * 256
                self.MM(ps[:, bank, c0:c0 + 256].rearrange("p (j t) -> p j t", j=2), kr[gp, kt * 128:(kt + 1) * 128], qrhs, r=["qkA"], w=[f"ps{bank}"])
            psflat = ps[:, b0:b0 + 3, :].rearrange("p b n -> p (b n)")
            self.ACT(E[:, 0:nk, :].rearrange("p k n -> p (k n)"), psflat[:, 0:nk * 256], AF.Exp, r=[f"ps{b0}", f"ps{b0 + 1}", f"ps{b0 + 2}"], w=[ek])
            for idx, (kt, mk) in enumerate(kts):
                if mk is not None:
                    ev = E[:, idx, :].rearrange("p (j t) -> p j t", j=2)
                    self.TT("gpsimd", ev, ev, mk.rearrange("p (a t) -> p a t", a=1).broadcast_to([128, 2, 128]), ALU.mult, r=[ek, "const"], w=[ek])
            for idx, (kt, mk) in enumerate(kts):
                self.MM(ps[0:64, 6, 0:256], V[:, kt, gi * 64:(gi + 1) * 64], E[:, idx, :], start=(idx == 0), stop=(idx == nk - 1), r=["VA", ek], w=["ps6"])
            for idx, (kt, mk) in enumerate(kts):
                self.MM(ps[0:64, 7, 0:256], self.ones_b[:, 0:64], E[:, idx, :], start=(idx == 0), stop=(idx == nk - 1), r=["constb", ek], w=["ps7"])
            for j in range(2):
                self.TS("vector", dn[0:64, j * 128:(j + 1) * 128], ps[0:64, 7, j * 128:(j + 1) * 128], sk[0:64, 2 * gi + j:2 * gi + j + 1], ALU.add, r=["ps7", "sk"], w=["dnA"])
            self.RECIP(dn[0:64, :], dn[0:64, :], r=["dnA"], w=["dnA"])
            self.TT("vector", mixg[0:64, :, ti * 128:(ti + 1) * 128], ps[0:64, 6, 0:256].rearrange("p (j t) -> p j t", j=2),
                    dn[0:64, :].rearrange("p (j t) -> p j t", j=2), ALU.mult, r=["ps6", "dnA"], w=["mixg"])
        if self.dbg and self.dbg_what == f"A{gi}":
            self.dump(mixg[0:64, 0, :], T, ["mixg"], rows=64)
        self.out_proj(l, WoA, 64, mixg, "mixg", "WoA", nacc=2)
    P.barrier()
    P.release(m)


K.mixer_A = _mixer_A


def _mixer_B(self, l):
    P, ps = self.P, self.ps
    m = P.mark()
    psq = ps.rearrange("p b (q n) -> p (b q) n", q=4)
    allh = [f"hT{b}" for b in range(5)]
    gm = P.alloc(512, BF16)
    self.load_cast(gm, self.gmask_d, 512, dkey="gmask")
    self.NSL, self.NSU, self.PMU, self.PML = gm[:, 0:128], gm[:, 128:256], gm[:, 256:384], gm[:, 384:512]
    Wab = P.alloc(8 * 16, BF16).rearrange("p (k n) -> p k n", k=8)
    self.load_w_cols(Wab, self.w_in[l], 2560, 16, "Wab")
    for ti in range(NT):
        for k in range(8):
            self.MM(ps[:, 0, ti * 16:(ti + 1) * 16], self.hT[:, k, ti * 128:(ti + 1) * 128], Wab[:, k, :], start=(k == 0), stop=(k == 7), r=["Wab"] + allh, w=["ps0"])
    abv = ps[:, 0, 0:NT * 16].rearrange("p (t j) -> p t j", j=16)
    one = P.alloc(1, F32)
    self.MEMSET("vector", one, 1.0, w=["one"])
    arr = lambda: P.alloc(NT * 8, F32).rearrange("p (t j) -> p t j", j=8)
    dtb, alog, X, AX, G, BETA, GC, GCL, BEG, KD = [arr() for _ in range(10)]
    self.DMA(dtb, self.gdn_dt_bias[l].rearrange("(a b j) -> a b j", a=1, b=1).broadcast_to([128, NT, 8]), w=["dtb"])
    self.DMA(alog, self.gdn_a_log[l].rearrange("(a b j) -> a b j", a=1, b=1).broadcast_to([128, NT, 8]), w=["alog"])
    self.TT("vector", X, abv[:, :, 0:8], dtb, ALU.add, r=["ps0", "dtb"], w=["gX"])
    self.TS("vector", AX, X, -1.0, ALU.mult, r=["gX"], w=["gAX"])
    self.TT("vector", AX, AX, X, ALU.max, r=["gX", "gAX"], w=["gAX"])
    self.ACT(AX, AX, AF.Exp, scale=-1.0, r=["gAX"], w=["gAX"])
    self.ACT(AX, AX, AF.Ln, bias=one, r=["gAX", "one"], w=["gAX"])
    self.STT(X, X, 0.0, AX, ALU.max, ALU.add, r=["gX", "gAX"], w=["gX"])
    self.ACT(alog, alog, AF.Exp, r=["alog"], w=["alog"])
    self.TS("vector", alog, alog, -1.0, ALU.mult, r=["alog"], w=["alog"])
    self.TT("vector", G, X, alog, ALU.mult, r=["gX", "alog"], w=["gG"])
    self.ACT(BETA, abv[:, :, 8:16], AF.Sigmoid, r=["ps0"], w=["gB"])
    if self.dbg_what == "BG3a":
        P.barrier()
        self.dump(G.rearrange("p t j -> p (t j)"), NT * 8, ["gG"], col0=256)
        self.dump(BETA.rearrange("p t j -> p (t j)"), NT * 8, ["gB"], col0=768)
        P.release(m)
        return
    for ti in range(NT):
        self.MM(ps[:, 1, ti * 8:ti * 8 + 4], self.U, G[:, ti, 0:4], r=["gG", "const"], w=["ps1"])
        self.MM(ps[:, 1, ti * 8 + 4:ti * 8 + 8], self.L, G[:, ti, 4:8], r=["gG", "const"], w=["ps1"])
        self.MM(ps[:, 2, ti * 8:ti * 8 + 8], self.ones, G[:, ti, :], r=["gG", "const"], w=["ps2"])
    self.CP("vector", GC, ps[:, 1, 0:NT * 8].rearrange("p (t j) -> p t j", j=8), r=["ps1"], w=["gGC"])
    self.CP("vector", GCL, ps[:, 2, 0:NT * 8].rearrange("p (t j) -> p t j", j=8), r=["ps2"], w=["gGCL"])
    if self.dbg_what == "BG3b":
        P.barrier()
        self.dump(GC.rearrange("p t j -> p (t j)"), NT * 8, ["gGC"], col0=256)
        self.dump(GCL.rearrange("p t j -> p (t j)"), NT * 8, ["gGCL"], col0=768)
        P.release(m)
        return
    self.ACT(BEG, GC, AF.Exp, r=["gGC"], w=["gBEG"])
    self.TT("vector", BEG, BEG, BETA, ALU.mult, r=["gBEG", "gB"], w=["gBEG"])
    self.TT("vector", KD, GCL, GC, ALU.subtract, r=["gGC", "gGCL"], w=["gKD"])
    self.ACT(KD, KD, AF.Exp, r=["gKD"], w=["gKD"])
    self.ACT(GCL, GCL, AF.Exp, r=["gGCL", "gKD"], w=["gGCL"])
    P.barrier()
    if self.dbg_what == "BG3":
        self.dump(KD.rearrange("p t j -> p (t j)"), NT * 8, ["gKD"])
        self.dump(G.rearrange("p t j -> p (t j)"), NT * 8, ["gG"], col0=256)
        self.dump(GC.rearrange("p t j -> p (t j)"), NT * 8, ["gGC"], col0=512)
        self.dump(BETA.rearrange("p t j -> p (t j)"), NT * 8, ["gB"], col0=768)
        P.release(m)
        return
    gains = P.alloc(2, F32)
    self.DMA(gains[:, 0:1], self.gdn_out_norm[l].rearrange("(p o) -> p o", o=1), w=["gains"])
    WG = P.alloc(8 * 128, BF16).rearrange("p (k n) -> p k n", k=8)
    raw = P.alloc(T, BF16)
    kT = P.alloc(T, F32)
    qT = P.alloc(T, BF16)
    vT = P.alloc(T, BF16)
    Oacc = P.alloc(T, BF16)
    cw = P.alloc(16, F32)
    y = P.alloc(512, F32)
    ys = P.alloc(512, F32)
    sqb = P.alloc(512, BF16)
    Wo = P.alloc(1024, BF16)
    names = ["qf", "vf", "Gtri", "t1", "e2m", "QKDT", "N", "M", "eg", "qdec", "kbg", "kdec", "bv", "R0", "R1", "N2", "M2", "U_", "WT", "VN", "S0", "S1"]
    Bs = [{nm: P.alloc(128, F32) for nm in names} for _ in range(2)]
    allraw = [f"raw{b}" for b in range(5)]
    for h in range(4):
        for Xi, (dstT, colbase) in enumerate([(qT, 512), (kT, 1024), (vT, 1536)]):
            self.load_w_cols(WG, self.w_in[l], colbase + h * 128, 128, "WG")
            self.DMA(cw[:, Xi * 5:(Xi + 1) * 5], self.gdn_conv[l, :, Xi * 512 + h * 128:Xi * 512 + (h + 1) * 128].rearrange("w p -> p w"), w=["cw"])
            for bi, (t0, n) in enumerate(BLKS):
                bank = bi % 2
                for k in range(8):
                    self.MM(ps[:, bank, 0:n], WG[:, k, :], self.hT[:, k, t0:t0 + n], start=(k == 0), stop=(k == 7), r=["WG", f"hT{bi}"], w=[f"pq{bank * 4}"])
                self.CP("scalar", raw[:, t0:t0 + n], ps[:, bank, 0:n], r=[f"pq{bank * 4}"], w=[f"raw{bi}"])
            for bi, (t0, n) in enumerate(BLKS):
                s0, s1 = (0, CTX) if bi == 0 else (CTX, T)
                self.TS("vector", y[:, 0:n], raw[:, t0:t0 + n], cw[:, Xi * 5 + 2:Xi * 5 + 3], ALU.mult, r=allraw + ["cw"], w=["cy"])
                for j in (0, 1, 3, 4):
                    a, b = max(t0, s0 - (j - 2)), min(t0 + n, s1 - (j - 2))
                    self.STT(y[:, a - t0:b - t0], raw[:, a + j - 2:b + j - 2], cw[:, Xi * 5 + j:Xi * 5 + j + 1], y[:, a - t0:b - t0], ALU.mult, ALU.add, r=allraw + ["cw", "cy"], w=["cy"])
                if Xi == 2:
                    self.ACT(vT[:, t0:t0 + n], y[:, 0:n], AF.Silu, r=["cy"], w=["gv"])
                    continue
                self.ACT(ys[:, 0:n], y[:, 0:n], AF.Silu, r=["cy"], w=["cys"])
                self.ACT(sqb[:, 0:n], ys[:, 0:n], AF.Square, r=["cys"], w=["csq"])
                self.MM(ps[:, 2, 0:n], self.ones_b, sqb[:, 0:n], r=["csq", "constb"], w=["pq8"])
                self.ACT(y[:, 0:n], ps[:, 2, 0:n], AF.Sqrt, bias=self.eps, r=["pq8", "const2", "cy"], w=["cy"])
                self.RECIP(y[:, 0:n], y[:, 0:n], r=["cy"], w=["cy"])
                self.STT(dstT[:, t0:t0 + n], ys[:, 0:n], (128.0 ** -0.5 if Xi == 0 else 1.0), y[:, 0:n], ALU.mult, ALU.mult, r=["cys", "cy"], w=["gq" if Xi == 0 else "gk"])
        P.barrier()
        if self.dbg_what == "Bproj":
            self.dump(kT, T, ["gk"])
            P.release(m)
            return
        order = [list(range(NT)), [1, 0] + list(range(NT - 1, 1, -1))]
        Scur = [Bs[0]["S0"], Bs[1]["S0"]]
        Snxt = [Bs[0]["S1"], Bs[1]["S1"]]
        for d in range(2):
            self.MEMSET("vector", Scur[d], 0.0, w=[f"S{d}"])
        touched = set()
        for step in range(NT):
            bufs = []
            for d in range(2):
                P.rec = []
                bufs.append(P.rec)
                ti = order[d][step]
                B = Bs[d]
                j = d * 4 + h
                q0 = d * 16
                cs = slice(ti * 128, (ti + 1) * 128)
                kt = kT[:, cs]
                Tri = self.U if d == 0 else self.L
                MS = self.SL if d == 0 else self.SU
                MIT = self.U if d == 0 else self.L
                gcol, gccol, bcol = G[:, ti, j:j + 1], GC[:, ti, j:j + 1], BETA[:, ti, j:j + 1]
                K_ = lambda nm: f"b{d}{nm}"
                SLT = lambda i: psq[:, q0 + i, :]
                SK = lambda i: f"pq{q0 + i}"
                self.CP("gpsimd", B["qf"], qT[:, cs], r=["gq"], w=[K_("qf")])
                self.CP("gpsimd", B["vf"], vT[:, cs], r=["gv"], w=[K_("vf")])
                self.MM(SLT(0), kt, kt, r=["gk"], w=[SK(0)])
                self.MM(SLT(1), kt, B["qf"], r=["gk", K_("qf")], w=[SK(1)])
                self.TS("vector", B["Gtri"], Tri, gcol, ALU.mult, r=["const", "gG"], w=[K_("Gtri")])
                self.MM(SLT(13), self.ones, B["Gtri"], r=["const", K_("Gtri")], w=[SK(13)])
                self.STT(B["t1"], SLT(13), gccol, (self.NSL if d == 0 else self.NSU), ALU.subtract, ALU.max, r=[SK(13), "gGC", "gmask"], w=[K_("t1")])
                self.ACT(B["t1"], B["t1"], AF.Exp, scale=-1.0, r=[K_("t1")], w=[K_("t1")])
                self.STT(B["e2m"], SLT(13), gccol, (self.PMU if d == 0 else self.PML), ALU.subtract, ALU.min, r=[SK(13), "gGC", "gmask"], w=[K_("e2m")])
                self.ACT(B["e2m"], B["e2m"], AF.Exp, r=[K_("e2m")], w=[K_("e2m")])
                self.TT("vector", B["QKDT"], SLT(1), B["e2m"], ALU.mult, r=[SK(1), K_("e2m")], w=[K_("QKDT")])
                self.STT(B["N"], SLT(0), bcol, B["t1"], ALU.mult, ALU.mult, r=[SK(0), "gB", K_("t1")], w=[K_("N")])
                self.TR(SLT(3), B["N"], r=[K_("N")], w=[SK(3)])
                self.CP("scalar", B["M"], SLT(3), r=[SK(3)], w=[K_("M")])
                self.ACT(B["eg"], SLT(13), AF.Exp, r=[SK(13)], w=[K_("eg")])
                self.TT("vector", B["qdec"], B["qf"], B["eg"], ALU.mult, r=[K_("qf"), K_("eg")], w=[K_("qdec")])
                self.TR(SLT(4), kt, r=["gk"], w=[SK(4)])
                self.TR(SLT(5), B["vf"], r=[K_("vf")], w=[SK(5)])
                self.TS("vector", B["kbg"], SLT(4), BEG[:, ti, j:j + 1], ALU.mult, r=[SK(4), "gBEG"], w=[K_("kbg")])
                self.TS("vector", B["kdec"], SLT(4), KD[:, ti, j:j + 1], ALU.mult, r=[SK(4), "gKD"], w=[K_("kdec")])
                self.TS("vector", B["bv"], SLT(5), bcol, ALU.mult, r=[SK(5), "gB"], w=[K_("bv")])
                self.TT("vector", B["R0"], self.ident, B["M"], ALU.subtract, r=["const", K_("M")], w=[K_("R0")])
                Nc, Mc, Rc = ("N", "M", "R0")
                Nn, Mn, Rn = ("N2", "M2", "R1")
                for lev in range(1, 7):
                    self.MM(SLT(6), B[Mc], B[Nc], r=[K_(Mc), K_(Nc)], w=[SK(6)])
                    self.CP("vector", B[Nn], SLT(6), r=[SK(6)], w=[K_(Nn)])
                    if lev < 6:
                        self.MM(SLT(14), B[Nc], B[Mc], r=[K_(Mc), K_(Nc)], w=[SK(14)])
                        self.CP("scalar", B[Mn], SLT(14), r=[SK(14)], w=[K_(Mn)])
                    self.MM(SLT(8), B[Nn], B[Rc], r=[K_(Nn), K_(Rc)], w=[SK(8)])
                    self.TT("vector", B[Rn], SLT(8), B[Rc], ALU.add, r=[SK(8), K_(Rc)], w=[K_(Rn)])
                    Nc, Nn = Nn, Nc
                    Mc, Mn = Mn, Mc
                    Rc, Rn = Rn, Rc
                TTk = Rc
                self.MM(SLT(9), B[TTk], B["bv"], r=[K_(TTk), K_("bv")], w=[SK(9)])
                self.CP("scalar", B["U_"], SLT(9), r=[SK(9)], w=[K_("U_")])
                self.MM(SLT(15), B["kbg"], B[TTk], r=[K_(TTk), K_("kbg")], w=[SK(15)])
                self.CP("vector", B["WT"], SLT(15), r=[SK(15)], w=[K_("WT")])
                S_, Sn_ = Scur[d], Snxt[d]
                self.MM(SLT(11), B["WT"], S_, r=[K_("WT"), f"S{d}"], w=[SK(11)])
                self.TT("vector", B["VN"], B["U_"], SLT(11), ALU.subtract, r=[K_("U_"), SK(11)], w=[K_("VN")])
                self.MM(SLT(12), S_, B["qdec"], start=True, stop=False, r=[f"S{d}", K_("qdec")], w=[SK(12)])
                self.MM(SLT(12), B["VN"], B["QKDT"], start=False, stop=True, r=[K_("VN"), K_("QKDT")], w=[SK(12)])
                if ti not in touched:
                    touched.add(ti)
                    self.CP("scalar", Oacc[:, cs], SLT(12), r=[SK(12)], w=[f"Oacc{ti}"])
                else:
                    self.TT("vector", Oacc[:, cs], SLT(12), Oacc[:, cs], ALU.add, r=[SK(12), f"Oacc{ti}"], w=[f"Oacc{ti}"])
                self.MM(SLT(9 + 0), B["kdec"], B["VN"], r=[K_("kdec"), K_("VN")], w=[SK(9 + 0)])
                self.STT(Sn_, S_, GCL[:, ti, j:j + 1], SLT(9 + 0), ALU.mult, ALU.add, r=[f"S{d}", "gGCL", SK(9 + 0)], w=[f"S{d}"])
                Scur[d], Snxt[d] = Sn_, S_
            P.replay_interleaved(bufs)
        P.barrier()
        self.load_w_cols(WG, self.w_in[l], 2048 + h * 128, 128, "WG")
        self.load_cast(Wo, self.w_out[l, 256 + h * 128:256 + (h + 1) * 128, :], 1024, dkey="WoB")
        MOD = self.MOD[l]
        for bi, (t0, n) in enumerate(BLKS):
            yy = 1 if bi == 0 else 0
            oa = Oacc[:, t0:t0 + n]
            self.ACT(sqb[:, 0:n], oa, AF.Square, w=["csq"])
            self.MM(ps[:, 2, 0:n], self.ones_b, sqb[:, 0:n], r=["csq", "constb"], w=["pq8"])
            self.ACT(y[:, 0:n], ps[:, 2, 0:n], AF.Sqrt, scale=1.0 / 128, bias=self.eps, r=["pq8", "const2"], w=["cy"])
            self.RECIP(y[:, 0:n], y[:, 0:n], r=["cy"], w=["cy"])
            self.STT(ys[:, 0:n], oa, gains[:, 0:1], y[:, 0:n], ALU.mult, ALU.mult, r=["cy", "gains"], w=["cys"])
            for k in range(8):
                self.MM(ps[:, 3, 0:n], WG[:, k, :], self.hT[:, k, t0:t0 + n], start=(k == 0), stop=(k == 7), r=["WG", f"hT{bi}"], w=["pq12"])
            self.ACT(y[:, 0:n], ps[:, 3, 0:n], AF.Silu, r=["pq12", "cy"], w=["cy"])
            self.TT("vector", sqb[:, 0:n], ys[:, 0:n], y[:, 0:n], ALU.mult, r=["cys", "cy"], w=["csq"])
            if self.dbg and self.dbg_what == f"B{h}":
                self.dump(sqb[:, 0:n], n, ["csq"], col0=t0)
            for dc in range(8):
                bank = 4 + dc % 4
                self.MM(ps[:, bank, 0:n], Wo[:, dc * 128:(dc + 1) * 128], sqb[:, 0:n], r=["WoB", "csq"], w=[f"pq{bank * 4}"])
                self.STT(self.xT[:, dc, t0:t0 + n], ps[:, bank, 0:n], MOD[:, 16 + dc, yy:yy + 1], self.xT[:, dc, t0:t0 + n],
                         ALU.mult, ALU.add, r=[f"pq{bank * 4}", f"MOD{l}", f"xT{bi}"], w=[f"xT{bi}"])
        P.barrier()
    P.release(m)


K.mixer_B = _mixer_B


def _moe(self, l):
    P, ps = self.P, self.ps
    m0 = P.mark()
    gateT = P.alloc(T, BF16)
    m = P.mark()
    RW = P.alloc(8 * 32, F32).rearrange("p (k e) -> p k e", k=8)
    self.DMA(RW, self.router_w[l].rearrange("(k p) e -> p k e", p=128), w=["RW"])
    rb = P.alloc(32, F32)
    self.DMA(rb, self.router_b[l].rearrange("(a e) -> a e", a=1).broadcast_to([128, 32]), w=["rb"])
    h2f = P.alloc(8 * 512, F32).rearrange("p (c t) -> p c t", c=8)
    lg, msk, ex = P.alloc(32, F32), P.alloc(32, F32), P.alloc(32, F32)
    top8, dn = P.alloc(8, F32), P.alloc(1, F32)

    def router_fn(bi, t0, n):
        for tt in range(n // 128):
            cs = slice(tt * 128, (tt + 1) * 128)
            for c in range(8):
                self.MM(ps[:, 1, 0:32], h2f[:, c, cs], RW[:, c, :], start=(c == 0), stop=(c == 7), r=[f"h2f{c}", "RW"], w=["ps1"])
            self.TT("vector", lg, ps[:, 1, 0:32], rb, ALU.add, r=["ps1", "rb"], w=["lg"])
            self.P.op("vector", lambda e: e.max(out=top8, in_=lg), ["lg"], ["top8"])
            self.TS("vector", msk, lg, top8[:, 3:4], ALU.is_ge, r=["lg", "top8"], w=["msk"])
            self.TS("vector", ex, lg, top8[:, 0:1], ALU.subtract, r=["lg", "top8"], w=["ex"])
            self.ACT(ex, ex, AF.Exp, r=["ex"], w=["ex"])
            self.TT("vector", ex, ex, msk, ALU.mult, r=["ex", "msk"], w=["ex"])
            self.P.op("vector", lambda e: e.reduce_sum(out=dn, in_=ex, axis=mybir.AxisListType.X), ["ex"], ["dn"])
            self.RECIP(dn, dn, r=["dn"], w=["dn"])
            self.TS("vector", ex, ex, dn[:, 0:1], ALU.mult, r=["ex", "dn"], w=["ex"])
            self.TR(ps[0:32, 2, 0:128], ex, r=["ex"], w=["ps2"])
            self.CP("scalar", gateT[0:32, t0 + tt * 128:t0 + (tt + 1) * 128], ps[0:32, 2, 0:128], r=["ps2"], w=["gateT"])

    self.norm_mod(l, self.A2[l], 24, router=dict(h2f=h2f, fn=router_fn))
    P.release(m)
    if self.dbg_what == "gate":
        self.dump(gateT[0:32, :], T, ["gateT"], rows=32)
        P.release(m0)
        return
    bsb = P.alloc(2048, F32)
    self.DMA(bsb[0:32, :], self.exp_b_gu[l], w=["bsb"])
    bv = bsb[0:32, :].rearrange("e (j p s) -> e j s p", j=8, s=2)
    for j in range(8):
        for s_ in range(2):
            self.TR(ps[:, 3, (j * 2 + s_) * 32:(j * 2 + s_ + 1) * 32], bv[:, j, s_, :], r=["bsb"], w=["ps3"])
    bguT = P.alloc(512, F32).rearrange("p (a e) -> p a e", e=32)
    self.CP("vector", bguT, ps[:, 3, :].rearrange("p (a e) -> p a e", e=32), r=["ps3"], w=["bguT"])
    bdn = P.alloc(1024, BF16)
    self.load_cast(bdn[0:32, :], self.exp_b_dn[l], 1024, dkey="bdn", parts=32)
    MOD = self.MOD[l]
    for bi, (t0, n) in enumerate(BLKS):
        yy = 1 if bi == 0 else 0
        for dc in range(8):
            bank = 4 + dc % 2
            self.MM(ps[:, bank, 0:n], bdn[0:32, dc * 128:(dc + 1) * 128], gateT[0:32, t0:t0 + n], r=["bdn", "gateT"], w=[f"ps{bank}"])
            self.STT(self.xT[:, dc, t0:t0 + n], ps[:, bank, 0:n], MOD[:, 40 + dc, yy:yy + 1], self.xT[:, dc, t0:t0 + n],
                     ALU.mult, ALU.add, r=[f"ps{bank}", f"MOD{l}", f"xT{bi}"], w=[f"xT{bi}"])
    Wgu = [P.alloc(8 * 512, BF16).rearrange("p (k s f) -> p k s f", k=8, s=2) for _ in range(2)]
    Wdn = [P.alloc(2 * 1024, BF16).rearrange("p (j n) -> p j n", j=2) for _ in range(2)]
    act = [P.alloc(2 * 512, BF16).rearrange("p (j n) -> p j n", j=2) for _ in range(3)]
    gsel = P.alloc(512, BF16)
    tg = [[P.alloc(512, BF16) for _ in range(2)] for _ in range(2)]
    tsg = [[P.alloc(512, BF16) for _ in range(2)] for _ in range(2)]
    tl = [[P.alloc(512, BF16) for _ in range(2)] for _ in range(2)]
    bl1 = P.alloc(8 * 32, F32).rearrange("p (j e) -> p j e", e=32)
    self.TS("vector", bl1, bguT.rearrange("p (j s) e -> p j s e", s=2)[:, :, 1, :], 1.0, ALU.add, r=["bguT"], w=["bl1"])
    nexp = NEXP if self.moe_experts is None else self.moe_experts
    units = [(e_, q) for e_ in range(nexp) for q in range(4)]

    def load_unit(ui):
        e_, q = units[ui]
        wi = ui % 2
        for k0 in range(0, 8, 2):
            st, sk = self.stage()
            sv = st[:, 0:1024].rearrange("p (k n) -> p k n", k=2)
            self.DMA(sv, self.exp_w_gu[l, e_, k0 * 128:(k0 + 2) * 128, q * 512:(q + 1) * 512].rearrange("(k p) n -> p k n", p=128), w=[sk])
            self.CP("gpsimd", Wgu[wi][:, k0:k0 + 2, :, :], sv.rearrange("p k (f s) -> p k s f", s=2), r=[sk], w=[f"Wgu{wi}"])
        for jj in range(2):
            st, sk = self.stage()
            self.DMA(st[:, 0:1024], self.exp_w_dn[l, e_, q * 256 + jj * 128:q * 256 + (jj + 1) * 128, :], w=[sk])
            self.CP("scalar", Wdn[wi][:, jj, :], st[:, 0:1024], r=[sk], w=[f"Wdn{wi}"])

    def gu_swiglu(ui, bi, ai):
        e_, q = units[ui]
        wi = ui % 2
        t0, n = BLKS[bi]
        wgk = f"Wgu{wi}"
        self.TS("vector", gsel[0:32, 0:n], gateT[0:32, t0:t0 + n], self.ident[0:32, e_:e_ + 1], ALU.mult, r=["gateT", "const"], w=["gsel"])
        for jj in range(2):
            bg_, bl_ = 2 * jj, 2 * jj + 1
            for s_ in range(2):
                for k in range(8):
                    self.MM(ps[:, 2 * jj + s_, 0:n], Wgu[wi][:, k, s_, jj * 128:(jj + 1) * 128], self.hT[:, k, t0:t0 + n],
                            start=(k == 0), stop=(k == 7), r=[wgk, f"hT{bi}"], w=[f"ps{2 * jj + s_}"])
            if jj == 0:
                self.MM(ps[:, 6, 0:n], self.ones_b[0:32, :], gsel[0:32, 0:n], r=["gsel", "constb"], w=["ps6"])
            fj = 2 * q + jj
            G_, S_, L_ = tg[ai % 2][jj], tsg[ai % 2][jj], tl[ai % 2][jj]
            kk = f"{ai % 2}{jj}"
            self.TS("vector", G_[:, 0:n], ps[:, bg_, 0:n], bguT[:, fj * 2, e_:e_ + 1], ALU.add, 7.0, ALU.min, r=[f"ps{bg_}", "bguT"], w=["tg" + kk])
            self.ACT(S_[:, 0:n], G_[:, 0:n], AF.Gelu_apprx_sigmoid, r=["tg" + kk], w=["tsg" + kk])
            self.TS("vector", L_[:, 0:n], ps[:, bl_, 0:n], bl1[:, fj, e_:e_ + 1], ALU.add, 8.0, ALU.min, r=[f"ps{bl_}", "bl1"], w=["tl" + kk])
            self.STT(L_[:, 0:n], L_[:, 0:n], -6.0, ps[:, 6, 0:n], ALU.max, ALU.mult, r=["tl" + kk, "ps6"], w=["tl" + kk])
            self.TT("gpsimd", act[ai][:, jj, 0:n], S_[:, 0:n], L_[:, 0:n], ALU.mult, r=["tsg" + kk, "tl" + kk], w=[f"act{ai}"])

    def dn_update(ui, bi, ai):
        wi = ui % 2
        t0, n = BLKS[bi]
        yy = 1 if bi == 0 else 0
        for dc in range(8):
            bank = 4 + dc % 2
            for jj in range(2):
                self.MM(ps[:, bank, 0:n], Wdn[wi][:, jj, dc * 128:(dc + 1) * 128], act[ai][:, jj, 0:n], start=(jj == 0), stop=(jj == 1), r=[f"Wdn{wi}", f"act{ai}"], w=[f"ps{bank}"])
            if dc in ():
                xi = xcnt[0] % 2
                xcnt[0] += 1
                self.ACT(xtmp[xi][:, 0:n], ps[:, bank, 0:n], AF.Copy, scale=MOD[:, 40 + dc, yy:yy + 1], r=[f"ps{bank}", f"MOD{l}"], w=[f"xtmp{xi}"])
                self.TT("gpsimd", self.xT[:, dc, t0:t0 + n], self.xT[:, dc, t0:t0 + n], xtmp[xi][:, 0:n], ALU.add, r=[f"xtmp{xi}", f"xTm{bi}_{dc}"], w=[f"xTm{bi}_{dc}"])
            else:
                self.STT(self.xT[:, dc, t0:t0 + n], ps[:, bank, 0:n], MOD[:, 40 + dc, yy:yy + 1], self.xT[:, dc, t0:t0 + n],
                         ALU.mult, ALU.add, r=[f"ps{bank}", f"MOD{l}", f"xTm{bi}_{dc}"], w=[f"xTm{bi}_{dc}"])

    xtmp = [P.alloc(512, F32), P.alloc(512, F32)]
    xcnt = [0]
    load_unit(0)
    pend = []
    step = 0
    blist = list(range(len(BLKS)))
    if l == self.nlayers - 1 and self.dbg_what is None:
        blist = blist[1:]
    for ui in range(len(units)):
        for bi in blist:
            ai = step % 2
            step += 1
            ai = step % 3
            P.rec = bufA = []
            gu_swiglu(ui, bi, ai)
            P.rec = bufB = []
            if len(pend) == 2:
                dn_update(*pend.pop(0))
            P.rec = None
            groups = [bufB[i:i + 3] for i in range(0, len(bufB), 3)]
            nmm = 0
            for kind, args in bufA:
                P.op(*args)
                if args[0] == "tensor":
                    nmm += 1
                    if nmm % 4 == 0 and groups:
                        for k2, a2 in groups.pop(0):
                            P.op(*a2)
            for g_ in groups:
                for k2, a2 in g_:
                    P.op(*a2)
            if bi == blist[1] and ui + 1 < len(units):
                load_unit(ui + 1)
            pend.append((ui, bi, ai))
    for p_ in pend:
        dn_update(*p_)
    P.barrier()
    P.release(m0)


K.moe = _moe
K.moe_experts = None
```

```python
import contextlib
import numpy as np
import concourse.bass as bass
import concourse.mybir as mybir
from concourse.bass_utils import run_bass_kernel_spmd

F32 = mybir.dt.float32
BF16 = mybir.dt.bfloat16
AF = mybir.ActivationFunctionType
ALU = mybir.AluOpType

ENG = ["tensor", "vector", "scalar", "gpsimd", "sync"]
SEM_ROLL = 30000
NDSEM = 24


class Prog:
    def __init__(self, nc):
        self.nc = nc
        self.ops = {e: [] for e in ENG}
        self.cnt = {e: 0 for e in ENG}
        self.seen = {e: {p: 0 for p in ENG} for e in ENG}
        self.dseen = {e: set() for e in ENG}
        self.res = {}
        self.ndma = 0
        self.dma_issuer = {}
        self.arena = None
        self.off = 0
        self.peak = 0

    def set_arena(self, arena, nbytes):
        self.arena = arena
        self.arena_bytes = nbytes
        self.off = 0

    def alloc(self, n, dtype):
        esz = 2 if dtype == BF16 else 4
        nb = (n * esz + 63) // 64 * 64
        assert self.off + nb <= self.arena_bytes, ("arena overflow", self.off, nb)
        a = self.arena[:, self.off // 4:(self.off + nb) // 4]
        self.off += nb
        self.peak = max(self.peak, self.off)
        if dtype != F32:
            a = a.bitcast(dtype)
        return a[:, 0:n]

    def mark(self):
        return self.off

    def release(self, m):
        self.off = m

    def _need(self, reads, writes):
        toks = []
        for r in reads:
            s = self.res.get(r)
            if s and s["w"] is not None:
                toks.append(s["w"])
        for w in writes:
            s = self.res.get(w)
            if s:
                if s["w"] is not None:
                    toks.append(s["w"])
                for e, n in s["rc"].items():
                    toks.append(("c", e, n))
                for d in s["rd"]:
                    toks.append(("d", d))
        return toks

    def _emit_waits(self, eng, toks):
        lst = self.ops[eng]
        best = {}
        for t in toks:
            if t[0] == "c":
                _, p, n = t
                if p == "tensor" and eng == "tensor":
                    continue
                if self.seen[eng][p] < n:
                    best[p] = max(best.get(p, 0), n)
            else:
                d = t[1]
                if d not in self.dseen[eng]:
                    self.dseen[eng].add(d)
                    k, v = d % NDSEM, 16 * (d // NDSEM + 1)
                    lst.append(lambda e, S, k=k, v=v: e.wait_ge(S["d"][k], v))
        for p, n in best.items():
            self.seen[eng][p] = n
            k, v = (n - 1) // SEM_ROLL, (n - 1) % SEM_ROLL + 1
            lst.append(lambda e, S, p=p, k=k, v=v: e.wait_ge(S[p][k], v))

    def _commit(self, tok, reads, writes):
        for r in reads:
            s = self.res.setdefault(r, {"w": None, "rc": {}, "rd": set()})
            if tok[0] == "c":
                s["rc"][tok[1]] = tok[2]
            else:
                s["rd"].add(tok[1])
        for w in writes:
            self.res[w] = {"w": tok, "rc": {}, "rd": set()}

    @staticmethod
    def _bank_keys(reads, writes):
        reads, writes = list(reads), list(writes)
        for k in reads + writes:
            b = None
            if k.startswith("pq") and k[2:].isdigit():
                b = int(k[2:]) // 4
            elif k.startswith("ps") and k[2:].isdigit():
                b = int(k[2:])
            if b is not None and f"PB{b}" not in writes:
                writes.append(f"PB{b}")
        return reads, writes

    rec = None

    def replay_interleaved(self, bufs):
        self.rec = None
        n = max(len(b) for b in bufs)
        for i in range(n):
            for b in bufs:
                if i < len(b):
                    kind, args = b[i]
                    if kind == "op":
                        self.op(*args)
                    else:
                        self.dma(*args[:-1], **args[-1])

    def op(self, eng, fn, reads=(), writes=()):
        if self.rec is not None:
            self.rec.append(("op", (eng, fn, list(reads), list(writes))))
            return
        reads, writes = self._bank_keys(reads, writes)
        self._emit_waits(eng, self._need(reads, writes))
        self.cnt[eng] += 1
        n = self.cnt[eng]
        k = (n - 1) // SEM_ROLL
        self.ops[eng].append(lambda e, S, fn=fn, p=eng, k=k: fn(e).then_inc(S[p][k], 1))
        self._commit(("c", eng, n), reads, writes)

    def dma(self, eng, out, in_, reads=(), writes=(), **kw):
        if self.rec is not None:
            self.rec.append(("dma", (eng, out, in_, list(reads), list(writes), kw)))
            return
        d = self.ndma
        self.ndma += 1
        toks = self._need(reads, writes)
        if d >= NDSEM:
            toks.append(("d", d - NDSEM))
        self._emit_waits(eng, toks)
        k = d % NDSEM
        self.ops[eng].append(lambda e, S, k=k, out=out, in_=in_, kw=kw: e.dma_start(out=out, in_=in_, **kw).then_inc(S["d"][k], 16))
        self.dma_issuer[d] = eng
        self._commit(("d", d), reads, writes)
        return d

    def barrier(self):
        for e in ENG:
            toks = [("c", p, self.cnt[p]) for p in ENG if p != e and self.cnt[p] > 0]
            toks += [("d", d) for d in range(max(0, self.ndma - NDSEM), self.ndma)]
            self._emit_waits(e, toks)

    def finish(self):
        self._emit_waits("sync", [("d", d) for d in range(max(0, self.ndma - NDSEM), self.ndma)])
        nc = self.nc
        with contextlib.ExitStack() as st:
            S = {}
            for e in ENG:
                ns = self.cnt[e] // SEM_ROLL + 1
                S[e] = [st.enter_context(nc.semaphore(f"s_{e}_{i}")) for i in range(ns)]
            S["d"] = [st.enter_context(nc.semaphore(f"s_d_{i}")) for i in range(NDSEM)]
            block = st.enter_context(nc.Block())

            def run(engname):
                def _f(eng):
                    for f in self.ops[engname]:
                        f(eng, S)
                return _f

            block.sync(run("sync"))
            block.tensor(run("tensor"))
            block.vector(run("vector"))
            block.scalar(run("scalar"))
            block.gpsimd(run("gpsimd"))


D = 1024
T = 2304
NT = 18
CTX = 256
SEQ = 2048
EPS = 1e-6
BLKS = [(0, 256), (256, 512), (768, 512), (1280, 512), (1792, 512)]
NIN = 2992
NEXP = 32


def tile_blk(ti):
    return 0 if ti < 2 else 1 + (ti - 2) // 4


def host_consts():
    c = {}
    i = np.arange(128)
    U = (i[:, None] <= i[None, :]).astype(np.float32)
    L = (i[:, None] >= i[None, :]).astype(np.float32)
    SU = (i[:, None] < i[None, :]).astype(np.float32)
    SL = (i[:, None] > i[None, :]).astype(np.float32)
    ident = np.eye(128, dtype=np.float32)
    ones = np.ones((128, 128), np.float32)
    bd64 = np.kron(np.eye(2, dtype=np.float32), np.ones((64, 64), np.float32))

    def rmat(rot, off, size):
        R = np.zeros((size, size), np.float32)
        q = rot // 4
        for ax in range(2):
            b = off + ax * 2 * q
            for k in range(q):
                R[b + k, b + q + k] = -1.0
                R[b + q + k, b + k] = 1.0
        return R
    RA = np.kron(np.eye(2, dtype=np.float32), rmat(64, 0, 64))
    RC = np.zeros((128, 128), np.float32)
    RC[:96, :96] = rmat(32, 64, 96)
    E32 = np.zeros((128, 128), np.float32)
    for k in range(32):
        E32[k, 64 + k] = 1.0
    BIG = np.float32(1.0e4)
    c["cf"] = np.concatenate([ident, U, L, SU, SL, ones, bd64, RA.T.copy(), RC.T.copy(), E32], axis=1).astype(np.float32)
    c["gmask"] = np.concatenate([BIG * (1 - SL), BIG * (1 - SU), -BIG * (1 - U), -BIG * (1 - L)], axis=1).astype(np.float32)

    def tables(rot):
        nf = rot // 4
        t = np.arange(SEQ)
        row = (t // 64).astype(np.float32)
        col = (t % 64).astype(np.float32)
        freq = (np.float32(10000.0) ** (-np.arange(nf, dtype=np.float32) / np.float32(nf))).astype(np.float32)
        ang = np.stack([row[:, None] * freq, col[:, None] * freq], axis=1)
        cs, sn = np.cos(ang).astype(np.float32), np.sin(ang).astype(np.float32)
        C = np.concatenate([cs[:, 0], cs[:, 0], cs[:, 1], cs[:, 1]], axis=1).T
        S = np.concatenate([sn[:, 0], sn[:, 0], sn[:, 1], sn[:, 1]], axis=1).T
        return C, S
    CA, SA = tables(64)
    c["ropeA"] = np.concatenate([np.concatenate([CA, CA], 0), np.concatenate([SA, SA], 0)], axis=1).astype(np.float32)
    CC, SC = tables(32)
    cc = np.ones((128, SEQ), np.float32)
    sc = np.zeros((128, SEQ), np.float32)
    cc[64:96] = CC
    sc[64:96] = SC
    c["ropeC"] = np.concatenate([cc, sc], axis=1).astype(np.float32)
    return c


class K:
    def __init__(self, nseq=2, nlayers=2, phases="CABM", dbg=None, moe=True):
        self.nseq, self.nlayers, self.phases, self.dbg = nseq, nlayers, phases, dbg
        nc = self.nc = bass.Bass("TRN2", target_bir_lowering=False)
        self.P = Prog(nc)
        self.all_in = []

        def dt(n, s):
            a = nc.dram_tensor(n, list(s), F32, kind="ExternalInput").ap()
            self.all_in.append(a)
            return a
        self.x2 = dt("x2", (2, SEQ, D))
        self.ctx2 = dt("ctx2", (2, CTX, D))
        self.vp1 = dt("vp1", (128, 128))
        self.vp2 = dt("vp2", (32, 128))
        self.w_mod = dt("w_mod", (2, D, 6 * D))
        self.w_in = dt("w_in", (2, D, NIN))
        self.w_out = dt("w_out", (2, 1024, D))
        self.swa_q_norm = dt("swa_q_norm", (2, 64))
        self.swa_k_norm = dt("swa_k_norm", (2, 64))
        self.swa_sink = dt("swa_sink", (2, 4))
        self.gdn_conv = dt("gdn_conv", (2, 5, 1536))
        self.gdn_a_log = dt("gdn_a_log", (2, 8))
        self.gdn_dt_bias = dt("gdn_dt_bias", (2, 8))
        self.gdn_out_norm = dt("gdn_out_norm", (2, 128))
        self.mla_q_a_norm = dt("mla_q_a_norm", (2, 256))
        self.mla_w_uq = dt("mla_w_uq", (2, 256, 384))
        self.mla_kv_a_norm = dt("mla_kv_a_norm", (2, 128))
        self.mla_w_ukv = dt("mla_w_ukv", (2, 128, 512))
        self.mla_q_norm = dt("mla_q_norm", (2, 96))
        self.mla_k_norm = dt("mla_k_norm", (2, 96))
        self.router_w = dt("router_w", (2, D, NEXP))
        self.router_b = dt("router_b", (2, NEXP))
        self.exp_w_gu = dt("exp_w_gu", (2, NEXP, D, 2048) if moe else (2, 1, 8, 8))
        self.exp_b_gu = dt("exp_b_gu", (2, NEXP, 2048))
        self.exp_w_dn = dt("exp_w_dn", (2, NEXP, D, D) if moe else (2, 1, 8, 8))
        self.exp_b_dn = dt("exp_b_dn", (2, NEXP, D))
        self.cf = dt("cf", (128, 1280))
        self.gmask_d = dt("gmask", (128, 512))
        self.ropeA = dt("ropeA", (128, 4096))
        self.ropeC = dt("ropeC", (128, 4096))
        self.out = nc.dram_tensor("out", [2, SEQ, D], F32, kind="ExternalOutput").ap()
        self.dbg_out = None
        if dbg:
            self.dbg_out = nc.dram_tensor("dbg", [128, dbg], F32, kind="ExternalOutput").ap()
        self.uid = 0

    def MM(self, out, lhsT, rhs, start=True, stop=True, r=(), w=()):
        self.P.op("tensor", lambda e, o=out, a=lhsT, b=rhs, s=start, t=stop: e.matmul(o, a, b, start=s, stop=t), r, w)

    def TR(self, out, in_, r=(), w=()):
        npar = in_.shape[0]
        idn = self.ident[0:npar, 0:npar]
        self.P.op("tensor", lambda e, o=out, a=in_: e.transpose(o, a, idn), list(r) + ["const"], w)

    def ACT(self, out, in_, func, r=(), w=(), scale=None, bias=None, eng="scalar"):
        kw = {}
        if scale is not None:
            kw["scale"] = scale
        if bias is not None:
            kw["bias"] = bias
        self.P.op("scalar", lambda e, o=out, a=in_, f=func, kw=kw: e.activation(o, a, f, **kw), r, w)

    def TS(self, eng, out, in0, s1, op0, s2=None, op1=None, r=(), w=()):
        if op1 is None:
            self.P.op(eng, lambda e, o=out, a=in0: e.tensor_scalar(out=o, in0=a, scalar1=s1, scalar2=None, op0=op0), r, w)
        else:
            self.P.op(eng, lambda e, o=out, a=in0: e.tensor_scalar(out=o, in0=a, scalar1=s1, scalar2=s2, op0=op0, op1=op1), r, w)

    def TT(self, eng, out, a, b, op, r=(), w=()):
        self.P.op(eng, lambda e, o=out, x=a, y=b: e.tensor_tensor(out=o, in0=x, in1=y, op=op), r, w)

    def STT(self, out, a, s, b, op0, op1, r=(), w=()):
        self.P.op("vector", lambda e, o=out, x=a, y=b: e.scalar_tensor_tensor(out=o, in0=x, scalar=s, in1=y, op0=op0, op1=op1), r, w)

    def CP(self, eng, out, in_, r=(), w=()):
        if eng == "scalar":
            self.P.op(eng, lambda e, o=out, a=in_: e.copy(o, a), r, w)
        else:
            self.P.op(eng, lambda e, o=out, a=in_: e.tensor_copy(o, a), r, w)

    def RECIP(self, out, in_, r=(), w=()):
        self.P.op("vector", lambda e, o=out, a=in_: e.reciprocal(o, a), r, w)

    def MEMSET(self, eng, ap, val, w=()):
        self.P.op(eng, lambda e, a=ap: e.memset(a, val), (), w)

    def DMA(self, out, in_, r=(), w=(), eng="sync"):
        self.P.dma(eng, out, in_, r, w, allow_slow_non_contiguous=True)

    def key(self, base):
        self.uid += 1
        return f"{base}#{self.uid}"

    def dump(self, ap, ncols, r, col0=0, rows=128):
        P = self.P
        m = P.mark()
        tmp = P.alloc(64, F32)
        for c0 in range(0, ncols, 64):
            n = min(64, ncols - c0)
            k = self.key("dump")
            self.CP("vector", tmp[0:rows, 0:n], ap[:, c0:c0 + n], r=list(r) + ["dumptmp"], w=[k, "dumptmp"])
            self.DMA(self.dbg_out[0:rows, col0 + c0:col0 + c0 + n], tmp[0:rows, 0:n], r=[k], w=[self.key("dbgout"), "dumptmp"])
        P.barrier()
        P.release(m)

    def setup(self):
        P = self.P
        cf = P.alloc(1280, F32)
        self.DMA(cf, self.cf, w=["const"])
        self.ident = cf[:, 0:128]
        self.U, self.L = cf[:, 128:256], cf[:, 256:384]
        self.SU, self.SL = cf[:, 384:512], cf[:, 512:640]
        self.ones = cf[:, 640:768]
        cb = P.alloc(640, BF16)
        self.CP("vector", cb, cf[:, 640:1280], r=["const"], w=["constb"])
        self.ones_b, self.bd64_b = cb[:, 0:128], cb[:, 128:256]
        self.RAT_b, self.RCT_b, self.E32_b = cb[:, 256:384], cb[:, 384:512], cb[:, 512:640]
        junk = P.alloc(64, F32)
        for i, a in enumerate(self.all_in):
            fl = a
            while len(fl.shape) > 1:
                fl = fl[0]
            self.DMA(junk[0:1, i:i + 1], fl[0:1].rearrange("(a b) -> a b", a=1), w=["junk"])
        self.eps = P.alloc(1, F32)
        self.MEMSET("vector", self.eps, EPS, w=["const2"])
        vt = P.alloc(128, F32)
        self.DMA(vt, self.vp1, w=["vt"])
        self.VP = P.alloc(128, F32)
        self.TR(self.ps[:, 0, 0:128], vt, r=["vt"], w=["ps0"])
        self.CP("vector", self.VP, self.ps[:, 0, 0:128], r=["ps0"], w=["VP"])
        vt2 = P.alloc(128, F32)
        self.DMA(vt2[0:32, :], self.vp2, w=["vt2"])
        self.CS = P.alloc(32, F32)
        self.TR(self.ps[:, 1, 0:32], vt2[0:32, :], r=["vt2"], w=["ps1"])
        self.ACT(self.CS, self.ps[:, 1, 0:32], AF.Silu, r=["ps1"], w=["CS"])
        self.xT = P.alloc(8 * T, F32).rearrange("p (c t) -> p c t", c=8)
        self.hT = P.alloc(8 * T, BF16).rearrange("p (c t) -> p c t", c=8)
        self.stg = [P.alloc(1024, F32), P.alloc(1024, F32)]
        self.stg_i = 0
        self.MOD = [P.alloc(96, F32).rearrange("p (j y) -> p j y", y=2) for _ in range(2)]
        self.A1 = [P.alloc(16, F32).rearrange("p (c y) -> p c y", y=2) for _ in range(2)]
        self.A2 = [P.alloc(16, F32).rearrange("p (c y) -> p c y", y=2) for _ in range(2)]

    def stage(self):
        i = self.stg_i
        self.stg_i ^= 1
        return self.stg[i], f"stg{i}"

    def load_cast(self, dst, src, ncols, eng="gpsimd", dkey=None, parts=128):
        st, sk = self.stage()
        self.DMA(st[0:parts, 0:ncols], src, w=[sk])
        self.CP(eng, dst, st[0:parts, 0:ncols], r=[sk], w=[dkey])

    def load_w_cols(self, dst, wsrc, c0, ncols, dkey, kc=8):
        per = max(1, 1024 // ncols)
        for k0 in range(0, kc, per):
            k1 = min(kc, k0 + per)
            st, sk = self.stage()
            sv = st[:, 0:(k1 - k0) * ncols].rearrange("p (k n) -> p k n", n=ncols)
            self.DMA(sv, wsrc[k0 * 128:k1 * 128, c0:c0 + ncols].rearrange("(k p) n -> p k n", p=128), w=[sk])
            self.CP("gpsimd", dst[:, k0:k1, :], sv, r=[sk], w=[dkey])

    def load_x(self, s):
        P = self.P
        m = P.mark()
        bufs = [P.alloc(1024, F32), P.alloc(1024, F32)]
        for ti in range(NT):
            b = bufs[ti % 2]
            bk = f"xl{ti % 2}"
            src = self.ctx2[s, ti * 128:(ti + 1) * 128, :] if ti < 2 else self.x2[s, (ti - 2) * 128:(ti - 1) * 128, :]
            self.DMA(b, src, w=[bk])
            for half in range(2):
                bank = 2 * (ti % 2) + half
                for c4 in range(4):
                    c = half * 4 + c4
                    self.TR(self.ps[:, bank, c4 * 128:(c4 + 1) * 128], b[:, c * 128:(c + 1) * 128], r=[bk], w=[f"ps{bank}"])
                eng = "vector" if half == 0 else "scalar"
                self.CP(eng, self.xT[:, half * 4:half * 4 + 4, ti * 128:(ti + 1) * 128],
                        self.ps[:, bank, :].rearrange("p (c t) -> p c t", c=4), r=[f"ps{bank}"], w=[f"xT{tile_blk(ti)}"])
        P.barrier()
        P.release(m)

    def store_x(self, s):
        P = self.P
        m = P.mark()
        bufs = [P.alloc(1024, F32), P.alloc(1024, F32)]
        for ti in range(2, NT):
            b = bufs[ti % 2]
            bk = f"xs{ti % 2}"
            for half in range(2):
                bank = 2 * (ti % 2) + half
                for c4 in range(4):
                    c = half * 4 + c4
                    self.TR(self.ps[:, bank, c4 * 128:(c4 + 1) * 128], self.xT[:, c, ti * 128:(ti + 1) * 128], r=[f"xT{tile_blk(ti)}"], w=[f"ps{bank}"])
                eng = "vector" if half == 0 else "scalar"
                self.CP(eng, b[:, half * 512:(half + 1) * 512], self.ps[:, bank, :], r=[f"ps{bank}"], w=[bk])
            self.DMA(self.out[s, (ti - 2) * 128:(ti - 1) * 128, :], b, r=[bk], w=[self.key("out")])
        P.barrier()
        P.release(m)

    def mods(self, s, l):
        P = self.P
        m = P.mark()
        sc = P.alloc(16, F32).rearrange("p (k y) -> p k y", y=2)
        kk = self.key("sc")
        self.CP("vector", sc[:, :, 0], self.CS[:, s * 8:s * 8 + 8], r=["CS"], w=[kk])
        self.CP("vector", sc[:, :, 1], self.CS[:, 16:24], r=["CS"], w=[kk])
        MOD = self.MOD[l]
        for j in range(48):
            st, sk = self.stage()
            sv = st[:, 0:1024].rearrange("p (k n) -> p k n", n=128)
            self.DMA(sv, self.w_mod[l, :, j * 128:(j + 1) * 128].rearrange("(k p) n -> p k n", p=128), w=[sk])
            for k in range(8):
                self.MM(self.ps[:, 7, 2 * j:2 * j + 2], sv[:, k, :], sc[:, k, :], start=(k == 0), stop=(k == 7), r=[sk, kk], w=["ps7"])
        mk = f"MOD{l}"
        bm = self.VP[:, l * 64:l * 64 + 48]
        pv = self.ps[:, 7, 0:96].rearrange("p (j y) -> p j y", y=2)
        for y in range(2):
            self.TT("vector", MOD[:, :, y], pv[:, :, y], bm, ALU.add, r=["ps7", "VP"], w=[mk])
        n1 = self.VP[:, l * 64 + 48:l * 64 + 56]
        n2 = self.VP[:, l * 64 + 56:l * 64 + 64]
        for y in range(2):
            self.STT(self.A1[l][:, :, y], MOD[:, 8:16, y], 1.0, n1, ALU.add, ALU.mult, r=[mk, "VP"], w=[mk + "a"])
            self.STT(self.A2[l][:, :, y], MOD[:, 32:40, y], 1.0, n2, ALU.add, ALU.mult, r=[mk, "VP"], w=[mk + "a"])
        P.barrier()
        P.release(m)

    def norm_mod(self, l, A, shift_j, router=None):
        P = self.P
        m = P.mark()
        sq = P.alloc(8 * 512, BF16).rearrange("p (c t) -> p c t", c=8)
        sd = P.alloc(512, F32)
        rs = P.alloc(512, F32)
        tmp = [P.alloc(512, F32), P.alloc(512, F32)]
        MOD = self.MOD[l]
        for bi, (t0, n) in enumerate(BLKS):
            if router is not None and self.skip_ctx and bi == 0:
                continue
            y = 1 if bi == 0 else 0
            xk, hk = f"xT{bi}", f"hT{bi}"
            for c in range(8):
                self.ACT(sq[:, c, 0:n], self.xT[:, c, t0:t0 + n], AF.Square, r=[xk], w=[f"sq{c}"])
            for c in range(8):
                self.MM(self.ps[:, 0, 0:n], self.ones_b, sq[:, c, 0:n], start=(c == 0), stop=(c == 7), r=[f"sq{c}", "constb"], w=["ps0"])
            self.ACT(sd[:, 0:n], self.ps[:, 0, 0:n], AF.Sqrt, scale=1.0 / D, bias=self.eps, r=["ps0", "const2"], w=["sd"])
            self.RECIP(rs[:, 0:n], sd[:, 0:n], r=["sd"], w=["rs"])
            for c in range(8):
                tb, tk = tmp[c % 2], f"nt{c % 2}"
                if router is not None:
                    tb, tk = router["h2f"][:, c, :], f"h2f{c}"
                self.STT(tb[:, 0:n], self.xT[:, c, t0:t0 + n], A[:, c, y:y + 1], rs[:, 0:n], ALU.mult, ALU.mult, r=[xk, "rs", f"MOD{l}a"], w=[tk])
                if router is not None:
                    self.TS("vector", tb[:, 0:n], tb[:, 0:n], MOD[:, shift_j + c, y:y + 1], ALU.add, r=[tk, f"MOD{l}"], w=[tk])
                    self.CP("scalar", self.hT[:, c, t0:t0 + n], tb[:, 0:n], r=[tk], w=[hk])
                else:
                    self.ACT(self.hT[:, c, t0:t0 + n], tb[:, 0:n], AF.Identity, bias=MOD[:, shift_j + c, y:y + 1], r=[tk, f"MOD{l}"], w=[hk])
            if router is not None:
                router["fn"](bi, t0, n)
        P.barrier()
        P.release(m)

    def head_norm_rope(self, src, npar, n, dst, onesl, inv_dim, gain, RT, rope_dram, t0, sk, dk, W):
        ps = self.ps
        sq, sd, rs, qn, t1, t2, rb = W["sq"], W["sd"], W["rs"], W["qn"], W["t1"], W["t2"], W["rb"]
        self.ACT(sq[0:npar, 0:n], src, AF.Square, r=[sk], w=["w_sq"])
        self.MM(ps[0:npar, 5, 0:n], onesl[0:npar, 0:npar], sq[0:npar, 0:n], r=["w_sq", "constb"], w=["ps5"])
        self.ACT(sd[0:npar, 0:n], ps[0:npar, 5, 0:n], AF.Sqrt, scale=inv_dim, bias=self.eps[0:npar, :], r=["ps5", "const2"], w=["w_sd"])
        self.RECIP(rs[0:npar, 0:n], sd[0:npar, 0:n], r=["w_sd"], w=["w_rs"])
        if t0 < CTX:
            self.STT(dst, src, gain, rs[0:npar, 0:n], ALU.mult, ALU.mult, r=[sk, "w_rs", "gains"], w=[dk])
            return
        self.STT(qn[0:npar, 0:n], src, gain, rs[0:npar, 0:n], ALU.mult, ALU.mult, r=[sk, "w_rs", "gains"], w=["w_qn"])
        self.MM(ps[0:npar, 6, 0:n], RT[0:npar, 0:npar], qn[0:npar, 0:n], r=["w_qn", "constb"], w=["ps6"])
        self.DMA(rb[:, 0, 0:n], rope_dram[:, t0 - CTX:t0 - CTX + n], w=["w_rb"])
        self.DMA(rb[:, 1, 0:n], rope_dram[:, SEQ + t0 - CTX:SEQ + t0 - CTX + n], w=["w_rb"])
        self.TT("gpsimd", t1[0:npar, 0:n], qn[0:npar, 0:n], rb[0:npar, 0, 0:n], ALU.mult, r=["w_qn", "w_rb"], w=["w_t1"])
        self.TT("vector", t2[0:npar, 0:n], ps[0:npar, 6, 0:n], rb[0:npar, 1, 0:n], ALU.mult, r=["ps6", "w_rb"], w=["w_t2"])
        self.TT("vector", dst, t1[0:npar, 0:n], t2[0:npar, 0:n], ALU.add, r=["w_t1", "w_t2"], w=[dk])

    def norm_temps(self):
        P = self.P
        return dict(sq=P.alloc(512, BF16), sd=P.alloc(512, F32), rs=P.alloc(512, F32), qn=P.alloc(512, BF16),
                    t1=P.alloc(512, BF16), t2=P.alloc(512, BF16), rb=P.alloc(1024, F32).rearrange("p (a n) -> p a n", a=2))

    def out_proj(self, l, Wo, kparts, mix, mixk, wk, nacc=1):
        ps = self.ps
        MOD = self.MOD[l]
        cnt = 0
        for bi, (t0, n) in enumerate(BLKS):
            if self.skip_ctx and bi == 0:
                continue
            y = 1 if bi == 0 else 0
            for dc in range(8):
                bank = cnt % 4
                cnt += 1
                for a in range(nacc):
                    self.MM(ps[:, bank, 0:n], Wo[0:kparts, a, dc * 128:(dc + 1) * 128], mix[0:kparts, a, t0:t0 + n],
                            start=(a == 0), stop=(a == nacc - 1), r=[wk, mixk], w=[f"ps{bank}"])
                self.STT(self.xT[:, dc, t0:t0 + n], ps[:, bank, 0:n], MOD[:, 16 + dc, y:y + 1], self.xT[:, dc, t0:t0 + n],
                         ALU.mult, ALU.add, r=[f"ps{bank}", f"MOD{l}", f"xT{bi}"], w=[f"xT{bi}"])

    def mixer_C(self, l):
        P, ps = self.P, self.ps
        m = P.mark()
        WC = P.alloc(8 * 416, BF16).rearrange("p (k n) -> p k n", k=8)
        self.load_w_cols(WC, self.w_in[l], 2576, 416, "WC")
        Wuq = P.alloc(2 * 384, BF16).rearrange("p (k n) -> p k n", k=2)
        self.load_w_cols(Wuq, self.mla_w_uq[l], 0, 384, "Wuq", kc=2)
        Wk = P.alloc(4 * 96, BF16).rearrange("p (h n) -> p h n", h=4)
        Wv = P.alloc(256, BF16).rearrange("p (h n) -> p h n", h=4)
        self.MEMSET("vector", Wk, 0.0, w=["Wk"])
        st, sk = self.stage()
        self.DMA(st[:, 0:512], self.mla_w_ukv[l], w=[sk])
        sv = st[:, 0:512].rearrange("p (h n) -> p h n", h=4)
        self.CP("gpsimd", Wk[:, :, 0:64], sv[:, :, 0:64], r=[sk], w=["Wk"])
        self.CP("gpsimd", Wv, sv[:, :, 64:128], r=[sk], w=["Wv"])
        g = P.alloc(8, F32)
        self.DMA(g[:, 0:2], self.mla_q_a_norm[l].rearrange("(c p) -> p c", p=128), w=["gains"])
        self.DMA(g[:, 2:3], self.mla_kv_a_norm[l].rearrange("(p o) -> p o", o=1), w=["gains"])
        self.DMA(g[0:96, 5:6], self.mla_q_norm[l].rearrange("(p o) -> p o", o=1), w=["gains"])
        self.DMA(g[0:96, 4:5], self.mla_k_norm[l].rearrange("(p o) -> p o", o=1), w=["gains"])
        self.TS("vector", g[0:96, 3:4], g[0:96, 5:6], 96.0 ** -0.5, ALU.mult, r=["gains"], w=["gains"])
        cqn = P.alloc(2 * T, BF16).rearrange("p (c t) -> p c t", c=2)
        ckvn = P.alloc(T, BF16)
        krT = P.alloc(T, BF16)
        W = self.norm_temps()
        for bi, (t0, n) in enumerate(BLKS):
            hk = f"hT{bi}"
            for o, (c0, npar) in enumerate([(0, 128), (128, 128), (256, 128), (384, 32)]):
                for k in range(8):
                    self.MM(ps[0:npar, o, 0:n], WC[:, k, c0:c0 + npar], self.hT[:, k, t0:t0 + n], start=(k == 0), stop=(k == 7), r=["WC", hk], w=[f"ps{o}"])
            for c in range(2):
                self.ACT(W["sq"][:, 0:n], ps[:, c, 0:n], AF.Square, r=[f"ps{c}"], w=["w_sq"])
                self.MM(ps[:, 5, 0:n], self.ones_b, W["sq"][:, 0:n], start=(c == 0), stop=(c == 1), r=["w_sq", "constb"], w=["ps5"])
            self.ACT(W["sd"][:, 0:n], ps[:, 5, 0:n], AF.Sqrt, scale=1.0 / 256, bias=self.eps, r=["ps5", "const2"], w=["w_sd"])
            self.RECIP(W["rs"][:, 0:n], W["sd"][:, 0:n], r=["w_sd"], w=["w_rs"])
            for c in range(2):
                self.STT(cqn[:, c, t0:t0 + n], ps[:, c, 0:n], g[:, c:c + 1], W["rs"][:, 0:n], ALU.mult, ALU.mult, r=[f"ps{c}", "w_rs", "gains"], w=[f"cqn{bi}"])
            self.ACT(W["sq"][:, 0:n], ps[:, 2, 0:n], AF.Square, r=["ps2"], w=["w_sq"])
            self.MM(ps[:, 5, 0:n], self.ones_b, W["sq"][:, 0:n], r=["w_sq", "constb"], w=["ps5"])
            self.ACT(W["sd"][:, 0:n], ps[:, 5, 0:n], AF.Sqrt, scale=1.0 / 128, bias=self.eps, r=["ps5", "const2"], w=["w_sd"])
            self.RECIP(W["rs"][:, 0:n], W["sd"][:, 0:n], r=["w_sd"], w=["w_rs"])
            self.STT(ckvn[:, t0:t0 + n], ps[:, 2, 0:n], g[:, 2:3], W["rs"][:, 0:n], ALU.mult, ALU.mult, r=["ps2", "w_rs", "gains"], w=[f"ckvn{bi}"])
            self.CP("scalar", krT[0:32, t0:t0 + n], ps[0:32, 3, 0:n], r=["ps3"], w=[f"krT{bi}"])
        allc = [f"cqn{b}" for b in range(5)]
        allkv = [f"ckvn{b}" for b in range(5)]
        qr = P.alloc(T, BF16)
        kr = P.alloc(T, BF16)
        Vh = P.alloc(NT * 64, BF16).rearrange("p (t d) -> p t d", d=64)
        mixh = P.alloc(T, BF16)
        WoC = P.alloc(1024, BF16)
        Eb = [P.alloc(512, BF16) for _ in range(3)]
        rd = P.alloc(512, F32)
        for h in range(4):
            self.load_cast(WoC[0:64, :], self.w_out[l, 768 + h * 64:768 + (h + 1) * 64, :], 1024, dkey="WoC", parts=64)
            for bi, (t0, n) in enumerate(BLKS):
                for k in range(2):
                    self.MM(ps[0:96, 0, 0:n], Wuq[:, k, h * 96:(h + 1) * 96], cqn[:, k, t0:t0 + n], start=(k == 0), stop=(k == 1), r=["Wuq", f"cqn{bi}"], w=["ps0"])
                self.head_norm_rope(ps[0:96, 0, 0:n], 96, n, qr[0:96, t0:t0 + n], self.ones_b, 1.0 / 96, g[0:96, 3:4], self.RCT_b, self.ropeC, t0, "ps0", f"qr{bi}", W)
                self.MM(ps[0:96, 1, 0:n], Wk[:, h, :], ckvn[:, t0:t0 + n], start=True, stop=False, r=["Wk", f"ckvn{bi}"], w=["ps1"])
                self.MM(ps[0:96, 1, 0:n], self.E32_b[0:32, 0:96], krT[0:32, t0:t0 + n], start=False, stop=True, r=["constb", f"krT{bi}"], w=["ps1"])
                self.head_norm_rope(ps[0:96, 1, 0:n], 96, n, kr[0:96, t0:t0 + n], self.ones_b, 1.0 / 96, g[0:96, 4:5], self.RCT_b, self.ropeC, t0, "ps1", f"kr{bi}", W)
            for ti in range(NT):
                bank = 2 + ti % 2
                self.MM(ps[:, bank, 0:64], ckvn[:, ti * 128:(ti + 1) * 128], Wv[:, h, :], r=allkv + ["Wv"], w=[f"ps{bank}"])
                self.CP("scalar", Vh[:, ti, :], ps[:, bank, 0:64], r=[f"ps{bank}"], w=["Vh"])
            allq = [f"qr{b}" for b in range(5)]
            allk = [f"kr{b}" for b in range(5)]
            for qi, (t0, n) in enumerate(BLKS):
                if self.skip_ctx and qi == 0:
                    continue
                kts = [0, 1] if qi == 0 else list(range(NT))
                ob, db = (4, 5) if qi % 2 == 0 else (6, 7)
                for idx, kt in enumerate(kts):
                    sb = idx % 4
                    E, ek = Eb[idx % 3], f"E{idx % 3}"
                    self.MM(ps[:, sb, 0:n], kr[0:96, kt * 128:(kt + 1) * 128], qr[0:96, t0:t0 + n], r=allk + [f"qr{qi}"], w=[f"ps{sb}"])
                    self.ACT(E[:, 0:n], ps[:, sb, 0:n], AF.Exp, r=[f"ps{sb}"], w=[ek])
                    self.MM(ps[0:64, ob, 0:n], Vh[:, kt, :], E[:, 0:n], start=(idx == 0), stop=(idx == len(kts) - 1), r=["Vh", ek], w=[f"ps{ob}"])
                    self.MM(ps[0:64, db, 0:n], self.ones_b[:, 0:64], E[:, 0:n], start=(idx == 0), stop=(idx == len(kts) - 1), r=["constb", ek], w=[f"ps{db}"])
                self.RECIP(rd[0:64, 0:n], ps[0:64, db, 0:n], r=[f"ps{db}"], w=["rd"])
                self.TT("vector", mixh[0:64, t0:t0 + n], ps[0:64, ob, 0:n], rd[0:64, 0:n], ALU.mult, r=[f"ps{ob}", "rd"], w=[f"mixh{qi}"])
            if self.dbg and self.dbg_what == f"C{h}":
                self.dump(mixh[0:64, :], T, [f"mixh{b}" for b in range(5)], rows=64)
            self.out_proj_heads(l, WoC, mixh)
        P.barrier()
        P.release(m)

    def out_proj_heads(self, l, Wo, mixh):
        ps = self.ps
        MOD = self.MOD[l]
        cnt = 0
        for bi, (t0, n) in enumerate(BLKS):
            if self.skip_ctx and bi == 0:
                continue
            y = 1 if bi == 0 else 0
            for dc in range(8):
                bank = cnt % 4
                cnt += 1
                self.MM(ps[:, bank, 0:n], Wo[0:64, dc * 128:(dc + 1) * 128], mixh[0:64, t0:t0 + n], r=["WoC", f"mixh{bi}"], w=[f"ps{bank}"])
                self.STT(self.xT[:, dc, t0:t0 + n], ps[:, bank, 0:n], MOD[:, 16 + dc, y:y + 1], self.xT[:, dc, t0:t0 + n],
                         ALU.mult, ALU.add, r=[f"ps{bank}", f"MOD{l}", f"xT{bi}"], w=[f"xT{bi}"])

    def build(self, dbg_what=None):
        nc, P = self.nc, self.P
        self.dbg_what = dbg_what
        NB = 48800
        with nc.sbuf_tensor("arena", [128, NB], F32) as arena, nc.psum_tensor("ps", [128, 8, 512], F32) as ps:
            self.ps = ps
            P.set_arena(arena, NB * 4)
            self.setup()
            if dbg_what == "setup":
                self.dump(self.VP, 128, ["VP"])
                P.finish()
                return nc
            for s in range(self.nseq):
                self.load_x(s)
                if dbg_what == "loadx":
                    self.dump(self.xT[:, 0, :], T, [f"xT{b}" for b in range(5)])
                    P.finish()
                    return nc
                for l in range(self.nlayers):
                    self.mods(s, l)
                if dbg_what == "mods":
                    self.dump(self.MOD[0].rearrange("p j y -> p (j y)"), 96, ["MOD0"])
                    P.finish()
                    return nc
                for l in range(self.nlayers):
                    self.layer(s, l)
                    if self.stop:
                        break
                if self.stop:
                    break
                self.store_x(s)
            P.finish()
        return nc

    stop = False

    def layer(self, s, l):
        ph = self.phases
        self.skip_ctx = (l == self.nlayers - 1 and self.dbg_what is None)
        allx = [f"xT{b}" for b in range(5)]
        allh = [f"hT{b}" for b in range(5)]
        self.norm_mod(l, self.A1[l], 0)
        if self.dbg_what == "h":
            self.dump(self.hT[:, 0, :], T, allh)
            self.stop = True
            return
        if "C" in ph:
            self.mixer_C(l)
        if "A" in ph:
            self.mixer_A(l)
        if "B" in ph:
            self.mixer_B(l)
        if self.dbg_what == "xa":
            self.dump(self.xT[:, 0, :], T, allx)
            self.stop = True
            return
        if "M" in ph:
            self.moe(l)
        if self.dbg_what == "xb":
            self.dump(self.xT[:, 0, :], T, allx)
            self.stop = True
            return


def make_inputs(inputs, core, consts):
    b0 = 2 * core
    f = lambda k: np.ascontiguousarray(inputs[k], dtype=np.float32)
    vp1 = np.concatenate([np.concatenate([f("b_mod")[l].reshape(48, 128), f("norm1")[l].reshape(8, 128), f("norm2")[l].reshape(8, 128)], 0) for l in range(2)], 0)
    vp2 = np.zeros((32, 128), np.float32)
    vp2[0:8] = f("c")[b0].reshape(8, 128)
    vp2[8:16] = f("c")[b0 + 1].reshape(8, 128)
    vp2[16:24] = f("c_ctx").reshape(8, 128)
    m = {"x2": f("x")[b0:b0 + 2], "ctx2": f("ctx")[b0:b0 + 2], "vp1": vp1, "vp2": vp2}
    for k in ["w_mod", "w_in", "w_out", "swa_q_norm", "swa_k_norm", "swa_sink", "gdn_conv", "gdn_out_norm", "mla_q_a_norm", "mla_w_uq",
              "mla_kv_a_norm", "mla_w_ukv", "mla_q_norm", "mla_k_norm", "router_w", "router_b", "exp_w_gu", "exp_b_gu", "exp_w_dn", "exp_b_dn"]:
        m[k] = f(k)
    m["gdn_a_log"] = f("gdn_a_log").reshape(2, 8)
    m["gdn_dt_bias"] = f("gdn_dt_bias").reshape(2, 8)
    m.update(consts)
    return m


def kernel(**inputs):
    k = K(nseq=2, nlayers=2, phases="CABM")
    nc = k.build()
    consts = host_consts()
    in_maps = [make_inputs(inputs, c, consts) for c in range(8)]
    res = run_bass_kernel_spmd(nc, in_maps, core_ids=list(range(8)))
    return np.concatenate([r["out"] for r in res.results], axis=0)


def _mixer_A(self, l):
    P, ps = self.P, self.ps
    m = P.mark()
    WA = P.alloc(8 * 512, BF16).rearrange("p (k n) -> p k n", k=8)
    for d0, c0, nc_ in [(0, 0, 64), (64, 128, 64), (128, 64, 64), (192, 192, 64), (256, 256, 256)]:
        self.load_w_cols(WA[:, :, d0:d0 + nc_], self.w_in[l], c0, nc_, "WA")
    g = P.alloc(8, F32)
    for hh in range(2):
        self.DMA(g[hh * 64:(hh + 1) * 64, 2:3], self.swa_q_norm[l].rearrange("(p o) -> p o", o=1), w=["gains"])
        self.DMA(g[hh * 64:(hh + 1) * 64, 1:2], self.swa_k_norm[l].rearrange("(p o) -> p o", o=1), w=["gains"])
    self.TS("vector", g[:, 0:1], g[:, 2:3], 0.125, ALU.mult, r=["gains"], w=["gains"])
    sk = P.alloc(4, F32)
    self.DMA(sk, self.swa_sink[l].rearrange("(a h) -> a h", a=1).broadcast_to([128, 4]), w=["sk0"])
    self.ACT(sk, sk, AF.Exp, r=["sk0"], w=["sk"])
    qr = P.alloc(2 * T, BF16).rearrange("p (c t) -> p c t", c=2)
    kr = P.alloc(T, BF16)
    V = P.alloc(NT * 128, BF16).rearrange("p (t d) -> p t d", d=128)
    W = self.norm_temps()
    for bi, (t0, n) in enumerate(BLKS):
        for o, c0 in enumerate([0, 128, 256]):
            for k in range(8):
                self.MM(ps[:, o, 0:n], WA[:, k, c0:c0 + 128], self.hT[:, k, t0:t0 + n], start=(k == 0), stop=(k == 7), r=["WA", f"hT{bi}"], w=[f"ps{o}"])
            dst = kr[:, t0:t0 + n] if o == 2 else qr[:, o, t0:t0 + n]
            self.head_norm_rope(ps[:, o, 0:n], 128, n, dst, self.bd64_b, 1.0 / 64, g[:, (1 if o == 2 else 0):(2 if o == 2 else 1)], self.RAT_b, self.ropeA, t0, f"ps{o}", "qkA", W)
    for ti in range(NT):
        bank = 3 + ti % 2
        for k in range(8):
            self.MM(ps[:, bank, 0:128], self.hT[:, k, ti * 128:(ti + 1) * 128], WA[:, k, 384:512], start=(k == 0), stop=(k == 7), r=["WA", f"hT{tile_blk(ti)}"], w=[f"ps{bank}"])
        self.CP("scalar", V[:, ti, :], ps[:, bank, 0:128], r=[f"ps{bank}"], w=["VA"])
    mixg = P.alloc(2 * T, BF16).rearrange("p (c t) -> p c t", c=2)
    WoA = P.alloc(2 * 1024, BF16).rearrange("p (c n) -> p c n", c=2)
    Eb = [P.alloc(5 * 256, BF16).rearrange("p (k n) -> p k n", k=5) for _ in range(2)]
    dn = P.alloc(256, F32)
    for gi in range(2):
        gp = slice(gi * 64, (gi + 1) * 64)
        for j in range(2):
            hd = 2 * gi + j
            self.load_cast(WoA[0:64, j, :], self.w_out[l, hd * 64:(hd + 1) * 64, :], 1024, dkey="WoA", parts=64)
        for ti in range(NT):
            if self.skip_ctx and ti < 2:
                continue
            if ti < 2:
                kts = [(0, None), (1, None)]
            else:
                kts = ([(ti - 1, self.L)] if ti - 1 >= 2 else []) + [(ti, None)] + ([(ti + 1, self.U)] if ti + 1 < NT else []) + [(0, None), (1, None)]
            b0 = 0 if ti % 2 == 0 else 3
            E, ek = Eb[ti % 2], f"EA{ti % 2}"
            qrhs = qr[gp, :, ti * 128:(ti + 1) * 128]
            nk = len(kts)
            for idx, (kt, mk) in enumerate(kts):
                bank = b0 + idx // 2
                c0 = (idx % 2) * 256
                self.MM(ps[:, bank, c0:c0 + 256].rearrange("p (j t) -> p j t", j=2), kr[gp, kt * 128:(kt + 1) * 128], qrhs, r=["qkA"], w=[f"ps{bank}"])
            psflat = ps[:, b0:b0 + 3, :].rearrange("p b n -> p (b n)")
            self.ACT(E[:, 0:nk, :].rearrange("p k n -> p (k n)"), psflat[:, 0:nk * 256], AF.Exp, r=[f"ps{b0}", f"ps{b0 + 1}", f"ps{b0 + 2}"], w=[ek])
            for idx, (kt, mk) in enumerate(kts):
                if mk is not None:
                    ev = E[:, idx, :].rearrange("p (j t) -> p j t", j=2)
                    self.TT("gpsimd", ev, ev, mk.rearrange("p (a t) -> p a t", a=1).broadcast_to([128, 2, 128]), ALU.mult, r=[ek, "const"], w=[ek])
            for idx, (kt, mk) in enumerate(kts):
                self.MM(ps[0:64, 6, 0:256], V[:, kt, gi * 64:(gi + 1) * 64], E[:, idx, :], start=(idx == 0), stop=(idx == nk - 1), r=["VA", ek], w=["ps6"])
            for idx, (kt, mk) in enumerate(kts):
                self.MM(ps[0:64, 7, 0:256], self.ones_b[:, 0:64], E[:, idx, :], start=(idx == 0), stop=(idx == nk - 1), r=["constb", ek], w=["ps7"])
            for j in range(2):
                self.TS("vector", dn[0:64, j * 128:(j + 1) * 128], ps[0:64, 7, j * 128:(j + 1) * 128], sk[0:64, 2 * gi + j:2 * gi + j + 1], ALU.add, r=["ps7", "sk"], w=["dnA"])
            self.RECIP(dn[0:64, :], dn[0:64, :], r=["dnA"], w=["dnA"])
            self.TT("vector", mixg[0:64, :, ti * 128:(ti + 1) * 128], ps[0:64, 6, 0:256].rearrange("p (j t) -> p j t", j=2),
                    dn[0:64, :].rearrange("p (j t) -> p j t", j=2), ALU.mult, r=["ps6", "dnA"], w=["mixg"])
        if self.dbg and self.dbg_what == f"A{gi}":
            self.dump(mixg[0:64, 0, :], T, ["mixg"], rows=64)
        self.out_proj(l, WoA, 64, mixg, "mixg", "WoA", nacc=2)
    P.barrier()
    P.release(m)


K.mixer_A = _mixer_A


def _mixer_B(self, l):
    P, ps = self.P, self.ps
    m = P.mark()
    psq = ps.rearrange("p b (q n) -> p (b q) n", q=4)
    allh = [f"hT{b}" for b in range(5)]
    gm = P.alloc(512, BF16)
    self.load_cast(gm, self.gmask_d, 512, dkey="gmask")
    self.NSL, self.NSU, self.PMU, self.PML = gm[:, 0:128], gm[:, 128:256], gm[:, 256:384], gm[:, 384:512]
    Wab = P.alloc(8 * 16, BF16).rearrange("p (k n) -> p k n", k=8)
    self.load_w_cols(Wab, self.w_in[l], 2560, 16, "Wab")
    for ti in range(NT):
        for k in range(8):
            self.MM(ps[:, 0, ti * 16:(ti + 1) * 16], self.hT[:, k, ti * 128:(ti + 1) * 128], Wab[:, k, :], start=(k == 0), stop=(k == 7), r=["Wab"] + allh, w=["ps0"])
    abv = ps[:, 0, 0:NT * 16].rearrange("p (t j) -> p t j", j=16)
    one = P.alloc(1, F32)
    self.MEMSET("vector", one, 1.0, w=["one"])
    arr = lambda: P.alloc(NT * 8, F32).rearrange("p (t j) -> p t j", j=8)
    dtb, alog, X, AX, G, BETA, GC, GCL, BEG, KD = [arr() for _ in range(10)]
    self.DMA(dtb, self.gdn_dt_bias[l].rearrange("(a b j) -> a b j", a=1, b=1).broadcast_to([128, NT, 8]), w=["dtb"])
    self.DMA(alog, self.gdn_a_log[l].rearrange("(a b j) -> a b j", a=1, b=1).broadcast_to([128, NT, 8]), w=["alog"])
    self.TT("vector", X, abv[:, :, 0:8], dtb, ALU.add, r=["ps0", "dtb"], w=["gX"])
    self.TS("vector", AX, X, -1.0, ALU.mult, r=["gX"], w=["gAX"])
    self.TT("vector", AX, AX, X, ALU.max, r=["gX", "gAX"], w=["gAX"])
    self.ACT(AX, AX, AF.Exp, scale=-1.0, r=["gAX"], w=["gAX"])
    self.ACT(AX, AX, AF.Ln, bias=one, r=["gAX", "one"], w=["gAX"])
    self.STT(X, X, 0.0, AX, ALU.max, ALU.add, r=["gX", "gAX"], w=["gX"])
    self.ACT(alog, alog, AF.Exp, r=["alog"], w=["alog"])
    self.TS("vector", alog, alog, -1.0, ALU.mult, r=["alog"], w=["alog"])
    self.TT("vector", G, X, alog, ALU.mult, r=["gX", "alog"], w=["gG"])
    self.ACT(BETA, abv[:, :, 8:16], AF.Sigmoid, r=["ps0"], w=["gB"])
    if self.dbg_what == "BG3a":
        P.barrier()
        self.dump(G.rearrange("p t j -> p (t j)"), NT * 8, ["gG"], col0=256)
        self.dump(BETA.rearrange("p t j -> p (t j)"), NT * 8, ["gB"], col0=768)
        P.release(m)
        return
    for ti in range(NT):
        self.MM(ps[:, 1, ti * 8:ti * 8 + 4], self.U, G[:, ti, 0:4], r=["gG", "const"], w=["ps1"])
        self.MM(ps[:, 1, ti * 8 + 4:ti * 8 + 8], self.L, G[:, ti, 4:8], r=["gG", "const"], w=["ps1"])
        self.MM(ps[:, 2, ti * 8:ti * 8 + 8], self.ones, G[:, ti, :], r=["gG", "const"], w=["ps2"])
    self.CP("vector", GC, ps[:, 1, 0:NT * 8].rearrange("p (t j) -> p t j", j=8), r=["ps1"], w=["gGC"])
    self.CP("vector", GCL, ps[:, 2, 0:NT * 8].rearrange("p (t j) -> p t j", j=8), r=["ps2"], w=["gGCL"])
    if self.dbg_what == "BG3b":
        P.barrier()
        self.dump(GC.rearrange("p t j -> p (t j)"), NT * 8, ["gGC"], col0=256)
        self.dump(GCL.rearrange("p t j -> p (t j)"), NT * 8, ["gGCL"], col0=768)
        P.release(m)
        return
    self.ACT(BEG, GC, AF.Exp, r=["gGC"], w=["gBEG"])
    self.TT("vector", BEG, BEG, BETA, ALU.mult, r=["gBEG", "gB"], w=["gBEG"])
    self.TT("vector", KD, GCL, GC, ALU.subtract, r=["gGC", "gGCL"], w=["gKD"])
    self.ACT(KD, KD, AF.Exp, r=["gKD"], w=["gKD"])
    self.ACT(GCL, GCL, AF.Exp, r=["gGCL", "gKD"], w=["gGCL"])
    P.barrier()
    if self.dbg_what == "BG3":
        self.dump(KD.rearrange("p t j -> p (t j)"), NT * 8, ["gKD"])
        self.dump(G.rearrange("p t j -> p (t j)"), NT * 8, ["gG"], col0=256)
        self.dump(GC.rearrange("p t j -> p (t j)"), NT * 8, ["gGC"], col0=512)
        self.dump(BETA.rearrange("p t j -> p (t j)"), NT * 8, ["gB"], col0=768)
        P.release(m)
        return
    gains = P.alloc(2, F32)
    self.DMA(gains[:, 0:1], self.gdn_out_norm[l].rearrange("(p o) -> p o", o=1), w=["gains"])
    WG = P.alloc(8 * 128, BF16).rearrange("p (k n) -> p k n", k=8)
    raw = P.alloc(T, BF16)
    kT = P.alloc(T, F32)
    qT = P.alloc(T, BF16)
    vT = P.alloc(T, BF16)
    Oacc = P.alloc(T, BF16)
    cw = P.alloc(16, F32)
    y = P.alloc(512, F32)
    ys = P.alloc(512, F32)
    sqb = P.alloc(512, BF16)
    Wo = P.alloc(1024, BF16)
    names = ["qf", "vf", "Gtri", "t1", "e2m", "QKDT", "N", "M", "eg", "qdec", "kbg", "kdec", "bv", "R0", "R1", "N2", "M2", "U_", "WT", "VN", "S0", "S1"]
    Bs = [{nm: P.alloc(128, F32) for nm in names} for _ in range(2)]
    allraw = [f"raw{b}" for b in range(5)]
    for h in range(4):
        for Xi, (dstT, colbase) in enumerate([(qT, 512), (kT, 1024), (vT, 1536)]):
            self.load_w_cols(WG, self.w_in[l], colbase + h * 128, 128, "WG")
            self.DMA(cw[:, Xi * 5:(Xi + 1) * 5], self.gdn_conv[l, :, Xi * 512 + h * 128:Xi * 512 + (h + 1) * 128].rearrange("w p -> p w"), w=["cw"])
            for bi, (t0, n) in enumerate(BLKS):
                bank = bi % 2
                for k in range(8):
                    self.MM(ps[:, bank, 0:n], WG[:, k, :], self.hT[:, k, t0:t0 + n], start=(k == 0), stop=(k == 7), r=["WG", f"hT{bi}"], w=[f"pq{bank * 4}"])
                self.CP("scalar", raw[:, t0:t0 + n], ps[:, bank, 0:n], r=[f"pq{bank * 4}"], w=[f"raw{bi}"])
            for bi, (t0, n) in enumerate(BLKS):
                s0, s1 = (0, CTX) if bi == 0 else (CTX, T)
                self.TS("vector", y[:, 0:n], raw[:, t0:t0 + n], cw[:, Xi * 5 + 2:Xi * 5 + 3], ALU.mult, r=allraw + ["cw"], w=["cy"])
                for j in (0, 1, 3, 4):
                    a, b = max(t0, s0 - (j - 2)), min(t0 + n, s1 - (j - 2))
                    self.STT(y[:, a - t0:b - t0], raw[:, a + j - 2:b + j - 2], cw[:, Xi * 5 + j:Xi * 5 + j + 1], y[:, a - t0:b - t0], ALU.mult, ALU.add, r=allraw + ["cw", "cy"], w=["cy"])
                if Xi == 2:
                    self.ACT(vT[:, t0:t0 + n], y[:, 0:n], AF.Silu, r=["cy"], w=["gv"])
                    continue
                self.ACT(ys[:, 0:n], y[:, 0:n], AF.Silu, r=["cy"], w=["cys"])
                self.ACT(sqb[:, 0:n], ys[:, 0:n], AF.Square, r=["cys"], w=["csq"])
                self.MM(ps[:, 2, 0:n], self.ones_b, sqb[:, 0:n], r=["csq", "constb"], w=["pq8"])
                self.ACT(y[:, 0:n], ps[:, 2, 0:n], AF.Sqrt, bias=self.eps, r=["pq8", "const2", "cy"], w=["cy"])
                self.RECIP(y[:, 0:n], y[:, 0:n], r=["cy"], w=["cy"])
                self.STT(dstT[:, t0:t0 + n], ys[:, 0:n], (128.0 ** -0.5 if Xi == 0 else 1.0), y[:, 0:n], ALU.mult, ALU.mult, r=["cys", "cy"], w=["gq" if Xi == 0 else "gk"])
        P.barrier()
        if self.dbg_what == "Bproj":
            self.dump(kT, T, ["gk"])
            P.release(m)
            return
        order = [list(range(NT)), [1, 0] + list(range(NT - 1, 1, -1))]
        Scur = [Bs[0]["S0"], Bs[1]["S0"]]
        Snxt = [Bs[0]["S1"], Bs[1]["S1"]]
        for d in range(2):
            self.MEMSET("vector", Scur[d], 0.0, w=[f"S{d}"])
        touched = set()
        for step in range(NT):
            bufs = []
            for d in range(2):
                P.rec = []
                bufs.append(P.rec)
                ti = order[d][step]
                B = Bs[d]
                j = d * 4 + h
                q0 = d * 16
                cs = slice(ti * 128, (ti + 1) * 128)
                kt = kT[:, cs]
                Tri = self.U if d == 0 else self.L
                MS = self.SL if d == 0 else self.SU
                MIT = self.U if d == 0 else self.L
                gcol, gccol, bcol = G[:, ti, j:j + 1], GC[:, ti, j:j + 1], BETA[:, ti, j:j + 1]
                K_ = lambda nm: f"b{d}{nm}"
                SLT = lambda i: psq[:, q0 + i, :]
                SK = lambda i: f"pq{q0 + i}"
                self.CP("gpsimd", B["qf"], qT[:, cs], r=["gq"], w=[K_("qf")])
                self.CP("gpsimd", B["vf"], vT[:, cs], r=["gv"], w=[K_("vf")])
                self.MM(SLT(0), kt, kt, r=["gk"], w=[SK(0)])
                self.MM(SLT(1), kt, B["qf"], r=["gk", K_("qf")], w=[SK(1)])
                self.TS("vector", B["Gtri"], Tri, gcol, ALU.mult, r=["const", "gG"], w=[K_("Gtri")])
                self.MM(SLT(13), self.ones, B["Gtri"], r=["const", K_("Gtri")], w=[SK(13)])
                self.STT(B["t1"], SLT(13), gccol, (self.NSL if d == 0 else self.NSU), ALU.subtract, ALU.max, r=[SK(13), "gGC", "gmask"], w=[K_("t1")])
                self.ACT(B["t1"], B["t1"], AF.Exp, scale=-1.0, r=[K_("t1")], w=[K_("t1")])
                self.STT(B["e2m"], SLT(13), gccol, (self.PMU if d == 0 else self.PML), ALU.subtract, ALU.min, r=[SK(13), "gGC", "gmask"], w=[K_("e2m")])
                self.ACT(B["e2m"], B["e2m"], AF.Exp, r=[K_("e2m")], w=[K_("e2m")])
                self.TT("vector", B["QKDT"], SLT(1), B["e2m"], ALU.mult, r=[SK(1), K_("e2m")], w=[K_("QKDT")])
                self.STT(B["N"], SLT(0), bcol, B["t1"], ALU.mult, ALU.mult, r=[SK(0), "gB", K_("t1")], w=[K_("N")])
                self.TR(SLT(3), B["N"], r=[K_("N")], w=[SK(3)])
                self.CP("scalar", B["M"], SLT(3), r=[SK(3)], w=[K_("M")])
                self.ACT(B["eg"], SLT(13), AF.Exp, r=[SK(13)], w=[K_("eg")])
                self.TT("vector", B["qdec"], B["qf"], B["eg"], ALU.mult, r=[K_("qf"), K_("eg")], w=[K_("qdec")])
                self.TR(SLT(4), kt, r=["gk"], w=[SK(4)])
                self.TR(SLT(5), B["vf"], r=[K_("vf")], w=[SK(5)])
                self.TS("vector", B["kbg"], SLT(4), BEG[:, ti, j:j + 1], ALU.mult, r=[SK(4), "gBEG"], w=[K_("kbg")])
                self.TS("vector", B["kdec"], SLT(4), KD[:, ti, j:j + 1], ALU.mult, r=[SK(4), "gKD"], w=[K_("kdec")])
                self.TS("vector", B["bv"], SLT(5), bcol, ALU.mult, r=[SK(5), "gB"], w=[K_("bv")])
                self.TT("vector", B["R0"], self.ident, B["M"], ALU.subtract, r=["const", K_("M")], w=[K_("R0")])
                Nc, Mc, Rc = ("N", "M", "R0")
                Nn, Mn, Rn = ("N2", "M2", "R1")
                for lev in range(1, 7):
                    self.MM(SLT(6), B[Mc], B[Nc], r=[K_(Mc), K_(Nc)], w=[SK(6)])
                    self.CP("vector", B[Nn], SLT(6), r=[SK(6)], w=[K_(Nn)])
                    if lev < 6:
                        self.MM(SLT(14), B[Nc], B[Mc], r=[K_(Mc), K_(Nc)], w=[SK(14)])
                        self.CP("scalar", B[Mn], SLT(14), r=[SK(14)], w=[K_(Mn)])
                    self.MM(SLT(8), B[Nn], B[Rc], r=[K_(Nn), K_(Rc)], w=[SK(8)])
                    self.TT("vector", B[Rn], SLT(8), B[Rc], ALU.add, r=[SK(8), K_(Rc)], w=[K_(Rn)])
                    Nc, Nn = Nn, Nc
                    Mc, Mn = Mn, Mc
                    Rc, Rn = Rn, Rc
                TTk = Rc
                self.MM(SLT(9), B[TTk], B["bv"], r=[K_(TTk), K_("bv")], w=[SK(9)])
                self.CP("scalar", B["U_"], SLT(9), r=[SK(9)], w=[K_("U_")])
                self.MM(SLT(15), B["kbg"], B[TTk], r=[K_(TTk), K_("kbg")], w=[SK(15)])
                self.CP("vector", B["WT"], SLT(15), r=[SK(15)], w=[K_("WT")])
                S_, Sn_ = Scur[d], Snxt[d]
                self.MM(SLT(11), B["WT"], S_, r=[K_("WT"), f"S{d}"], w=[SK(11)])
                self.TT("vector", B["VN"], B["U_"], SLT(11), ALU.subtract, r=[K_("U_"), SK(11)], w=[K_("VN")])
                self.MM(SLT(12), S_, B["qdec"], start=True, stop=False, r=[f"S{d}", K_("qdec")], w=[SK(12)])
                self.MM(SLT(12), B["VN"], B["QKDT"], start=False, stop=True, r=[K_("VN"), K_("QKDT")], w=[SK(12)])
                if ti not in touched:
                    touched.add(ti)
                    self.CP("scalar", Oacc[:, cs], SLT(12), r=[SK(12)], w=[f"Oacc{ti}"])
                else:
                    self.TT("vector", Oacc[:, cs], SLT(12), Oacc[:, cs], ALU.add, r=[SK(12), f"Oacc{ti}"], w=[f"Oacc{ti}"])
                self.MM(SLT(9 + 0), B["kdec"], B["VN"], r=[K_("kdec"), K_("VN")], w=[SK(9 + 0)])
                self.STT(Sn_, S_, GCL[:, ti, j:j + 1], SLT(9 + 0), ALU.mult, ALU.add, r=[f"S{d}", "gGCL", SK(9 + 0)], w=[f"S{d}"])
                Scur[d], Snxt[d] = Sn_, S_
            P.replay_interleaved(bufs)
        P.barrier()
        self.load_w_cols(WG, self.w_in[l], 2048 + h * 128, 128, "WG")
        self.load_cast(Wo, self.w_out[l, 256 + h * 128:256 + (h + 1) * 128, :], 1024, dkey="WoB")
        MOD = self.MOD[l]
        for bi, (t0, n) in enumerate(BLKS):
            if self.skip_ctx and bi == 0:
                continue
            yy = 1 if bi == 0 else 0
            oa = Oacc[:, t0:t0 + n]
            self.ACT(sqb[:, 0:n], oa, AF.Square, w=["csq"])
            self.MM(ps[:, 2, 0:n], self.ones_b, sqb[:, 0:n], r=["csq", "constb"], w=["pq8"])
            self.ACT(y[:, 0:n], ps[:, 2, 0:n], AF.Sqrt, scale=1.0 / 128, bias=self.eps, r=["pq8", "const2"], w=["cy"])
            self.RECIP(y[:, 0:n], y[:, 0:n], r=["cy"], w=["cy"])
            self.STT(ys[:, 0:n], oa, gains[:, 0:1], y[:, 0:n], ALU.mult, ALU.mult, r=["cy", "gains"], w=["cys"])
            for k in range(8):
                self.MM(ps[:, 3, 0:n], WG[:, k, :], self.hT[:, k, t0:t0 + n], start=(k == 0), stop=(k == 7), r=["WG", f"hT{bi}"], w=["pq12"])
            self.ACT(y[:, 0:n], ps[:, 3, 0:n], AF.Silu, r=["pq12", "cy"], w=["cy"])
            self.TT("vector", sqb[:, 0:n], ys[:, 0:n], y[:, 0:n], ALU.mult, r=["cys", "cy"], w=["csq"])
            if self.dbg and self.dbg_what == f"B{h}":
                self.dump(sqb[:, 0:n], n, ["csq"], col0=t0)
            for dc in range(8):
                bank = 4 + dc % 4
                self.MM(ps[:, bank, 0:n], Wo[:, dc * 128:(dc + 1) * 128], sqb[:, 0:n], r=["WoB", "csq"], w=[f"pq{bank * 4}"])
                self.STT(self.xT[:, dc, t0:t0 + n], ps[:, bank, 0:n], MOD[:, 16 + dc, yy:yy + 1], self.xT[:, dc, t0:t0 + n],
                         ALU.mult, ALU.add, r=[f"pq{bank * 4}", f"MOD{l}", f"xT{bi}"], w=[f"xT{bi}"])
        P.barrier()
    P.release(m)


K.mixer_B = _mixer_B


def _moe(self, l):
    P, ps = self.P, self.ps
    m0 = P.mark()
    gateT = P.alloc(T, BF16)
    m = P.mark()
    RW = P.alloc(8 * 32, F32).rearrange("p (k e) -> p k e", k=8)
    self.DMA(RW, self.router_w[l].rearrange("(k p) e -> p k e", p=128), w=["RW"])
    rb = P.alloc(32, F32)
    self.DMA(rb, self.router_b[l].rearrange("(a e) -> a e", a=1).broadcast_to([128, 32]), w=["rb"])
    h2f = P.alloc(8 * 512, F32).rearrange("p (c t) -> p c t", c=8)
    lg, msk, ex = P.alloc(32, F32), P.alloc(32, F32), P.alloc(32, F32)
    top8, dn = P.alloc(8, F32), P.alloc(1, F32)

    def router_fn(bi, t0, n):
        for tt in range(n // 128):
            cs = slice(tt * 128, (tt + 1) * 128)
            for c in range(8):
                self.MM(ps[:, 1, 0:32], h2f[:, c, cs], RW[:, c, :], start=(c == 0), stop=(c == 7), r=[f"h2f{c}", "RW"], w=["ps1"])
            self.TT("vector", lg, ps[:, 1, 0:32], rb, ALU.add, r=["ps1", "rb"], w=["lg"])
            self.P.op("vector", lambda e: e.max(out=top8, in_=lg), ["lg"], ["top8"])
            self.TS("vector", msk, lg, top8[:, 3:4], ALU.is_ge, r=["lg", "top8"], w=["msk"])
            self.TS("vector", ex, lg, top8[:, 0:1], ALU.subtract, r=["lg", "top8"], w=["ex"])
            self.ACT(ex, ex, AF.Exp, r=["ex"], w=["ex"])
            self.TT("vector", ex, ex, msk, ALU.mult, r=["ex", "msk"], w=["ex"])
            self.P.op("vector", lambda e: e.reduce_sum(out=dn, in_=ex, axis=mybir.AxisListType.X), ["ex"], ["dn"])
            self.RECIP(dn, dn, r=["dn"], w=["dn"])
            self.TS("vector", ex, ex, dn[:, 0:1], ALU.mult, r=["ex", "dn"], w=["ex"])
            self.TR(ps[0:32, 2, 0:128], ex, r=["ex"], w=["ps2"])
            self.CP("scalar", gateT[0:32, t0 + tt * 128:t0 + (tt + 1) * 128], ps[0:32, 2, 0:128], r=["ps2"], w=["gateT"])

    self.norm_mod(l, self.A2[l], 24, router=dict(h2f=h2f, fn=router_fn))
    P.release(m)
    if self.dbg_what == "gate":
        self.dump(gateT[0:32, :], T, ["gateT"], rows=32)
        P.release(m0)
        return
    bsb = P.alloc(2048, F32)
    self.DMA(bsb[0:32, :], self.exp_b_gu[l], w=["bsb"])
    bv = bsb[0:32, :].rearrange("e (j p s) -> e j s p", j=8, s=2)
    for j in range(8):
        for s_ in range(2):
            self.TR(ps[:, 3, (j * 2 + s_) * 32:(j * 2 + s_ + 1) * 32], bv[:, j, s_, :], r=["bsb"], w=["ps3"])
    bguT = P.alloc(512, F32).rearrange("p (a e) -> p a e", e=32)
    self.CP("vector", bguT, ps[:, 3, :].rearrange("p (a e) -> p a e", e=32), r=["ps3"], w=["bguT"])
    bdn = P.alloc(1024, BF16)
    self.load_cast(bdn[0:32, :], self.exp_b_dn[l], 1024, dkey="bdn", parts=32)
    MOD = self.MOD[l]
    for bi, (t0, n) in enumerate(BLKS):
        if self.skip_ctx and bi == 0:
            continue
        yy = 1 if bi == 0 else 0
        for dc in range(8):
            bank = 4 + dc % 2
            self.MM(ps[:, bank, 0:n], bdn[0:32, dc * 128:(dc + 1) * 128], gateT[0:32, t0:t0 + n], r=["bdn", "gateT"], w=[f"ps{bank}"])
            self.STT(self.xT[:, dc, t0:t0 + n], ps[:, bank, 0:n], MOD[:, 40 + dc, yy:yy + 1], self.xT[:, dc, t0:t0 + n],
                     ALU.mult, ALU.add, r=[f"ps{bank}", f"MOD{l}", f"xT{bi}"], w=[f"xT{bi}"])
    Wgu = [P.alloc(8 * 512, BF16).rearrange("p (k s f) -> p k s f", k=8, s=2) for _ in range(2)]
    Wdn = [P.alloc(2 * 1024, BF16).rearrange("p (j n) -> p j n", j=2) for _ in range(2)]
    act = [P.alloc(2 * 512, BF16).rearrange("p (j n) -> p j n", j=2) for _ in range(3)]
    gsel = P.alloc(512, BF16)
    tg = [[P.alloc(512, BF16) for _ in range(2)] for _ in range(2)]
    tsg = [[P.alloc(512, BF16) for _ in range(2)] for _ in range(2)]
    tl = [[P.alloc(512, BF16) for _ in range(2)] for _ in range(2)]
    bl1 = P.alloc(8 * 32, F32).rearrange("p (j e) -> p j e", e=32)
    self.TS("vector", bl1, bguT.rearrange("p (j s) e -> p j s e", s=2)[:, :, 1, :], 1.0, ALU.add, r=["bguT"], w=["bl1"])
    nexp = NEXP if self.moe_experts is None else self.moe_experts
    units = [(e_, q) for e_ in range(nexp) for q in range(4)]

    def load_unit(ui):
        e_, q = units[ui]
        wi = ui % 2
        for k0 in range(0, 8, 2):
            st, sk = self.stage()
            sv = st[:, 0:1024].rearrange("p (k n) -> p k n", k=2)
            self.DMA(sv, self.exp_w_gu[l, e_, k0 * 128:(k0 + 2) * 128, q * 512:(q + 1) * 512].rearrange("(k p) n -> p k n", p=128), w=[sk])
            self.CP("gpsimd", Wgu[wi][:, k0:k0 + 2, :, :], sv.rearrange("p k (f s) -> p k s f", s=2), r=[sk], w=[f"Wgu{wi}"])
        for jj in range(2):
            st, sk = self.stage()
            self.DMA(st[:, 0:1024], self.exp_w_dn[l, e_, q * 256 + jj * 128:q * 256 + (jj + 1) * 128, :], w=[sk])
            self.CP("scalar", Wdn[wi][:, jj, :], st[:, 0:1024], r=[sk], w=[f"Wdn{wi}"])

    def gu_swiglu(ui, bi, ai):
        e_, q = units[ui]
        wi = ui % 2
        t0, n = BLKS[bi]
        wgk = f"Wgu{wi}"
        self.TS("vector", gsel[0:32, 0:n], gateT[0:32, t0:t0 + n], self.ident[0:32, e_:e_ + 1], ALU.mult, r=["gateT", "const"], w=["gsel"])
        for jj in range(2):
            bg_, bl_ = 2 * jj, 2 * jj + 1
            for s_ in range(2):
                for k in range(8):
                    self.MM(ps[:, 2 * jj + s_, 0:n], Wgu[wi][:, k, s_, jj * 128:(jj + 1) * 128], self.hT[:, k, t0:t0 + n],
                            start=(k == 0), stop=(k == 7), r=[wgk, f"hT{bi}"], w=[f"ps{2 * jj + s_}"])
            if jj == 0:
                self.MM(ps[:, 6, 0:n], self.ones_b[0:32, :], gsel[0:32, 0:n], r=["gsel", "constb"], w=["ps6"])
            fj = 2 * q + jj
            G_, S_, L_ = tg[ai % 2][jj], tsg[ai % 2][jj], tl[ai % 2][jj]
            kk = f"{ai % 2}{jj}"
            self.TS("vector", G_[:, 0:n], ps[:, bg_, 0:n], bguT[:, fj * 2, e_:e_ + 1], ALU.add, 7.0, ALU.min, r=[f"ps{bg_}", "bguT"], w=["tg" + kk])
            self.ACT(S_[:, 0:n], G_[:, 0:n], AF.Gelu_apprx_sigmoid, r=["tg" + kk], w=["tsg" + kk])
            self.TS("vector", L_[:, 0:n], ps[:, bl_, 0:n], bl1[:, fj, e_:e_ + 1], ALU.add, 8.0, ALU.min, r=[f"ps{bl_}", "bl1"], w=["tl" + kk])
            self.STT(L_[:, 0:n], L_[:, 0:n], -6.0, ps[:, 6, 0:n], ALU.max, ALU.mult, r=["tl" + kk, "ps6"], w=["tl" + kk])
            self.TT("gpsimd", act[ai][:, jj, 0:n], S_[:, 0:n], L_[:, 0:n], ALU.mult, r=["tsg" + kk, "tl" + kk], w=[f"act{ai}"])

    def dn_update(ui, bi, ai):
        wi = ui % 2
        t0, n = BLKS[bi]
        yy = 1 if bi == 0 else 0
        for dc in range(8):
            bank = 4 + dc % 2
            for jj in range(2):
                self.MM(ps[:, bank, 0:n], Wdn[wi][:, jj, dc * 128:(dc + 1) * 128], act[ai][:, jj, 0:n], start=(jj == 0), stop=(jj == 1), r=[f"Wdn{wi}", f"act{ai}"], w=[f"ps{bank}"])
            if dc in ():
                xi = xcnt[0] % 2
                xcnt[0] += 1
                self.ACT(xtmp[xi][:, 0:n], ps[:, bank, 0:n], AF.Copy, scale=MOD[:, 40 + dc, yy:yy + 1], r=[f"ps{bank}", f"MOD{l}"], w=[f"xtmp{xi}"])
                self.TT("gpsimd", self.xT[:, dc, t0:t0 + n], self.xT[:, dc, t0:t0 + n], xtmp[xi][:, 0:n], ALU.add, r=[f"xtmp{xi}", f"xTm{bi}_{dc}"], w=[f"xTm{bi}_{dc}"])
            else:
                self.STT(self.xT[:, dc, t0:t0 + n], ps[:, bank, 0:n], MOD[:, 40 + dc, yy:yy + 1], self.xT[:, dc, t0:t0 + n],
                         ALU.mult, ALU.add, r=[f"ps{bank}", f"MOD{l}", f"xTm{bi}_{dc}"], w=[f"xTm{bi}_{dc}"])

    xtmp = [P.alloc(512, F32), P.alloc(512, F32)]
    xcnt = [0]
    load_unit(0)
    pend = []
    step = 0
    blist = list(range(len(BLKS)))
    if l == self.nlayers - 1 and self.dbg_what is None:
        blist = blist[1:]
    for ui in range(len(units)):
        for bi in blist:
            ai = step % 2
            step += 1
            ai = step % 3
            P.rec = bufA = []
            gu_swiglu(ui, bi, ai)
            P.rec = bufB = []
            if len(pend) == 2:
                dn_update(*pend.pop(0))
            P.rec = None
            groups = [bufB[i:i + 3] for i in range(0, len(bufB), 3)]
            nmm = 0
            for kind, args in bufA:
                P.op(*args)
                if args[0] == "tensor":
                    nmm += 1
                    if nmm % 4 == 0 and groups:
                        for k2, a2 in groups.pop(0):
                            P.op(*a2)
            for g_ in groups:
                for k2, a2 in g_:
                    P.op(*a2)
            if bi == blist[1] and ui + 1 < len(units):
                load_unit(ui + 1)
            pend.append((ui, bi, ai))
    for p_ in pend:
        dn_update(*p_)
    P.barrier()
    P.release(m0)


K.moe = _moe
K.moe_experts = None
```
